# Optimizing a Trainium2 kernel written in Bass

```python
import math
import jax, jax.numpy as jnp
from jax import lax
import numpy as np

D_MODEL = 1024
BATCH = 4
SEQ = 8192
DEPTH = 4

N_MIXERS = 2
N_MLSTM_LAYERS = (DEPTH + 1) // 2
N_MLA_LAYERS = DEPTH // 2

MLSTM_HEADS = 8
MLSTM_DV = D_MODEL // MLSTM_HEADS
MLSTM_DQK = MLSTM_DV // 2
MLSTM_CHUNK = 64
MLSTM_IN = 2 * MLSTM_HEADS * MLSTM_DQK + 2 * MLSTM_HEADS * MLSTM_DV + 2 * MLSTM_HEADS

MLA_HEADS = 8
MLA_Q_LORA = 384
MLA_KV_LORA = 256
MLA_NOPE = 128
MLA_ROPE = 64
MLA_V = 128
MLA_IN = MLA_Q_LORA + MLA_KV_LORA + MLA_ROPE
ROPE_THETA = 10000.0
ATTN_QBLOCK = 128

N_EXPERTS = 32
TOP_K = 4
D_FF = D_MODEL
SWIGLU_LIMIT = 7.0
SWIGLU_ALPHA = 1.702
MOE_BLOCK = 128

DEEPNORM_ALPHA = (2.0 * DEPTH) ** 0.25
DEEPNORM_BETA = (8.0 * DEPTH) ** -0.25
LN_EPS = 1e-5
RMS_EPS = 1e-6

kernel_name = 'hybrid_mlstm_mla_moe_deepnorm'


def layer_norm(x, g, b):
    xf = x.astype(jnp.float32)
    mu = jnp.mean(xf, axis=-1, keepdims=True)
    var = jnp.mean(jnp.square(xf - mu), axis=-1, keepdims=True)
    return ((xf - mu) * lax.rsqrt(var + LN_EPS) * g + b).astype(x.dtype)


def rms_norm(x, g):
    xf = x.astype(jnp.float32)
    return (xf * lax.rsqrt(jnp.mean(jnp.square(xf), axis=-1, keepdims=True) + RMS_EPS) * g).astype(x.dtype)


def rope_tables(positions):
    inv_freq = ROPE_THETA ** (-jnp.arange(0, MLA_ROPE, 2, dtype=jnp.float32) / MLA_ROPE)
    ang = positions.astype(jnp.float32)[..., None] * inv_freq
    return jnp.cos(ang), jnp.sin(ang)


def apply_rope(x, cos, sin):
    x1, x2 = jnp.split(x, 2, axis=-1)
    return jnp.concatenate([x1 * cos - x2 * sin, x2 * cos + x1 * sin], axis=-1).astype(x.dtype)


def mlstm_cell(q, k, v, ig, lf):
    B, H, S, dk = q.shape
    dv = v.shape[-1]
    L = MLSTM_CHUNK
    nc = S // L
    q = q.reshape(B, H, nc, L, dk)
    k = k.reshape(B, H, nc, L, dk)
    v = v.reshape(B, H, nc, L, dv)
    ig = ig.reshape(B, H, nc, L)
    lf = lf.reshape(B, H, nc, L)
    b = jnp.cumsum(lf, axis=-1)
    g_end = b[..., -1:] - b + ig

    def step(carry, inp):
        C, n, m = carry
        k_c, v_c, g_c, bl_c = inp
        m_new = jnp.maximum(bl_c + m, jnp.max(g_c, axis=-1))
        decay = jnp.exp(bl_c + m - m_new)
        w = jnp.exp(g_c - m_new[..., None])
        C_new = decay[..., None, None] * C + jnp.einsum('bhlv,bhlk->bhvk', w[..., None] * v_c, k_c)
        n_new = decay[..., None] * n + jnp.einsum('bhl,bhlk->bhk', w, k_c)
        return (C_new, n_new, m_new), (C, n, m)

    init = (jnp.zeros((B, H, dv, dk), jnp.float32), jnp.zeros((B, H, dk), jnp.float32),
            jnp.zeros((B, H), jnp.float32))
    xs = (jnp.moveaxis(k, 2, 0), jnp.moveaxis(v, 2, 0), jnp.moveaxis(g_end, 2, 0),
          jnp.moveaxis(b[..., -1], 2, 0))
    _, (C_prev, n_prev, m_prev) = lax.scan(step, init, xs)
    C_prev = jnp.moveaxis(C_prev, 0, 2)
    n_prev = jnp.moveaxis(n_prev, 0, 2)
    m_prev = jnp.moveaxis(m_prev, 0, 2)

    causal = jnp.tril(jnp.ones((L, L), dtype=bool))
    D = jnp.where(causal, b[..., :, None] - b[..., None, :] + ig[..., None, :], -jnp.inf)
    inter = b + m_prev[..., None]
    m_row = jnp.maximum(inter, jnp.max(D, axis=-1))
    s = jnp.einsum('bhcqk,bhcsk->bhcqs', q, k) * jnp.exp(D - m_row[..., None])
    w_inter = jnp.exp(inter - m_row)
    num = (jnp.einsum('bhcqs,bhcsv->bhcqv', s, v)
           + w_inter[..., None] * jnp.einsum('bhcqk,bhcvk->bhcqv', q, C_prev))
    den = jnp.sum(s, axis=-1) + w_inter * jnp.einsum('bhcqk,bhck->bhcq', q, n_prev)
    h = num / jnp.maximum(jnp.abs(den), jnp.exp(-m_row))[..., None]
    return h.reshape(B, H, S, dv)


def mlstm_mixer(x, w_in, b_gates, norm_gain, w_out):
    B, S, _ = x.shape
    H, dk, dv = MLSTM_HEADS, MLSTM_DQK, MLSTM_DV
    proj = x @ w_in
    cuts = [H * dk, 2 * H * dk, 2 * H * dk + H * dv, 2 * H * dk + 2 * H * dv]
    q, k, v, o, g = jnp.split(proj, cuts, axis=-1)

    def heads(t, d):
        return t.reshape(B, S, H, d).transpose(0, 2, 1, 3).astype(jnp.float32)

    gates = (g + b_gates).astype(jnp.float32)
    ig = gates[..., :H].transpose(0, 2, 1)
    lf = jax.nn.log_sigmoid(gates[..., H:]).transpose(0, 2, 1)
    h = mlstm_cell(heads(q, dk) * (dk ** -0.5), heads(k, dk), heads(v, dv), ig, lf)
    h = rms_norm(h.transpose(0, 2, 1, 3), norm_gain.reshape(H, dv)).reshape(B, S, H * dv)
    h = (jax.nn.sigmoid(o.astype(jnp.float32)) * h).astype(x.dtype)
    return h @ w_out


def causal_block_attention(q_nope, q_rope, k_nope, k_rope, v):
    B, S, H, dn = q_nope.shape
    QB = ATTN_QBLOCK
    nqb = S // QB
    scale = (MLA_NOPE + MLA_ROPE) ** -0.5
    qn = q_nope.reshape(B, nqb, QB, H, dn).transpose(1, 0, 3, 2, 4)
    qr = q_rope.reshape(B, nqb, QB, H, MLA_ROPE).transpose(1, 0, 3, 2, 4)
    kn = k_nope.transpose(0, 2, 1, 3)
    vh = v.transpose(0, 2, 1, 3)
    key_pos = jnp.arange(S)

    def block(args):
        qn_b, qr_b, blk = args
        s = jnp.einsum('bhqd,bhkd->bhqk', qn_b, kn) + jnp.einsum('bhqr,bkr->bhqk', qr_b, k_rope)
        s = s.astype(jnp.float32) * scale
        q_pos = blk * QB + jnp.arange(QB)
        mask = key_pos[None, :] <= q_pos[:, None]
        p = jax.nn.softmax(jnp.where(mask, s, -jnp.inf), axis=-1)
        return jnp.einsum('bhqk,bhkv->bhqv', p.astype(vh.dtype), vh)

    o = lax.map(block, (qn, qr, jnp.arange(nqb)))
    return o.transpose(1, 0, 3, 2, 4).reshape(B, S, H, MLA_V)


def mla_mixer(x, cos, sin, w_in, q_norm, kv_norm, w_qb, w_kvb, w_out):
    B, S, _ = x.shape
    H = MLA_HEADS
    a = x @ w_in
    c_q, c_kv, k_r = jnp.split(a, [MLA_Q_LORA, MLA_Q_LORA + MLA_KV_LORA], axis=-1)
    c_q = rms_norm(c_q, q_norm)
    c_kv = rms_norm(c_kv, kv_norm)
    q = (c_q @ w_qb).reshape(B, S, H, MLA_NOPE + MLA_ROPE)
    kv = (c_kv @ w_kvb).reshape(B, S, H, MLA_NOPE + MLA_V)
    q_nope, q_rope = q[..., :MLA_NOPE], q[..., MLA_NOPE:]
    k_nope, v = kv[..., :MLA_NOPE], kv[..., MLA_NOPE:]
    q_rope = apply_rope(q_rope, cos[:, :, None, :], sin[:, :, None, :])
    k_rope = apply_rope(k_r, cos, sin)
    o = causal_block_attention(q_nope, q_rope, k_nope, k_rope, v)
    return o.reshape(B, S, H * MLA_V) @ w_out


def clamped_swiglu(gu):
    g = jnp.minimum(gu[..., ::2], SWIGLU_LIMIT)
    l = jnp.clip(gu[..., 1::2], -SWIGLU_LIMIT, SWIGLU_LIMIT)
    return (l + 1.0) * g * jax.nn.sigmoid(SWIGLU_ALPHA * g)


def moe(x, w_router, b_router, w_gu, b_gu, w_down, b_down):
    B, S, D = x.shape
    T = B * S
    A = T * TOP_K
    xt = x.reshape(T, D)
    logits = (xt @ w_router + b_router).astype(jnp.float32)
    top_val, top_idx = lax.top_k(logits, TOP_K)
    gate = jax.nn.softmax(top_val, axis=-1)
    e_flat = top_idx.reshape(-1)
    tok_flat = jnp.repeat(jnp.arange(T, dtype=jnp.int32), TOP_K)
    order = jnp.argsort(e_flat)
    e_sorted = e_flat[order]
    tok_sorted = tok_flat[order]
    gate_sorted = gate.reshape(-1)[order]
    counts = jnp.bincount(e_flat, length=N_EXPERTS)
    seg_start = jnp.cumsum(counts) - counts
    padded = (counts + MOE_BLOCK - 1) // MOE_BLOCK * MOE_BLOCK
    pad_end = jnp.cumsum(padded)
    pad_start = pad_end - padded
    dest = pad_start[e_sorted] + jnp.arange(A) - seg_start[e_sorted]
    n_blocks = -(-A // MOE_BLOCK) + N_EXPERTS
    P = n_blocks * MOE_BLOCK
    row_tok = jnp.full((P,), T, dtype=jnp.int32).at[dest].set(tok_sorted)
    xpad = jnp.concatenate([xt, jnp.zeros((1, D), xt.dtype)], axis=0)
    x_blocks = xpad[row_tok].reshape(n_blocks, MOE_BLOCK, D)
    block_expert = jnp.minimum(
        jnp.searchsorted(pad_end, jnp.arange(n_blocks) * MOE_BLOCK, side='right'), N_EXPERTS - 1)

    def expert_block(args):
        xb, e = args
        h = clamped_swiglu(xb @ w_gu[e] + b_gu[e])
        return h @ w_down[e] + b_down[e]

    y_blocks = lax.map(expert_block, (x_blocks, block_expert))
    y_sorted = y_blocks.reshape(P, D)[dest]
    y = jax.ops.segment_sum(y_sorted * gate_sorted[:, None].astype(y_sorted.dtype), tok_sorted,
                            num_segments=T)
    return y.reshape(B, S, D).astype(x.dtype)


def setup_inputs(seed: int = 0) -> dict:
    key = jax.random.key(seed)
    ks = jax.random.split(key, 24)

    def nrm(k, shape, scale):
        return jax.random.normal(k, shape, jnp.float32) * scale

    H = MLSTM_HEADS
    x = nrm(ks[0], (BATCH, SEQ, D_MODEL), 1.0)
    positions = (jax.random.randint(ks[1], (BATCH, 1), 0, 4096, dtype=jnp.int32)
                 + jnp.arange(SEQ, dtype=jnp.int32)[None, :])
    ln_gain = 1.0 + nrm(ks[2], (DEPTH, 2, D_MODEL), 0.02)
    ln_bias = nrm(ks[3], (DEPTH, 2, D_MODEL), 0.02)
    mlstm_w_in = nrm(ks[4], (N_MLSTM_LAYERS, D_MODEL, MLSTM_IN), D_MODEL ** -0.5)
    mlstm_b_gates = jnp.concatenate([nrm(ks[5], (N_MLSTM_LAYERS, H), 0.1),
                                     3.0 + nrm(ks[6], (N_MLSTM_LAYERS, H), 0.5)], axis=-1)
    mlstm_norm_gain = 1.0 + nrm(ks[7], (N_MLSTM_LAYERS, H * MLSTM_DV), 0.02)
    mlstm_w_out = nrm(ks[8], (N_MLSTM_LAYERS, H * MLSTM_DV, D_MODEL),
                      (H * MLSTM_DV) ** -0.5 * DEEPNORM_BETA)
    mla_w_in = nrm(ks[9], (N_MLA_LAYERS, D_MODEL, MLA_IN), D_MODEL ** -0.5)
    mla_q_norm = 1.0 + nrm(ks[10], (N_MLA_LAYERS, MLA_Q_LORA), 0.02)
    mla_kv_norm = 1.0 + nrm(ks[11], (N_MLA_LAYERS, MLA_KV_LORA), 0.02)
    mla_w_qb = nrm(ks[12], (N_MLA_LAYERS, MLA_Q_LORA, MLA_HEADS * (MLA_NOPE + MLA_ROPE)), MLA_Q_LORA ** -0.5)
    mla_w_kvb = nrm(ks[13], (N_MLA_LAYERS, MLA_KV_LORA, MLA_HEADS * (MLA_NOPE + MLA_V)), MLA_KV_LORA ** -0.5)
    mla_w_out = nrm(ks[14], (N_MLA_LAYERS, MLA_HEADS * MLA_V, D_MODEL),
                    (MLA_HEADS * MLA_V) ** -0.5 * DEEPNORM_BETA)
    moe_w_router = nrm(ks[15], (DEPTH, D_MODEL, N_EXPERTS), D_MODEL ** -0.5)
    moe_b_router = nrm(ks[16], (DEPTH, N_EXPERTS), 0.01)
    moe_w_gate_up = nrm(ks[17], (DEPTH, N_EXPERTS, D_MODEL, 2 * D_FF), D_MODEL ** -0.5)
    moe_b_gate_up = nrm(ks[18], (DEPTH, N_EXPERTS, 2 * D_FF), 0.02)
    moe_w_down = nrm(ks[19], (DEPTH, N_EXPERTS, D_FF, D_MODEL), D_FF ** -0.5 * DEEPNORM_BETA)
    moe_b_down = nrm(ks[20], (DEPTH, N_EXPERTS, D_MODEL), 0.02)
    return {'x': x, 'positions': positions, 'ln_gain': ln_gain, 'ln_bias': ln_bias,
            'mlstm_w_in': mlstm_w_in, 'mlstm_b_gates': mlstm_b_gates,
            'mlstm_norm_gain': mlstm_norm_gain, 'mlstm_w_out': mlstm_w_out,
            'mla_w_in': mla_w_in, 'mla_q_norm': mla_q_norm, 'mla_kv_norm': mla_kv_norm,
            'mla_w_qb': mla_w_qb, 'mla_w_kvb': mla_w_kvb, 'mla_w_out': mla_w_out,
            'moe_w_router': moe_w_router, 'moe_b_router': moe_b_router,
            'moe_w_gate_up': moe_w_gate_up, 'moe_b_gate_up': moe_b_gate_up,
            'moe_w_down': moe_w_down, 'moe_b_down': moe_b_down}


def reference(x, positions, ln_gain, ln_bias, mlstm_w_in, mlstm_b_gates, mlstm_norm_gain,
              mlstm_w_out, mla_w_in, mla_q_norm, mla_kv_norm, mla_w_qb, mla_w_kvb, mla_w_out,
              moe_w_router, moe_b_router, moe_w_gate_up, moe_b_gate_up, moe_w_down, moe_b_down):
    cos, sin = rope_tables(positions)
    for layer in range(DEPTH):
        j = layer // N_MIXERS
        if layer % N_MIXERS == 0:
            mix = mlstm_mixer(x, mlstm_w_in[j], mlstm_b_gates[j], mlstm_norm_gain[j], mlstm_w_out[j])
        else:
            mix = mla_mixer(x, cos, sin, mla_w_in[j], mla_q_norm[j], mla_kv_norm[j],
                            mla_w_qb[j], mla_w_kvb[j], mla_w_out[j])
        x = layer_norm(DEEPNORM_ALPHA * x + mix, ln_gain[layer, 0], ln_bias[layer, 0])
        ff = moe(x, moe_w_router[layer], moe_b_router[layer], moe_w_gate_up[layer],
                 moe_b_gate_up[layer], moe_w_down[layer], moe_b_down[layer])
        x = layer_norm(DEEPNORM_ALPHA * x + ff, ln_gain[layer, 1], ln_bias[layer, 1])
    return x
```

```python
import math
from contextlib import ExitStack
import numpy as np
import concourse.bass as bass
import concourse.mybir as mybir
from concourse.bass_utils import run_bass_kernel_spmd

F32 = mybir.dt.float32
BF16 = mybir.dt.bfloat16
I32 = mybir.dt.int32
U32 = mybir.dt.uint32
AF = mybir.ActivationFunctionType
ALU = mybir.AluOpType
AX = mybir.AxisListType


class Buf:
    __slots__ = ("t", "w", "r", "name")

    def __init__(self, t=None, name=""):
        self.t = t
        self.w = {}
        self.r = {}
        self.name = name

    def __getitem__(self, idx):
        return self.t[idx]


class _Eng:
    def __init__(self, name, eng, sem):
        self.name = name
        self.eng = eng
        self.sem = sem
        self.count = 0
        self.waited = {}


class Sched:
    def __init__(self, nc, ctx, n_dma_sems=12, same_engine_sync=True):
        self.nc = nc
        self.ctx = ctx
        self.same_engine_sync = same_engine_sync
        self.E = {}
        for name, eng in (("pe", nc.tensor), ("dve", nc.vector), ("act", nc.scalar),
                          ("pool", nc.gpsimd), ("sp", nc.sync)):
            sem = ctx.enter_context(nc.semaphore("s_" + name))
            self.E[name] = _Eng(name, eng, sem)
        self.dma_sems = {}
        for q in ("sp", "act", "pool"):
            lst = []
            for i in range(n_dma_sems):
                s = ctx.enter_context(nc.semaphore(f"d_{q}{i}"))
                lst.append([s, 0])
            self.dma_sems[q] = [lst, 0]
        self.ninst = 0
        self.pfx = ""

    def sbuf(self, name, shape, dt):
        t = self.ctx.enter_context(self.nc.sbuf_tensor(self.pfx + name, list(shape), dt))
        return Buf(t, name)

    def psum(self, name, shape, dt):
        t = self.ctx.enter_context(self.nc.psum_tensor(name, list(shape), dt))
        return Buf(t, name)

    @staticmethod
    def _key(sem):
        return id(sem)

    def _need(self, reads, writes):
        need = {}
        def add(d):
            for k, (s, v) in d.items():
                if k not in need or need[k][1] < v:
                    need[k] = (s, v)
        for b in reads:
            add(b.w)
        for b in writes:
            add(b.w)
            add(b.r)
        return need

    def _do_waits(self, e, need):
        for k, (s, v) in need.items():
            if s is e.sem and (e.name == "pe" or not self.same_engine_sync):
                continue
            if e.waited.get(k, 0) < v:
                e.eng.wait_ge(s, v)
                e.waited[k] = v

    def _record(self, dep_sem, dep_val, reads, writes):
        k = self._key(dep_sem)
        for b in writes:
            b.w = {k: (dep_sem, dep_val)}
            b.r = {}
        for b in reads:
            if b in writes:
                continue
            if k not in b.r or b.r[k][1] < dep_val:
                b.r[k] = (dep_sem, dep_val)

    def op(self, eng, fn, reads=(), writes=(), inc=True):
        e = self.E[eng]
        self._do_waits(e, self._need(reads, writes))
        inst = fn(e.eng)
        self.ninst += 1
        if inc:
            inst.then_inc(e.sem, 1)
            e.count += 1
            self._record(e.sem, e.count, reads, writes)
        else:
            self._record(e.sem, e.count + 1, reads, writes)
        return inst

    def dma(self, q, out, in_, reads=(), writes=(), indirect=None, **kw):
        e = self.E[q]
        lst, pos = self.dma_sems[q]
        ent = lst[pos]
        self.dma_sems[q][1] = (pos + 1) % len(lst)
        s, cnt = ent
        need = self._need(reads, writes)
        if cnt > 0:
            need[self._key(s)] = (s, cnt)
        for k, (ss, v) in need.items():
            if e.waited.get(k, 0) < v:
                e.eng.wait_ge(ss, v)
                e.waited[k] = v
        if indirect is None:
            inst = e.eng.dma_start(out=out, in_=in_, **kw)
        else:
            inst = e.eng.indirect_dma_start(out=out, in_=in_, **indirect, **kw)
        self.ninst += 1
        inst.then_inc(s, 16)
        ent[1] = cnt + 16
        self._record(s, cnt + 16, reads, writes)
        return inst

    def cc(self, kind, groups, in_ap, out_ap, reads=(), writes=(), inc=16):
        e = self.E["pool"]
        lst, pos = self.dma_sems["pool"]
        ent = lst[pos]
        self.dma_sems["pool"][1] = (pos + 1) % len(lst)
        s, cnt = ent
        need = self._need(reads, writes)
        if cnt > 0:
            need[self._key(s)] = (s, cnt)
        for k, (ss, v) in need.items():
            if e.waited.get(k, 0) < v:
                e.eng.wait_ge(ss, v)
                e.waited[k] = v
        inst = e.eng.collective_compute(kind, ALU.bypass, replica_groups=groups, ins=[in_ap], outs=[out_ap])
        self.ninst += 1
        inst.then_inc(s, inc)
        ent[1] = cnt + inc
        self._record(s, cnt + inc, reads, writes)
        return inst

    def barrier(self):
        targets = []
        for name, o in self.E.items():
            if o.count > 0:
                targets.append((o, o.sem, o.count))
        for q, (lst, _) in self.dma_sems.items():
            for s, cnt in lst:
                if cnt > 0:
                    targets.append((None, s, cnt))
        for name in ("sp", "pool", "act", "dve", "pe"):
            e = self.E[name]
            for (o, s, v) in targets:
                if o is e:
                    continue
                k = self._key(s)
                if e.waited.get(k, 0) < v:
                    e.eng.wait_ge(s, v)
                    e.waited[k] = v

    def finish(self, bufs):
        need = self._need((), bufs)
        for name in ("sp", "pool", "act", "dve", "pe"):
            e = self.E[name]
            for k, (s, v) in need.items():
                if s is e.sem:
                    continue
                if e.waited.get(k, 0) < v:
                    e.eng.wait_ge(s, v)
                    e.waited[k] = v


D = 1024
E = 32
ALPHA = 8.0 ** 0.25
LN_EPS = 1e-5
RMS_EPS = 1e-6
SCALE = 192.0 ** -0.5
STAGE = SUB = SUBA = SUBB = SUBC = 99
SEQ_FULL = 8192
TOK_CORE = 4096
CAP = 640

def emit_ln(S, z, gain, bias, out, tmp):
    st, mv, sd, rstd, xn = tmp
    for h in range(2):
        S.op("dve", lambda e: e.bn_stats(out=st[:, h, :], in_=z[:, h * 512:(h + 1) * 512]),
             reads=[z], writes=[st])
    S.op("dve", lambda e: e.bn_aggr(out=mv[:], in_=st[:].rearrange("p a b -> p (a b)")),
         reads=[st], writes=[mv])
    S.op("act", lambda e: e.activation(out=sd[:], in_=mv[:, 1:2], func=AF.Sqrt, bias=tmp_eps(S)[:], scale=1.0),
         reads=[mv], writes=[sd])
    S.op("dve", lambda e: e.reciprocal(out=rstd[:], in_=sd[:]), reads=[sd], writes=[rstd])
    S.op("dve", lambda e: e.tensor_scalar(out=xn[:], in0=z[:], scalar1=mv[:, 0:1], scalar2=rstd[:],
                                          op0=ALU.subtract, op1=ALU.mult),
         reads=[z, mv, rstd], writes=[xn])
    S.op("pool", lambda e: e.tensor_tensor(out=xn[:], in0=xn[:], in1=gain[:], op=ALU.mult),
         reads=[xn, gain], writes=[xn])
    S.op("pool", lambda e: e.tensor_tensor(out=out[:], in0=xn[:], in1=bias[:], op=ALU.add),
         reads=[xn, bias], writes=[out])


_EPS = {}


def tmp_eps(S):
    return _EPS[id(S)]


def emit_post(S, nc, T, CAP, dr, ps):
    NT = T // 128
    RB = CAP // 128
    parts = []
    lo = 0
    while lo < CAP:
        n = min(512, CAP - lo)
        parts.append((lo, n))
        lo += n
    ctx = S.ctx
    identF = S.sbuf("identF", [128, 128], F32)
    identB = S.sbuf("identB", [128, 128], BF16)
    triS = S.sbuf("triS", [128, 128], F32)
    onesM = S.sbuf("onesM", [128, 128], F32)
    offs = S.sbuf("offs", [128, E], F32)
    epsb = S.sbuf("epsb", [128, 1], F32)
    _EPS[id(S)] = epsb
    idx_all = S.sbuf("idx_all", [128, NT, 4], I32)
    gate_all = S.sbuf("gate_all", [128, NT, 4], F32)
    wgu = [S.sbuf(f"wgu{i}", [128, 8, 2048], BF16) for i in range(2)]
    wd = [S.sbuf(f"wd{i}", [128, 8, 1024], BF16) for i in range(2)]
    bgu = [S.sbuf(f"bgu{i}", [128, 8, 2], F32) for i in range(2)]
    bdn = [S.sbuf(f"bdn{i}", [128, 1024], F32) for i in range(2)]
    lnp = [S.sbuf(f"lnp{i}", [128, 1024], F32) for i in range(4)]
    st = S.sbuf("st", [128, 2, 6], F32)
    mv = S.sbuf("mv", [128, 2], F32)
    sd = S.sbuf("sd", [128, 1], F32)
    rstd = S.sbuf("rstd", [128, 1], F32)
    xn = S.sbuf("xn", [128, 1024], F32)
    lntmp = (st, mv, sd, rstd, xn)
    xg_d = Buf(None, "xg")
    yg_d = Buf(None, "yg")
    x1_d = Buf(None, "x1s")
    out_d = Buf(None, "out")

    S.op("pool", lambda e: e.memset(identF[:], 0.0), writes=[identF])
    S.op("pool", lambda e: e.affine_select(out=identF[:], in_=identF[:], pattern=[[-1, 128]],
                                           compare_op=ALU.not_equal, fill=1.0, base=0,
                                           channel_multiplier=1), reads=[identF], writes=[identF])
    S.op("dve", lambda e: e.tensor_copy(out=identB[:], in_=identF[:]), reads=[identF], writes=[identB])
    S.op("pool", lambda e: e.memset(onesM[:], 1.0), writes=[onesM])
    S.op("pool", lambda e: e.memset(epsb[:], LN_EPS), writes=[epsb])
    S.op("pool", lambda e: e.affine_select(out=triS[:], in_=onesM[:], pattern=[[1, 128]],
                                           compare_op=ALU.is_gt, fill=0.0, base=0,
                                           channel_multiplier=-1), reads=[onesM], writes=[triS])
    S.op("pool", lambda e: e.iota(offs[:], pattern=[[CAP, E]], base=0, channel_multiplier=0,
                                  allow_small_or_imprecise_dtypes=True), writes=[offs])
    for j, nm in enumerate(("g1", "b1", "g2", "b2")):
        S.dma("sp", lnp[j][:], dr[nm].partition_broadcast(128), writes=[lnp[j]])

    def load_expert(e_):
        b = e_ % 2
        for c in range(8):
            S.dma("pool", wgu[b][:, c, :], dr["w_gu"][e_, c * 128:(c + 1) * 128, :], writes=[wgu[b]])
        for c in range(0, 8, 2):
            S.dma("pool", wd[b][:, c:c + 2, :],
                  dr["w_dn"][e_, c * 128:(c + 2) * 128, :].rearrange("(c p) n -> p c n", p=128),
                  writes=[wd[b]])
        with nc.allow_non_contiguous_dma(reason="tiny bias"):
            S.dma("sp", bgu[b][:], dr["b_gu"][e_, :].rearrange("(c p t) -> p c t", p=128, t=2), writes=[bgu[b]])
        S.dma("sp", bdn[b][:], dr["b_dn"][e_, :].partition_broadcast(128), writes=[bdn[b]])

    load_expert(0)

    with ExitStack() as pctx:
        def sb(name, shape, dt):
            return Buf(pctx.enter_context(nc.sbuf_tensor(S.pfx + name, list(shape), dt)), name)
        wout = sb("wout", [128, 8, 1024], BF16)
        wrt = sb("wrt", [128, 8, E], F32)
        brt = sb("brt", [128, E], F32)
        baseoffs = sb("baseoffs", [128, E], F32)
        hgt = [sb(f"hgt{i}", [128, 1024], BF16) for i in range(2)]
        xt = [sb(f"xt{i}", [128, 1024], F32) for i in range(2)]
        hgT = [sb(f"hgT{i}", [128, 8, 128], BF16) for i in range(2)]
        z = [sb(f"z{i}", [128, 1024], F32) for i in range(2)]
        x1 = [sb(f"x1{i}", [128, 1024], F32) for i in range(2)]
        x1b = [sb(f"x1b{i}", [128, 1024], BF16) for i in range(2)]
        x1T = [sb(f"x1T{i}", [128, 8, 128], F32) for i in range(2)]
        lg = [sb(f"lg{i}", [128, E], F32) for i in range(2)]
        top8 = [sb(f"top8{i}", [128, 8], F32) for i in range(2)]
        mask = [sb(f"mask{i}", [128, E], F32) for i in range(2)]
        nmx = [sb(f"nmx{i}", [128, 1], F32) for i in range(2)]
        ex = [sb(f"ex{i}", [128, 4], F32) for i in range(2)]
        sm = [sb(f"sm{i}", [128, 1], F32) for i in range(2)]
        rs = [sb(f"rs{i}", [128, 1], F32) for i in range(2)]
        posf = [sb(f"posf{i}", [128, E], F32) for i in range(2)]
        oh = [sb(f"oh{i}", [128, E], F32) for i in range(2)]
        junk = [sb(f"junk{i}", [128, E], F32) for i in range(2)]
        destf = [sb(f"destf{i}", [128, 4], F32) for i in range(2)]

        if "hg_gath" in dr:
            hgidx = sb("hgidx", [128, NT, 2], I32)
            S.dma("sp", hgidx[:], dr["hgidx"], writes=[hgidx])
        for c in range(0, 8, 2):
            S.dma("pool", wout[:, c:c + 2, :],
                  dr["w_out"][c * 128:(c + 2) * 128, :].rearrange("(c p) n -> p c n", p=128), writes=[wout])
        S.dma("sp", wrt[:], dr["w_rt"].rearrange("(c p) n -> p c n", p=128), writes=[wrt])
        S.dma("sp", brt[:], dr["b_rt"].partition_broadcast(128), writes=[brt])
        S.op("dve", lambda e: e.tensor_copy(out=baseoffs[:], in_=offs[:]), reads=[offs], writes=[baseoffs])

        ptB, P = ps["ptB"], ps["P"]
        for i in range(NT):
            b = i % 2
            sl = slice(i * 128, (i + 1) * 128)
            if "hg_gath" in dr:
                for r in range(2):
                    S.dma("pool", hgt[b][:, r * 512:(r + 1) * 512], dr["hg_gath"], reads=[hgidx], writes=[hgt[b]],
                          indirect=dict(out_offset=None,
                                        in_offset=bass.IndirectOffsetOnAxis(ap=hgidx[:, i, r:r + 1], axis=0)))
            else:
                S.dma("sp", hgt[b][:], dr["hg"][sl, :], writes=[hgt[b]])
            S.dma("sp", xt[b][:], dr["x"][sl, :], writes=[xt[b]])
            for c in range(8):
                S.op("pe", lambda e: e.transpose(out=ptB[:, c * 128:(c + 1) * 128],
                                                 in_=hgt[b][:, c * 128:(c + 1) * 128], identity=identB[:]),
                     reads=[hgt[b], identB], writes=[ptB], inc=(c == 7))
            S.op("act", lambda e: e.copy(out=hgT[b][:], in_=ptB[:].rearrange("p (c n) -> p c n", c=8)),
                 reads=[ptB], writes=[hgT[b]])
            for h in range(2):
                pm = P[1 + h]
                for c in range(8):
                    S.op("pe", lambda e: e.matmul(pm[:], lhsT=hgT[b][:, c, :], rhs=wout[:, c, h * 512:(h + 1) * 512],
                                                  start=(c == 0), stop=(c == 7)),
                         reads=[hgT[b], wout], writes=[pm], inc=(c == 7))
                S.op("dve", lambda e: e.scalar_tensor_tensor(out=z[b][:, h * 512:(h + 1) * 512],
                                                             in0=xt[b][:, h * 512:(h + 1) * 512], scalar=ALPHA,
                                                             in1=pm[:], op0=ALU.mult, op1=ALU.add),
                     reads=[xt[b], pm], writes=[z[b]])
            emit_ln(S, z[b], lnp[0], lnp[1], x1[b], lntmp)
            S.dma("sp", dr["x1s"][sl, :], x1[b][:], reads=[x1[b]], writes=[x1_d])
            S.op("act", lambda e: e.copy(out=x1b[b][:], in_=x1[b][:]), reads=[x1[b]], writes=[x1b[b]])
            for h in range(2):
                pT = P[3 + h]
                for c in range(4):
                    cc = h * 4 + c
                    S.op("pe", lambda e: e.transpose(out=pT[:, c * 128:(c + 1) * 128],
                                                     in_=x1[b][:, cc * 128:(cc + 1) * 128], identity=identF[:]),
                         reads=[x1[b], identF], writes=[pT], inc=(c == 3))
                S.op("act", lambda e: e.copy(out=x1T[b][:, h * 4:(h + 1) * 4, :],
                                             in_=pT[:].rearrange("p (c n) -> p c n", c=4)),
                     reads=[pT], writes=[x1T[b]])
            pq = P[5]
            for c in range(8):
                S.op("pe", lambda e: e.matmul(pq[:, 0:E], lhsT=x1T[b][:, c, :], rhs=wrt[:, c, :],
                                              start=(c == 0), stop=(c == 7)),
                     reads=[x1T[b], wrt], writes=[pq], inc=(c == 7))
            S.op("dve", lambda e: e.tensor_tensor(out=lg[b][:], in0=pq[:, 0:E], in1=brt[:], op=ALU.add),
                 reads=[pq, brt], writes=[lg[b]])
            S.op("dve", lambda e: e.max(out=top8[b][:], in_=lg[b][:]), reads=[lg[b]], writes=[top8[b]])
            S.op("dve", lambda e: e.tensor_scalar(out=mask[b][:], in0=lg[b][:], scalar1=top8[b][:, 3:4], scalar2=None,
                                                  op0=ALU.is_ge), reads=[lg[b], top8[b]], writes=[mask[b]])
            S.op("act", lambda e: e.mul(out=nmx[b][:], in_=top8[b][:, 0:1], mul=-1.0), reads=[top8[b]], writes=[nmx[b]])
            S.op("act", lambda e: e.activation(out=ex[b][:], in_=top8[b][:, 0:4], func=AF.Exp, bias=nmx[b][:],
                                               scale=1.0, accum_out=sm[b][:]),
                 reads=[top8[b], nmx[b]], writes=[ex[b], sm[b]])
            S.op("dve", lambda e: e.reciprocal(out=rs[b][:], in_=sm[b][:]), reads=[sm[b]], writes=[rs[b]])
            S.op("dve", lambda e: e.tensor_scalar(out=gate_all[:, i, :], in0=ex[b][:], scalar1=rs[b][:], scalar2=None,
                                                  op0=ALU.mult), reads=[ex[b], rs[b]], writes=[gate_all])
            S.op("pe", lambda e: e.matmul(pq[:, 32:32 + E], lhsT=triS[:], rhs=mask[b][:], start=True, stop=True),
                 reads=[triS, mask[b]], writes=[pq], inc=False)
            S.op("pe", lambda e: e.matmul(pq[:, 64:64 + E], lhsT=onesM[:], rhs=mask[b][:], start=True, stop=True),
                 reads=[onesM, mask[b]], writes=[pq])
            S.op("dve", lambda e: e.tensor_tensor(out=posf[b][:], in0=pq[:, 32:32 + E], in1=baseoffs[:], op=ALU.add),
                 reads=[pq, baseoffs], writes=[posf[b]])
            S.op("dve", lambda e: e.tensor_tensor(out=baseoffs[:], in0=pq[:, 64:64 + E], in1=baseoffs[:], op=ALU.add),
                 reads=[pq, baseoffs], writes=[baseoffs])
            for k in range(4):
                S.op("dve", lambda e: e.scalar_tensor_tensor(out=junk[b][:], in0=lg[b][:], scalar=top8[b][:, k:k + 1],
                                                             in1=posf[b][:], op0=ALU.is_equal, op1=ALU.mult,
                                                             accum_out=destf[b][:, k:k + 1]),
                     reads=[lg[b], top8[b], posf[b]], writes=[junk[b], destf[b]])
            S.op("dve", lambda e: e.tensor_copy(out=idx_all[:, i, :], in_=destf[b][:]), reads=[destf[b]], writes=[idx_all])
            for k in range(4):
                S.dma("pool", dr["xg"], x1b[b][:], reads=[x1b[b], idx_all], writes=[],
                      indirect=dict(out_offset=bass.IndirectOffsetOnAxis(ap=idx_all[:, i, k:k + 1], axis=0),
                                    in_offset=None))
        S.barrier()

    with ExitStack() as pctx:
        def sb(name, shape, dt):
            return Buf(pctx.enter_context(nc.sbuf_tensor(S.pfx + name, list(shape), dt)), name)
        xgr = [sb(f"xgr{i}", [128, 1024], BF16) for i in range(2)]
        xgT = sb("xgT", [128, 8, CAP], BF16)
        hT = sb("hT", [128, 8, CAP], BF16)
        tg = [sb(f"tg{i}", [128, 512], F32) for i in range(2)]
        tsg = [sb(f"tsg{i}", [128, 512], F32) for i in range(2)]
        tu = [sb(f"tu{i}", [128, 512], F32) for i in range(2)]
        yo = [sb(f"yo{i}", [128, 1024], F32) for i in range(2)]
        ptB, P = ps["ptB"], ps["P"]
        cnt = 0
        for e_ in range(E):
            wb = e_ % 2
            if e_ + 1 < E:
                load_expert(e_ + 1)
            for rb in range(RB):
                b = rb % 2
                r0 = e_ * CAP + rb * 128
                S.dma("sp", xgr[b][:], dr["xg"][r0:r0 + 128, :], writes=[xgr[b]])
                for c in range(8):
                    S.op("pe", lambda e: e.transpose(out=ptB[:, c * 128:(c + 1) * 128],
                                                     in_=xgr[b][:, c * 128:(c + 1) * 128], identity=identB[:]),
                         reads=[xgr[b], identB], writes=[ptB], inc=(c == 7))
                S.op("act", lambda e: e.copy(out=xgT[:, :, rb * 128:(rb + 1) * 128],
                                             in_=ptB[:].rearrange("p (c n) -> p c n", c=8)),
                     reads=[ptB], writes=[xgT])
            for fc in range(8):
                for (lo, n) in parts:
                    b = cnt % 2
                    cnt += 1
                    pg, pu = P[1 + b], P[3 + b]
                    for gi, pp in ((0, pg), (1, pu)):
                        for c in range(8):
                            S.op("pe", lambda e: e.matmul(pp[:, 0:n],
                                                          lhsT=wgu[wb][:, c, fc * 256 + gi:fc * 256 + 256:2],
                                                          rhs=xgT[:, c, lo:lo + n], start=(c == 0), stop=(c == 7)),
                                 reads=[wgu[wb], xgT], writes=[pp], inc=(c == 7))
                    S.op("dve", lambda e: e.tensor_scalar(out=tg[b][:, 0:n], in0=pg[:, 0:n], scalar1=bgu[wb][:, fc, 0:1],
                                                          scalar2=7.0, op0=ALU.add, op1=ALU.min),
                         reads=[pg, bgu[wb]], writes=[tg[b]])
                    S.op("act", lambda e: e.activation(out=tsg[b][:, 0:n], in_=tg[b][:, 0:n], func=AF.Sigmoid,
                                                       scale=1.702), reads=[tg[b]], writes=[tsg[b]])
                    S.op("dve", lambda e: e.tensor_scalar(out=tu[b][:, 0:n], in0=pu[:, 0:n], scalar1=bgu[wb][:, fc, 1:2],
                                                          scalar2=-7.0, op0=ALU.add, op1=ALU.max),
                         reads=[pu, bgu[wb]], writes=[tu[b]])
                    S.op("pool", lambda e: e.tensor_scalar(out=tu[b][:, 0:n], in0=tu[b][:, 0:n], scalar1=7.0, scalar2=1.0,
                                                           op0=ALU.min, op1=ALU.add), reads=[tu[b]], writes=[tu[b]])
                    S.op("pool", lambda e: e.tensor_tensor(out=tg[b][:, 0:n], in0=tg[b][:, 0:n], in1=tsg[b][:, 0:n],
                                                           op=ALU.mult), reads=[tg[b], tsg[b]], writes=[tg[b]])
                    S.op("dve", lambda e: e.tensor_tensor(out=hT[:, fc, lo:lo + n], in0=tg[b][:, 0:n], in1=tu[b][:, 0:n],
                                                          op=ALU.mult), reads=[tg[b], tu[b]], writes=[hT])
            for rb in range(RB):
                b = rb % 2
                for h in range(2):
                    py = P[5 + h]
                    for fc in range(8):
                        S.op("pe", lambda e: e.matmul(py[:], lhsT=hT[:, fc, rb * 128:(rb + 1) * 128],
                                                      rhs=wd[wb][:, fc, h * 512:(h + 1) * 512],
                                                      start=(fc == 0), stop=(fc == 7)),
                             reads=[hT, wd[wb]], writes=[py], inc=(fc == 7))
                    S.op("dve", lambda e: e.tensor_tensor(out=yo[b][:, h * 512:(h + 1) * 512], in0=py[:],
                                                          in1=bdn[wb][:, h * 512:(h + 1) * 512], op=ALU.add),
                         reads=[py, bdn[wb]], writes=[yo[b]])
                r0 = e_ * CAP + rb * 128
                S.dma("sp", dr["yg"][r0:r0 + 128, :], yo[b][:], reads=[yo[b]], writes=[yg_d])
        S.barrier()

    with ExitStack() as pctx:
        def sb(name, shape, dt):
            return Buf(pctx.enter_context(nc.sbuf_tensor(S.pfx + name, list(shape), dt)), name)
        xc = [sb(f"xc{i}", [128, 1024], F32) for i in range(2)]
        yk = [[sb(f"yk{i}_{k}", [128, 1024], F32) for k in range(4)] for i in range(2)]
        acc = [sb(f"acc{i}", [128, 1024], F32) for i in range(2)]
        x2 = [sb(f"x2{i}", [128, 1024], F32) for i in range(2)]
        for i in range(NT):
            b = i % 2
            sl = slice(i * 128, (i + 1) * 128)
            S.dma("sp", xc[b][:], dr["x1s"][sl, :], writes=[xc[b]])
            for k in range(4):
                S.dma("pool", yk[b][k][:], dr["yg"], reads=[idx_all], writes=[yk[b][k]],
                      indirect=dict(out_offset=None,
                                    in_offset=bass.IndirectOffsetOnAxis(ap=idx_all[:, i, k:k + 1], axis=0)))
            S.op("dve", lambda e: e.tensor_scalar(out=acc[b][:], in0=yk[b][0][:], scalar1=gate_all[:, i, 0:1],
                                                  scalar2=None, op0=ALU.mult),
                 reads=[yk[b][0], gate_all], writes=[acc[b]])
            for k in range(1, 4):
                S.op("dve", lambda e: e.scalar_tensor_tensor(out=acc[b][:], in0=yk[b][k][:], scalar=gate_all[:, i, k:k + 1],
                                                             in1=acc[b][:], op0=ALU.mult, op1=ALU.add),
                     reads=[yk[b][k], gate_all, acc[b]], writes=[acc[b]])
            S.op("dve", lambda e: e.scalar_tensor_tensor(out=acc[b][:], in0=xc[b][:], scalar=ALPHA, in1=acc[b][:],
                                                         op0=ALU.mult, op1=ALU.add),
                 reads=[xc[b], acc[b]], writes=[acc[b]])
            emit_ln(S, acc[b], lnp[2], lnp[3], x2[b], lntmp)
            S.dma("sp", dr["out"][sl, :], x2[b][:], reads=[x2[b]], writes=[out_d])
        S.barrier()


def build_k3(T, CAP):
    nc = bass.Bass("TRN2", target_bir_lowering=False)
    dr = {}
    def din(name, shape, dt=F32):
        dr[name] = nc.dram_tensor(name, list(shape), dt, kind="ExternalInput").ap()
    din("x", [T, D]); din("hg", [T, D], BF16)
    din("w_out", [D, D]); din("g1", [D]); din("b1", [D]); din("g2", [D]); din("b2", [D])
    din("w_rt", [D, E]); din("b_rt", [E]); din("w_gu", [E, D, 2 * D]); din("b_gu", [E, 2 * D])
    din("w_dn", [E, D, D]); din("b_dn", [E, D])
    dr["out"] = nc.dram_tensor("out", [T, D], F32, kind="ExternalOutput").ap()
    dr["xg"] = nc.dram_tensor("xg", [E * CAP, D], BF16, kind="Internal").ap()
    dr["yg"] = nc.dram_tensor("yg", [E * CAP, D], F32, kind="Internal").ap()
    dr["x1s"] = nc.dram_tensor("x1s", [T, D], F32, kind="Internal").ap()
    with ExitStack() as ctx:
        S = Sched(nc, ctx)
        ps = {"ptB": S.psum("ptB", [128, 1024], BF16), "P": [None] + [S.psum(f"P{i}", [128, 512], F32) for i in range(1, 8)]}
        emit_post(S, nc, T, CAP, dr, ps)
        pass
    return nc


def emit_mlstm(S, nc, SEQ, dr, ps):
    NT = SEQ // 128
    ptB, PA, PB, PC, PQ, PST, PN = ps["ptB"], ps["P"][1], ps["P"][2], ps["P"][3], ps["P"][4], ps["P"][5], ps["P"][6:8]
    identF = S.sbuf("identF", [128, 128], F32)
    identB = S.sbuf("identB", [128, 128], BF16)
    triI = S.sbuf("triI", [128, 128], F32)
    onesM = S.sbuf("onesM", [128, 128], F32)
    epsb = S.sbuf("epsb", [128, 1], F32)
    wfm = S.sbuf("wfm", [128, 8, 512], BF16)
    wtm = S.sbuf("wtm", [128, 8, 1288], BF16)
    bg = S.sbuf("bg", [128, 8], F32)
    gain = S.sbuf("gain_sb", [128, 512], F32)
    Cn = [S.sbuf(f"Cn{h}", [128, 132], F32) for h in range(4)]
    Cnb = [S.sbuf(f"Cnb{h}", [128, 132], BF16) for h in range(4)]
    out_d = Buf(None, "out")

    S.op("pool", lambda e: e.memset(identF[:], 0.0), writes=[identF])
    S.op("pool", lambda e: e.affine_select(out=identF[:], in_=identF[:], pattern=[[-1, 128]],
                                           compare_op=ALU.not_equal, fill=1.0, base=0,
                                           channel_multiplier=1), reads=[identF], writes=[identF])
    S.op("dve", lambda e: e.tensor_copy(out=identB[:], in_=identF[:]), reads=[identF], writes=[identB])
    S.op("pool", lambda e: e.memset(onesM[:], 1.0), writes=[onesM])
    S.op("pool", lambda e: e.memset(epsb[:], RMS_EPS), writes=[epsb])
    oneb = S.sbuf("oneb", [128, 1], F32)
    S.op("pool", lambda e: e.memset(oneb[:], 1.0), writes=[oneb])
    S.op("pool", lambda e: e.affine_select(out=triI[:], in_=onesM[:], pattern=[[1, 128]],
                                           compare_op=ALU.is_ge, fill=0.0, base=0,
                                           channel_multiplier=-1), reads=[onesM], writes=[triI])
    hm = S.sbuf("hm", [128, 2], F32)
    S.op("pool", lambda e: e.affine_select(out=hm[:, 0:1], in_=onesM[:, 0:1], pattern=[[0, 1]],
                                           compare_op=ALU.is_ge, fill=0.0, base=63,
                                           channel_multiplier=-1), reads=[onesM], writes=[hm])
    S.op("pool", lambda e: e.affine_select(out=hm[:, 1:2], in_=onesM[:, 0:1], pattern=[[0, 1]],
                                           compare_op=ALU.is_ge, fill=0.0, base=-64,
                                           channel_multiplier=1), reads=[onesM], writes=[hm])
    for h in range(4):
        S.op("pool", lambda e: e.memset(Cn[h][:], 0.0), writes=[Cn[h]])
        S.op("pool", lambda e: e.memset(Cnb[h][:], 0.0), writes=[Cnb[h]])
    for c in range(0, 8, 2):
        S.dma("pool", wfm[:, c:c + 2, :], dr["w_fm"][c * 128:(c + 2) * 128, :].rearrange("(c p) n -> p c n", p=128),
              writes=[wfm])
        S.dma("pool", wtm[:, c:c + 2, :], dr["w_tm"][c * 128:(c + 2) * 128, :].rearrange("(c p) n -> p c n", p=128),
              writes=[wtm])
    S.dma("sp", bg[:], dr["b_g"].partition_broadcast(128), writes=[bg])
    S.dma("sp", gain[:], dr["gain"].partition_broadcast(128), writes=[gain])

    def sb2(name, shape, dt):
        return [S.sbuf(f"{name}{i}", shape, dt) for i in range(2)]
    xt = sb2("xt", [128, 1024], F32)
    xb = sb2("xb", [128, 1024], BF16)
    xT = sb2("xT", [128, 8, 128], BF16)
    qTz = [[S.sbuf(f"qTz{i}_{h}", [128, 128], BF16) for h in range(4)] for i in range(2)]
    kT = sb2("kT", [128, 2, 128], BF16)
    gts = sb2("gts", [128, 8], F32)
    e1 = sb2("e1", [128, 4], F32)
    l1 = sb2("l1", [128, 4], F32)
    eq = sb2("eq", [128, 4], F32)
    ginb = sb2("ginb", [128, 4], F32)
    u = sb2("u", [128, 4], F32)
    ebl = sb2("ebl", [128, 4], F32)
    ktm = sb2("ktm", [128, 256], BF16)
    vx = sb2("vx", [128, 4, 132], BF16)
    og = sb2("og", [128, 512], F32)
    Sm = sb2("Sm", [128, 4, 128], BF16)
    dd = sb2("dd", [128, 4], F32)
    rr = sb2("rr", [128, 4], F32)
    fac = sb2("fac", [128, 4], F32)
    ss = sb2("ss", [128, 4], F32)
    rms = sb2("rms", [128, 4], F32)
    rinv = sb2("rinv", [128, 4], F32)
    fac2 = sb2("fac2", [128, 4], F32)
    sqj = sb2("sqj", [128, 128], F32)
    hn = sb2("hn", [128, 512], F32)
    hgo = sb2("hgo", [128, 512], BF16)
    for i in range(2):
        for h in range(4):
            S.op("pool", lambda e: e.memset(qTz[i][h][:], 0.0), writes=[qTz[i][h]])

    for t in range(NT):
        b = t % 2
        sl = slice(t * 128, (t + 1) * 128)
        S.dma("sp", xt[b][:], (dr["xmap"](t) if "xmap" in dr else dr["x"][sl, :]), writes=[xt[b]])
        S.op("act", lambda e: e.copy(out=xb[b][:], in_=xt[b][:]), reads=[xt[b]], writes=[xb[b]])
        for c in range(8):
            S.op("pe", lambda e: e.transpose(out=ptB[:, c * 128:(c + 1) * 128], in_=xb[b][:, c * 128:(c + 1) * 128],
                                             identity=identB[:]), reads=[xb[b], identB], writes=[ptB], inc=(c == 7))
        S.op("dve", lambda e: e.tensor_copy(out=xT[b][:], in_=ptB[:].rearrange("p (c n) -> p c n", c=8)),
             reads=[ptB], writes=[xT[b]])
        if STAGE < 1:
            continue
        for j in range(4):
            for c in range(8):
                S.op("pe", lambda e: e.matmul(PQ[:, j * 128:(j + 1) * 128], lhsT=wfm[:, c, j * 128:(j + 1) * 128],
                                              rhs=xT[b][:, c, :], start=(c == 0), stop=(c == 7)),
                     reads=[wfm, xT[b]], writes=[PQ], inc=(c == 7))
        for h in range(4):
            hb, jj = h // 2, h % 2
            S.op("dve", lambda e: e.tensor_scalar(out=qTz[b][h][:], in0=PQ[:, hb * 128:(hb + 1) * 128],
                                                  scalar1=hm[:, jj:jj + 1], scalar2=0.125, op0=ALU.mult, op1=ALU.mult),
                 reads=[PQ, hm], writes=[qTz[b][h]])
        S.op("dve", lambda e: e.tensor_copy(out=kT[b][:], in_=PQ[:, 256:512].rearrange("p (c n) -> p c n", c=2)),
             reads=[PQ], writes=[kT[b]])
        if STAGE < 2:
            continue
        for (pp, lo, n) in ((PA, 0, 264), (PB, 264, 512), (PC, 776, 512))[:SUB]:
            for c in range(8):
                S.op("pe", lambda e: e.matmul(pp[:, 0:n], lhsT=xT[b][:, c, :], rhs=wtm[:, c, lo:lo + n],
                                              start=(c == 0), stop=(c == 7)),
                     reads=[wtm, xT[b]], writes=[pp], inc=(c == 7))
        if SUB < 4:
            continue
        S.op("dve", lambda e: e.tensor_tensor(out=gts[b][:], in0=PA[:, 256:264], in1=bg[:], op=ALU.add),
             reads=[PA, bg], writes=[gts[b]])
        if SUB < 5:
            continue
        S.op("dve", lambda e: e.tensor_copy(out=ktm[b][:], in_=PA[:, 0:256]), reads=[PA], writes=[ktm[b]])
        if SUB < 6:
            continue
        S.op("act", lambda e: e.activation(out=e1[b][:], in_=gts[b][:, 4:8], func=AF.Exp, scale=-1.0),
             reads=[gts[b]], writes=[e1[b]])
        S.op("act", lambda e: e.activation(out=l1[b][:], in_=e1[b][:], func=AF.Ln, bias=oneb[:], scale=1.0),
             reads=[e1[b], oneb], writes=[l1[b]])
        if STAGE < 3:
            continue
        S.op("pe", lambda e: e.matmul(PA[:, 272:276], lhsT=triI[:], rhs=l1[b][:], start=True, stop=True),
             reads=[triI, l1[b]], writes=[PA], inc=False)
        S.op("pe", lambda e: e.matmul(PA[:, 280:284], lhsT=onesM[:], rhs=l1[b][:], start=True, stop=True),
             reads=[onesM, l1[b]], writes=[PA])
        S.op("act", lambda e: e.activation(out=eq[b][:], in_=PA[:, 272:276], func=AF.Exp, scale=-1.0),
             reads=[PA], writes=[eq[b]])
        S.op("dve", lambda e: e.tensor_tensor(out=ginb[b][:], in0=PA[:, 272:276], in1=gts[b][:, 0:4], op=ALU.add),
             reads=[PA, gts[b]], writes=[ginb[b]])
        S.op("act", lambda e: e.activation(out=u[b][:], in_=ginb[b][:], func=AF.Exp), reads=[ginb[b]], writes=[u[b]])
        S.op("act", lambda e: e.activation(out=ebl[b][:], in_=PA[:, 280:284], func=AF.Exp, scale=-1.0),
             reads=[PA], writes=[ebl[b]])
        for h in range(4):
            S.op("dve", lambda e: e.tensor_scalar(out=vx[b][:, h, 0:128], in0=PB[:, h * 128:(h + 1) * 128],
                                                  scalar1=u[b][:, h:h + 1], scalar2=None, op0=ALU.mult),
                 reads=[PB, u[b]], writes=[vx[b]])
        S.op("dve", lambda e: e.tensor_copy(out=vx[b][:, :, 128], in_=u[b][:]), reads=[u[b]], writes=[vx[b]])
        S.op("act", lambda e: e.activation(out=og[b][:], in_=PC[:], func=AF.Sigmoid), reads=[PC], writes=[og[b]])
        if STAGE < 4:
            continue
        for h in range(4):
            hb = h // 2
            S.op("pe", lambda e: e.matmul(PST[:, h * 128:(h + 1) * 128], lhsT=kT[b][:, hb, :], rhs=qTz[b][h][:],
                                          start=True, stop=True),
                 reads=[kT[b], qTz[b][h]], writes=[PST], inc=(h == 3))
        for h in range(4):
            S.op("dve", lambda e: e.tensor_tensor(out=Sm[b][:, h, :], in0=PST[:, h * 128:(h + 1) * 128], in1=triI[:],
                                                  op=ALU.mult), reads=[PST, triI], writes=[Sm[b]])
        for h in range(4):
            hb, jj = h // 2, h % 2
            pn = PN[hb]
            S.op("pe", lambda e: e.matmul(pn[:, jj * 129:(jj + 1) * 129], lhsT=Sm[b][:, h, :], rhs=vx[b][:, h, 0:129],
                                          start=True, stop=False),
                 reads=[Sm[b], vx[b]], writes=[pn], inc=False)
            S.op("pe", lambda e: e.matmul(pn[:, jj * 129:(jj + 1) * 129], lhsT=qTz[b][h][:], rhs=Cnb[h][:, 0:129],
                                          start=False, stop=True),
                 reads=[qTz[b][h], Cnb[h]], writes=[pn])
        if STAGE < 5:
            continue
        for h in range(4):
            hb, jj = h // 2, h % 2
            R = slice(0, 128)
            pd = PQ if hb == 0 else PST
            S.op("pe", lambda e: e.matmul(pd[:, jj * 129:(jj + 1) * 129], lhsT=ktm[b][:, hb * 128:(hb + 1) * 128],
                                          rhs=vx[b][:, h, 0:129], start=True, stop=True),
                 reads=[ktm[b], vx[b]], writes=[pd])
            S.op("dve", lambda e: e.tensor_tensor(out=Cn[h][R, 0:129], in0=pd[R, jj * 129:(jj + 1) * 129], in1=Cn[h][R, 0:129],
                                                  op=ALU.add), reads=[pd, Cn[h]], writes=[Cn[h]])
            S.op("act", lambda e: e.activation(out=Cn[h][R, 0:129], in_=Cn[h][R, 0:129], func=AF.Identity,
                                               scale=ebl[b][R, h:h + 1]), reads=[Cn[h], ebl[b]], writes=[Cn[h]])
            S.op("pool", lambda e: e.tensor_copy(out=Cnb[h][R, 0:129], in_=Cn[h][R, 0:129]), reads=[Cn[h]], writes=[Cnb[h]])
        if STAGE < 6:
            continue
        for h in range(4):
            hb, jj = h // 2, h % 2
            pn = PN[hb]
            c0 = jj * 129
            hs = slice(h, h + 1)
            S.op("act", lambda e: e.activation(out=dd[b][:, hs], in_=pn[:, c0 + 128:c0 + 129], func=AF.Abs,
                                               scale=eq[b][:, hs]), reads=[pn, eq[b]], writes=[dd[b]])
            S.op("dve", lambda e: e.tensor_scalar(out=dd[b][:, hs], in0=dd[b][:, hs], scalar1=1.0, scalar2=None,
                                                  op0=ALU.max), reads=[dd[b]], writes=[dd[b]])
            S.op("dve", lambda e: e.reciprocal(out=rr[b][:, hs], in_=dd[b][:, hs]), reads=[dd[b]], writes=[rr[b]])
            S.op("dve", lambda e: e.tensor_tensor(out=fac[b][:, hs], in0=rr[b][:, hs], in1=eq[b][:, hs], op=ALU.mult),
                 reads=[rr[b], eq[b]], writes=[fac[b]])
            S.op("act", lambda e: e.activation(out=sqj[b][:], in_=pn[:, c0:c0 + 128], func=AF.Square,
                                               scale=fac[b][:, hs], accum_out=ss[b][:, hs]),
                 reads=[pn, fac[b]], writes=[sqj[b], ss[b]])
            S.op("act", lambda e: e.activation(out=rms[b][:, hs], in_=ss[b][:, hs], func=AF.Sqrt, bias=epsb[:],
                                               scale=1.0 / 128.0), reads=[ss[b], epsb], writes=[rms[b]])
            S.op("dve", lambda e: e.reciprocal(out=rinv[b][:, hs], in_=rms[b][:, hs]), reads=[rms[b]], writes=[rinv[b]])
            S.op("dve", lambda e: e.tensor_tensor(out=fac2[b][:, hs], in0=fac[b][:, hs], in1=rinv[b][:, hs], op=ALU.mult),
                 reads=[fac[b], rinv[b]], writes=[fac2[b]])
            S.op("dve", lambda e: e.scalar_tensor_tensor(out=hn[b][:, h * 128:(h + 1) * 128], in0=pn[:, c0:c0 + 128],
                                                         scalar=fac2[b][:, hs], in1=gain[:, h * 128:(h + 1) * 128],
                                                         op0=ALU.mult, op1=ALU.mult),
                 reads=[pn, fac2[b], gain], writes=[hn[b]])
        S.op("pool", lambda e: e.tensor_tensor(out=hgo[b][:], in0=hn[b][:], in1=og[b][:], op=ALU.mult),
             reads=[hn[b], og[b]], writes=[hgo[b]])
        S.dma("sp", dr["out"][sl, :], hgo[b][:], reads=[hgo[b]], writes=[out_d])
    S.barrier()


def build_k1(SEQ):
    nc = bass.Bass("TRN2", target_bir_lowering=False)
    dr = {}
    def din(name, shape, dt=F32):
        dr[name] = nc.dram_tensor(name, list(shape), dt, kind="ExternalInput").ap()
    din("x", [SEQ, D]); din("w_fm", [D, 512]); din("w_tm", [D, 1288]); din("b_g", [8]); din("gain", [512])
    dr["out"] = nc.dram_tensor("out", [SEQ, 512], BF16, kind="ExternalOutput").ap()
    with ExitStack() as ctx:
        S = Sched(nc, ctx)
        ps = {"ptB": S.psum("ptB", [128, 1024], BF16), "P": [None] + [S.psum(f"P{i}", [128, 512], F32) for i in range(1, 8)]}
        emit_mlstm(S, nc, SEQ, dr, ps)
        pass
    return nc


def mlstm_inputs(x_b, w_in, b_gates, norm_gain, g):
    H, dk, dv = 8, 64, 128
    hs = slice(g * 4, g * 4 + 4)
    wq = w_in[:, 0:512].reshape(D, H, dk)[:, hs].reshape(D, 256)
    wk = w_in[:, 512:1024].reshape(D, H, dk)[:, hs].reshape(D, 256)
    wv = w_in[:, 1024:2048].reshape(D, H, dv)[:, hs].reshape(D, 512)
    wo = w_in[:, 2048:3072].reshape(D, H, dv)[:, hs].reshape(D, 512)
    wgi = w_in[:, 3072:3080][:, hs]
    wgf = w_in[:, 3080:3088][:, hs]
    w_fm = np.ascontiguousarray(np.concatenate([wq, wk], axis=1))
    w_tm = np.ascontiguousarray(np.concatenate([wk, wgi, wgf, wv, wo], axis=1))
    b_g = np.ascontiguousarray(np.concatenate([b_gates[0:8][hs], b_gates[8:16][hs]]))
    gain = np.ascontiguousarray(norm_gain.reshape(H, dv)[hs].reshape(512))
    return {"x": np.ascontiguousarray(x_b), "w_fm": w_fm, "w_tm": w_tm, "b_g": b_g, "gain": gain}


def emit_mla(S, nc, SEQ, dr, ps):
    NT = SEQ // 128
    ptB, ptT = ps["ptB"], ps["ptT"]
    P = ps["P"]
    identF = S.sbuf("identF", [128, 128], F32)
    identB = S.sbuf("identB", [128, 128], BF16)
    onesM = S.sbuf("onesM", [128, 128], F32)
    negF = S.sbuf("negF", [128, 128], F32)
    NEG = S.sbuf("NEG", [128, 128], BF16)
    hm = S.sbuf("hm", [128, 2], F32)
    epsb = S.sbuf("epsb", [128, 1], F32)
    wqn = S.sbuf("wqn", [128, 3, 512], BF16)
    cqT_all = S.sbuf("cqT_all", [128, 3, SEQ], BF16)
    krT2 = S.sbuf("krT2", [128, SEQ], BF16)
    qrT_all = S.sbuf("qrT_all", [128, 2, SEQ], BF16)
    out_d = Buf(None, "out")
    knT_dd = Buf(None, "knT_d")
    v_dd = Buf(None, "v_d")

    S.op("pool", lambda e: e.memset(identF[:], 0.0), writes=[identF])
    S.op("pool", lambda e: e.affine_select(out=identF[:], in_=identF[:], pattern=[[-1, 128]],
                                           compare_op=ALU.not_equal, fill=1.0, base=0,
                                           channel_multiplier=1), reads=[identF], writes=[identF])
    S.op("dve", lambda e: e.tensor_copy(out=identB[:], in_=identF[:]), reads=[identF], writes=[identB])
    S.op("pool", lambda e: e.memset(onesM[:], 1.0), writes=[onesM])
    S.op("pool", lambda e: e.memset(epsb[:], RMS_EPS), writes=[epsb])
    S.op("pool", lambda e: e.memset(negF[:], -30000.0), writes=[negF])
    S.op("pool", lambda e: e.affine_select(out=negF[:], in_=negF[:], pattern=[[1, 128]],
                                           compare_op=ALU.is_gt, fill=0.0, base=0,
                                           channel_multiplier=-1), reads=[negF], writes=[negF])
    S.op("dve", lambda e: e.tensor_copy(out=NEG[:], in_=negF[:]), reads=[negF], writes=[NEG])
    S.op("pool", lambda e: e.affine_select(out=hm[:, 0:1], in_=onesM[:, 0:1], pattern=[[0, 1]],
                                           compare_op=ALU.is_ge, fill=0.0, base=63,
                                           channel_multiplier=-1), reads=[onesM], writes=[hm])
    S.op("pool", lambda e: e.affine_select(out=hm[:, 1:2], in_=onesM[:, 0:1], pattern=[[0, 1]],
                                           compare_op=ALU.is_ge, fill=0.0, base=-64,
                                           channel_multiplier=1), reads=[onesM], writes=[hm])
    S.dma("pool", wqn[:], dr["wqn"].rearrange("(c p) n -> p c n", p=128), writes=[wqn])

    with ExitStack() as pctx:
        def sb(name, shape, dt):
            return Buf(pctx.enter_context(nc.sbuf_tensor(S.pfx + name, list(shape), dt)), name)
        def sb2(name, shape, dt):
            return [sb(f"{name}{i}", shape, dt) for i in range(2)]
        win = sb("win", [128, 8, 704], BF16)
        wqr = sb("wqr", [128, 3, 256], BF16)
        wkn = sb("wkn", [128, 2, 512], BF16)
        wkv = sb("wkv", [128, 2, 512], BF16)
        qn_t = sb("qn_t", [128, 384], F32)
        kvn_t = sb("kvn_t", [128, 256], F32)
        cosT = sb("cosT", [128, NT, 32], F32)
        sinT = sb("sinT", [128, NT, 32], F32)
        for c in range(0, 8, 4):
            S.dma("pool", win[:, c:c + 4, :], dr["w_in"][c * 128:(c + 4) * 128, :].rearrange("(c p) n -> p c n", p=128),
                  writes=[win])
        S.dma("pool", wqr[:], dr["wqr"].rearrange("(c p) n -> p c n", p=128), writes=[wqr])
        S.dma("pool", wkn[:], dr["wkn"].rearrange("(c p) n -> p c n", p=128), writes=[wkn])
        S.dma("pool", wkv[:], dr["wkv"].rearrange("(c p) n -> p c n", p=128), writes=[wkv])
        S.dma("sp", qn_t[:], dr["qn"].partition_broadcast(128), writes=[qn_t])
        S.dma("sp", kvn_t[:], dr["kvn"].partition_broadcast(128), writes=[kvn_t])

        with ExitStack() as tctx:
            def tb(name, shape, dt):
                return Buf(tctx.enter_context(nc.sbuf_tensor(S.pfx + name, list(shape), dt)), name)
            posi = tb("posi", [128, NT], I32)
            posf = tb("posf", [128, NT], F32)
            iof = tb("iof", [128, 32], F32)
            invf = tb("invf", [128, 32], F32)
            rr = tb("rr", [128, NT, 32], F32)
            ri = tb("ri", [128, NT * 32], I32)
            rf = tb("rf", [128, NT * 32], F32)
            ff = tb("ff", [128, NT * 32], F32)
            mk = tb("mk", [128, NT * 32], F32)
            S.dma("sp", posi[:], dr["pos"], writes=[posi])
            S.op("dve", lambda e: e.tensor_copy(out=posf[:], in_=posi[:]), reads=[posi], writes=[posf])
            S.op("pool", lambda e: e.iota(iof[:], pattern=[[1, 32]], base=0, channel_multiplier=0,
                                          allow_small_or_imprecise_dtypes=True), writes=[iof])
            S.op("act", lambda e: e.activation(out=invf[:], in_=iof[:], func=AF.Exp, scale=-math.log(10000.0) / 32.0),
                 reads=[iof], writes=[invf])
            S.op("dve", lambda e: e.tensor_scalar(out=invf[:], in0=invf[:], scalar1=1.0 / (2.0 * math.pi), scalar2=None,
                                                  op0=ALU.mult), reads=[invf], writes=[invf])
            for t in range(NT):
                S.op("dve", lambda e: e.tensor_scalar(out=rr[:, t, :], in0=invf[:], scalar1=posf[:, t:t + 1], scalar2=None,
                                                      op0=ALU.mult), reads=[invf, posf], writes=[rr])
            rrf = rr[:].rearrange("p t i -> p (t i)")
            for (shift, outT) in ((0.0, sinT), (0.25, cosT)):
                if shift != 0.0:
                    S.op("dve", lambda e: e.tensor_scalar(out=rrf, in0=rrf, scalar1=shift, scalar2=None, op0=ALU.add),
                         reads=[rr], writes=[rr])
                S.op("dve", lambda e: e.tensor_copy(out=ri[:], in_=rrf), reads=[rr], writes=[ri])
                S.op("dve", lambda e: e.tensor_copy(out=rf[:], in_=ri[:]), reads=[ri], writes=[rf])
                S.op("dve", lambda e: e.tensor_tensor(out=ff[:], in0=rrf, in1=rf[:], op=ALU.subtract),
                     reads=[rr, rf], writes=[ff])
                S.op("dve", lambda e: e.tensor_scalar(out=mk[:], in0=ff[:], scalar1=0.5, scalar2=None, op0=ALU.is_gt),
                     reads=[ff], writes=[mk])
                S.op("dve", lambda e: e.tensor_tensor(out=ff[:], in0=ff[:], in1=mk[:], op=ALU.subtract),
                     reads=[ff, mk], writes=[ff])
                S.op("dve", lambda e: e.tensor_scalar(out=mk[:], in0=ff[:], scalar1=-0.5, scalar2=None, op0=ALU.is_lt),
                     reads=[ff], writes=[mk])
                S.op("dve", lambda e: e.tensor_tensor(out=ff[:], in0=ff[:], in1=mk[:], op=ALU.add),
                     reads=[ff, mk], writes=[ff])
                S.op("dve", lambda e: e.tensor_scalar(out=ff[:], in0=ff[:], scalar1=-0.49999, scalar2=0.49999,
                                                      op0=ALU.max, op1=ALU.min), reads=[ff], writes=[ff])
                S.op("act", lambda e: e.activation(out=outT[:].rearrange("p t i -> p (t i)"), in_=ff[:], func=AF.Sin,
                                                   scale=2.0 * math.pi), reads=[ff], writes=[outT])
            S.barrier()

        xt = sb2("xt", [128, 1024], F32)
        xb = sb2("xb", [128, 1024], BF16)
        xT = sb2("xT", [128, 8, 128], BF16)
        junk = sb2("junk", [128, 384], F32)
        ssq = sb2("ssq", [128, 2], F32)
        rms = sb2("rms", [128, 2], F32)
        rinv = sb2("rinv", [128, 2], F32)
        ckv = sb2("ckv", [128, 256], BF16)
        cq = sb2("cq", [128, 384], BF16)
        kr2 = sb2("kr2", [128, 128], BF16)
        rt = [sb2(f"rt{k}", [128, 32], F32) for k in range(4)]
        qrt = [sb2(f"qrt{k}", [128, 4, 32], F32) for k in range(4)]
        qr = sb2("qr", [128, 4, 64], BF16)
        stg1 = sb2("stg1", [128, 6, 128], BF16)
        stg2 = sb2("stg2", [128, 2, 128], BF16)
        knT_sb = sb2("knT_sb", [128, 4, 128], BF16)
        v_sb = sb2("v_sb", [128, 512], BF16)
        PA1, PA2, PQR, PK, PV = P[1], P[2], P[3], P[4], P[5]

        for t in range(NT):
            if SUBA < 1:
                continue
            b = t % 2
            sl = slice(t * 128, (t + 1) * 128)
            S.dma("sp", xt[b][:], (dr["xmap"](t) if "xmap" in dr else dr["x"][sl, :]), writes=[xt[b]])
            S.op("act", lambda e: e.copy(out=xb[b][:], in_=xt[b][:]), reads=[xt[b]], writes=[xb[b]])
            for c in range(8):
                S.op("pe", lambda e: e.transpose(out=ptB[:, c * 128:(c + 1) * 128], in_=xb[b][:, c * 128:(c + 1) * 128],
                                                 identity=identB[:]), reads=[xb[b], identB], writes=[ptB], inc=(c == 7))
            S.op("dve", lambda e: e.tensor_copy(out=xT[b][:], in_=ptB[:].rearrange("p (c n) -> p c n", c=8)),
                 reads=[ptB], writes=[xT[b]])
            for (pp, lo, n) in ((PA1, 0, 320), (PA2, 320, 384)):
                for c in range(8):
                    S.op("pe", lambda e: e.matmul(pp[:, 0:n], lhsT=xT[b][:, c, :], rhs=win[:, c, lo:lo + n],
                                                  start=(c == 0), stop=(c == 7)),
                         reads=[win, xT[b]], writes=[pp], inc=(c == 7))
            S.op("act", lambda e: e.activation(out=junk[b][:, 0:256], in_=PA1[:, 0:256], func=AF.Square,
                                               accum_out=ssq[b][:, 0:1]), reads=[PA1], writes=[junk[b], ssq[b]])
            S.op("act", lambda e: e.activation(out=junk[b][:, 0:384], in_=PA2[:, 0:384], func=AF.Square,
                                               accum_out=ssq[b][:, 1:2]), reads=[PA2], writes=[junk[b], ssq[b]])
            S.op("act", lambda e: e.activation(out=rms[b][:, 0:1], in_=ssq[b][:, 0:1], func=AF.Sqrt, bias=epsb[:],
                                               scale=1.0 / 256.0), reads=[ssq[b], epsb], writes=[rms[b]])
            S.op("act", lambda e: e.activation(out=rms[b][:, 1:2], in_=ssq[b][:, 1:2], func=AF.Sqrt, bias=epsb[:],
                                               scale=1.0 / 384.0), reads=[ssq[b], epsb], writes=[rms[b]])
            S.op("dve", lambda e: e.reciprocal(out=rinv[b][:], in_=rms[b][:]), reads=[rms[b]], writes=[rinv[b]])
            S.op("dve", lambda e: e.scalar_tensor_tensor(out=ckv[b][:], in0=PA1[:, 0:256], scalar=rinv[b][:, 0:1],
                                                         in1=kvn_t[:], op0=ALU.mult, op1=ALU.mult),
                 reads=[PA1, rinv[b], kvn_t], writes=[ckv[b]])
            S.op("dve", lambda e: e.scalar_tensor_tensor(out=cq[b][:], in0=PA2[:, 0:384], scalar=rinv[b][:, 1:2],
                                                         in1=qn_t[:], op0=ALU.mult, op1=ALU.mult),
                 reads=[PA2, rinv[b], qn_t], writes=[cq[b]])
            if SUBA < 2:
                continue
            cs, sn = cosT[:, t, :], sinT[:, t, :]
            x1, x2 = PA1[:, 256:288], PA1[:, 288:320]
            S.op("dve", lambda e: e.tensor_tensor(out=rt[0][b][:], in0=x1, in1=cs, op=ALU.mult), reads=[PA1, cosT], writes=[rt[0][b]])
            S.op("dve", lambda e: e.tensor_tensor(out=rt[1][b][:], in0=x2, in1=sn, op=ALU.mult), reads=[PA1, sinT], writes=[rt[1][b]])
            S.op("dve", lambda e: e.tensor_tensor(out=rt[2][b][:], in0=x2, in1=cs, op=ALU.mult), reads=[PA1, cosT], writes=[rt[2][b]])
            S.op("dve", lambda e: e.tensor_tensor(out=rt[3][b][:], in0=x1, in1=sn, op=ALU.mult), reads=[PA1, sinT], writes=[rt[3][b]])
            if SUBB < 2:
                continue
            for off in (0, 64):
                S.op("pool", lambda e: e.tensor_tensor(out=kr2[b][:, off:off + 32], in0=rt[0][b][:], in1=rt[1][b][:],
                                                       op=ALU.subtract), reads=[rt[0][b], rt[1][b]], writes=[kr2[b]])
                S.op("pool", lambda e: e.tensor_tensor(out=kr2[b][:, off + 32:off + 64], in0=rt[2][b][:], in1=rt[3][b][:],
                                                       op=ALU.add), reads=[rt[2][b], rt[3][b]], writes=[kr2[b]])
            if SUBB < 3:
                continue
            srcs = [(cq[b], k * 128) for k in range(3)] + [(ckv[b], k * 128) for k in range(2)] + [(kr2[b], 0)]
            for k, (src, o0) in enumerate(srcs):
                S.op("pe", lambda e: e.transpose(out=ptT[:, k * 128:(k + 1) * 128], in_=src[:, o0:o0 + 128], identity=identB[:]),
                     reads=[src, identB], writes=[ptT], inc=(k == 5))
            if SUBB < 4:
                continue
            S.op("act", lambda e: e.copy(out=stg1[b][:], in_=ptT[:, 0:768].rearrange("p (c n) -> p c n", c=6)),
                 reads=[ptT], writes=[stg1[b]])
            for c in range(3):
                S.op("pool", lambda e: e.tensor_copy(out=cqT_all[:, c, sl], in_=stg1[b][:, c, :]),
                     reads=[stg1[b]], writes=[cqT_all])
            S.op("pool", lambda e: e.tensor_copy(out=krT2[:, sl], in_=stg1[b][:, 5, :]), reads=[stg1[b]], writes=[krT2])
            if SUBA < 3:
                continue
            for c in range(3):
                S.op("pe", lambda e: e.matmul(PQR[:, 0:256], lhsT=stg1[b][:, c, :], rhs=wqr[:, c, :],
                                              start=(c == 0), stop=(c == 2)),
                     reads=[stg1[b], wqr], writes=[PQR], inc=(c == 2))
            for h in range(4):
                q1, q2 = PQR[:, h * 64:h * 64 + 32], PQR[:, h * 64 + 32:h * 64 + 64]
                S.op("dve", lambda e: e.tensor_tensor(out=qrt[0][b][:, h, :], in0=q1, in1=cs, op=ALU.mult), reads=[PQR, cosT], writes=[qrt[0][b]])
                S.op("dve", lambda e: e.tensor_tensor(out=qrt[1][b][:, h, :], in0=q2, in1=sn, op=ALU.mult), reads=[PQR, sinT], writes=[qrt[1][b]])
                S.op("dve", lambda e: e.tensor_tensor(out=qrt[2][b][:, h, :], in0=q2, in1=cs, op=ALU.mult), reads=[PQR, cosT], writes=[qrt[2][b]])
                S.op("dve", lambda e: e.tensor_tensor(out=qrt[3][b][:, h, :], in0=q1, in1=sn, op=ALU.mult), reads=[PQR, sinT], writes=[qrt[3][b]])
            S.op("pool", lambda e: e.tensor_tensor(out=qr[b][:, :, 0:32], in0=qrt[0][b][:], in1=qrt[1][b][:], op=ALU.subtract),
                 reads=[qrt[0][b], qrt[1][b]], writes=[qr[b]])
            S.op("pool", lambda e: e.tensor_tensor(out=qr[b][:, :, 32:64], in0=qrt[2][b][:], in1=qrt[3][b][:], op=ALU.add),
                 reads=[qrt[2][b], qrt[3][b]], writes=[qr[b]])
            qrf = qr[b][:].rearrange("p h d -> p (h d)")
            for k in range(2):
                S.op("pe", lambda e: e.transpose(out=ptT[:, 768 + k * 128:768 + (k + 1) * 128], in_=qrf[:, k * 128:(k + 1) * 128],
                                                 identity=identB[:]), reads=[qr[b], identB], writes=[ptT], inc=(k == 1))
            S.op("act", lambda e: e.copy(out=stg2[b][:], in_=ptT[:, 768:1024].rearrange("p (c n) -> p c n", c=2)),
                 reads=[ptT], writes=[stg2[b]])
            for c in range(2):
                S.op("pool", lambda e: e.tensor_copy(out=qrT_all[:, c, sl], in_=stg2[b][:, c, :]),
                     reads=[stg2[b]], writes=[qrT_all])
            if SUBA < 4:
                continue
            for h in range(4):
                for c in range(2):
                    S.op("pe", lambda e: e.matmul(PK[:, h * 128:(h + 1) * 128], lhsT=wkn[:, c, h * 128:(h + 1) * 128],
                                                  rhs=stg1[b][:, 3 + c, :], start=(c == 0), stop=(c == 1)),
                         reads=[wkn, stg1[b]], writes=[PK], inc=(c == 1))
            S.op("dve", lambda e: e.tensor_copy(out=knT_sb[b][:], in_=PK[:].rearrange("p (h n) -> p h n", h=4)),
                 reads=[PK], writes=[knT_sb[b]])
            S.dma("sp", dr["knT_d"][:, :, sl].rearrange("h p n -> p h n"), knT_sb[b][:], reads=[knT_sb[b]], writes=[knT_dd])
            for c in range(2):
                S.op("pe", lambda e: e.matmul(PV[:], lhsT=stg1[b][:, 3 + c, :], rhs=wkv[:, c, :], start=(c == 0), stop=(c == 1)),
                     reads=[wkv, stg1[b]], writes=[PV], inc=(c == 1))
            S.op("dve", lambda e: e.tensor_copy(out=v_sb[b][:], in_=PV[:]), reads=[PV], writes=[v_sb[b]])
            S.dma("sp", dr["v_d"][sl, :], v_sb[b][:], reads=[v_sb[b]], writes=[v_dd])
        S.barrier()

    if STAGE < 2:
        return
    with ExitStack() as pctx:
        def sb(name, shape, dt):
            return Buf(pctx.enter_context(nc.sbuf_tensor(S.pfx + name, list(shape), dt)), name)
        def sb2(name, shape, dt):
            return [sb(f"{name}{i}", shape, dt) for i in range(2)]
        knT = sb("knT", [128, SEQ], BF16)
        vh = sb("vh", [128, NT, 128], BF16)
        sc = sb("sc", [128, SEQ], F32)
        PT = sb("PT", [128, NT, 128], BF16)
        qnT = sb2("qnT", [128, 128], BF16)
        qrz = sb2("qrz", [128, 128], BF16)
        mx = sb2("mx", [128, 16], F32)
        mrow = sb2("mrow", [128, 1], F32)
        negm = sb2("negm", [128, 1], F32)
        rsum = sb2("rsum", [128, 1], F32)
        rinv2 = sb2("rinv2", [128, 1], F32)
        osb = sb2("osb", [128, 128], BF16)
        PQN = P[1]
        PS = [P[2], P[3]]
        PO = [P[4], P[5]]
        ptP = [(P[6], P[6][:]), (ptT, ptT[:].bitcast(F32))]
        it = 0
        for h in range(4):
            hb, jj = h // 2, h % 2
            S.dma("sp", knT[:], dr["knT_d"][h, :, :], writes=[knT])
            for t0 in range(0, NT, 8):
                t1 = min(NT, t0 + 8)
                S.dma("sp", vh[:, t0:t1, :],
                      dr["v_d"][t0 * 128:t1 * 128, h * 128:(h + 1) * 128].rearrange("(t p) d -> p t d", p=128),
                      writes=[vh])
            for qb in range(NT):
                b = it % 2
                it += 1
                qs = slice(qb * 128, (qb + 1) * 128)
                nkeys = (qb + 1) * 128
                for c in range(3):
                    S.op("pe", lambda e: e.matmul(PQN[:, 0:128], lhsT=wqn[:, c, h * 128:(h + 1) * 128], rhs=cqT_all[:, c, qs],
                                                  start=(c == 0), stop=(c == 2)),
                         reads=[wqn, cqT_all], writes=[PQN], inc=(c == 2))
                S.op("dve", lambda e: e.tensor_copy(out=qnT[b][:], in_=PQN[:, 0:128]), reads=[PQN], writes=[qnT[b]])
                S.op("pool", lambda e: e.tensor_scalar(out=qrz[b][:], in0=qrT_all[:, hb, qs], scalar1=hm[:, jj:jj + 1],
                                                       scalar2=None, op0=ALU.mult), reads=[qrT_all, hm], writes=[qrz[b]])
                nch = (nkeys + 511) // 512
                for kc in range(nch):
                    k0 = kc * 512
                    n = min(512, nkeys - k0)
                    pp = PS[kc % 2]
                    last = (kc == nch - 1)
                    S.op("pe", lambda e: e.matmul(pp[:, 0:n], lhsT=qnT[b][:], rhs=knT[:, k0:k0 + n], start=True, stop=False),
                         reads=[qnT[b], knT], writes=[pp], inc=False)
                    S.op("pe", lambda e: e.matmul(pp[:, 0:n], lhsT=qrz[b][:], rhs=krT2[:, k0:k0 + n], start=False, stop=(not last)),
                         reads=[qrz[b], krT2], writes=[pp], inc=(not last))
                    if last:
                        S.op("pe", lambda e: e.matmul(pp[:, n - 128:n], lhsT=identB[:], rhs=NEG[:], start=False, stop=True),
                             reads=[identB, NEG], writes=[pp])
                    S.op("dve", lambda e: e.tensor_scalar(out=sc[:, k0:k0 + n], in0=pp[:, 0:n], scalar1=1.0, scalar2=None,
                                                          op0=ALU.mult, op1=ALU.max, accum_out=mx[b][:, kc:kc + 1]),
                         reads=[pp], writes=[sc, mx[b]])
                S.op("dve", lambda e: e.tensor_reduce(out=mrow[b][:], in_=mx[b][:, 0:nch], axis=AX.X, op=ALU.max),
                     reads=[mx[b]], writes=[mrow[b]])
                S.op("dve", lambda e: e.tensor_scalar(out=negm[b][:], in0=mrow[b][:], scalar1=-SCALE, scalar2=None, op0=ALU.mult),
                     reads=[mrow[b]], writes=[negm[b]])
                S.op("act", lambda e: e.activation(out=sc[:, 0:nkeys], in_=sc[:, 0:nkeys], func=AF.Exp, scale=SCALE,
                                                   bias=negm[b][:], accum_out=rsum[b][:]),
                     reads=[sc, negm[b]], writes=[sc, rsum[b]])
                nkb = qb + 1
                for g0 in range(0, nkb, 4):
                    g1 = min(nkb, g0 + 4)
                    ptb_, ptap = ptP[(g0 // 4) % 2]
                    for kb in range(g0, g1):
                        S.op("pe", lambda e: e.transpose(out=ptap[:, (kb - g0) * 128:(kb - g0 + 1) * 128],
                                                         in_=sc[:, kb * 128:(kb + 1) * 128], identity=identF[:]),
                             reads=[sc, identF], writes=[ptb_], inc=(kb == g1 - 1))
                    S.op("dve", lambda e: e.tensor_copy(out=PT[:, g0:g1, :],
                                                        in_=ptap[:, 0:(g1 - g0) * 128].rearrange("p (c n) -> p c n", n=128)),
                         reads=[ptb_], writes=[PT])
                po = PO[b]
                for kb in range(nkb):
                    S.op("pe", lambda e: e.matmul(po[:, 0:128], lhsT=PT[:, kb, :], rhs=vh[:, kb, :],
                                                  start=(kb == 0), stop=(kb == nkb - 1)),
                         reads=[PT, vh], writes=[po], inc=(kb == nkb - 1))
                S.op("dve", lambda e: e.reciprocal(out=rinv2[b][:], in_=rsum[b][:]), reads=[rsum[b]], writes=[rinv2[b]])
                S.op("dve", lambda e: e.tensor_scalar(out=osb[b][:], in0=po[:, 0:128], scalar1=rinv2[b][:], scalar2=None,
                                                      op0=ALU.mult), reads=[po, rinv2[b]], writes=[osb[b]])
                S.dma("sp", dr["out"][qs, h * 128:(h + 1) * 128], osb[b][:], reads=[osb[b]], writes=[out_d])
        S.barrier()


def build_k2(SEQ):
    nc = bass.Bass("TRN2", target_bir_lowering=False)
    dr = {}
    def din(name, shape, dt=F32):
        dr[name] = nc.dram_tensor("i_" + name, list(shape), dt, kind="ExternalInput").ap()
    NT = SEQ // 128
    din("x", [SEQ, D]); din("pos", [128, NT], I32); din("w_in", [D, 704]); din("qn", [384]); din("kvn", [256])
    din("wqn", [384, 512]); din("wqr", [384, 256]); din("wkn", [256, 512]); din("wkv", [256, 512])
    dr["out"] = nc.dram_tensor("out", [SEQ, 512], BF16, kind="ExternalOutput").ap()
    dr["knT_d"] = nc.dram_tensor("knT_d", [4, 128, SEQ], BF16, kind="Internal").ap()
    dr["v_d"] = nc.dram_tensor("v_d", [SEQ, 512], BF16, kind="Internal").ap()
    with ExitStack() as ctx:
        S = Sched(nc, ctx)
        ps = {"ptB": S.psum("ptB", [128, 1024], BF16), "ptT": S.psum("ptT", [128, 1024], BF16),
              "P": [None] + [S.psum(f"P{i}", [128, 512], F32) for i in range(1, 7)]}
        emit_mla(S, nc, SEQ, dr, ps)
        pass
    return nc


def mla_inputs(x_b, pos_b, w_in, q_norm, kv_norm, w_qb, w_kvb, g):
    SEQ = x_b.shape[0]
    H = 8
    hs = slice(g * 4, g * 4 + 4)
    w_in2 = np.ascontiguousarray(np.concatenate([w_in[:, 384:640], w_in[:, 640:704], w_in[:, 0:384]], axis=1))
    wq = w_qb.reshape(384, H, 192)[:, hs]
    wqn = np.ascontiguousarray(wq[:, :, 0:128].reshape(384, 512))
    wqr = np.ascontiguousarray(wq[:, :, 128:192].reshape(384, 256))
    wkv_ = w_kvb.reshape(256, H, 256)[:, hs]
    wkn = np.ascontiguousarray(wkv_[:, :, 0:128].reshape(256, 512))
    wkv = np.ascontiguousarray(wkv_[:, :, 128:256].reshape(256, 512))
    pos2 = np.ascontiguousarray(pos_b.reshape(SEQ // 128, 128).T.astype(np.int32))
    return {"i_x": np.ascontiguousarray(x_b), "i_pos": pos2, "i_w_in": w_in2, "i_qn": np.ascontiguousarray(q_norm),
            "i_kvn": np.ascontiguousarray(kv_norm), "i_wqn": wqn, "i_wqr": wqr, "i_wkn": wkn, "i_wkv": wkv}


def run_phase(S, pfx, fn):
    with ExitStack() as pctx:
        old = S.ctx
        S.ctx = pctx
        S.pfx = pfx
        fn()
        S.ctx = old
        S.pfx = ""


def build_fused(SEQ, CAP_, NB, depth=4):
    TOK = SEQ // 2
    NT3 = TOK // 128
    nc = bass.Bass("TRN2", target_bir_lowering=False)
    ext = {}

    def din(name, shape, dt=F32):
        ext[name] = nc.dram_tensor(name, list(shape), dt, kind="ExternalInput").ap()

    def dint(name, shape, dt):
        return nc.dram_tensor(name, list(shape), dt, kind="Internal").ap()

    din("x_full", [SEQ, D]); din("x_own", [TOK, D]); din("hgidx", [128, NT3, 2], I32)
    din("pos", [128, SEQ // 128], I32)
    for j in range((depth + 1) // 2):
        din(f"m{j}_w_fm", [D, 512]); din(f"m{j}_w_tm", [D, 1288]); din(f"m{j}_b_g", [8]); din(f"m{j}_gain", [512])
    for j in range(depth // 2):
        din(f"a{j}_w_in", [D, 704]); din(f"a{j}_qn", [384]); din(f"a{j}_kvn", [256])
        din(f"a{j}_wqn", [384, 512]); din(f"a{j}_wqr", [384, 256]); din(f"a{j}_wkn", [256, 512]); din(f"a{j}_wkv", [256, 512])
    for l in range(depth):
        din(f"l{l}_w_out", [D, D])
        for nm in ("g1", "b1", "g2", "b2"):
            din(f"l{l}_{nm}", [D])
        din(f"l{l}_w_rt", [D, E]); din(f"l{l}_b_rt", [E]); din(f"l{l}_w_gu", [E, D, 2 * D]); din(f"l{l}_b_gu", [E, 2 * D])
        din(f"l{l}_w_dn", [E, D, D]); din(f"l{l}_b_dn", [E, D])
    out_ap = nc.dram_tensor("out", [TOK, D], F32, kind="ExternalOutput").ap()
    hg_own = dint("hg_own", [SEQ, 512], BF16)
    hg_gath = dint("hg_gath", [2 * SEQ, 512], BF16)
    xn = [dint(f"xn{i}", [TOK, D], F32) for i in range(2)]
    xfull_g = dint("xfull_g", [SEQ, D], F32)
    knT_d = dint("knT_d", [4, 128, SEQ], BF16)
    v_d = dint("v_d", [SEQ, 512], BF16)
    xg = dint("xg", [E * CAP_, D], BF16)
    yg = dint("yg", [E * CAP_, D], F32)
    x1s = dint("x1s", [TOK, D], F32)
    groups = [[2 * b, 2 * b + 1] for b in range(NB)]
    CHX = min(512, TOK)
    CHH = min(2048, SEQ)

    def xmap(t):
        row = t * 128
        r = row // TOK
        lrow = row - r * TOK
        k, off = lrow // CHX, lrow % CHX
        r0 = k * 2 * CHX + r * CHX + off
        return xfull_g[r0:r0 + 128, :]
    with ExitStack() as ctx:
        S = Sched(nc, ctx)
        ptB = S.psum("ptB", [128, 1024], BF16)
        Pf = [S.psum(f"P{i}", [128, 512], F32) for i in range(1, 7)]
        ptT = S.psum("ptT", [128, 1024], BF16)
        P7 = Buf(ptT.t[:].bitcast(F32), "P7")
        ps13 = {"ptB": ptB, "P": [None] + Pf + [P7]}
        ps2 = {"ptB": ptB, "ptT": ptT, "P": [None] + Pf}
        for l in range(depth):
            j = l // 2
            xsrc = ext["x_full"] if l == 0 else xfull_g
            if l % 2 == 0:
                dr = {"x": xsrc, **({"xmap": xmap} if l > 0 else {}), "w_fm": ext[f"m{j}_w_fm"], "w_tm": ext[f"m{j}_w_tm"], "b_g": ext[f"m{j}_b_g"],
                      "gain": ext[f"m{j}_gain"], "out": hg_own}
                run_phase(S, f"L{l}m_", lambda: emit_mlstm(S, nc, SEQ, dr, ps13))
            else:
                dr = {"x": xsrc, **({"xmap": xmap} if l > 0 else {}), "pos": ext["pos"], "w_in": ext[f"a{j}_w_in"], "qn": ext[f"a{j}_qn"], "kvn": ext[f"a{j}_kvn"],
                      "wqn": ext[f"a{j}_wqn"], "wqr": ext[f"a{j}_wqr"], "wkn": ext[f"a{j}_wkn"], "wkv": ext[f"a{j}_wkv"],
                      "out": hg_own, "knT_d": knT_d, "v_d": v_d}
                run_phase(S, f"L{l}a_", lambda: emit_mla(S, nc, SEQ, dr, ps2))
            for k in range(SEQ // CHH):
                S.cc("AllGather", groups, hg_own[k * CHH:(k + 1) * CHH, :], hg_gath[k * 2 * CHH:(k + 1) * 2 * CHH, :], inc=1)
            S.barrier()
            dr = {"x": ext["x_own"] if l == 0 else xn[(l - 1) % 2], "hg_gath": hg_gath, "hgidx": ext["hgidx"],
                  "out": out_ap if l == depth - 1 else xn[l % 2], "xg": xg, "yg": yg, "x1s": x1s}
            for nm in ("w_out", "g1", "b1", "g2", "b2", "w_rt", "b_rt", "w_gu", "b_gu", "w_dn", "b_dn"):
                dr[nm] = ext[f"l{l}_{nm}"]
            run_phase(S, f"L{l}p_", lambda: emit_post(S, nc, TOK, CAP_, dr, ps13))
            if l < depth - 1:
                for k in range(TOK // CHX):
                    S.cc("AllGather", groups, xn[l % 2][k * CHX:(k + 1) * CHX, :],
                         xfull_g[k * 2 * CHX:(k + 1) * 2 * CHX, :], inc=1)
                S.barrier()
    return nc


def fused_in_maps(x, positions, ln_gain, ln_bias, mlstm_w_in, mlstm_b_gates, mlstm_norm_gain, mlstm_w_out,
                  mla_w_in, mla_q_norm, mla_kv_norm, mla_w_qb, mla_w_kvb, mla_w_out,
                  moe_w_router, moe_b_router, moe_w_gate_up, moe_b_gate_up, moe_w_down, moe_b_down):
    f32 = np.float32
    A = lambda a: np.ascontiguousarray(np.asarray(a, dtype=f32))
    x = A(x)
    positions = np.asarray(positions)
    B, S_, _ = x.shape
    TOK = S_ // 2
    NT3 = TOK // 128
    depth = ln_gain.shape[0]
    shared = {}
    for l in range(depth):
        j = l // 2
        shared[f"l{l}_w_out"] = A(mlstm_w_out[j]) if l % 2 == 0 else A(mla_w_out[j])
        shared[f"l{l}_g1"] = A(ln_gain[l, 0]); shared[f"l{l}_b1"] = A(ln_bias[l, 0])
        shared[f"l{l}_g2"] = A(ln_gain[l, 1]); shared[f"l{l}_b2"] = A(ln_bias[l, 1])
        shared[f"l{l}_w_rt"] = A(moe_w_router[l]); shared[f"l{l}_b_rt"] = A(moe_b_router[l])
        shared[f"l{l}_w_gu"] = A(moe_w_gate_up[l]); shared[f"l{l}_b_gu"] = A(moe_b_gate_up[l])
        shared[f"l{l}_w_dn"] = A(moe_w_down[l]); shared[f"l{l}_b_dn"] = A(moe_b_down[l])
    in_maps = []
    for c in range(2 * B):
        b, g = c // 2, c % 2
        m = dict(shared)
        m["x_full"] = np.ascontiguousarray(x[b])
        m["x_own"] = np.ascontiguousarray(x[b, g * TOK:(g + 1) * TOK])
        p = np.arange(128, dtype=np.int64)[:, None, None]
        i = np.arange(NT3, dtype=np.int64)[None, :, None]
        r = np.arange(2, dtype=np.int64)[None, None, :]
        chh = min(2048, S_)
        tok = g * TOK + i * 128 + p
        m["hgidx"] = np.ascontiguousarray(((tok // chh) * 2 * chh + r * chh + tok % chh).astype(np.int32))
        for j in range((depth + 1) // 2):
            mi = mlstm_inputs(x[b], A(mlstm_w_in[j]), A(mlstm_b_gates[j]), A(mlstm_norm_gain[j]), g)
            for k in ("w_fm", "w_tm", "b_g", "gain"):
                m[f"m{j}_{k}"] = mi[k]
        for j in range(depth // 2):
            ai = mla_inputs(x[b], positions[b], A(mla_w_in[j]), A(mla_q_norm[j]), A(mla_kv_norm[j]),
                            A(mla_w_qb[j]), A(mla_w_kvb[j]), g)
            m["pos"] = ai["i_pos"]
            for k in ("w_in", "qn", "kvn", "wqn", "wqr", "wkn", "wkv"):
                m[f"a{j}_{k}"] = ai["i_" + k]
        in_maps.append(m)
    return in_maps


_NC_CACHE = {}


def kernel(x, positions, ln_gain, ln_bias, mlstm_w_in, mlstm_b_gates, mlstm_norm_gain, mlstm_w_out,
           mla_w_in, mla_q_norm, mla_kv_norm, mla_w_qb, mla_w_kvb, mla_w_out,
           moe_w_router, moe_b_router, moe_w_gate_up, moe_b_gate_up, moe_w_down, moe_b_down):
    x = np.asarray(x)
    B, S_, _ = x.shape
    TOK = S_ // 2
    depth = ln_gain.shape[0]
    cap = CAP if S_ == SEQ_FULL else 128
    key = (S_, B, depth)
    if key not in _NC_CACHE:
        _NC_CACHE[key] = build_fused(S_, cap, B, depth)
    nc = _NC_CACHE[key]
    in_maps = fused_in_maps(x, positions, ln_gain, ln_bias, mlstm_w_in, mlstm_b_gates, mlstm_norm_gain, mlstm_w_out,
                            mla_w_in, mla_q_norm, mla_kv_norm, mla_w_qb, mla_w_kvb, mla_w_out,
                            moe_w_router, moe_b_router, moe_w_gate_up, moe_b_gate_up, moe_w_down, moe_b_down)
    res = run_bass_kernel_spmd(nc, in_maps, core_ids=list(range(2 * B)))
    out = np.empty((B, S_, D), dtype=np.float32)
    for c in range(2 * B):
        b, g = c // 2, c % 2
        out[b, g * TOK:(g + 1) * TOK] = res.results[c]["out"]
    return out
```

```python
import math
from contextlib import ExitStack
import numpy as np
import concourse.bass as bass
import concourse.mybir as mybir
from concourse.bass_utils import run_bass_kernel_spmd

F32 = mybir.dt.float32
BF16 = mybir.dt.bfloat16
I32 = mybir.dt.int32
U32 = mybir.dt.uint32
AF = mybir.ActivationFunctionType
ALU = mybir.AluOpType
AX = mybir.AxisListType


class Buf:
    __slots__ = ("t", "w", "r", "name")

    def __init__(self, t=None, name=""):
        self.t = t
        self.w = {}
        self.r = {}
        self.name = name

    def __getitem__(self, idx):
        return self.t[idx]


class _Eng:
    def __init__(self, name, eng, sem):
        self.name = name
        self.eng = eng
        self.sem = sem
        self.count = 0
        self.waited = {}


class Sched:
    def __init__(self, nc, ctx, n_dma_sems=12, same_engine_sync=True):
        self.nc = nc
        self.ctx = ctx
        self.same_engine_sync = same_engine_sync
        self.E = {}
        for name, eng in (("pe", nc.tensor), ("dve", nc.vector), ("act", nc.scalar),
                          ("pool", nc.gpsimd), ("sp", nc.sync)):
            sem = ctx.enter_context(nc.semaphore("s_" + name))
            self.E[name] = _Eng(name, eng, sem)
        self.dma_sems = {}
        for q in ("sp", "act", "pool"):
            lst = []
            for i in range(n_dma_sems):
                s = ctx.enter_context(nc.semaphore(f"d_{q}{i}"))
                lst.append([s, 0])
            self.dma_sems[q] = [lst, 0]
        self.ninst = 0
        self.pfx = ""

    def sbuf(self, name, shape, dt):
        t = self.ctx.enter_context(self.nc.sbuf_tensor(self.pfx + name, list(shape), dt))
        return Buf(t, name)

    def psum(self, name, shape, dt):
        t = self.ctx.enter_context(self.nc.psum_tensor(name, list(shape), dt))
        return Buf(t, name)

    @staticmethod
    def _key(sem):
        return id(sem)

    def _need(self, reads, writes):
        need = {}
        def add(d):
            for k, (s, v) in d.items():
                if k not in need or need[k][1] < v:
                    need[k] = (s, v)
        for b in reads:
            add(b.w)
        for b in writes:
            add(b.w)
            add(b.r)
        return need

    def _do_waits(self, e, need):
        for k, (s, v) in need.items():
            if s is e.sem and (e.name == "pe" or not self.same_engine_sync):
                continue
            if e.waited.get(k, 0) < v:
                e.eng.wait_ge(s, v)
                e.waited[k] = v

    def _record(self, dep_sem, dep_val, reads, writes):
        k = self._key(dep_sem)
        for b in writes:
            b.w = {k: (dep_sem, dep_val)}
            b.r = {}
        for b in reads:
            if b in writes:
                continue
            if k not in b.r or b.r[k][1] < dep_val:
                b.r[k] = (dep_sem, dep_val)

    def op(self, eng, fn, reads=(), writes=(), inc=True):
        e = self.E[eng]
        self._do_waits(e, self._need(reads, writes))
        inst = fn(e.eng)
        self.ninst += 1
        if inc:
            inst.then_inc(e.sem, 1)
            e.count += 1
            self._record(e.sem, e.count, reads, writes)
        else:
            self._record(e.sem, e.count + 1, reads, writes)
        return inst

    def dma(self, q, out, in_, reads=(), writes=(), indirect=None, **kw):
        e = self.E[q]
        lst, pos = self.dma_sems[q]
        ent = lst[pos]
        self.dma_sems[q][1] = (pos + 1) % len(lst)
        s, cnt = ent
        need = self._need(reads, writes)
        if cnt > 0:
            need[self._key(s)] = (s, cnt)
        for k, (ss, v) in need.items():
            if e.waited.get(k, 0) < v:
                e.eng.wait_ge(ss, v)
                e.waited[k] = v
        if indirect is None:
            inst = e.eng.dma_start(out=out, in_=in_, **kw)
        else:
            inst = e.eng.indirect_dma_start(out=out, in_=in_, **indirect, **kw)
        self.ninst += 1
        inst.then_inc(s, 16)
        ent[1] = cnt + 16
        self._record(s, cnt + 16, reads, writes)
        return inst

    def cc(self, kind, groups, in_ap, out_ap, reads=(), writes=(), inc=16):
        e = self.E["pool"]
        lst, pos = self.dma_sems["pool"]
        ent = lst[pos]
        self.dma_sems["pool"][1] = (pos + 1) % len(lst)
        s, cnt = ent
        need = self._need(reads, writes)
        if cnt > 0:
            need[self._key(s)] = (s, cnt)
        for k, (ss, v) in need.items():
            if e.waited.get(k, 0) < v:
                e.eng.wait_ge(ss, v)
                e.waited[k] = v
        inst = e.eng.collective_compute(kind, ALU.bypass, replica_groups=groups, ins=[in_ap], outs=[out_ap])
        self.ninst += 1
        inst.then_inc(s, inc)
        ent[1] = cnt + inc
        self._record(s, cnt + inc, reads, writes)
        return inst

    def barrier(self):
        targets = []
        for name, o in self.E.items():
            if o.count > 0:
                targets.append((o, o.sem, o.count))
        for q, (lst, _) in self.dma_sems.items():
            for s, cnt in lst:
                if cnt > 0:
                    targets.append((None, s, cnt))
        for name in ("sp", "pool", "act", "dve", "pe"):
            e = self.E[name]
            for (o, s, v) in targets:
                if o is e:
                    continue
                k = self._key(s)
                if e.waited.get(k, 0) < v:
                    e.eng.wait_ge(s, v)
                    e.waited[k] = v

    def finish(self, bufs):
        need = self._need((), bufs)
        for name in ("sp", "pool", "act", "dve", "pe"):
            e = self.E[name]
            for k, (s, v) in need.items():
                if s is e.sem:
                    continue
                if e.waited.get(k, 0) < v:
                    e.eng.wait_ge(s, v)
                    e.waited[k] = v


D = 1024
E = 32
ALPHA = 8.0 ** 0.25
LN_EPS = 1e-5
RMS_EPS = 1e-6
SCALE = 192.0 ** -0.5
STAGE = SUB = SUBA = SUBB = SUBC = 99
SEQ_FULL = 8192
TOK_CORE = 4096
CAP = 640

def emit_ln(S, z, gain, bias, out, tmp):
    st, mv, sd, rstd, xn = tmp
    for h in range(2):
        S.op("dve", lambda e: e.bn_stats(out=st[:, h, :], in_=z[:, h * 512:(h + 1) * 512]),
             reads=[z], writes=[st])
    S.op("dve", lambda e: e.bn_aggr(out=mv[:], in_=st[:].rearrange("p a b -> p (a b)")),
         reads=[st], writes=[mv])
    S.op("act", lambda e: e.activation(out=sd[:], in_=mv[:, 1:2], func=AF.Sqrt, bias=tmp_eps(S)[:], scale=1.0),
         reads=[mv], writes=[sd])
    S.op("dve", lambda e: e.reciprocal(out=rstd[:], in_=sd[:]), reads=[sd], writes=[rstd])
    S.op("dve", lambda e: e.tensor_scalar(out=sd[:], in0=mv[:, 0:1], scalar1=rstd[:], scalar2=-1.0,
                                          op0=ALU.mult, op1=ALU.mult), reads=[mv, rstd], writes=[sd])
    S.op("act", lambda e: e.activation(out=xn[:], in_=z[:], func=AF.Identity, scale=rstd[:], bias=sd[:]),
         reads=[z, rstd, sd], writes=[xn])
    S.op("dve", lambda e: e.tensor_tensor(out=xn[:], in0=xn[:], in1=gain[:], op=ALU.mult),
         reads=[xn, gain], writes=[xn])
    S.op("dve", lambda e: e.tensor_tensor(out=out[:], in0=xn[:], in1=bias[:], op=ALU.add),
         reads=[xn, bias], writes=[out])


_EPS = {}


def tmp_eps(S):
    return _EPS[id(S)]


def emit_post(S, nc, T, CAP, dr, ps):
    NT = T // 128
    RB = CAP // 128
    parts = []
    lo = 0
    while lo < CAP:
        n = min(512, CAP - lo)
        parts.append((lo, n))
        lo += n
    ctx = S.ctx
    identF = S.sbuf("identF", [128, 128], F32)
    identB = S.sbuf("identB", [128, 128], BF16)
    triS = S.sbuf("triS", [128, 128], F32)
    onesM = S.sbuf("onesM", [128, 128], F32)
    offs = S.sbuf("offs", [128, E], F32)
    epsb = S.sbuf("epsb", [128, 1], F32)
    _EPS[id(S)] = epsb
    idx_all = S.sbuf("idx_all", [128, NT, 4], I32)
    gate_all = S.sbuf("gate_all", [128, NT, 4], F32)
    wgu = [S.sbuf(f"wgu{i}", [128, 8, 2048], BF16) for i in range(2)]
    wd = [S.sbuf(f"wd{i}", [128, 8, 1024], BF16) for i in range(2)]
    bgu = [S.sbuf(f"bgu{i}", [128, 8, 2], F32) for i in range(2)]
    bdn = [S.sbuf(f"bdn{i}", [128, 1024], F32) for i in range(2)]
    lnp = [S.sbuf(f"lnp{i}", [128, 1024], F32) for i in range(4)]
    st = S.sbuf("st", [128, 2, 6], F32)
    mv = S.sbuf("mv", [128, 2], F32)
    sd = S.sbuf("sd", [128, 1], F32)
    rstd = S.sbuf("rstd", [128, 1], F32)
    xn = S.sbuf("xn", [128, 1024], F32)
    lntmp = (st, mv, sd, rstd, xn)
    xg_d = Buf(None, "xg")
    yg_d = Buf(None, "yg")
    x1_d = Buf(None, "x1s")
    out_d = Buf(None, "out")

    S.op("pool", lambda e: e.memset(identF[:], 0.0), writes=[identF])
    S.op("pool", lambda e: e.affine_select(out=identF[:], in_=identF[:], pattern=[[-1, 128]],
                                           compare_op=ALU.not_equal, fill=1.0, base=0,
                                           channel_multiplier=1), reads=[identF], writes=[identF])
    S.op("dve", lambda e: e.tensor_copy(out=identB[:], in_=identF[:]), reads=[identF], writes=[identB])
    S.op("pool", lambda e: e.memset(onesM[:], 1.0), writes=[onesM])
    S.op("pool", lambda e: e.memset(epsb[:], LN_EPS), writes=[epsb])
    S.op("pool", lambda e: e.affine_select(out=triS[:], in_=onesM[:], pattern=[[1, 128]],
                                           compare_op=ALU.is_gt, fill=0.0, base=0,
                                           channel_multiplier=-1), reads=[onesM], writes=[triS])
    S.op("pool", lambda e: e.iota(offs[:], pattern=[[CAP, E]], base=0, channel_multiplier=0,
                                  allow_small_or_imprecise_dtypes=True), writes=[offs])
    for j, nm in enumerate(("g1", "b1", "g2", "b2")):
        S.dma("sp", lnp[j][:], dr[nm].partition_broadcast(128), writes=[lnp[j]])

    with ExitStack() as pctx:
        def sb(name, shape, dt):
            return Buf(pctx.enter_context(nc.sbuf_tensor(S.pfx + name, list(shape), dt)), name)
        wout = sb("wout", [128, 8, 1024], BF16)
        wrt = sb("wrt", [128, 8, E], F32)
        brt = sb("brt", [128, E], F32)
        baseoffs = sb("baseoffs", [128, E], F32)
        hgt = [sb(f"hgt{i}", [128, 1024], BF16) for i in range(2)]
        xt = [sb(f"xt{i}", [128, 1024], F32) for i in range(2)]
        hgT = [sb(f"hgT{i}", [128, 8, 128], BF16) for i in range(2)]
        z = [sb(f"z{i}", [128, 1024], F32) for i in range(2)]
        x1 = [sb(f"x1{i}", [128, 1024], F32) for i in range(2)]
        x1b = [sb(f"x1b{i}", [128, 1024], BF16) for i in range(2)]
        x1T = [sb(f"x1T{i}", [128, 8, 128], F32) for i in range(2)]
        lg = [sb(f"lg{i}", [128, E], F32) for i in range(2)]
        top8 = [sb(f"top8{i}", [128, 8], F32) for i in range(2)]
        mask = [sb(f"mask{i}", [128, E], F32) for i in range(2)]
        nmx = [sb(f"nmx{i}", [128, 1], F32) for i in range(2)]
        ex = [sb(f"ex{i}", [128, 4], F32) for i in range(2)]
        sm = [sb(f"sm{i}", [128, 1], F32) for i in range(2)]
        rs = [sb(f"rs{i}", [128, 1], F32) for i in range(2)]
        posf = [sb(f"posf{i}", [128, E], F32) for i in range(2)]
        oh = [sb(f"oh{i}", [128, E], F32) for i in range(2)]
        junk = [sb(f"junk{i}", [128, E], F32) for i in range(2)]
        destf = [sb(f"destf{i}", [128, 4], F32) for i in range(2)]

        if "hg_gath" in dr:
            hgidx = sb("hgidx", [128, NT, 2], I32)
            S.dma("sp", hgidx[:], dr["hgidx"], writes=[hgidx])
        for c in range(0, 8, 2):
            S.dma("pool", wout[:, c:c + 2, :],
                  dr["w_out"][c * 128:(c + 2) * 128, :].rearrange("(c p) n -> p c n", p=128), writes=[wout])
        S.dma("sp", wrt[:], dr["w_rt"].rearrange("(c p) n -> p c n", p=128), writes=[wrt])
        S.dma("sp", brt[:], dr["b_rt"].partition_broadcast(128), writes=[brt])
        S.op("dve", lambda e: e.tensor_copy(out=baseoffs[:], in_=offs[:]), reads=[offs], writes=[baseoffs])

        ptB, P = ps["ptB"], ps["P"]
        for i in range(NT):
            b = i % 2
            sl = slice(i * 128, (i + 1) * 128)
            if "hg_gath" in dr:
                for r in range(2):
                    S.dma("pool", hgt[b][:, r * 512:(r + 1) * 512], dr["hg_gath"], reads=[hgidx], writes=[hgt[b]],
                          indirect=dict(out_offset=None,
                                        in_offset=bass.IndirectOffsetOnAxis(ap=hgidx[:, i, r:r + 1], axis=0)))
            else:
                S.dma("sp", hgt[b][:], dr["hg"][sl, :], writes=[hgt[b]])
            S.dma("sp", xt[b][:], dr["x"][sl, :], writes=[xt[b]])
            for c in range(8):
                S.op("pe", lambda e: e.transpose(out=ptB[:, c * 128:(c + 1) * 128],
                                                 in_=hgt[b][:, c * 128:(c + 1) * 128], identity=identB[:]),
                     reads=[hgt[b], identB], writes=[ptB], inc=(c == 7))
            S.op("act", lambda e: e.copy(out=hgT[b][:], in_=ptB[:].rearrange("p (c n) -> p c n", c=8)),
                 reads=[ptB], writes=[hgT[b]])
            for h in range(2):
                pm = P[1 + h]
                for c in range(8):
                    S.op("pe", lambda e: e.matmul(pm[:], lhsT=hgT[b][:, c, :], rhs=wout[:, c, h * 512:(h + 1) * 512],
                                                  start=(c == 0), stop=(c == 7)),
                         reads=[hgT[b], wout], writes=[pm], inc=(c == 7))
                S.op("dve", lambda e: e.scalar_tensor_tensor(out=z[b][:, h * 512:(h + 1) * 512],
                                                             in0=xt[b][:, h * 512:(h + 1) * 512], scalar=ALPHA,
                                                             in1=pm[:], op0=ALU.mult, op1=ALU.add),
                     reads=[xt[b], pm], writes=[z[b]])
            emit_ln(S, z[b], lnp[0], lnp[1], x1[b], lntmp)
            S.dma("sp", dr["x1s"][sl, :], x1[b][:], reads=[x1[b]], writes=[x1_d])
            S.op("act", lambda e: e.copy(out=x1b[b][:], in_=x1[b][:]), reads=[x1[b]], writes=[x1b[b]])
            for h in range(2):
                pT = P[3 + h]
                for c in range(4):
                    cc = h * 4 + c
                    S.op("pe", lambda e: e.transpose(out=pT[:, c * 128:(c + 1) * 128],
                                                     in_=x1[b][:, cc * 128:(cc + 1) * 128], identity=identF[:]),
                         reads=[x1[b], identF], writes=[pT], inc=(c == 3))
                S.op("act", lambda e: e.copy(out=x1T[b][:, h * 4:(h + 1) * 4, :],
                                             in_=pT[:].rearrange("p (c n) -> p c n", c=4)),
                     reads=[pT], writes=[x1T[b]])
            pq = P[5]
            for c in range(8):
                S.op("pe", lambda e: e.matmul(pq[:, 0:E], lhsT=x1T[b][:, c, :], rhs=wrt[:, c, :],
                                              start=(c == 0), stop=(c == 7)),
                     reads=[x1T[b], wrt], writes=[pq], inc=(c == 7))
            S.op("dve", lambda e: e.tensor_tensor(out=lg[b][:], in0=pq[:, 0:E], in1=brt[:], op=ALU.add),
                 reads=[pq, brt], writes=[lg[b]])
            S.op("dve", lambda e: e.max(out=top8[b][:], in_=lg[b][:]), reads=[lg[b]], writes=[top8[b]])
            S.op("dve", lambda e: e.tensor_scalar(out=mask[b][:], in0=lg[b][:], scalar1=top8[b][:, 3:4], scalar2=None,
                                                  op0=ALU.is_ge), reads=[lg[b], top8[b]], writes=[mask[b]])
            S.op("act", lambda e: e.mul(out=nmx[b][:], in_=top8[b][:, 0:1], mul=-1.0), reads=[top8[b]], writes=[nmx[b]])
            S.op("act", lambda e: e.activation(out=ex[b][:], in_=top8[b][:, 0:4], func=AF.Exp, bias=nmx[b][:],
                                               scale=1.0, accum_out=sm[b][:]),
                 reads=[top8[b], nmx[b]], writes=[ex[b], sm[b]])
            S.op("dve", lambda e: e.reciprocal(out=rs[b][:], in_=sm[b][:]), reads=[sm[b]], writes=[rs[b]])
            S.op("dve", lambda e: e.tensor_scalar(out=gate_all[:, i, :], in0=ex[b][:], scalar1=rs[b][:], scalar2=None,
                                                  op0=ALU.mult), reads=[ex[b], rs[b]], writes=[gate_all])
            S.op("pe", lambda e: e.matmul(pq[:, 32:32 + E], lhsT=triS[:], rhs=mask[b][:], start=True, stop=True),
                 reads=[triS, mask[b]], writes=[pq], inc=False)
            S.op("pe", lambda e: e.matmul(pq[:, 64:64 + E], lhsT=onesM[:], rhs=mask[b][:], start=True, stop=True),
                 reads=[onesM, mask[b]], writes=[pq])
            S.op("dve", lambda e: e.tensor_tensor(out=posf[b][:], in0=pq[:, 32:32 + E], in1=baseoffs[:], op=ALU.add),
                 reads=[pq, baseoffs], writes=[posf[b]])
            S.op("dve", lambda e: e.tensor_tensor(out=baseoffs[:], in0=pq[:, 64:64 + E], in1=baseoffs[:], op=ALU.add),
                 reads=[pq, baseoffs], writes=[baseoffs])
            for k in range(4):
                S.op("dve", lambda e: e.scalar_tensor_tensor(out=junk[b][:], in0=lg[b][:], scalar=top8[b][:, k:k + 1],
                                                             in1=posf[b][:], op0=ALU.is_equal, op1=ALU.mult,
                                                             accum_out=destf[b][:, k:k + 1]),
                     reads=[lg[b], top8[b], posf[b]], writes=[junk[b], destf[b]])
            S.op("dve", lambda e: e.tensor_copy(out=idx_all[:, i, :], in_=destf[b][:]), reads=[destf[b]], writes=[idx_all])
            for k in range(4):
                S.dma("pool", dr["xg"], x1b[b][:], reads=[x1b[b], idx_all], writes=[],
                      indirect=dict(out_offset=bass.IndirectOffsetOnAxis(ap=idx_all[:, i, k:k + 1], axis=0),
                                    in_offset=None))
        S.barrier()

    with ExitStack() as pctx:
        def sb(name, shape, dt):
            return Buf(pctx.enter_context(nc.sbuf_tensor(S.pfx + name, list(shape), dt)), name)
        xgr = [sb(f"xgr{i}", [128, 1024], BF16) for i in range(2)]
        xgT = sb("xgT", [128, 8, CAP], BF16)
        hT = sb("hT", [128, 8, CAP], BF16)
        tg = [sb(f"tg{i}", [128, 512], F32) for i in range(2)]
        tsg = [sb(f"tsg{i}", [128, 512], F32) for i in range(2)]
        tu = [sb(f"tu{i}", [128, 512], F32) for i in range(2)]
        yo = [sb(f"yo{i}", [128, 1024], F32) for i in range(2)]
        ptB, P = ps["ptB"], ps["P"]
        stg = [sb(f"stg{i}", [128, 2048], F32) for i in range(3)]
        wguc = [[Buf(wgu[i].t, f"wguc{i}_{c}") for c in range(8)] for i in range(2)]
        wdc = [[Buf(wd[i].t, f"wdc{i}_{c}") for c in range(8)] for i in range(2)]
        cast_gu = ["act", "act", "dve", "act", "act", "dve", "act", "act"]
        cast_dn = ["act", "dve", "act", "dve"]
        stk = [0]

        def cast(eng, out, in_, reads, writes):
            if eng == "act":
                S.op("act", lambda e: e.copy(out=out, in_=in_), reads=reads, writes=writes)
            else:
                S.op(eng, lambda e: e.tensor_copy(out=out, in_=in_), reads=reads, writes=writes)

        def load_expert(e_):
            b = e_ % 2
            for c in range(8):
                st = stg[stk[0] % 3]
                stk[0] += 1
                S.dma("sp", st[:], dr["w_gu"][e_, c * 128:(c + 1) * 128, :], writes=[st])
                cast(cast_gu[c], wgu[b][:, c, :], st[:], [st], [wguc[b][c]])
            for k2, c in enumerate(range(0, 8, 2)):
                st = stg[stk[0] % 3]
                stk[0] += 1
                S.dma("sp", st[:].rearrange("p (c n) -> p c n", c=2),
                      dr["w_dn"][e_, c * 128:(c + 2) * 128, :].rearrange("(c p) n -> p c n", p=128), writes=[st])
                cast(cast_dn[k2], wd[b][:, c:c + 2, :], st[:].rearrange("p (c n) -> p c n", c=2), [st],
                     [wdc[b][c], wdc[b][c + 1]])
            with nc.allow_non_contiguous_dma(reason="tiny bias"):
                S.dma("sp", bgu[b][:], dr["b_gu"][e_, :].rearrange("(c p t) -> p c t", p=128, t=2), writes=[bgu[b]])
            S.dma("sp", bdn[b][:], dr["b_dn"][e_, :].partition_broadcast(128), writes=[bdn[b]])

        load_expert(0)
        cnt = 0
        for e_ in range(E):
            wb = e_ % 2
            for rb in range(RB):
                b = rb % 2
                r0 = e_ * CAP + rb * 128
                S.dma("sp", xgr[b][:], dr["xg"][r0:r0 + 128, :], writes=[xgr[b]])
                for c in range(8):
                    S.op("pe", lambda e: e.transpose(out=ptB[:, c * 128:(c + 1) * 128],
                                                     in_=xgr[b][:, c * 128:(c + 1) * 128], identity=identB[:]),
                         reads=[xgr[b], identB], writes=[ptB], inc=(c == 7))
                S.op("act", lambda e: e.copy(out=xgT[:, :, rb * 128:(rb + 1) * 128],
                                             in_=ptB[:].rearrange("p (c n) -> p c n", c=8)),
                     reads=[ptB], writes=[xgT])
            if e_ + 1 < E:
                load_expert(e_ + 1)
            for fc in range(8):
                for (lo, n) in parts:
                    b = cnt % 2
                    cnt += 1
                    pg, pu = P[1 + b], P[3 + b]
                    for gi, pp in ((0, pg), (1, pu)):
                        for c in range(8):
                            S.op("pe", lambda e: e.matmul(pp[:, 0:n],
                                                          lhsT=wgu[wb][:, c, fc * 256 + gi:fc * 256 + 256:2],
                                                          rhs=xgT[:, c, lo:lo + n], start=(c == 0), stop=(c == 7)),
                                 reads=[wguc[wb][c], xgT], writes=[pp], inc=(c == 7))
                    S.op("dve", lambda e: e.tensor_scalar(out=tg[b][:, 0:n], in0=pg[:, 0:n], scalar1=bgu[wb][:, fc, 0:1],
                                                          scalar2=7.0, op0=ALU.add, op1=ALU.min),
                         reads=[pg, bgu[wb]], writes=[tg[b]])
                    S.op("act", lambda e: e.activation(out=tsg[b][:, 0:n], in_=tg[b][:, 0:n], func=AF.Gelu_apprx_sigmoid),
                         reads=[tg[b]], writes=[tsg[b]])
                    S.op("dve", lambda e: e.tensor_scalar(out=tu[b][:, 0:n], in0=pu[:, 0:n], scalar1=bgu[wb][:, fc, 1:2],
                                                          scalar2=-7.0, op0=ALU.add, op1=ALU.max),
                         reads=[pu, bgu[wb]], writes=[tu[b]])
                    S.op("dve", lambda e: e.scalar_tensor_tensor(out=tu[b][:, 0:n], in0=tu[b][:, 0:n], scalar=7.0,
                                                                 in1=tsg[b][:, 0:n], op0=ALU.min, op1=ALU.mult),
                         reads=[tu[b], tsg[b]], writes=[tu[b]])
                    S.op("dve", lambda e: e.tensor_tensor(out=hT[:, fc, lo:lo + n], in0=tu[b][:, 0:n], in1=tsg[b][:, 0:n],
                                                          op=ALU.add), reads=[tu[b], tsg[b]], writes=[hT])
            for rb in range(RB):
                b = rb % 2
                for h in range(2):
                    py = P[5 + h]
                    for fc in range(8):
                        S.op("pe", lambda e: e.matmul(py[:], lhsT=hT[:, fc, rb * 128:(rb + 1) * 128],
                                                      rhs=wd[wb][:, fc, h * 512:(h + 1) * 512],
                                                      start=(fc == 0), stop=(fc == 7)),
                             reads=[hT, wdc[wb][fc]], writes=[py], inc=(fc == 7))
                    S.op("dve", lambda e: e.tensor_tensor(out=yo[b][:, h * 512:(h + 1) * 512], in0=py[:],
                                                          in1=bdn[wb][:, h * 512:(h + 1) * 512], op=ALU.add),
                         reads=[py, bdn[wb]], writes=[yo[b]])
                r0 = e_ * CAP + rb * 128
                S.dma("sp", dr["yg"][r0:r0 + 128, :], yo[b][:], reads=[yo[b]], writes=[yg_d])
        S.barrier()

    with ExitStack() as pctx:
        def sb(name, shape, dt):
            return Buf(pctx.enter_context(nc.sbuf_tensor(S.pfx + name, list(shape), dt)), name)
        xc = [sb(f"xc{i}", [128, 1024], F32) for i in range(2)]
        yk = [[sb(f"yk{i}_{k}", [128, 1024], F32) for k in range(4)] for i in range(2)]
        acc = [sb(f"acc{i}", [128, 1024], F32) for i in range(2)]
        x2 = [sb(f"x2{i}", [128, 1024], F32) for i in range(2)]
        for i in range(NT):
            b = i % 2
            sl = slice(i * 128, (i + 1) * 128)
            S.dma("sp", xc[b][:], dr["x1s"][sl, :], writes=[xc[b]])
            for k in range(4):
                S.dma("pool", yk[b][k][:], dr["yg"], reads=[idx_all], writes=[yk[b][k]],
                      indirect=dict(out_offset=None,
                                    in_offset=bass.IndirectOffsetOnAxis(ap=idx_all[:, i, k:k + 1], axis=0)))
            S.op("dve", lambda e: e.tensor_scalar(out=acc[b][:], in0=yk[b][0][:], scalar1=gate_all[:, i, 0:1],
                                                  scalar2=None, op0=ALU.mult),
                 reads=[yk[b][0], gate_all], writes=[acc[b]])
            for k in range(1, 4):
                S.op("dve", lambda e: e.scalar_tensor_tensor(out=acc[b][:], in0=yk[b][k][:], scalar=gate_all[:, i, k:k + 1],
                                                             in1=acc[b][:], op0=ALU.mult, op1=ALU.add),
                     reads=[yk[b][k], gate_all, acc[b]], writes=[acc[b]])
            S.op("dve", lambda e: e.scalar_tensor_tensor(out=acc[b][:], in0=xc[b][:], scalar=ALPHA, in1=acc[b][:],
                                                         op0=ALU.mult, op1=ALU.add),
                 reads=[xc[b], acc[b]], writes=[acc[b]])
            emit_ln(S, acc[b], lnp[2], lnp[3], x2[b], lntmp)
            S.dma("sp", dr["out"][sl, :], x2[b][:], reads=[x2[b]], writes=[out_d])
        S.barrier()


def build_k3(T, CAP):
    nc = bass.Bass("TRN2", target_bir_lowering=False)
    dr = {}
    def din(name, shape, dt=F32):
        dr[name] = nc.dram_tensor(name, list(shape), dt, kind="ExternalInput").ap()
    din("x", [T, D]); din("hg", [T, D], BF16)
    din("w_out", [D, D]); din("g1", [D]); din("b1", [D]); din("g2", [D]); din("b2", [D])
    din("w_rt", [D, E]); din("b_rt", [E]); din("w_gu", [E, D, 2 * D]); din("b_gu", [E, 2 * D])
    din("w_dn", [E, D, D]); din("b_dn", [E, D])
    dr["out"] = nc.dram_tensor("out", [T, D], F32, kind="ExternalOutput").ap()
    dr["xg"] = nc.dram_tensor("xg", [E * CAP, D], BF16, kind="Internal").ap()
    dr["yg"] = nc.dram_tensor("yg", [E * CAP, D], F32, kind="Internal").ap()
    dr["x1s"] = nc.dram_tensor("x1s", [T, D], F32, kind="Internal").ap()
    with ExitStack() as ctx:
        S = Sched(nc, ctx)
        ps = {"ptB": S.psum("ptB", [128, 1024], BF16), "P": [None] + [S.psum(f"P{i}", [128, 512], F32) for i in range(1, 8)]}
        emit_post(S, nc, T, CAP, dr, ps)
        pass
    return nc


def emit_mlstm(S, nc, SEQ, dr, ps):
    NT = SEQ // 128
    ptB, PA, PB, PC, PQ, PST, PN = ps["ptB"], ps["P"][1], ps["P"][2], ps["P"][3], ps["P"][4], ps["P"][5], ps["P"][6:8]
    identF = S.sbuf("identF", [128, 128], F32)
    identB = S.sbuf("identB", [128, 128], BF16)
    triI = S.sbuf("triI", [128, 128], F32)
    onesM = S.sbuf("onesM", [128, 128], F32)
    epsb = S.sbuf("epsb", [128, 1], F32)
    wfm = S.sbuf("wfm", [128, 8, 512], BF16)
    wtm = S.sbuf("wtm", [128, 8, 1288], BF16)
    bg = S.sbuf("bg", [128, 8], F32)
    gain = S.sbuf("gain_sb", [128, 512], F32)
    Cn = [S.sbuf(f"Cn{h}", [128, 132], F32) for h in range(4)]
    Cnb = [S.sbuf(f"Cnb{h}", [128, 132], BF16) for h in range(4)]
    out_d = Buf(None, "out")

    S.op("pool", lambda e: e.memset(identF[:], 0.0), writes=[identF])
    S.op("pool", lambda e: e.affine_select(out=identF[:], in_=identF[:], pattern=[[-1, 128]],
                                           compare_op=ALU.not_equal, fill=1.0, base=0,
                                           channel_multiplier=1), reads=[identF], writes=[identF])
    S.op("dve", lambda e: e.tensor_copy(out=identB[:], in_=identF[:]), reads=[identF], writes=[identB])
    S.op("pool", lambda e: e.memset(onesM[:], 1.0), writes=[onesM])
    S.op("pool", lambda e: e.memset(epsb[:], RMS_EPS), writes=[epsb])
    oneb = S.sbuf("oneb", [128, 1], F32)
    S.op("pool", lambda e: e.memset(oneb[:], 1.0), writes=[oneb])
    S.op("pool", lambda e: e.affine_select(out=triI[:], in_=onesM[:], pattern=[[1, 128]],
                                           compare_op=ALU.is_ge, fill=0.0, base=0,
                                           channel_multiplier=-1), reads=[onesM], writes=[triI])
    hm = S.sbuf("hm", [128, 2], F32)
    S.op("pool", lambda e: e.affine_select(out=hm[:, 0:1], in_=onesM[:, 0:1], pattern=[[0, 1]],
                                           compare_op=ALU.is_ge, fill=0.0, base=63,
                                           channel_multiplier=-1), reads=[onesM], writes=[hm])
    S.op("pool", lambda e: e.affine_select(out=hm[:, 1:2], in_=onesM[:, 0:1], pattern=[[0, 1]],
                                           compare_op=ALU.is_ge, fill=0.0, base=-64,
                                           channel_multiplier=1), reads=[onesM], writes=[hm])
    for h in range(4):
        S.op("pool", lambda e: e.memset(Cn[h][:], 0.0), writes=[Cn[h]])
        S.op("pool", lambda e: e.memset(Cnb[h][:], 0.0), writes=[Cnb[h]])
    for c in range(0, 8, 2):
        S.dma("pool", wfm[:, c:c + 2, :], dr["w_fm"][c * 128:(c + 2) * 128, :].rearrange("(c p) n -> p c n", p=128),
              writes=[wfm])
        S.dma("pool", wtm[:, c:c + 2, :], dr["w_tm"][c * 128:(c + 2) * 128, :].rearrange("(c p) n -> p c n", p=128),
              writes=[wtm])
    S.dma("sp", bg[:], dr["b_g"].partition_broadcast(128), writes=[bg])
    S.dma("sp", gain[:], dr["gain"].partition_broadcast(128), writes=[gain])

    def sb2(name, shape, dt):
        return [S.sbuf(f"{name}{i}", shape, dt) for i in range(2)]
    xt = sb2("xt", [128, 1024], F32)
    xb = sb2("xb", [128, 1024], BF16)
    xT = sb2("xT", [128, 8, 128], BF16)
    qTz = [[S.sbuf(f"qTz{i}_{h}", [128, 128], BF16) for h in range(4)] for i in range(2)]
    kT = sb2("kT", [128, 2, 128], BF16)
    gts = sb2("gts", [128, 8], F32)
    e1 = sb2("e1", [128, 4], F32)
    l1 = sb2("l1", [128, 4], F32)
    eq = sb2("eq", [128, 4], F32)
    ginb = sb2("ginb", [128, 4], F32)
    u = sb2("u", [128, 4], F32)
    ebl = sb2("ebl", [128, 4], F32)
    ktm = sb2("ktm", [128, 256], BF16)
    vx = sb2("vx", [128, 4, 132], BF16)
    og = sb2("og", [128, 512], F32)
    Sm = sb2("Sm", [128, 4, 128], BF16)
    dd = sb2("dd", [128, 4], F32)
    rr = sb2("rr", [128, 4], F32)
    fac = sb2("fac", [128, 4], F32)
    ss = sb2("ss", [128, 4], F32)
    rms = sb2("rms", [128, 4], F32)
    rinv = sb2("rinv", [128, 4], F32)
    fac2 = sb2("fac2", [128, 4], F32)
    sqj = sb2("sqj", [128, 128], F32)
    hn = sb2("hn", [128, 512], F32)
    hgo = sb2("hgo", [128, 512], BF16)
    for i in range(2):
        for h in range(4):
            S.op("pool", lambda e: e.memset(qTz[i][h][:], 0.0), writes=[qTz[i][h]])

    for t in range(NT):
        b = t % 2
        sl = slice(t * 128, (t + 1) * 128)
        S.dma("sp", xt[b][:], (dr["xmap"](t) if "xmap" in dr else dr["x"][sl, :]), writes=[xt[b]])
        S.op("act", lambda e: e.copy(out=xb[b][:], in_=xt[b][:]), reads=[xt[b]], writes=[xb[b]])
        for c in range(8):
            S.op("pe", lambda e: e.transpose(out=ptB[:, c * 128:(c + 1) * 128], in_=xb[b][:, c * 128:(c + 1) * 128],
                                             identity=identB[:]), reads=[xb[b], identB], writes=[ptB], inc=(c == 7))
        S.op("dve", lambda e: e.tensor_copy(out=xT[b][:], in_=ptB[:].rearrange("p (c n) -> p c n", c=8)),
             reads=[ptB], writes=[xT[b]])
        if STAGE < 1:
            continue
        for j in range(4):
            for c in range(8):
                S.op("pe", lambda e: e.matmul(PQ[:, j * 128:(j + 1) * 128], lhsT=wfm[:, c, j * 128:(j + 1) * 128],
                                              rhs=xT[b][:, c, :], start=(c == 0), stop=(c == 7)),
                     reads=[wfm, xT[b]], writes=[PQ], inc=(c == 7))
        for h in range(4):
            hb, jj = h // 2, h % 2
            S.op("dve", lambda e: e.tensor_scalar(out=qTz[b][h][:], in0=PQ[:, hb * 128:(hb + 1) * 128],
                                                  scalar1=hm[:, jj:jj + 1], scalar2=0.125, op0=ALU.mult, op1=ALU.mult),
                 reads=[PQ, hm], writes=[qTz[b][h]])
        S.op("dve", lambda e: e.tensor_copy(out=kT[b][:], in_=PQ[:, 256:512].rearrange("p (c n) -> p c n", c=2)),
             reads=[PQ], writes=[kT[b]])
        if STAGE < 2:
            continue
        for (pp, lo, n) in ((PA, 0, 264), (PB, 264, 512), (PC, 776, 512))[:SUB]:
            for c in range(8):
                S.op("pe", lambda e: e.matmul(pp[:, 0:n], lhsT=xT[b][:, c, :], rhs=wtm[:, c, lo:lo + n],
                                              start=(c == 0), stop=(c == 7)),
                     reads=[wtm, xT[b]], writes=[pp], inc=(c == 7))
        if SUB < 4:
            continue
        S.op("dve", lambda e: e.tensor_tensor(out=gts[b][:], in0=PA[:, 256:264], in1=bg[:], op=ALU.add),
             reads=[PA, bg], writes=[gts[b]])
        if SUB < 5:
            continue
        S.op("dve", lambda e: e.tensor_copy(out=ktm[b][:], in_=PA[:, 0:256]), reads=[PA], writes=[ktm[b]])
        if SUB < 6:
            continue
        S.op("act", lambda e: e.activation(out=e1[b][:], in_=gts[b][:, 4:8], func=AF.Exp, scale=-1.0),
             reads=[gts[b]], writes=[e1[b]])
        S.op("act", lambda e: e.activation(out=l1[b][:], in_=e1[b][:], func=AF.Ln, bias=oneb[:], scale=1.0),
             reads=[e1[b], oneb], writes=[l1[b]])
        if STAGE < 3:
            continue
        S.op("pe", lambda e: e.matmul(PA[:, 272:276], lhsT=triI[:], rhs=l1[b][:], start=True, stop=True),
             reads=[triI, l1[b]], writes=[PA], inc=False)
        S.op("pe", lambda e: e.matmul(PA[:, 280:284], lhsT=onesM[:], rhs=l1[b][:], start=True, stop=True),
             reads=[onesM, l1[b]], writes=[PA])
        S.op("act", lambda e: e.activation(out=eq[b][:], in_=PA[:, 272:276], func=AF.Exp, scale=-1.0),
             reads=[PA], writes=[eq[b]])
        S.op("dve", lambda e: e.tensor_tensor(out=ginb[b][:], in0=PA[:, 272:276], in1=gts[b][:, 0:4], op=ALU.add),
             reads=[PA, gts[b]], writes=[ginb[b]])
        S.op("act", lambda e: e.activation(out=u[b][:], in_=ginb[b][:], func=AF.Exp), reads=[ginb[b]], writes=[u[b]])
        S.op("act", lambda e: e.activation(out=ebl[b][:], in_=PA[:, 280:284], func=AF.Exp, scale=-1.0),
             reads=[PA], writes=[ebl[b]])
        for h in range(4):
            S.op("dve", lambda e: e.tensor_scalar(out=vx[b][:, h, 0:128], in0=PB[:, h * 128:(h + 1) * 128],
                                                  scalar1=u[b][:, h:h + 1], scalar2=None, op0=ALU.mult),
                 reads=[PB, u[b]], writes=[vx[b]])
        S.op("dve", lambda e: e.tensor_copy(out=vx[b][:, :, 128], in_=u[b][:]), reads=[u[b]], writes=[vx[b]])
        S.op("act", lambda e: e.activation(out=og[b][:], in_=PC[:], func=AF.Sigmoid), reads=[PC], writes=[og[b]])
        if STAGE < 4:
            continue
        for h in range(4):
            hb = h // 2
            S.op("pe", lambda e: e.matmul(PST[:, h * 128:(h + 1) * 128], lhsT=kT[b][:, hb, :], rhs=qTz[b][h][:],
                                          start=True, stop=True),
                 reads=[kT[b], qTz[b][h]], writes=[PST], inc=(h == 3))
        for h in range(4):
            S.op("dve", lambda e: e.tensor_tensor(out=Sm[b][:, h, :], in0=PST[:, h * 128:(h + 1) * 128], in1=triI[:],
                                                  op=ALU.mult), reads=[PST, triI], writes=[Sm[b]])
        for h in range(4):
            hb, jj = h // 2, h % 2
            pn = PN[hb]
            S.op("pe", lambda e: e.matmul(pn[:, jj * 129:(jj + 1) * 129], lhsT=Sm[b][:, h, :], rhs=vx[b][:, h, 0:129],
                                          start=True, stop=False),
                 reads=[Sm[b], vx[b]], writes=[pn], inc=False)
            S.op("pe", lambda e: e.matmul(pn[:, jj * 129:(jj + 1) * 129], lhsT=qTz[b][h][:], rhs=Cnb[h][:, 0:129],
                                          start=False, stop=True),
                 reads=[qTz[b][h], Cnb[h]], writes=[pn])
        if STAGE < 5:
            continue
        for h in range(4):
            hb, jj = h // 2, h % 2
            R = slice(0, 128)
            pd = PQ if hb == 0 else PST
            S.op("pe", lambda e: e.matmul(pd[:, jj * 129:(jj + 1) * 129], lhsT=ktm[b][:, hb * 128:(hb + 1) * 128],
                                          rhs=vx[b][:, h, 0:129], start=True, stop=True),
                 reads=[ktm[b], vx[b]], writes=[pd])
            S.op("dve", lambda e: e.tensor_tensor(out=Cn[h][R, 0:129], in0=pd[R, jj * 129:(jj + 1) * 129], in1=Cn[h][R, 0:129],
                                                  op=ALU.add), reads=[pd, Cn[h]], writes=[Cn[h]])
            S.op("act", lambda e: e.activation(out=Cn[h][R, 0:129], in_=Cn[h][R, 0:129], func=AF.Identity,
                                               scale=ebl[b][R, h:h + 1]), reads=[Cn[h], ebl[b]], writes=[Cn[h]])
            S.op("pool", lambda e: e.tensor_copy(out=Cnb[h][R, 0:129], in_=Cn[h][R, 0:129]), reads=[Cn[h]], writes=[Cnb[h]])
        if STAGE < 6:
            continue
        for h in range(4):
            hb, jj = h // 2, h % 2
            pn = PN[hb]
            c0 = jj * 129
            hs = slice(h, h + 1)
            S.op("act", lambda e: e.activation(out=dd[b][:, hs], in_=pn[:, c0 + 128:c0 + 129], func=AF.Abs,
                                               scale=eq[b][:, hs]), reads=[pn, eq[b]], writes=[dd[b]])
            S.op("dve", lambda e: e.tensor_scalar(out=dd[b][:, hs], in0=dd[b][:, hs], scalar1=1.0, scalar2=None,
                                                  op0=ALU.max), reads=[dd[b]], writes=[dd[b]])
            S.op("dve", lambda e: e.reciprocal(out=rr[b][:, hs], in_=dd[b][:, hs]), reads=[dd[b]], writes=[rr[b]])
            S.op("dve", lambda e: e.tensor_tensor(out=fac[b][:, hs], in0=rr[b][:, hs], in1=eq[b][:, hs], op=ALU.mult),
                 reads=[rr[b], eq[b]], writes=[fac[b]])
            S.op("act", lambda e: e.activation(out=sqj[b][:], in_=pn[:, c0:c0 + 128], func=AF.Square,
                                               scale=fac[b][:, hs], accum_out=ss[b][:, hs]),
                 reads=[pn, fac[b]], writes=[sqj[b], ss[b]])
            S.op("act", lambda e: e.activation(out=rms[b][:, hs], in_=ss[b][:, hs], func=AF.Sqrt, bias=epsb[:],
                                               scale=1.0 / 128.0), reads=[ss[b], epsb], writes=[rms[b]])
            S.op("dve", lambda e: e.reciprocal(out=rinv[b][:, hs], in_=rms[b][:, hs]), reads=[rms[b]], writes=[rinv[b]])
            S.op("dve", lambda e: e.tensor_tensor(out=fac2[b][:, hs], in0=fac[b][:, hs], in1=rinv[b][:, hs], op=ALU.mult),
                 reads=[fac[b], rinv[b]], writes=[fac2[b]])
            S.op("dve", lambda e: e.scalar_tensor_tensor(out=hn[b][:, h * 128:(h + 1) * 128], in0=pn[:, c0:c0 + 128],
                                                         scalar=fac2[b][:, hs], in1=gain[:, h * 128:(h + 1) * 128],
                                                         op0=ALU.mult, op1=ALU.mult),
                 reads=[pn, fac2[b], gain], writes=[hn[b]])
        S.op("pool", lambda e: e.tensor_tensor(out=hgo[b][:], in0=hn[b][:], in1=og[b][:], op=ALU.mult),
             reads=[hn[b], og[b]], writes=[hgo[b]])
        S.dma("sp", dr["out"][sl, :], hgo[b][:], reads=[hgo[b]], writes=[out_d])
    S.barrier()


def build_k1(SEQ):
    nc = bass.Bass("TRN2", target_bir_lowering=False)
    dr = {}
    def din(name, shape, dt=F32):
        dr[name] = nc.dram_tensor(name, list(shape), dt, kind="ExternalInput").ap()
    din("x", [SEQ, D]); din("w_fm", [D, 512]); din("w_tm", [D, 1288]); din("b_g", [8]); din("gain", [512])
    dr["out"] = nc.dram_tensor("out", [SEQ, 512], BF16, kind="ExternalOutput").ap()
    with ExitStack() as ctx:
        S = Sched(nc, ctx)
        ps = {"ptB": S.psum("ptB", [128, 1024], BF16), "P": [None] + [S.psum(f"P{i}", [128, 512], F32) for i in range(1, 8)]}
        emit_mlstm(S, nc, SEQ, dr, ps)
        pass
    return nc


def mlstm_inputs(x_b, w_in, b_gates, norm_gain, g):
    H, dk, dv = 8, 64, 128
    hs = slice(g * 4, g * 4 + 4)
    wq = w_in[:, 0:512].reshape(D, H, dk)[:, hs].reshape(D, 256)
    wk = w_in[:, 512:1024].reshape(D, H, dk)[:, hs].reshape(D, 256)
    wv = w_in[:, 1024:2048].reshape(D, H, dv)[:, hs].reshape(D, 512)
    wo = w_in[:, 2048:3072].reshape(D, H, dv)[:, hs].reshape(D, 512)
    wgi = w_in[:, 3072:3080][:, hs]
    wgf = w_in[:, 3080:3088][:, hs]
    w_fm = np.ascontiguousarray(np.concatenate([wq, wk], axis=1))
    w_tm = np.ascontiguousarray(np.concatenate([wk, wgi, wgf, wv, wo], axis=1))
    b_g = np.ascontiguousarray(np.concatenate([b_gates[0:8][hs], b_gates[8:16][hs]]))
    gain = np.ascontiguousarray(norm_gain.reshape(H, dv)[hs].reshape(512))
    return {"x": np.ascontiguousarray(x_b), "w_fm": w_fm, "w_tm": w_tm, "b_g": b_g, "gain": gain}


def emit_mla(S, nc, SEQ, dr, ps):
    NT = SEQ // 128
    ptB, ptT = ps["ptB"], ps["ptT"]
    P = ps["P"]
    identF = S.sbuf("identF", [128, 128], F32)
    identB = S.sbuf("identB", [128, 128], BF16)
    onesM = S.sbuf("onesM", [128, 128], F32)
    negF = S.sbuf("negF", [128, 128], F32)
    NEG = S.sbuf("NEG", [128, 128], BF16)
    hm = S.sbuf("hm", [128, 2], F32)
    epsb = S.sbuf("epsb", [128, 1], F32)
    wqn = S.sbuf("wqn", [128, 3, 512], BF16)
    cqT_all = S.sbuf("cqT_all", [128, 3, SEQ], BF16)
    krT2 = S.sbuf("krT2", [128, SEQ], BF16)
    qrT_all = S.sbuf("qrT_all", [128, 2, SEQ], BF16)
    out_d = Buf(None, "out")
    knT_dd = Buf(None, "knT_d")
    v_dd = Buf(None, "v_d")

    S.op("pool", lambda e: e.memset(identF[:], 0.0), writes=[identF])
    S.op("pool", lambda e: e.affine_select(out=identF[:], in_=identF[:], pattern=[[-1, 128]],
                                           compare_op=ALU.not_equal, fill=1.0, base=0,
                                           channel_multiplier=1), reads=[identF], writes=[identF])
    S.op("dve", lambda e: e.tensor_copy(out=identB[:], in_=identF[:]), reads=[identF], writes=[identB])
    S.op("pool", lambda e: e.memset(onesM[:], 1.0), writes=[onesM])
    S.op("pool", lambda e: e.memset(epsb[:], RMS_EPS), writes=[epsb])
    S.op("pool", lambda e: e.memset(negF[:], -30000.0), writes=[negF])
    S.op("pool", lambda e: e.affine_select(out=negF[:], in_=negF[:], pattern=[[1, 128]],
                                           compare_op=ALU.is_gt, fill=0.0, base=0,
                                           channel_multiplier=-1), reads=[negF], writes=[negF])
    S.op("dve", lambda e: e.tensor_copy(out=NEG[:], in_=negF[:]), reads=[negF], writes=[NEG])
    S.op("pool", lambda e: e.affine_select(out=hm[:, 0:1], in_=onesM[:, 0:1], pattern=[[0, 1]],
                                           compare_op=ALU.is_ge, fill=0.0, base=63,
                                           channel_multiplier=-1), reads=[onesM], writes=[hm])
    S.op("pool", lambda e: e.affine_select(out=hm[:, 1:2], in_=onesM[:, 0:1], pattern=[[0, 1]],
                                           compare_op=ALU.is_ge, fill=0.0, base=-64,
                                           channel_multiplier=1), reads=[onesM], writes=[hm])
    S.dma("pool", wqn[:], dr["wqn"].rearrange("(c p) n -> p c n", p=128), writes=[wqn])

    with ExitStack() as pctx:
        def sb(name, shape, dt):
            return Buf(pctx.enter_context(nc.sbuf_tensor(S.pfx + name, list(shape), dt)), name)
        def sb2(name, shape, dt):
            return [sb(f"{name}{i}", shape, dt) for i in range(2)]
        win = sb("win", [128, 8, 704], BF16)
        wqr = sb("wqr", [128, 3, 256], BF16)
        wkn = sb("wkn", [128, 2, 512], BF16)
        wkv = sb("wkv", [128, 2, 512], BF16)
        qn_t = sb("qn_t", [128, 384], F32)
        kvn_t = sb("kvn_t", [128, 256], F32)
        cosT = sb("cosT", [128, NT, 32], F32)
        sinT = sb("sinT", [128, NT, 32], F32)
        for c in range(0, 8, 4):
            S.dma("pool", win[:, c:c + 4, :], dr["w_in"][c * 128:(c + 4) * 128, :].rearrange("(c p) n -> p c n", p=128),
                  writes=[win])
        S.dma("pool", wqr[:], dr["wqr"].rearrange("(c p) n -> p c n", p=128), writes=[wqr])
        S.dma("pool", wkn[:], dr["wkn"].rearrange("(c p) n -> p c n", p=128), writes=[wkn])
        S.dma("pool", wkv[:], dr["wkv"].rearrange("(c p) n -> p c n", p=128), writes=[wkv])
        S.dma("sp", qn_t[:], dr["qn"].partition_broadcast(128), writes=[qn_t])
        S.dma("sp", kvn_t[:], dr["kvn"].partition_broadcast(128), writes=[kvn_t])

        with ExitStack() as tctx:
            def tb(name, shape, dt):
                return Buf(tctx.enter_context(nc.sbuf_tensor(S.pfx + name, list(shape), dt)), name)
            posi = tb("posi", [128, NT], I32)
            posf = tb("posf", [128, NT], F32)
            iof = tb("iof", [128, 32], F32)
            invf = tb("invf", [128, 32], F32)
            rr = tb("rr", [128, NT, 32], F32)
            ri = tb("ri", [128, NT * 32], I32)
            rf = tb("rf", [128, NT * 32], F32)
            ff = tb("ff", [128, NT * 32], F32)
            mk = tb("mk", [128, NT * 32], F32)
            S.dma("sp", posi[:], dr["pos"], writes=[posi])
            S.op("dve", lambda e: e.tensor_copy(out=posf[:], in_=posi[:]), reads=[posi], writes=[posf])
            S.op("pool", lambda e: e.iota(iof[:], pattern=[[1, 32]], base=0, channel_multiplier=0,
                                          allow_small_or_imprecise_dtypes=True), writes=[iof])
            S.op("act", lambda e: e.activation(out=invf[:], in_=iof[:], func=AF.Exp, scale=-math.log(10000.0) / 32.0),
                 reads=[iof], writes=[invf])
            S.op("dve", lambda e: e.tensor_scalar(out=invf[:], in0=invf[:], scalar1=1.0 / (2.0 * math.pi), scalar2=None,
                                                  op0=ALU.mult), reads=[invf], writes=[invf])
            for t in range(NT):
                S.op("dve", lambda e: e.tensor_scalar(out=rr[:, t, :], in0=invf[:], scalar1=posf[:, t:t + 1], scalar2=None,
                                                      op0=ALU.mult), reads=[invf, posf], writes=[rr])
            rrf = rr[:].rearrange("p t i -> p (t i)")
            for (shift, outT) in ((0.0, sinT), (0.25, cosT)):
                if shift != 0.0:
                    S.op("dve", lambda e: e.tensor_scalar(out=rrf, in0=rrf, scalar1=shift, scalar2=None, op0=ALU.add),
                         reads=[rr], writes=[rr])
                S.op("dve", lambda e: e.tensor_copy(out=ri[:], in_=rrf), reads=[rr], writes=[ri])
                S.op("dve", lambda e: e.tensor_copy(out=rf[:], in_=ri[:]), reads=[ri], writes=[rf])
                S.op("dve", lambda e: e.tensor_tensor(out=ff[:], in0=rrf, in1=rf[:], op=ALU.subtract),
                     reads=[rr, rf], writes=[ff])
                S.op("dve", lambda e: e.tensor_scalar(out=mk[:], in0=ff[:], scalar1=0.5, scalar2=None, op0=ALU.is_gt),
                     reads=[ff], writes=[mk])
                S.op("dve", lambda e: e.tensor_tensor(out=ff[:], in0=ff[:], in1=mk[:], op=ALU.subtract),
                     reads=[ff, mk], writes=[ff])
                S.op("dve", lambda e: e.tensor_scalar(out=mk[:], in0=ff[:], scalar1=-0.5, scalar2=None, op0=ALU.is_lt),
                     reads=[ff], writes=[mk])
                S.op("dve", lambda e: e.tensor_tensor(out=ff[:], in0=ff[:], in1=mk[:], op=ALU.add),
                     reads=[ff, mk], writes=[ff])
                S.op("dve", lambda e: e.tensor_scalar(out=ff[:], in0=ff[:], scalar1=-0.49999, scalar2=0.49999,
                                                      op0=ALU.max, op1=ALU.min), reads=[ff], writes=[ff])
                S.op("act", lambda e: e.activation(out=outT[:].rearrange("p t i -> p (t i)"), in_=ff[:], func=AF.Sin,
                                                   scale=2.0 * math.pi), reads=[ff], writes=[outT])
            S.barrier()

        xt = sb2("xt", [128, 1024], F32)
        xb = sb2("xb", [128, 1024], BF16)
        xT = sb2("xT", [128, 8, 128], BF16)
        junk = sb2("junk", [128, 384], F32)
        ssq = sb2("ssq", [128, 2], F32)
        rms = sb2("rms", [128, 2], F32)
        rinv = sb2("rinv", [128, 2], F32)
        ckv = sb2("ckv", [128, 256], BF16)
        cq = sb2("cq", [128, 384], BF16)
        kr2 = sb2("kr2", [128, 128], BF16)
        rt = [sb2(f"rt{k}", [128, 32], F32) for k in range(4)]
        qrt = [sb2(f"qrt{k}", [128, 4, 32], F32) for k in range(4)]
        qr = sb2("qr", [128, 4, 64], BF16)
        stg1 = sb2("stg1", [128, 6, 128], BF16)
        stg2 = sb2("stg2", [128, 2, 128], BF16)
        knT_sb = sb2("knT_sb", [128, 4, 128], BF16)
        v_sb = sb2("v_sb", [128, 512], BF16)
        PA1, PA2, PQR, PK, PV = P[1], P[2], P[3], P[4], P[5]

        for t in range(NT):
            if SUBA < 1:
                continue
            b = t % 2
            sl = slice(t * 128, (t + 1) * 128)
            S.dma("sp", xt[b][:], (dr["xmap"](t) if "xmap" in dr else dr["x"][sl, :]), writes=[xt[b]])
            S.op("act", lambda e: e.copy(out=xb[b][:], in_=xt[b][:]), reads=[xt[b]], writes=[xb[b]])
            for c in range(8):
                S.op("pe", lambda e: e.transpose(out=ptB[:, c * 128:(c + 1) * 128], in_=xb[b][:, c * 128:(c + 1) * 128],
                                                 identity=identB[:]), reads=[xb[b], identB], writes=[ptB], inc=(c == 7))
            S.op("dve", lambda e: e.tensor_copy(out=xT[b][:], in_=ptB[:].rearrange("p (c n) -> p c n", c=8)),
                 reads=[ptB], writes=[xT[b]])
            for (pp, lo, n) in ((PA1, 0, 320), (PA2, 320, 384)):
                for c in range(8):
                    S.op("pe", lambda e: e.matmul(pp[:, 0:n], lhsT=xT[b][:, c, :], rhs=win[:, c, lo:lo + n],
                                                  start=(c == 0), stop=(c == 7)),
                         reads=[win, xT[b]], writes=[pp], inc=(c == 7))
            S.op("act", lambda e: e.activation(out=junk[b][:, 0:256], in_=PA1[:, 0:256], func=AF.Square,
                                               accum_out=ssq[b][:, 0:1]), reads=[PA1], writes=[junk[b], ssq[b]])
            S.op("act", lambda e: e.activation(out=junk[b][:, 0:384], in_=PA2[:, 0:384], func=AF.Square,
                                               accum_out=ssq[b][:, 1:2]), reads=[PA2], writes=[junk[b], ssq[b]])
            S.op("act", lambda e: e.activation(out=rms[b][:, 0:1], in_=ssq[b][:, 0:1], func=AF.Sqrt, bias=epsb[:],
                                               scale=1.0 / 256.0), reads=[ssq[b], epsb], writes=[rms[b]])
            S.op("act", lambda e: e.activation(out=rms[b][:, 1:2], in_=ssq[b][:, 1:2], func=AF.Sqrt, bias=epsb[:],
                                               scale=1.0 / 384.0), reads=[ssq[b], epsb], writes=[rms[b]])
            S.op("dve", lambda e: e.reciprocal(out=rinv[b][:], in_=rms[b][:]), reads=[rms[b]], writes=[rinv[b]])
            S.op("dve", lambda e: e.scalar_tensor_tensor(out=ckv[b][:], in0=PA1[:, 0:256], scalar=rinv[b][:, 0:1],
                                                         in1=kvn_t[:], op0=ALU.mult, op1=ALU.mult),
                 reads=[PA1, rinv[b], kvn_t], writes=[ckv[b]])
            S.op("dve", lambda e: e.scalar_tensor_tensor(out=cq[b][:], in0=PA2[:, 0:384], scalar=rinv[b][:, 1:2],
                                                         in1=qn_t[:], op0=ALU.mult, op1=ALU.mult),
                 reads=[PA2, rinv[b], qn_t], writes=[cq[b]])
            if SUBA < 2:
                continue
            cs, sn = cosT[:, t, :], sinT[:, t, :]
            x1, x2 = PA1[:, 256:288], PA1[:, 288:320]
            S.op("dve", lambda e: e.tensor_tensor(out=rt[0][b][:], in0=x1, in1=cs, op=ALU.mult), reads=[PA1, cosT], writes=[rt[0][b]])
            S.op("dve", lambda e: e.tensor_tensor(out=rt[1][b][:], in0=x2, in1=sn, op=ALU.mult), reads=[PA1, sinT], writes=[rt[1][b]])
            S.op("dve", lambda e: e.tensor_tensor(out=rt[2][b][:], in0=x2, in1=cs, op=ALU.mult), reads=[PA1, cosT], writes=[rt[2][b]])
            S.op("dve", lambda e: e.tensor_tensor(out=rt[3][b][:], in0=x1, in1=sn, op=ALU.mult), reads=[PA1, sinT], writes=[rt[3][b]])
            if SUBB < 2:
                continue
            for off in (0, 64):
                S.op("pool", lambda e: e.tensor_tensor(out=kr2[b][:, off:off + 32], in0=rt[0][b][:], in1=rt[1][b][:],
                                                       op=ALU.subtract), reads=[rt[0][b], rt[1][b]], writes=[kr2[b]])
                S.op("pool", lambda e: e.tensor_tensor(out=kr2[b][:, off + 32:off + 64], in0=rt[2][b][:], in1=rt[3][b][:],
                                                       op=ALU.add), reads=[rt[2][b], rt[3][b]], writes=[kr2[b]])
            if SUBB < 3:
                continue
            srcs = [(cq[b], k * 128) for k in range(3)] + [(ckv[b], k * 128) for k in range(2)] + [(kr2[b], 0)]
            for k, (src, o0) in enumerate(srcs):
                S.op("pe", lambda e: e.transpose(out=ptT[:, k * 128:(k + 1) * 128], in_=src[:, o0:o0 + 128], identity=identB[:]),
                     reads=[src, identB], writes=[ptT], inc=(k == 5))
            if SUBB < 4:
                continue
            S.op("act", lambda e: e.copy(out=stg1[b][:], in_=ptT[:, 0:768].rearrange("p (c n) -> p c n", c=6)),
                 reads=[ptT], writes=[stg1[b]])
            for c in range(3):
                S.op("dve", lambda e: e.tensor_copy(out=cqT_all[:, c, sl], in_=stg1[b][:, c, :]),
                     reads=[stg1[b]], writes=[cqT_all])
            S.op("dve", lambda e: e.tensor_copy(out=krT2[:, sl], in_=stg1[b][:, 5, :]), reads=[stg1[b]], writes=[krT2])
            if SUBA < 3:
                continue
            for c in range(3):
                S.op("pe", lambda e: e.matmul(PQR[:, 0:256], lhsT=stg1[b][:, c, :], rhs=wqr[:, c, :],
                                              start=(c == 0), stop=(c == 2)),
                     reads=[stg1[b], wqr], writes=[PQR], inc=(c == 2))
            for h in range(4):
                q1, q2 = PQR[:, h * 64:h * 64 + 32], PQR[:, h * 64 + 32:h * 64 + 64]
                S.op("dve", lambda e: e.tensor_tensor(out=qrt[0][b][:, h, :], in0=q1, in1=cs, op=ALU.mult), reads=[PQR, cosT], writes=[qrt[0][b]])
                S.op("dve", lambda e: e.tensor_tensor(out=qrt[1][b][:, h, :], in0=q2, in1=sn, op=ALU.mult), reads=[PQR, sinT], writes=[qrt[1][b]])
                S.op("dve", lambda e: e.tensor_tensor(out=qrt[2][b][:, h, :], in0=q2, in1=cs, op=ALU.mult), reads=[PQR, cosT], writes=[qrt[2][b]])
                S.op("dve", lambda e: e.tensor_tensor(out=qrt[3][b][:, h, :], in0=q1, in1=sn, op=ALU.mult), reads=[PQR, sinT], writes=[qrt[3][b]])
            S.op("pool", lambda e: e.tensor_tensor(out=qr[b][:, :, 0:32], in0=qrt[0][b][:], in1=qrt[1][b][:], op=ALU.subtract),
                 reads=[qrt[0][b], qrt[1][b]], writes=[qr[b]])
            S.op("pool", lambda e: e.tensor_tensor(out=qr[b][:, :, 32:64], in0=qrt[2][b][:], in1=qrt[3][b][:], op=ALU.add),
                 reads=[qrt[2][b], qrt[3][b]], writes=[qr[b]])
            qrf = qr[b][:].rearrange("p h d -> p (h d)")
            for k in range(2):
                S.op("pe", lambda e: e.transpose(out=ptT[:, 768 + k * 128:768 + (k + 1) * 128], in_=qrf[:, k * 128:(k + 1) * 128],
                                                 identity=identB[:]), reads=[qr[b], identB], writes=[ptT], inc=(k == 1))
            S.op("act", lambda e: e.copy(out=stg2[b][:], in_=ptT[:, 768:1024].rearrange("p (c n) -> p c n", c=2)),
                 reads=[ptT], writes=[stg2[b]])
            for c in range(2):
                S.op("dve", lambda e: e.tensor_copy(out=qrT_all[:, c, sl], in_=stg2[b][:, c, :]),
                     reads=[stg2[b]], writes=[qrT_all])
            if SUBA < 4:
                continue
            for h in range(4):
                for c in range(2):
                    S.op("pe", lambda e: e.matmul(PK[:, h * 128:(h + 1) * 128], lhsT=wkn[:, c, h * 128:(h + 1) * 128],
                                                  rhs=stg1[b][:, 3 + c, :], start=(c == 0), stop=(c == 1)),
                         reads=[wkn, stg1[b]], writes=[PK], inc=(c == 1))
            S.op("dve", lambda e: e.tensor_copy(out=knT_sb[b][:], in_=PK[:].rearrange("p (h n) -> p h n", h=4)),
                 reads=[PK], writes=[knT_sb[b]])
            S.dma("sp", dr["knT_d"][:, :, sl].rearrange("h p n -> p h n"), knT_sb[b][:], reads=[knT_sb[b]], writes=[knT_dd])
            for c in range(2):
                S.op("pe", lambda e: e.matmul(PV[:], lhsT=stg1[b][:, 3 + c, :], rhs=wkv[:, c, :], start=(c == 0), stop=(c == 1)),
                     reads=[wkv, stg1[b]], writes=[PV], inc=(c == 1))
            S.op("dve", lambda e: e.tensor_copy(out=v_sb[b][:], in_=PV[:]), reads=[PV], writes=[v_sb[b]])
            S.dma("sp", dr["v_d"][sl, :], v_sb[b][:], reads=[v_sb[b]], writes=[v_dd])
        S.barrier()

    if STAGE < 2:
        return
    with ExitStack() as pctx:
        def sb(name, shape, dt):
            return Buf(pctx.enter_context(nc.sbuf_tensor(S.pfx + name, list(shape), dt)), name)
        def sb2(name, shape, dt):
            return [sb(f"{name}{i}", shape, dt) for i in range(2)]
        knT = sb("knT", [128, SEQ], BF16)
        vh = sb("vh", [128, NT, 128], BF16)
        sc = sb("sc", [128, SEQ], F32)
        Pb = sb("Pb", [128, SEQ], BF16)
        PT = sb("PT", [128, NT, 128], BF16)
        qnT = sb2("qnT", [128, 128], BF16)
        qrz = sb2("qrz", [128, 128], BF16)
        mx = sb2("mx", [128, 16], F32)
        mrow = sb2("mrow", [128, 1], F32)
        negm = sb2("negm", [128, 1], F32)
        rsum = sb2("rsum", [128, 1], F32)
        rinv2 = sb2("rinv2", [128, 1], F32)
        osb = sb2("osb", [128, 128], BF16)
        PQN = P[1]
        PS = [P[2], P[3]]
        PO = [P[4], P[5]]
        ptP = [ptB, ptT]
        it = 0
        for h in range(4):
            hb, jj = h // 2, h % 2
            S.dma("sp", knT[:], dr["knT_d"][h, :, :], writes=[knT])
            for t0 in range(0, NT, 8):
                t1 = min(NT, t0 + 8)
                S.dma("sp", vh[:, t0:t1, :],
                      dr["v_d"][t0 * 128:t1 * 128, h * 128:(h + 1) * 128].rearrange("(t p) d -> p t d", p=128),
                      writes=[vh])
            for qb in range(NT):
                b = it % 2
                it += 1
                qs = slice(qb * 128, (qb + 1) * 128)
                nkeys = (qb + 1) * 128
                for c in range(3):
                    S.op("pe", lambda e: e.matmul(PQN[:, 0:128], lhsT=wqn[:, c, h * 128:(h + 1) * 128], rhs=cqT_all[:, c, qs],
                                                  start=(c == 0), stop=(c == 2)),
                         reads=[wqn, cqT_all], writes=[PQN], inc=(c == 2))
                S.op("dve", lambda e: e.tensor_copy(out=qnT[b][:], in_=PQN[:, 0:128]), reads=[PQN], writes=[qnT[b]])
                S.op("pool", lambda e: e.tensor_scalar(out=qrz[b][:], in0=qrT_all[:, hb, qs], scalar1=hm[:, jj:jj + 1],
                                                       scalar2=None, op0=ALU.mult), reads=[qrT_all, hm], writes=[qrz[b]])
                nch = (nkeys + 511) // 512
                for kc in range(nch):
                    k0 = kc * 512
                    n = min(512, nkeys - k0)
                    pp = PS[kc % 2]
                    last = (kc == nch - 1)
                    S.op("pe", lambda e: e.matmul(pp[:, 0:n], lhsT=qnT[b][:], rhs=knT[:, k0:k0 + n], start=True, stop=False),
                         reads=[qnT[b], knT], writes=[pp], inc=False)
                    S.op("pe", lambda e: e.matmul(pp[:, 0:n], lhsT=qrz[b][:], rhs=krT2[:, k0:k0 + n], start=False, stop=(not last)),
                         reads=[qrz[b], krT2], writes=[pp], inc=(not last))
                    if last:
                        S.op("pe", lambda e: e.matmul(pp[:, n - 128:n], lhsT=identB[:], rhs=NEG[:], start=False, stop=True),
                             reads=[identB, NEG], writes=[pp])
                    S.op("dve", lambda e: e.tensor_scalar(out=sc[:, k0:k0 + n], in0=pp[:, 0:n], scalar1=1.0, scalar2=None,
                                                          op0=ALU.mult, op1=ALU.max, accum_out=mx[b][:, kc:kc + 1]),
                         reads=[pp], writes=[sc, mx[b]])
                S.op("dve", lambda e: e.tensor_reduce(out=mrow[b][:], in_=mx[b][:, 0:nch], axis=AX.X, op=ALU.max),
                     reads=[mx[b]], writes=[mrow[b]])
                S.op("dve", lambda e: e.tensor_scalar(out=negm[b][:], in0=mrow[b][:], scalar1=-SCALE, scalar2=None, op0=ALU.mult),
                     reads=[mrow[b]], writes=[negm[b]])
                S.op("act", lambda e: e.activation(out=Pb[:, 0:nkeys], in_=sc[:, 0:nkeys], func=AF.Exp, scale=SCALE,
                                                   bias=negm[b][:], accum_out=rsum[b][:]),
                     reads=[sc, negm[b]], writes=[Pb, rsum[b]])
                nkb = qb + 1
                for g0 in range(0, nkb, 8):
                    g1 = min(nkb, g0 + 8)
                    pt = ptP[(g0 // 8) % 2]
                    for kb in range(g0, g1):
                        S.op("pe", lambda e: e.transpose(out=pt[:, (kb - g0) * 128:(kb - g0 + 1) * 128],
                                                         in_=Pb[:, kb * 128:(kb + 1) * 128], identity=identB[:]),
                             reads=[Pb, identB], writes=[pt], inc=(kb == g1 - 1))
                    S.op("act", lambda e: e.copy(out=PT[:, g0:g1, :],
                                                 in_=pt[:, 0:(g1 - g0) * 128].rearrange("p (c n) -> p c n", n=128)),
                         reads=[pt], writes=[PT])
                po = PO[b]
                for kb in range(nkb):
                    S.op("pe", lambda e: e.matmul(po[:, 0:128], lhsT=PT[:, kb, :], rhs=vh[:, kb, :],
                                                  start=(kb == 0), stop=(kb == nkb - 1)),
                         reads=[PT, vh], writes=[po], inc=(kb == nkb - 1))
                S.op("dve", lambda e: e.reciprocal(out=rinv2[b][:], in_=rsum[b][:]), reads=[rsum[b]], writes=[rinv2[b]])
                S.op("dve", lambda e: e.tensor_scalar(out=osb[b][:], in0=po[:, 0:128], scalar1=rinv2[b][:], scalar2=None,
                                                      op0=ALU.mult), reads=[po, rinv2[b]], writes=[osb[b]])
                S.dma("sp", dr["out"][qs, h * 128:(h + 1) * 128], osb[b][:], reads=[osb[b]], writes=[out_d])
        S.barrier()


def build_k2(SEQ):
    nc = bass.Bass("TRN2", target_bir_lowering=False)
    dr = {}
    def din(name, shape, dt=F32):
        dr[name] = nc.dram_tensor("i_" + name, list(shape), dt, kind="ExternalInput").ap()
    NT = SEQ // 128
    din("x", [SEQ, D]); din("pos", [128, NT], I32); din("w_in", [D, 704]); din("qn", [384]); din("kvn", [256])
    din("wqn", [384, 512]); din("wqr", [384, 256]); din("wkn", [256, 512]); din("wkv", [256, 512])
    dr["out"] = nc.dram_tensor("out", [SEQ, 512], BF16, kind="ExternalOutput").ap()
    dr["knT_d"] = nc.dram_tensor("knT_d", [4, 128, SEQ], BF16, kind="Internal").ap()
    dr["v_d"] = nc.dram_tensor("v_d", [SEQ, 512], BF16, kind="Internal").ap()
    with ExitStack() as ctx:
        S = Sched(nc, ctx)
        ps = {"ptB": S.psum("ptB", [128, 1024], BF16), "ptT": S.psum("ptT", [128, 1024], BF16),
              "P": [None] + [S.psum(f"P{i}", [128, 512], F32) for i in range(1, 7)]}
        emit_mla(S, nc, SEQ, dr, ps)
        pass
    return nc


def mla_inputs(x_b, pos_b, w_in, q_norm, kv_norm, w_qb, w_kvb, g):
    SEQ = x_b.shape[0]
    H = 8
    hs = slice(g * 4, g * 4 + 4)
    w_in2 = np.ascontiguousarray(np.concatenate([w_in[:, 384:640], w_in[:, 640:704], w_in[:, 0:384]], axis=1))
    wq = w_qb.reshape(384, H, 192)[:, hs]
    wqn = np.ascontiguousarray(wq[:, :, 0:128].reshape(384, 512))
    wqr = np.ascontiguousarray(wq[:, :, 128:192].reshape(384, 256))
    wkv_ = w_kvb.reshape(256, H, 256)[:, hs]
    wkn = np.ascontiguousarray(wkv_[:, :, 0:128].reshape(256, 512))
    wkv = np.ascontiguousarray(wkv_[:, :, 128:256].reshape(256, 512))
    pos2 = np.ascontiguousarray(pos_b.reshape(SEQ // 128, 128).T.astype(np.int32))
    return {"i_x": np.ascontiguousarray(x_b), "i_pos": pos2, "i_w_in": w_in2, "i_qn": np.ascontiguousarray(q_norm),
            "i_kvn": np.ascontiguousarray(kv_norm), "i_wqn": wqn, "i_wqr": wqr, "i_wkn": wkn, "i_wkv": wkv}


def run_phase(S, pfx, fn):
    with ExitStack() as pctx:
        old = S.ctx
        S.ctx = pctx
        S.pfx = pfx
        fn()
        S.ctx = old
        S.pfx = ""


def build_fused(SEQ, CAP_, NB, depth=4):
    TOK = SEQ // 2
    NT3 = TOK // 128
    nc = bass.Bass("TRN2", target_bir_lowering=False)
    ext = {}

    def din(name, shape, dt=F32):
        ext[name] = nc.dram_tensor(name, list(shape), dt, kind="ExternalInput").ap()

    def dint(name, shape, dt):
        return nc.dram_tensor(name, list(shape), dt, kind="Internal").ap()

    din("x_full", [SEQ, D]); din("x_own", [TOK, D]); din("hgidx", [128, NT3, 2], I32)
    din("pos", [128, SEQ // 128], I32)
    for j in range((depth + 1) // 2):
        din(f"m{j}_w_fm", [D, 512]); din(f"m{j}_w_tm", [D, 1288]); din(f"m{j}_b_g", [8]); din(f"m{j}_gain", [512])
    for j in range(depth // 2):
        din(f"a{j}_w_in", [D, 704]); din(f"a{j}_qn", [384]); din(f"a{j}_kvn", [256])
        din(f"a{j}_wqn", [384, 512]); din(f"a{j}_wqr", [384, 256]); din(f"a{j}_wkn", [256, 512]); din(f"a{j}_wkv", [256, 512])
    for l in range(depth):
        din(f"l{l}_w_out", [D, D])
        for nm in ("g1", "b1", "g2", "b2"):
            din(f"l{l}_{nm}", [D])
        din(f"l{l}_w_rt", [D, E]); din(f"l{l}_b_rt", [E]); din(f"l{l}_w_gu", [E, D, 2 * D]); din(f"l{l}_b_gu", [E, 2 * D])
        din(f"l{l}_w_dn", [E, D, D]); din(f"l{l}_b_dn", [E, D])
    out_ap = nc.dram_tensor("out", [TOK, D], F32, kind="ExternalOutput").ap()
    hg_own = dint("hg_own", [SEQ, 512], BF16)
    hg_gath = dint("hg_gath", [2 * SEQ, 512], BF16)
    xn = [dint(f"xn{i}", [TOK, D], F32) for i in range(2)]
    xfull_g = dint("xfull_g", [SEQ, D], F32)
    knT_d = dint("knT_d", [4, 128, SEQ], BF16)
    v_d = dint("v_d", [SEQ, 512], BF16)
    xg = dint("xg", [E * CAP_, D], BF16)
    yg = dint("yg", [E * CAP_, D], F32)
    x1s = dint("x1s", [TOK, D], F32)
    groups = [[2 * b, 2 * b + 1] for b in range(NB)]
    CHX = min(512, TOK)
    CHH = min(2048, SEQ)

    def xmap(t):
        row = t * 128
        r = row // TOK
        lrow = row - r * TOK
        k, off = lrow // CHX, lrow % CHX
        r0 = k * 2 * CHX + r * CHX + off
        return xfull_g[r0:r0 + 128, :]
    with ExitStack() as ctx:
        S = Sched(nc, ctx)
        ptB = S.psum("ptB", [128, 1024], BF16)
        Pf = [S.psum(f"P{i}", [128, 512], F32) for i in range(1, 7)]
        ptT = S.psum("ptT", [128, 1024], BF16)
        P7 = Buf(ptT.t[:].bitcast(F32), "P7")
        ps13 = {"ptB": ptB, "P": [None] + Pf + [P7]}
        ps2 = {"ptB": ptB, "ptT": ptT, "P": [None] + Pf}
        for l in range(depth):
            j = l // 2
            xsrc = ext["x_full"] if l == 0 else xfull_g
            if l % 2 == 0:
                dr = {"x": xsrc, **({"xmap": xmap} if l > 0 else {}), "w_fm": ext[f"m{j}_w_fm"], "w_tm": ext[f"m{j}_w_tm"], "b_g": ext[f"m{j}_b_g"],
                      "gain": ext[f"m{j}_gain"], "out": hg_own}
                run_phase(S, f"L{l}m_", lambda: emit_mlstm(S, nc, SEQ, dr, ps13))
            else:
                dr = {"x": xsrc, **({"xmap": xmap} if l > 0 else {}), "pos": ext["pos"], "w_in": ext[f"a{j}_w_in"], "qn": ext[f"a{j}_qn"], "kvn": ext[f"a{j}_kvn"],
                      "wqn": ext[f"a{j}_wqn"], "wqr": ext[f"a{j}_wqr"], "wkn": ext[f"a{j}_wkn"], "wkv": ext[f"a{j}_wkv"],
                      "out": hg_own, "knT_d": knT_d, "v_d": v_d}
                run_phase(S, f"L{l}a_", lambda: emit_mla(S, nc, SEQ, dr, ps2))
            for k in range(SEQ // CHH):
                S.cc("AllGather", groups, hg_own[k * CHH:(k + 1) * CHH, :], hg_gath[k * 2 * CHH:(k + 1) * 2 * CHH, :], inc=1)
            S.barrier()
            dr = {"x": ext["x_own"] if l == 0 else xn[(l - 1) % 2], "hg_gath": hg_gath, "hgidx": ext["hgidx"],
                  "out": out_ap if l == depth - 1 else xn[l % 2], "xg": xg, "yg": yg, "x1s": x1s}
            for nm in ("w_out", "g1", "b1", "g2", "b2", "w_rt", "b_rt", "w_gu", "b_gu", "w_dn", "b_dn"):
                dr[nm] = ext[f"l{l}_{nm}"]
            run_phase(S, f"L{l}p_", lambda: emit_post(S, nc, TOK, CAP_, dr, ps13))
            if l < depth - 1:
                for k in range(TOK // CHX):
                    S.cc("AllGather", groups, xn[l % 2][k * CHX:(k + 1) * CHX, :],
                         xfull_g[k * 2 * CHX:(k + 1) * 2 * CHX, :], inc=1)
                S.barrier()
    return nc


def fused_in_maps(x, positions, ln_gain, ln_bias, mlstm_w_in, mlstm_b_gates, mlstm_norm_gain, mlstm_w_out,
                  mla_w_in, mla_q_norm, mla_kv_norm, mla_w_qb, mla_w_kvb, mla_w_out,
                  moe_w_router, moe_b_router, moe_w_gate_up, moe_b_gate_up, moe_w_down, moe_b_down):
    f32 = np.float32
    A = lambda a: np.ascontiguousarray(np.asarray(a, dtype=f32))
    x = A(x)
    positions = np.asarray(positions)
    B, S_, _ = x.shape
    TOK = S_ // 2
    NT3 = TOK // 128
    depth = ln_gain.shape[0]
    shared = {}
    for l in range(depth):
        j = l // 2
        shared[f"l{l}_w_out"] = A(mlstm_w_out[j]) if l % 2 == 0 else A(mla_w_out[j])
        shared[f"l{l}_g1"] = A(ln_gain[l, 0]); shared[f"l{l}_b1"] = A(ln_bias[l, 0])
        shared[f"l{l}_g2"] = A(ln_gain[l, 1]); shared[f"l{l}_b2"] = A(ln_bias[l, 1])
        shared[f"l{l}_w_rt"] = A(moe_w_router[l]); shared[f"l{l}_b_rt"] = A(moe_b_router[l])
        shared[f"l{l}_w_gu"] = A(moe_w_gate_up[l]); shared[f"l{l}_b_gu"] = A(moe_b_gate_up[l])
        shared[f"l{l}_w_dn"] = A(moe_w_down[l]); shared[f"l{l}_b_dn"] = A(moe_b_down[l])
    in_maps = []
    for c in range(2 * B):
        b, g = c // 2, c % 2
        m = dict(shared)
        m["x_full"] = np.ascontiguousarray(x[b])
        m["x_own"] = np.ascontiguousarray(x[b, g * TOK:(g + 1) * TOK])
        p = np.arange(128, dtype=np.int64)[:, None, None]
        i = np.arange(NT3, dtype=np.int64)[None, :, None]
        r = np.arange(2, dtype=np.int64)[None, None, :]
        chh = min(2048, S_)
        tok = g * TOK + i * 128 + p
        m["hgidx"] = np.ascontiguousarray(((tok // chh) * 2 * chh + r * chh + tok % chh).astype(np.int32))
        for j in range((depth + 1) // 2):
            mi = mlstm_inputs(x[b], A(mlstm_w_in[j]), A(mlstm_b_gates[j]), A(mlstm_norm_gain[j]), g)
            for k in ("w_fm", "w_tm", "b_g", "gain"):
                m[f"m{j}_{k}"] = mi[k]
        for j in range(depth // 2):
            ai = mla_inputs(x[b], positions[b], A(mla_w_in[j]), A(mla_q_norm[j]), A(mla_kv_norm[j]),
                            A(mla_w_qb[j]), A(mla_w_kvb[j]), g)
            m["pos"] = ai["i_pos"]
            for k in ("w_in", "qn", "kvn", "wqn", "wqr", "wkn", "wkv"):
                m[f"a{j}_{k}"] = ai["i_" + k]
        in_maps.append(m)
    return in_maps


_NC_CACHE = {}


def kernel(x, positions, ln_gain, ln_bias, mlstm_w_in, mlstm_b_gates, mlstm_norm_gain, mlstm_w_out,
           mla_w_in, mla_q_norm, mla_kv_norm, mla_w_qb, mla_w_kvb, mla_w_out,
           moe_w_router, moe_b_router, moe_w_gate_up, moe_b_gate_up, moe_w_down, moe_b_down):
    x = np.asarray(x)
    B, S_, _ = x.shape
    TOK = S_ // 2
    depth = ln_gain.shape[0]
    cap = CAP if S_ == SEQ_FULL else 128
    key = (S_, B, depth)
    if key not in _NC_CACHE:
        _NC_CACHE[key] = build_fused(S_, cap, B, depth)
    nc = _NC_CACHE[key]
    in_maps = fused_in_maps(x, positions, ln_gain, ln_bias, mlstm_w_in, mlstm_b_gates, mlstm_norm_gain, mlstm_w_out,
                            mla_w_in, mla_q_norm, mla_kv_norm, mla_w_qb, mla_w_kvb, mla_w_out,
                            moe_w_router, moe_b_router, moe_w_gate_up, moe_b_gate_up, moe_w_down, moe_b_down)
    res = run_bass_kernel_spmd(nc, in_maps, core_ids=list(range(2 * B)))
    out = np.empty((B, S_, D), dtype=np.float32)
    for c in range(2 * B):
        b, g = c // 2, c % 2
        out[b, g * TOK:(g + 1) * TOK] = res.results[c]["out"]
    return out
```

```python
import math
from contextlib import ExitStack
import numpy as np
import concourse.bass as bass
import concourse.mybir as mybir
from concourse.bass_utils import run_bass_kernel_spmd

F32 = mybir.dt.float32
BF16 = mybir.dt.bfloat16
I32 = mybir.dt.int32
U32 = mybir.dt.uint32
AF = mybir.ActivationFunctionType
ALU = mybir.AluOpType
AX = mybir.AxisListType


class Buf:
    __slots__ = ("t", "w", "r", "name")

    def __init__(self, t=None, name=""):
        self.t = t
        self.w = {}
        self.r = {}
        self.name = name

    def __getitem__(self, idx):
        return self.t[idx]


class _Eng:
    def __init__(self, name, eng, sem):
        self.name = name
        self.eng = eng
        self.sem = sem
        self.count = 0
        self.waited = {}


class Sched:
    def __init__(self, nc, ctx, n_dma_sems=12, same_engine_sync=True):
        self.nc = nc
        self.ctx = ctx
        self.same_engine_sync = same_engine_sync
        self.E = {}
        for name, eng in (("pe", nc.tensor), ("dve", nc.vector), ("act", nc.scalar),
                          ("pool", nc.gpsimd), ("sp", nc.sync)):
            sem = ctx.enter_context(nc.semaphore("s_" + name))
            self.E[name] = _Eng(name, eng, sem)
        self.dma_sems = {}
        for q in ("sp", "act", "pool"):
            lst = []
            for i in range(n_dma_sems):
                s = ctx.enter_context(nc.semaphore(f"d_{q}{i}"))
                lst.append([s, 0])
            self.dma_sems[q] = [lst, 0]
        self.ninst = 0
        self.pfx = ""

    def sbuf(self, name, shape, dt):
        t = self.ctx.enter_context(self.nc.sbuf_tensor(self.pfx + name, list(shape), dt))
        return Buf(t, name)

    def psum(self, name, shape, dt):
        t = self.ctx.enter_context(self.nc.psum_tensor(name, list(shape), dt))
        return Buf(t, name)

    @staticmethod
    def _key(sem):
        return id(sem)

    def _need(self, reads, writes):
        need = {}
        def add(d):
            for k, (s, v) in d.items():
                if k not in need or need[k][1] < v:
                    need[k] = (s, v)
        for b in reads:
            add(b.w)
        for b in writes:
            add(b.w)
            add(b.r)
        return need

    def _do_waits(self, e, need):
        for k, (s, v) in need.items():
            if s is e.sem and (e.name == "pe" or not self.same_engine_sync):
                continue
            if e.waited.get(k, 0) < v:
                e.eng.wait_ge(s, v)
                e.waited[k] = v

    def _record(self, dep_sem, dep_val, reads, writes):
        k = self._key(dep_sem)
        for b in writes:
            b.w = {k: (dep_sem, dep_val)}
            b.r = {}
        for b in reads:
            if b in writes:
                continue
            if k not in b.r or b.r[k][1] < dep_val:
                b.r[k] = (dep_sem, dep_val)

    def op(self, eng, fn, reads=(), writes=(), inc=True):
        e = self.E[eng]
        self._do_waits(e, self._need(reads, writes))
        inst = fn(e.eng)
        self.ninst += 1
        if inc:
            inst.then_inc(e.sem, 1)
            e.count += 1
            self._record(e.sem, e.count, reads, writes)
        else:
            self._record(e.sem, e.count + 1, reads, writes)
        return inst

    def dma(self, q, out, in_, reads=(), writes=(), indirect=None, **kw):
        e = self.E[q]
        lst, pos = self.dma_sems[q]
        ent = lst[pos]
        self.dma_sems[q][1] = (pos + 1) % len(lst)
        s, cnt = ent
        need = self._need(reads, writes)
        if cnt > 0:
            need[self._key(s)] = (s, cnt)
        for k, (ss, v) in need.items():
            if e.waited.get(k, 0) < v:
                e.eng.wait_ge(ss, v)
                e.waited[k] = v
        if indirect is None:
            inst = e.eng.dma_start(out=out, in_=in_, **kw)
        else:
            inst = e.eng.indirect_dma_start(out=out, in_=in_, **indirect, **kw)
        self.ninst += 1
        inst.then_inc(s, 16)
        ent[1] = cnt + 16
        self._record(s, cnt + 16, reads, writes)
        return inst

    def cc(self, kind, groups, in_ap, out_ap, reads=(), writes=(), inc=16):
        e = self.E["pool"]
        lst, pos = self.dma_sems["pool"]
        ent = lst[pos]
        self.dma_sems["pool"][1] = (pos + 1) % len(lst)
        s, cnt = ent
        need = self._need(reads, writes)
        if cnt > 0:
            need[self._key(s)] = (s, cnt)
        for k, (ss, v) in need.items():
            if e.waited.get(k, 0) < v:
                e.eng.wait_ge(ss, v)
                e.waited[k] = v
        inst = e.eng.collective_compute(kind, ALU.bypass, replica_groups=groups, ins=[in_ap], outs=[out_ap])
        self.ninst += 1
        inst.then_inc(s, inc)
        ent[1] = cnt + inc
        self._record(s, cnt + inc, reads, writes)
        return inst

    def barrier(self):
        targets = []
        for name, o in self.E.items():
            if o.count > 0:
                targets.append((o, o.sem, o.count))
        for q, (lst, _) in self.dma_sems.items():
            for s, cnt in lst:
                if cnt > 0:
                    targets.append((None, s, cnt))
        for name in ("sp", "pool", "act", "dve", "pe"):
            e = self.E[name]
            for (o, s, v) in targets:
                if o is e:
                    continue
                k = self._key(s)
                if e.waited.get(k, 0) < v:
                    e.eng.wait_ge(s, v)
                    e.waited[k] = v

    def finish(self, bufs):
        need = self._need((), bufs)
        for name in ("sp", "pool", "act", "dve", "pe"):
            e = self.E[name]
            for k, (s, v) in need.items():
                if s is e.sem:
                    continue
                if e.waited.get(k, 0) < v:
                    e.eng.wait_ge(s, v)
                    e.waited[k] = v


def run_interleaved(gens, width=2):
    it = iter(gens)
    active = []
    while True:
        while len(active) < width:
            g = next(it, None)
            if g is None:
                break
            active.append(g)
        if not active:
            break
        for g in list(active):
            try:
                next(g)
            except StopIteration:
                active.remove(g)


D = 1024
E = 32
ALPHA = 8.0 ** 0.25
LN_EPS = 1e-5
RMS_EPS = 1e-6
SCALE = 192.0 ** -0.5
STAGE = SUB = SUBA = SUBB = SUBC = 99
SEQ_FULL = 8192
TOK_CORE = 4096
CAP = 640

def emit_ln(S, z, gain, bias, out, tmp):
    st, mv, sd, rstd, xn = tmp
    for h in range(2):
        S.op("dve", lambda e: e.bn_stats(out=st[:, h, :], in_=z[:, h * 512:(h + 1) * 512]),
             reads=[z], writes=[st])
    S.op("dve", lambda e: e.bn_aggr(out=mv[:], in_=st[:].rearrange("p a b -> p (a b)")),
         reads=[st], writes=[mv])
    S.op("act", lambda e: e.activation(out=sd[:], in_=mv[:, 1:2], func=AF.Sqrt, bias=tmp_eps(S)[:], scale=1.0),
         reads=[mv], writes=[sd])
    S.op("dve", lambda e: e.reciprocal(out=rstd[:], in_=sd[:]), reads=[sd], writes=[rstd])
    S.op("dve", lambda e: e.tensor_scalar(out=sd[:], in0=mv[:, 0:1], scalar1=rstd[:], scalar2=-1.0,
                                          op0=ALU.mult, op1=ALU.mult), reads=[mv, rstd], writes=[sd])
    S.op("act", lambda e: e.activation(out=xn[:], in_=z[:], func=AF.Identity, scale=rstd[:], bias=sd[:]),
         reads=[z, rstd, sd], writes=[xn])
    S.op("dve", lambda e: e.tensor_tensor(out=xn[:], in0=xn[:], in1=gain[:], op=ALU.mult),
         reads=[xn, gain], writes=[xn])
    S.op("dve", lambda e: e.tensor_tensor(out=out[:], in0=xn[:], in1=bias[:], op=ALU.add),
         reads=[xn, bias], writes=[out])


_EPS = {}


def tmp_eps(S):
    return _EPS[id(S)]


def emit_post(S, nc, T, CAP, dr, ps):
    NT = T // 128
    RB = CAP // 128
    parts = []
    lo = 0
    while lo < CAP:
        n = min(512, CAP - lo)
        parts.append((lo, n))
        lo += n
    ctx = S.ctx
    identF = S.sbuf("identF", [128, 128], F32)
    identB = S.sbuf("identB", [128, 128], BF16)
    triS = S.sbuf("triS", [128, 128], F32)
    onesM = S.sbuf("onesM", [128, 128], F32)
    offs = S.sbuf("offs", [128, E], F32)
    epsb = S.sbuf("epsb", [128, 1], F32)
    _EPS[id(S)] = epsb
    idx_all = S.sbuf("idx_all", [128, NT, 4], I32)
    gate_all = S.sbuf("gate_all", [128, NT, 4], F32)
    wgu = [S.sbuf(f"wgu{i}", [128, 8, 2048], BF16) for i in range(2)]
    wd = [S.sbuf(f"wd{i}", [128, 8, 1024], BF16) for i in range(2)]
    bgu = [S.sbuf(f"bgu{i}", [128, 8, 2], F32) for i in range(2)]
    bdn = [S.sbuf(f"bdn{i}", [128, 1024], F32) for i in range(2)]
    lnp = [S.sbuf(f"lnp{i}", [128, 1024], F32) for i in range(4)]
    st = S.sbuf("st", [128, 2, 6], F32)
    mv = S.sbuf("mv", [128, 2], F32)
    sd = S.sbuf("sd", [128, 1], F32)
    rstd = S.sbuf("rstd", [128, 1], F32)
    xn = S.sbuf("xn", [128, 1024], F32)
    lntmp = (st, mv, sd, rstd, xn)
    xg_d = Buf(None, "xg")
    yg_d = Buf(None, "yg")
    x1_d = Buf(None, "x1s")
    out_d = Buf(None, "out")

    S.op("pool", lambda e: e.memset(identF[:], 0.0), writes=[identF])
    S.op("pool", lambda e: e.affine_select(out=identF[:], in_=identF[:], pattern=[[-1, 128]],
                                           compare_op=ALU.not_equal, fill=1.0, base=0,
                                           channel_multiplier=1), reads=[identF], writes=[identF])
    S.op("dve", lambda e: e.tensor_copy(out=identB[:], in_=identF[:]), reads=[identF], writes=[identB])
    S.op("pool", lambda e: e.memset(onesM[:], 1.0), writes=[onesM])
    S.op("pool", lambda e: e.memset(epsb[:], LN_EPS), writes=[epsb])
    S.op("pool", lambda e: e.affine_select(out=triS[:], in_=onesM[:], pattern=[[1, 128]],
                                           compare_op=ALU.is_gt, fill=0.0, base=0,
                                           channel_multiplier=-1), reads=[onesM], writes=[triS])
    S.op("pool", lambda e: e.iota(offs[:], pattern=[[CAP, E]], base=0, channel_multiplier=0,
                                  allow_small_or_imprecise_dtypes=True), writes=[offs])
    for j, nm in enumerate(("g1", "b1", "g2", "b2")):
        S.dma("sp", lnp[j][:], dr[nm].partition_broadcast(128), writes=[lnp[j]])

    with ExitStack() as pctx:
        def sb(name, shape, dt):
            return Buf(pctx.enter_context(nc.sbuf_tensor(S.pfx + name, list(shape), dt)), name)
        wout = sb("wout", [128, 8, 1024], BF16)
        wrt = sb("wrt", [128, 8, E], F32)
        brt = sb("brt", [128, E], F32)
        baseoffs = sb("baseoffs", [128, E], F32)
        hgt = [sb(f"hgt{i}", [128, 1024], BF16) for i in range(2)]
        xt = [sb(f"xt{i}", [128, 1024], F32) for i in range(2)]
        hgT = [sb(f"hgT{i}", [128, 8, 128], BF16) for i in range(2)]
        z = [sb(f"z{i}", [128, 1024], F32) for i in range(2)]
        x1 = [sb(f"x1{i}", [128, 1024], F32) for i in range(2)]
        x1b = [sb(f"x1b{i}", [128, 1024], BF16) for i in range(2)]
        x1T = [sb(f"x1T{i}", [128, 8, 128], F32) for i in range(2)]
        lg = [sb(f"lg{i}", [128, E], F32) for i in range(2)]
        top8 = [sb(f"top8{i}", [128, 8], F32) for i in range(2)]
        mask = [sb(f"mask{i}", [128, E], F32) for i in range(2)]
        nmx = [sb(f"nmx{i}", [128, 1], F32) for i in range(2)]
        ex = [sb(f"ex{i}", [128, 4], F32) for i in range(2)]
        sm = [sb(f"sm{i}", [128, 1], F32) for i in range(2)]
        rs = [sb(f"rs{i}", [128, 1], F32) for i in range(2)]
        posf = [sb(f"posf{i}", [128, E], F32) for i in range(2)]
        oh = [sb(f"oh{i}", [128, E], F32) for i in range(2)]
        junk = [sb(f"junk{i}", [128, E], F32) for i in range(2)]
        destf = [sb(f"destf{i}", [128, 4], F32) for i in range(2)]

        if "hg_gath" in dr:
            hgidx = sb("hgidx", [128, NT, 2], I32)
            S.dma("sp", hgidx[:], dr["hgidx"], writes=[hgidx])
        for c in range(0, 8, 2):
            S.dma("pool", wout[:, c:c + 2, :],
                  dr["w_out"][c * 128:(c + 2) * 128, :].rearrange("(c p) n -> p c n", p=128), writes=[wout])
        S.dma("sp", wrt[:], dr["w_rt"].rearrange("(c p) n -> p c n", p=128), writes=[wrt])
        S.dma("sp", brt[:], dr["b_rt"].partition_broadcast(128), writes=[brt])
        S.op("dve", lambda e: e.tensor_copy(out=baseoffs[:], in_=offs[:]), reads=[offs], writes=[baseoffs])

        ptB, P = ps["ptB"], ps["P"]
        def _tile(i):
            b = i % 2
            sl = slice(i * 128, (i + 1) * 128)
            if "hg_gath" in dr:
                for r in range(2):
                    S.dma("pool", hgt[b][:, r * 512:(r + 1) * 512], dr["hg_gath"], reads=[hgidx], writes=[hgt[b]],
                          indirect=dict(out_offset=None,
                                        in_offset=bass.IndirectOffsetOnAxis(ap=hgidx[:, i, r:r + 1], axis=0)))
            else:
                S.dma("sp", hgt[b][:], dr["hg"][sl, :], writes=[hgt[b]])
            S.dma("sp", xt[b][:], dr["x"][sl, :], writes=[xt[b]])
            for c in range(8):
                S.op("pe", lambda e: e.transpose(out=ptB[:, c * 128:(c + 1) * 128],
                                                 in_=hgt[b][:, c * 128:(c + 1) * 128], identity=identB[:]),
                     reads=[hgt[b], identB], writes=[ptB], inc=(c == 7))
            S.op("act", lambda e: e.copy(out=hgT[b][:], in_=ptB[:].rearrange("p (c n) -> p c n", c=8)),
                 reads=[ptB], writes=[hgT[b]])
            for h in range(2):
                pm = P[1 + h]
                for c in range(8):
                    S.op("pe", lambda e: e.matmul(pm[:], lhsT=hgT[b][:, c, :], rhs=wout[:, c, h * 512:(h + 1) * 512],
                                                  start=(c == 0), stop=(c == 7)),
                         reads=[hgT[b], wout], writes=[pm], inc=(c == 7))
                S.op("dve", lambda e: e.scalar_tensor_tensor(out=z[b][:, h * 512:(h + 1) * 512],
                                                             in0=xt[b][:, h * 512:(h + 1) * 512], scalar=ALPHA,
                                                             in1=pm[:], op0=ALU.mult, op1=ALU.add),
                     reads=[xt[b], pm], writes=[z[b]])
            yield
            emit_ln(S, z[b], lnp[0], lnp[1], x1[b], lntmp)
            yield
            S.dma("sp", dr["x1s"][sl, :], x1[b][:], reads=[x1[b]], writes=[x1_d])
            S.op("act", lambda e: e.copy(out=x1b[b][:], in_=x1[b][:]), reads=[x1[b]], writes=[x1b[b]])
            for h in range(2):
                pT = P[3 + h]
                for c in range(4):
                    cc = h * 4 + c
                    S.op("pe", lambda e: e.transpose(out=pT[:, c * 128:(c + 1) * 128],
                                                     in_=x1[b][:, cc * 128:(cc + 1) * 128], identity=identF[:]),
                         reads=[x1[b], identF], writes=[pT], inc=(c == 3))
                S.op("act", lambda e: e.copy(out=x1T[b][:, h * 4:(h + 1) * 4, :],
                                             in_=pT[:].rearrange("p (c n) -> p c n", c=4)),
                     reads=[pT], writes=[x1T[b]])
            yield
            pq = P[5]
            for c in range(8):
                S.op("pe", lambda e: e.matmul(pq[:, 0:E], lhsT=x1T[b][:, c, :], rhs=wrt[:, c, :],
                                              start=(c == 0), stop=(c == 7)),
                     reads=[x1T[b], wrt], writes=[pq], inc=(c == 7))
            S.op("dve", lambda e: e.tensor_tensor(out=lg[b][:], in0=pq[:, 0:E], in1=brt[:], op=ALU.add),
                 reads=[pq, brt], writes=[lg[b]])
            S.op("dve", lambda e: e.max(out=top8[b][:], in_=lg[b][:]), reads=[lg[b]], writes=[top8[b]])
            S.op("dve", lambda e: e.tensor_scalar(out=mask[b][:], in0=lg[b][:], scalar1=top8[b][:, 3:4], scalar2=None,
                                                  op0=ALU.is_ge), reads=[lg[b], top8[b]], writes=[mask[b]])
            yield
            S.op("act", lambda e: e.mul(out=nmx[b][:], in_=top8[b][:, 0:1], mul=-1.0), reads=[top8[b]], writes=[nmx[b]])
            S.op("act", lambda e: e.activation(out=ex[b][:], in_=top8[b][:, 0:4], func=AF.Exp, bias=nmx[b][:],
                                               scale=1.0, accum_out=sm[b][:]),
                 reads=[top8[b], nmx[b]], writes=[ex[b], sm[b]])
            S.op("dve", lambda e: e.reciprocal(out=rs[b][:], in_=sm[b][:]), reads=[sm[b]], writes=[rs[b]])
            S.op("dve", lambda e: e.tensor_scalar(out=gate_all[:, i, :], in0=ex[b][:], scalar1=rs[b][:], scalar2=None,
                                                  op0=ALU.mult), reads=[ex[b], rs[b]], writes=[gate_all])
            yield
            S.op("pe", lambda e: e.matmul(pq[:, 32:32 + E], lhsT=triS[:], rhs=mask[b][:], start=True, stop=True),
                 reads=[triS, mask[b]], writes=[pq], inc=False)
            S.op("pe", lambda e: e.matmul(pq[:, 64:64 + E], lhsT=onesM[:], rhs=mask[b][:], start=True, stop=True),
                 reads=[onesM, mask[b]], writes=[pq])
            S.op("dve", lambda e: e.tensor_tensor(out=posf[b][:], in0=pq[:, 32:32 + E], in1=baseoffs[:], op=ALU.add),
                 reads=[pq, baseoffs], writes=[posf[b]])
            S.op("dve", lambda e: e.tensor_tensor(out=baseoffs[:], in0=pq[:, 64:64 + E], in1=baseoffs[:], op=ALU.add),
                 reads=[pq, baseoffs], writes=[baseoffs])
            for k in range(4):
                S.op("dve", lambda e: e.scalar_tensor_tensor(out=junk[b][:], in0=lg[b][:], scalar=top8[b][:, k:k + 1],
                                                             in1=posf[b][:], op0=ALU.is_equal, op1=ALU.mult,
                                                             accum_out=destf[b][:, k:k + 1]),
                     reads=[lg[b], top8[b], posf[b]], writes=[junk[b], destf[b]])
            yield
            S.op("dve", lambda e: e.tensor_copy(out=idx_all[:, i, :], in_=destf[b][:]), reads=[destf[b]], writes=[idx_all])
            for k in range(4):
                S.dma("pool", dr["xg"], x1b[b][:], reads=[x1b[b], idx_all], writes=[],
                      indirect=dict(out_offset=bass.IndirectOffsetOnAxis(ap=idx_all[:, i, k:k + 1], axis=0),
                                    in_offset=None))
        run_interleaved([_tile(i_) for i_ in range(NT)], 2)
        S.barrier()

    with ExitStack() as pctx:
        def sb(name, shape, dt):
            return Buf(pctx.enter_context(nc.sbuf_tensor(S.pfx + name, list(shape), dt)), name)
        xgr = [sb(f"xgr{i}", [128, 1024], BF16) for i in range(2)]
        xgT = sb("xgT", [128, 8, CAP], BF16)
        hT = sb("hT", [128, 8, CAP], BF16)
        tg = [sb(f"tg{i}", [128, 512], F32) for i in range(2)]
        tsg = [sb(f"tsg{i}", [128, 512], F32) for i in range(2)]
        tu = [sb(f"tu{i}", [128, 512], F32) for i in range(2)]
        yo = [sb(f"yo{i}", [128, 1024], F32) for i in range(2)]
        ptB, P = ps["ptB"], ps["P"]
        stg = [sb(f"stg{i}", [128, 2048], F32) for i in range(3)]
        wguc = [[Buf(wgu[i].t, f"wguc{i}_{c}") for c in range(8)] for i in range(2)]
        wdc = [[Buf(wd[i].t, f"wdc{i}_{c}") for c in range(8)] for i in range(2)]
        cast_gu = ["act", "act", "dve", "act", "act", "dve", "act", "act"]
        cast_dn = ["act", "dve", "act", "dve"]
        stk = [0]

        def cast(eng, out, in_, reads, writes):
            if eng == "act":
                S.op("act", lambda e: e.copy(out=out, in_=in_), reads=reads, writes=writes)
            else:
                S.op(eng, lambda e: e.tensor_copy(out=out, in_=in_), reads=reads, writes=writes)

        def load_expert(e_):
            b = e_ % 2
            for c in range(8):
                st = stg[stk[0] % 3]
                stk[0] += 1
                S.dma("sp", st[:], dr["w_gu"][e_, c * 128:(c + 1) * 128, :], writes=[st])
                cast(cast_gu[c], wgu[b][:, c, :], st[:], [st], [wguc[b][c]])
                yield
            for k2, c in enumerate(range(0, 8, 2)):
                st = stg[stk[0] % 3]
                stk[0] += 1
                S.dma("sp", st[:].rearrange("p (c n) -> p c n", c=2),
                      dr["w_dn"][e_, c * 128:(c + 2) * 128, :].rearrange("(c p) n -> p c n", p=128), writes=[st])
                cast(cast_dn[k2], wd[b][:, c:c + 2, :], st[:].rearrange("p (c n) -> p c n", c=2), [st],
                     [wdc[b][c], wdc[b][c + 1]])
                yield
            with nc.allow_non_contiguous_dma(reason="tiny bias"):
                S.dma("sp", bgu[b][:], dr["b_gu"][e_, :].rearrange("(c p t) -> p c t", p=128, t=2), writes=[bgu[b]])
            S.dma("sp", bdn[b][:], dr["b_dn"][e_, :].partition_broadcast(128), writes=[bdn[b]])

        for _ in load_expert(0):
            pass
        cnt = 0
        for e_ in range(E):
            wb = e_ % 2
            for rb in range(RB):
                b = rb % 2
                r0 = e_ * CAP + rb * 128
                S.dma("sp", xgr[b][:], dr["xg"][r0:r0 + 128, :], writes=[xgr[b]])
                for c in range(8):
                    S.op("pe", lambda e: e.transpose(out=ptB[:, c * 128:(c + 1) * 128],
                                                     in_=xgr[b][:, c * 128:(c + 1) * 128], identity=identB[:]),
                         reads=[xgr[b], identB], writes=[ptB], inc=(c == 7))
                S.op("act", lambda e: e.copy(out=xgT[:, :, rb * 128:(rb + 1) * 128],
                                             in_=ptB[:].rearrange("p (c n) -> p c n", c=8)),
                     reads=[ptB], writes=[xgT])
            pre = load_expert(e_ + 1) if e_ + 1 < E else iter(())
            for fc in range(8):
                for (lo, n) in parts:
                    b = cnt % 2
                    cnt += 1
                    next(pre, None)
                    pg, pu = P[1 + b], P[3 + b]
                    for gi, pp in ((0, pg), (1, pu)):
                        for c in range(8):
                            S.op("pe", lambda e: e.matmul(pp[:, 0:n],
                                                          lhsT=wgu[wb][:, c, fc * 256 + gi:fc * 256 + 256:2],
                                                          rhs=xgT[:, c, lo:lo + n], start=(c == 0), stop=(c == 7)),
                                 reads=[wguc[wb][c], xgT], writes=[pp], inc=(c == 7))
                    S.op("dve", lambda e: e.tensor_scalar(out=tg[b][:, 0:n], in0=pg[:, 0:n], scalar1=bgu[wb][:, fc, 0:1],
                                                          scalar2=7.0, op0=ALU.add, op1=ALU.min),
                         reads=[pg, bgu[wb]], writes=[tg[b]])
                    S.op("act", lambda e: e.activation(out=tsg[b][:, 0:n], in_=tg[b][:, 0:n], func=AF.Gelu_apprx_sigmoid),
                         reads=[tg[b]], writes=[tsg[b]])
                    S.op("dve", lambda e: e.tensor_scalar(out=tu[b][:, 0:n], in0=pu[:, 0:n], scalar1=bgu[wb][:, fc, 1:2],
                                                          scalar2=-7.0, op0=ALU.add, op1=ALU.max),
                         reads=[pu, bgu[wb]], writes=[tu[b]])
                    S.op("dve", lambda e: e.scalar_tensor_tensor(out=tu[b][:, 0:n], in0=tu[b][:, 0:n], scalar=7.0,
                                                                 in1=tsg[b][:, 0:n], op0=ALU.min, op1=ALU.mult),
                         reads=[tu[b], tsg[b]], writes=[tu[b]])
                    S.op("dve", lambda e: e.tensor_tensor(out=hT[:, fc, lo:lo + n], in0=tu[b][:, 0:n], in1=tsg[b][:, 0:n],
                                                          op=ALU.add), reads=[tu[b], tsg[b]], writes=[hT])
            for _ in pre:
                pass
            for rb in range(RB):
                b = rb % 2
                for h in range(2):
                    py = P[5 + h]
                    for fc in range(8):
                        S.op("pe", lambda e: e.matmul(py[:], lhsT=hT[:, fc, rb * 128:(rb + 1) * 128],
                                                      rhs=wd[wb][:, fc, h * 512:(h + 1) * 512],
                                                      start=(fc == 0), stop=(fc == 7)),
                             reads=[hT, wdc[wb][fc]], writes=[py], inc=(fc == 7))
                    S.op("dve", lambda e: e.tensor_tensor(out=yo[b][:, h * 512:(h + 1) * 512], in0=py[:],
                                                          in1=bdn[wb][:, h * 512:(h + 1) * 512], op=ALU.add),
                         reads=[py, bdn[wb]], writes=[yo[b]])
                r0 = e_ * CAP + rb * 128
                S.dma("sp", dr["yg"][r0:r0 + 128, :], yo[b][:], reads=[yo[b]], writes=[yg_d])
        S.barrier()

    with ExitStack() as pctx:
        def sb(name, shape, dt):
            return Buf(pctx.enter_context(nc.sbuf_tensor(S.pfx + name, list(shape), dt)), name)
        xc = [sb(f"xc{i}", [128, 1024], F32) for i in range(2)]
        yk = [[sb(f"yk{i}_{k}", [128, 1024], F32) for k in range(4)] for i in range(2)]
        acc = [sb(f"acc{i}", [128, 1024], F32) for i in range(2)]
        x2 = [sb(f"x2{i}", [128, 1024], F32) for i in range(2)]
        def _tile(i):
            b = i % 2
            sl = slice(i * 128, (i + 1) * 128)
            S.dma("sp", xc[b][:], dr["x1s"][sl, :], writes=[xc[b]])
            for k in range(4):
                S.dma("pool", yk[b][k][:], dr["yg"], reads=[idx_all], writes=[yk[b][k]],
                      indirect=dict(out_offset=None,
                                    in_offset=bass.IndirectOffsetOnAxis(ap=idx_all[:, i, k:k + 1], axis=0)))
            yield
            S.op("dve", lambda e: e.tensor_scalar(out=acc[b][:], in0=yk[b][0][:], scalar1=gate_all[:, i, 0:1],
                                                  scalar2=None, op0=ALU.mult),
                 reads=[yk[b][0], gate_all], writes=[acc[b]])
            for k in range(1, 4):
                S.op("dve", lambda e: e.scalar_tensor_tensor(out=acc[b][:], in0=yk[b][k][:], scalar=gate_all[:, i, k:k + 1],
                                                             in1=acc[b][:], op0=ALU.mult, op1=ALU.add),
                     reads=[yk[b][k], gate_all, acc[b]], writes=[acc[b]])
            S.op("dve", lambda e: e.scalar_tensor_tensor(out=acc[b][:], in0=xc[b][:], scalar=ALPHA, in1=acc[b][:],
                                                         op0=ALU.mult, op1=ALU.add),
                 reads=[xc[b], acc[b]], writes=[acc[b]])
            yield
            emit_ln(S, acc[b], lnp[2], lnp[3], x2[b], lntmp)
            S.dma("sp", dr["out"][sl, :], x2[b][:], reads=[x2[b]], writes=[out_d])
        run_interleaved([_tile(i_) for i_ in range(NT)], 2)
        S.barrier()


def build_k3(T, CAP):
    nc = bass.Bass("TRN2", target_bir_lowering=False)
    dr = {}
    def din(name, shape, dt=F32):
        dr[name] = nc.dram_tensor(name, list(shape), dt, kind="ExternalInput").ap()
    din("x", [T, D]); din("hg", [T, D], BF16)
    din("w_out", [D, D]); din("g1", [D]); din("b1", [D]); din("g2", [D]); din("b2", [D])
    din("w_rt", [D, E]); din("b_rt", [E]); din("w_gu", [E, D, 2 * D]); din("b_gu", [E, 2 * D])
    din("w_dn", [E, D, D]); din("b_dn", [E, D])
    dr["out"] = nc.dram_tensor("out", [T, D], F32, kind="ExternalOutput").ap()
    dr["xg"] = nc.dram_tensor("xg", [E * CAP, D], BF16, kind="Internal").ap()
    dr["yg"] = nc.dram_tensor("yg", [E * CAP, D], F32, kind="Internal").ap()
    dr["x1s"] = nc.dram_tensor("x1s", [T, D], F32, kind="Internal").ap()
    with ExitStack() as ctx:
        S = Sched(nc, ctx)
        ps = {"ptB": S.psum("ptB", [128, 1024], BF16), "P": [None] + [S.psum(f"P{i}", [128, 512], F32) for i in range(1, 8)]}
        emit_post(S, nc, T, CAP, dr, ps)
        pass
    return nc


def emit_mlstm(S, nc, SEQ, dr, ps):
    NT = SEQ // 128
    ptB, PA, PB, PC, PQ, PST, PN = ps["ptB"], ps["P"][1], ps["P"][2], ps["P"][3], ps["P"][4], ps["P"][5], ps["P"][6:8]
    identF = S.sbuf("identF", [128, 128], F32)
    identB = S.sbuf("identB", [128, 128], BF16)
    triI = S.sbuf("triI", [128, 128], F32)
    onesM = S.sbuf("onesM", [128, 128], F32)
    epsb = S.sbuf("epsb", [128, 1], F32)
    wfm = S.sbuf("wfm", [128, 8, 512], BF16)
    wtm = S.sbuf("wtm", [128, 8, 1288], BF16)
    bg = S.sbuf("bg", [128, 8], F32)
    gain = S.sbuf("gain_sb", [128, 512], F32)
    Cn = [S.sbuf(f"Cn{h}", [128, 132], F32) for h in range(4)]
    Cnb = [S.sbuf(f"Cnb{h}", [128, 132], BF16) for h in range(4)]
    out_d = Buf(None, "out")

    S.op("pool", lambda e: e.memset(identF[:], 0.0), writes=[identF])
    S.op("pool", lambda e: e.affine_select(out=identF[:], in_=identF[:], pattern=[[-1, 128]],
                                           compare_op=ALU.not_equal, fill=1.0, base=0,
                                           channel_multiplier=1), reads=[identF], writes=[identF])
    S.op("dve", lambda e: e.tensor_copy(out=identB[:], in_=identF[:]), reads=[identF], writes=[identB])
    S.op("pool", lambda e: e.memset(onesM[:], 1.0), writes=[onesM])
    S.op("pool", lambda e: e.memset(epsb[:], RMS_EPS), writes=[epsb])
    oneb = S.sbuf("oneb", [128, 1], F32)
    S.op("pool", lambda e: e.memset(oneb[:], 1.0), writes=[oneb])
    S.op("pool", lambda e: e.affine_select(out=triI[:], in_=onesM[:], pattern=[[1, 128]],
                                           compare_op=ALU.is_ge, fill=0.0, base=0,
                                           channel_multiplier=-1), reads=[onesM], writes=[triI])
    hm = S.sbuf("hm", [128, 2], F32)
    S.op("pool", lambda e: e.affine_select(out=hm[:, 0:1], in_=onesM[:, 0:1], pattern=[[0, 1]],
                                           compare_op=ALU.is_ge, fill=0.0, base=63,
                                           channel_multiplier=-1), reads=[onesM], writes=[hm])
    S.op("pool", lambda e: e.affine_select(out=hm[:, 1:2], in_=onesM[:, 0:1], pattern=[[0, 1]],
                                           compare_op=ALU.is_ge, fill=0.0, base=-64,
                                           channel_multiplier=1), reads=[onesM], writes=[hm])
    for h in range(4):
        S.op("pool", lambda e: e.memset(Cn[h][:], 0.0), writes=[Cn[h]])
        S.op("pool", lambda e: e.memset(Cnb[h][:], 0.0), writes=[Cnb[h]])
    for c in range(0, 8, 2):
        S.dma("pool", wfm[:, c:c + 2, :], dr["w_fm"][c * 128:(c + 2) * 128, :].rearrange("(c p) n -> p c n", p=128),
              writes=[wfm])
        S.dma("pool", wtm[:, c:c + 2, :], dr["w_tm"][c * 128:(c + 2) * 128, :].rearrange("(c p) n -> p c n", p=128),
              writes=[wtm])
    S.dma("sp", bg[:], dr["b_g"].partition_broadcast(128), writes=[bg])
    S.dma("sp", gain[:], dr["gain"].partition_broadcast(128), writes=[gain])

    def sb2(name, shape, dt):
        return [S.sbuf(f"{name}{i}", shape, dt) for i in range(2)]
    xt = sb2("xt", [128, 1024], F32)
    xb = sb2("xb", [128, 1024], BF16)
    xT = sb2("xT", [128, 8, 128], BF16)
    qTz = [[S.sbuf(f"qTz{i}_{h}", [128, 128], BF16) for h in range(4)] for i in range(2)]
    kT = sb2("kT", [128, 2, 128], BF16)
    gts = sb2("gts", [128, 8], F32)
    e1 = sb2("e1", [128, 4], F32)
    l1 = sb2("l1", [128, 4], F32)
    eq = sb2("eq", [128, 4], F32)
    ginb = sb2("ginb", [128, 4], F32)
    u = sb2("u", [128, 4], F32)
    ebl = sb2("ebl", [128, 4], F32)
    ktm = sb2("ktm", [128, 256], BF16)
    vx = sb2("vx", [128, 4, 132], BF16)
    og = sb2("og", [128, 512], F32)
    Sm = sb2("Sm", [128, 4, 128], BF16)
    dd = sb2("dd", [128, 4], F32)
    rr = sb2("rr", [128, 4], F32)
    fac = sb2("fac", [128, 4], F32)
    ss = sb2("ss", [128, 4], F32)
    rms = sb2("rms", [128, 4], F32)
    rinv = sb2("rinv", [128, 4], F32)
    fac2 = sb2("fac2", [128, 4], F32)
    sqj = sb2("sqj", [128, 128], F32)
    hn = sb2("hn", [128, 512], F32)
    hgo = sb2("hgo", [128, 512], BF16)
    for i in range(2):
        for h in range(4):
            S.op("pool", lambda e: e.memset(qTz[i][h][:], 0.0), writes=[qTz[i][h]])

    def _tile(t):
        b = t % 2
        sl = slice(t * 128, (t + 1) * 128)
        S.dma("sp", xt[b][:], (dr["xmap"](t) if "xmap" in dr else dr["x"][sl, :]), writes=[xt[b]])
        S.op("act", lambda e: e.copy(out=xb[b][:], in_=xt[b][:]), reads=[xt[b]], writes=[xb[b]])
        for c in range(8):
            S.op("pe", lambda e: e.transpose(out=ptB[:, c * 128:(c + 1) * 128], in_=xb[b][:, c * 128:(c + 1) * 128],
                                             identity=identB[:]), reads=[xb[b], identB], writes=[ptB], inc=(c == 7))
        S.op("dve", lambda e: e.tensor_copy(out=xT[b][:], in_=ptB[:].rearrange("p (c n) -> p c n", c=8)),
             reads=[ptB], writes=[xT[b]])
        if STAGE < 1:
            return
        yield
        for j in range(4):
            for c in range(8):
                S.op("pe", lambda e: e.matmul(PQ[:, j * 128:(j + 1) * 128], lhsT=wfm[:, c, j * 128:(j + 1) * 128],
                                              rhs=xT[b][:, c, :], start=(c == 0), stop=(c == 7)),
                     reads=[wfm, xT[b]], writes=[PQ], inc=(c == 7))
        for h in range(4):
            hb, jj = h // 2, h % 2
            S.op("dve", lambda e: e.tensor_scalar(out=qTz[b][h][:], in0=PQ[:, hb * 128:(hb + 1) * 128],
                                                  scalar1=hm[:, jj:jj + 1], scalar2=0.125, op0=ALU.mult, op1=ALU.mult),
                 reads=[PQ, hm], writes=[qTz[b][h]])
        S.op("dve", lambda e: e.tensor_copy(out=kT[b][:], in_=PQ[:, 256:512].rearrange("p (c n) -> p c n", c=2)),
             reads=[PQ], writes=[kT[b]])
        if STAGE < 2:
            return
        yield
        for (pp, lo, n) in ((PA, 0, 264), (PB, 264, 512), (PC, 776, 512))[:SUB]:
            for c in range(8):
                S.op("pe", lambda e: e.matmul(pp[:, 0:n], lhsT=xT[b][:, c, :], rhs=wtm[:, c, lo:lo + n],
                                              start=(c == 0), stop=(c == 7)),
                     reads=[wtm, xT[b]], writes=[pp], inc=(c == 7))
        if SUB < 4:
            return
        S.op("dve", lambda e: e.tensor_tensor(out=gts[b][:], in0=PA[:, 256:264], in1=bg[:], op=ALU.add),
             reads=[PA, bg], writes=[gts[b]])
        if SUB < 5:
            return
        S.op("dve", lambda e: e.tensor_copy(out=ktm[b][:], in_=PA[:, 0:256]), reads=[PA], writes=[ktm[b]])
        if SUB < 6:
            return
        S.op("act", lambda e: e.activation(out=e1[b][:], in_=gts[b][:, 4:8], func=AF.Exp, scale=-1.0),
             reads=[gts[b]], writes=[e1[b]])
        S.op("act", lambda e: e.activation(out=l1[b][:], in_=e1[b][:], func=AF.Ln, bias=oneb[:], scale=1.0),
             reads=[e1[b], oneb], writes=[l1[b]])
        if STAGE < 3:
            return
        S.op("pe", lambda e: e.matmul(PA[:, 272:276], lhsT=triI[:], rhs=l1[b][:], start=True, stop=True),
             reads=[triI, l1[b]], writes=[PA], inc=False)
        S.op("pe", lambda e: e.matmul(PA[:, 280:284], lhsT=onesM[:], rhs=l1[b][:], start=True, stop=True),
             reads=[onesM, l1[b]], writes=[PA])
        S.op("act", lambda e: e.activation(out=eq[b][:], in_=PA[:, 272:276], func=AF.Exp, scale=-1.0),
             reads=[PA], writes=[eq[b]])
        S.op("dve", lambda e: e.tensor_tensor(out=ginb[b][:], in0=PA[:, 272:276], in1=gts[b][:, 0:4], op=ALU.add),
             reads=[PA, gts[b]], writes=[ginb[b]])
        S.op("act", lambda e: e.activation(out=u[b][:], in_=ginb[b][:], func=AF.Exp), reads=[ginb[b]], writes=[u[b]])
        S.op("act", lambda e: e.activation(out=ebl[b][:], in_=PA[:, 280:284], func=AF.Exp, scale=-1.0),
             reads=[PA], writes=[ebl[b]])
        for h in range(4):
            S.op("dve", lambda e: e.tensor_scalar(out=vx[b][:, h, 0:128], in0=PB[:, h * 128:(h + 1) * 128],
                                                  scalar1=u[b][:, h:h + 1], scalar2=None, op0=ALU.mult),
                 reads=[PB, u[b]], writes=[vx[b]])
        S.op("dve", lambda e: e.tensor_copy(out=vx[b][:, :, 128], in_=u[b][:]), reads=[u[b]], writes=[vx[b]])
        S.op("act", lambda e: e.activation(out=og[b][:], in_=PC[:], func=AF.Sigmoid), reads=[PC], writes=[og[b]])
        if STAGE < 4:
            return
        for h in range(4):
            hb = h // 2
            S.op("pe", lambda e: e.matmul(PST[:, h * 128:(h + 1) * 128], lhsT=kT[b][:, hb, :], rhs=qTz[b][h][:],
                                          start=True, stop=True),
                 reads=[kT[b], qTz[b][h]], writes=[PST], inc=(h == 3))
        for h in range(4):
            S.op("dve", lambda e: e.tensor_tensor(out=Sm[b][:, h, :], in0=PST[:, h * 128:(h + 1) * 128], in1=triI[:],
                                                  op=ALU.mult), reads=[PST, triI], writes=[Sm[b]])
        for h in range(4):
            hb, jj = h // 2, h % 2
            pn = PN[hb]
            S.op("pe", lambda e: e.matmul(pn[:, jj * 129:(jj + 1) * 129], lhsT=Sm[b][:, h, :], rhs=vx[b][:, h, 0:129],
                                          start=True, stop=False),
                 reads=[Sm[b], vx[b]], writes=[pn], inc=False)
            S.op("pe", lambda e: e.matmul(pn[:, jj * 129:(jj + 1) * 129], lhsT=qTz[b][h][:], rhs=Cnb[h][:, 0:129],
                                          start=False, stop=True),
                 reads=[qTz[b][h], Cnb[h]], writes=[pn])
        if STAGE < 5:
            return
        for h in range(4):
            hb, jj = h // 2, h % 2
            R = slice(0, 128)
            pd = PQ if hb == 0 else PST
            S.op("pe", lambda e: e.matmul(pd[:, jj * 129:(jj + 1) * 129], lhsT=ktm[b][:, hb * 128:(hb + 1) * 128],
                                          rhs=vx[b][:, h, 0:129], start=True, stop=True),
                 reads=[ktm[b], vx[b]], writes=[pd])
            S.op("dve", lambda e: e.tensor_tensor(out=Cn[h][R, 0:129], in0=pd[R, jj * 129:(jj + 1) * 129], in1=Cn[h][R, 0:129],
                                                  op=ALU.add), reads=[pd, Cn[h]], writes=[Cn[h]])
            S.op("act", lambda e: e.activation(out=Cn[h][R, 0:129], in_=Cn[h][R, 0:129], func=AF.Identity,
                                               scale=ebl[b][R, h:h + 1]), reads=[Cn[h], ebl[b]], writes=[Cn[h]])
            S.op("pool", lambda e: e.tensor_copy(out=Cnb[h][R, 0:129], in_=Cn[h][R, 0:129]), reads=[Cn[h]], writes=[Cnb[h]])
        if STAGE < 6:
            return
        for h in range(4):
            hb, jj = h // 2, h % 2
            pn = PN[hb]
            c0 = jj * 129
            hs = slice(h, h + 1)
            S.op("act", lambda e: e.activation(out=dd[b][:, hs], in_=pn[:, c0 + 128:c0 + 129], func=AF.Abs,
                                               scale=eq[b][:, hs]), reads=[pn, eq[b]], writes=[dd[b]])
            S.op("dve", lambda e: e.tensor_scalar(out=dd[b][:, hs], in0=dd[b][:, hs], scalar1=1.0, scalar2=None,
                                                  op0=ALU.max), reads=[dd[b]], writes=[dd[b]])
            S.op("dve", lambda e: e.reciprocal(out=rr[b][:, hs], in_=dd[b][:, hs]), reads=[dd[b]], writes=[rr[b]])
            S.op("dve", lambda e: e.tensor_tensor(out=fac[b][:, hs], in0=rr[b][:, hs], in1=eq[b][:, hs], op=ALU.mult),
                 reads=[rr[b], eq[b]], writes=[fac[b]])
            S.op("act", lambda e: e.activation(out=sqj[b][:], in_=pn[:, c0:c0 + 128], func=AF.Square,
                                               scale=fac[b][:, hs], accum_out=ss[b][:, hs]),
                 reads=[pn, fac[b]], writes=[sqj[b], ss[b]])
            S.op("act", lambda e: e.activation(out=rms[b][:, hs], in_=ss[b][:, hs], func=AF.Sqrt, bias=epsb[:],
                                               scale=1.0 / 128.0), reads=[ss[b], epsb], writes=[rms[b]])
            S.op("dve", lambda e: e.reciprocal(out=rinv[b][:, hs], in_=rms[b][:, hs]), reads=[rms[b]], writes=[rinv[b]])
            S.op("dve", lambda e: e.tensor_tensor(out=fac2[b][:, hs], in0=fac[b][:, hs], in1=rinv[b][:, hs], op=ALU.mult),
                 reads=[fac[b], rinv[b]], writes=[fac2[b]])
            S.op("dve", lambda e: e.scalar_tensor_tensor(out=hn[b][:, h * 128:(h + 1) * 128], in0=pn[:, c0:c0 + 128],
                                                         scalar=fac2[b][:, hs], in1=gain[:, h * 128:(h + 1) * 128],
                                                         op0=ALU.mult, op1=ALU.mult),
                 reads=[pn, fac2[b], gain], writes=[hn[b]])
        S.op("pool", lambda e: e.tensor_tensor(out=hgo[b][:], in0=hn[b][:], in1=og[b][:], op=ALU.mult),
             reads=[hn[b], og[b]], writes=[hgo[b]])
        S.dma("sp", dr["out"][sl, :], hgo[b][:], reads=[hgo[b]], writes=[out_d])
    run_interleaved([_tile(t_) for t_ in range(NT)], 2)
    S.barrier()


def build_k1(SEQ):
    nc = bass.Bass("TRN2", target_bir_lowering=False)
    dr = {}
    def din(name, shape, dt=F32):
        dr[name] = nc.dram_tensor(name, list(shape), dt, kind="ExternalInput").ap()
    din("x", [SEQ, D]); din("w_fm", [D, 512]); din("w_tm", [D, 1288]); din("b_g", [8]); din("gain", [512])
    dr["out"] = nc.dram_tensor("out", [SEQ, 512], BF16, kind="ExternalOutput").ap()
    with ExitStack() as ctx:
        S = Sched(nc, ctx)
        ps = {"ptB": S.psum("ptB", [128, 1024], BF16), "P": [None] + [S.psum(f"P{i}", [128, 512], F32) for i in range(1, 8)]}
        emit_mlstm(S, nc, SEQ, dr, ps)
        pass
    return nc


def mlstm_inputs(x_b, w_in, b_gates, norm_gain, g):
    H, dk, dv = 8, 64, 128
    hs = slice(g * 4, g * 4 + 4)
    wq = w_in[:, 0:512].reshape(D, H, dk)[:, hs].reshape(D, 256)
    wk = w_in[:, 512:1024].reshape(D, H, dk)[:, hs].reshape(D, 256)
    wv = w_in[:, 1024:2048].reshape(D, H, dv)[:, hs].reshape(D, 512)
    wo = w_in[:, 2048:3072].reshape(D, H, dv)[:, hs].reshape(D, 512)
    wgi = w_in[:, 3072:3080][:, hs]
    wgf = w_in[:, 3080:3088][:, hs]
    w_fm = np.ascontiguousarray(np.concatenate([wq, wk], axis=1))
    w_tm = np.ascontiguousarray(np.concatenate([wk, wgi, wgf, wv, wo], axis=1))
    b_g = np.ascontiguousarray(np.concatenate([b_gates[0:8][hs], b_gates[8:16][hs]]))
    gain = np.ascontiguousarray(norm_gain.reshape(H, dv)[hs].reshape(512))
    return {"x": np.ascontiguousarray(x_b), "w_fm": w_fm, "w_tm": w_tm, "b_g": b_g, "gain": gain}


def emit_mla(S, nc, SEQ, dr, ps):
    NT = SEQ // 128
    ptB, ptT = ps["ptB"], ps["ptT"]
    P = ps["P"]
    identF = S.sbuf("identF", [128, 128], F32)
    identB = S.sbuf("identB", [128, 128], BF16)
    onesM = S.sbuf("onesM", [128, 128], F32)
    negF = S.sbuf("negF", [128, 128], F32)
    NEG = S.sbuf("NEG", [128, 128], BF16)
    hm = S.sbuf("hm", [128, 2], F32)
    epsb = S.sbuf("epsb", [128, 1], F32)
    wqn = S.sbuf("wqn", [128, 3, 512], BF16)
    cqT_all = S.sbuf("cqT_all", [128, 3, SEQ], BF16)
    krT2 = S.sbuf("krT2", [128, SEQ], BF16)
    qrT_all = S.sbuf("qrT_all", [128, 2, SEQ], BF16)
    out_d = Buf(None, "out")
    knT_dd = Buf(None, "knT_d")
    v_dd = Buf(None, "v_d")

    S.op("pool", lambda e: e.memset(identF[:], 0.0), writes=[identF])
    S.op("pool", lambda e: e.affine_select(out=identF[:], in_=identF[:], pattern=[[-1, 128]],
                                           compare_op=ALU.not_equal, fill=1.0, base=0,
                                           channel_multiplier=1), reads=[identF], writes=[identF])
    S.op("dve", lambda e: e.tensor_copy(out=identB[:], in_=identF[:]), reads=[identF], writes=[identB])
    S.op("pool", lambda e: e.memset(onesM[:], 1.0), writes=[onesM])
    S.op("pool", lambda e: e.memset(epsb[:], RMS_EPS), writes=[epsb])
    S.op("pool", lambda e: e.memset(negF[:], -30000.0), writes=[negF])
    S.op("pool", lambda e: e.affine_select(out=negF[:], in_=negF[:], pattern=[[1, 128]],
                                           compare_op=ALU.is_gt, fill=0.0, base=0,
                                           channel_multiplier=-1), reads=[negF], writes=[negF])
    S.op("dve", lambda e: e.tensor_copy(out=NEG[:], in_=negF[:]), reads=[negF], writes=[NEG])
    S.op("pool", lambda e: e.affine_select(out=hm[:, 0:1], in_=onesM[:, 0:1], pattern=[[0, 1]],
                                           compare_op=ALU.is_ge, fill=0.0, base=63,
                                           channel_multiplier=-1), reads=[onesM], writes=[hm])
    S.op("pool", lambda e: e.affine_select(out=hm[:, 1:2], in_=onesM[:, 0:1], pattern=[[0, 1]],
                                           compare_op=ALU.is_ge, fill=0.0, base=-64,
                                           channel_multiplier=1), reads=[onesM], writes=[hm])
    S.dma("pool", wqn[:], dr["wqn"].rearrange("(c p) n -> p c n", p=128), writes=[wqn])

    with ExitStack() as pctx:
        def sb(name, shape, dt):
            return Buf(pctx.enter_context(nc.sbuf_tensor(S.pfx + name, list(shape), dt)), name)
        def sb2(name, shape, dt):
            return [sb(f"{name}{i}", shape, dt) for i in range(2)]
        win = sb("win", [128, 8, 704], BF16)
        wqr = sb("wqr", [128, 3, 256], BF16)
        wkn = sb("wkn", [128, 2, 512], BF16)
        wkv = sb("wkv", [128, 2, 512], BF16)
        qn_t = sb("qn_t", [128, 384], F32)
        kvn_t = sb("kvn_t", [128, 256], F32)
        cosT = sb("cosT", [128, NT, 32], F32)
        sinT = sb("sinT", [128, NT, 32], F32)
        for c in range(0, 8, 4):
            S.dma("pool", win[:, c:c + 4, :], dr["w_in"][c * 128:(c + 4) * 128, :].rearrange("(c p) n -> p c n", p=128),
                  writes=[win])
        S.dma("pool", wqr[:], dr["wqr"].rearrange("(c p) n -> p c n", p=128), writes=[wqr])
        S.dma("pool", wkn[:], dr["wkn"].rearrange("(c p) n -> p c n", p=128), writes=[wkn])
        S.dma("pool", wkv[:], dr["wkv"].rearrange("(c p) n -> p c n", p=128), writes=[wkv])
        S.dma("sp", qn_t[:], dr["qn"].partition_broadcast(128), writes=[qn_t])
        S.dma("sp", kvn_t[:], dr["kvn"].partition_broadcast(128), writes=[kvn_t])

        with ExitStack() as tctx:
            def tb(name, shape, dt):
                return Buf(tctx.enter_context(nc.sbuf_tensor(S.pfx + name, list(shape), dt)), name)
            posi = tb("posi", [128, NT], I32)
            posf = tb("posf", [128, NT], F32)
            iof = tb("iof", [128, 32], F32)
            invf = tb("invf", [128, 32], F32)
            rr = tb("rr", [128, NT, 32], F32)
            ri = tb("ri", [128, NT * 32], I32)
            rf = tb("rf", [128, NT * 32], F32)
            ff = tb("ff", [128, NT * 32], F32)
            mk = tb("mk", [128, NT * 32], F32)
            S.dma("sp", posi[:], dr["pos"], writes=[posi])
            S.op("dve", lambda e: e.tensor_copy(out=posf[:], in_=posi[:]), reads=[posi], writes=[posf])
            S.op("pool", lambda e: e.iota(iof[:], pattern=[[1, 32]], base=0, channel_multiplier=0,
                                          allow_small_or_imprecise_dtypes=True), writes=[iof])
            S.op("act", lambda e: e.activation(out=invf[:], in_=iof[:], func=AF.Exp, scale=-math.log(10000.0) / 32.0),
                 reads=[iof], writes=[invf])
            S.op("dve", lambda e: e.tensor_scalar(out=invf[:], in0=invf[:], scalar1=1.0 / (2.0 * math.pi), scalar2=None,
                                                  op0=ALU.mult), reads=[invf], writes=[invf])
            for t in range(NT):
                S.op("dve", lambda e: e.tensor_scalar(out=rr[:, t, :], in0=invf[:], scalar1=posf[:, t:t + 1], scalar2=None,
                                                      op0=ALU.mult), reads=[invf, posf], writes=[rr])
            rrf = rr[:].rearrange("p t i -> p (t i)")
            for (shift, outT) in ((0.0, sinT), (0.25, cosT)):
                if shift != 0.0:
                    S.op("dve", lambda e: e.tensor_scalar(out=rrf, in0=rrf, scalar1=shift, scalar2=None, op0=ALU.add),
                         reads=[rr], writes=[rr])
                S.op("dve", lambda e: e.tensor_copy(out=ri[:], in_=rrf), reads=[rr], writes=[ri])
                S.op("dve", lambda e: e.tensor_copy(out=rf[:], in_=ri[:]), reads=[ri], writes=[rf])
                S.op("dve", lambda e: e.tensor_tensor(out=ff[:], in0=rrf, in1=rf[:], op=ALU.subtract),
                     reads=[rr, rf], writes=[ff])
                S.op("dve", lambda e: e.tensor_scalar(out=mk[:], in0=ff[:], scalar1=0.5, scalar2=None, op0=ALU.is_gt),
                     reads=[ff], writes=[mk])
                S.op("dve", lambda e: e.tensor_tensor(out=ff[:], in0=ff[:], in1=mk[:], op=ALU.subtract),
                     reads=[ff, mk], writes=[ff])
                S.op("dve", lambda e: e.tensor_scalar(out=mk[:], in0=ff[:], scalar1=-0.5, scalar2=None, op0=ALU.is_lt),
                     reads=[ff], writes=[mk])
                S.op("dve", lambda e: e.tensor_tensor(out=ff[:], in0=ff[:], in1=mk[:], op=ALU.add),
                     reads=[ff, mk], writes=[ff])
                S.op("dve", lambda e: e.tensor_scalar(out=ff[:], in0=ff[:], scalar1=-0.49999, scalar2=0.49999,
                                                      op0=ALU.max, op1=ALU.min), reads=[ff], writes=[ff])
                S.op("act", lambda e: e.activation(out=outT[:].rearrange("p t i -> p (t i)"), in_=ff[:], func=AF.Sin,
                                                   scale=2.0 * math.pi), reads=[ff], writes=[outT])
            S.barrier()

        xt = sb2("xt", [128, 1024], F32)
        xb = sb2("xb", [128, 1024], BF16)
        xT = sb2("xT", [128, 8, 128], BF16)
        junk = sb2("junk", [128, 384], F32)
        ssq = sb2("ssq", [128, 2], F32)
        rms = sb2("rms", [128, 2], F32)
        rinv = sb2("rinv", [128, 2], F32)
        ckv = sb2("ckv", [128, 256], BF16)
        cq = sb2("cq", [128, 384], BF16)
        kr2 = sb2("kr2", [128, 128], BF16)
        rt = [sb2(f"rt{k}", [128, 32], F32) for k in range(4)]
        qrt = [sb2(f"qrt{k}", [128, 4, 32], F32) for k in range(4)]
        qr = sb2("qr", [128, 4, 64], BF16)
        stg1 = sb2("stg1", [128, 6, 128], BF16)
        stg2 = sb2("stg2", [128, 2, 128], BF16)
        knT_sb = sb2("knT_sb", [128, 4, 128], BF16)
        v_sb = sb2("v_sb", [128, 512], BF16)
        PA1, PA2, PQR, PK, PV = P[1], P[2], P[3], P[4], P[5]

        def _tile(t):
            if SUBA < 1:
                return
            b = t % 2
            sl = slice(t * 128, (t + 1) * 128)
            S.dma("sp", xt[b][:], (dr["xmap"](t) if "xmap" in dr else dr["x"][sl, :]), writes=[xt[b]])
            S.op("act", lambda e: e.copy(out=xb[b][:], in_=xt[b][:]), reads=[xt[b]], writes=[xb[b]])
            for c in range(8):
                S.op("pe", lambda e: e.transpose(out=ptB[:, c * 128:(c + 1) * 128], in_=xb[b][:, c * 128:(c + 1) * 128],
                                                 identity=identB[:]), reads=[xb[b], identB], writes=[ptB], inc=(c == 7))
            S.op("dve", lambda e: e.tensor_copy(out=xT[b][:], in_=ptB[:].rearrange("p (c n) -> p c n", c=8)),
                 reads=[ptB], writes=[xT[b]])
            for (pp, lo, n) in ((PA1, 0, 320), (PA2, 320, 384)):
                for c in range(8):
                    S.op("pe", lambda e: e.matmul(pp[:, 0:n], lhsT=xT[b][:, c, :], rhs=win[:, c, lo:lo + n],
                                                  start=(c == 0), stop=(c == 7)),
                         reads=[win, xT[b]], writes=[pp], inc=(c == 7))
            S.op("act", lambda e: e.activation(out=junk[b][:, 0:256], in_=PA1[:, 0:256], func=AF.Square,
                                               accum_out=ssq[b][:, 0:1]), reads=[PA1], writes=[junk[b], ssq[b]])
            S.op("act", lambda e: e.activation(out=junk[b][:, 0:384], in_=PA2[:, 0:384], func=AF.Square,
                                               accum_out=ssq[b][:, 1:2]), reads=[PA2], writes=[junk[b], ssq[b]])
            S.op("act", lambda e: e.activation(out=rms[b][:, 0:1], in_=ssq[b][:, 0:1], func=AF.Sqrt, bias=epsb[:],
                                               scale=1.0 / 256.0), reads=[ssq[b], epsb], writes=[rms[b]])
            S.op("act", lambda e: e.activation(out=rms[b][:, 1:2], in_=ssq[b][:, 1:2], func=AF.Sqrt, bias=epsb[:],
                                               scale=1.0 / 384.0), reads=[ssq[b], epsb], writes=[rms[b]])
            S.op("dve", lambda e: e.reciprocal(out=rinv[b][:], in_=rms[b][:]), reads=[rms[b]], writes=[rinv[b]])
            S.op("dve", lambda e: e.scalar_tensor_tensor(out=ckv[b][:], in0=PA1[:, 0:256], scalar=rinv[b][:, 0:1],
                                                         in1=kvn_t[:], op0=ALU.mult, op1=ALU.mult),
                 reads=[PA1, rinv[b], kvn_t], writes=[ckv[b]])
            S.op("dve", lambda e: e.scalar_tensor_tensor(out=cq[b][:], in0=PA2[:, 0:384], scalar=rinv[b][:, 1:2],
                                                         in1=qn_t[:], op0=ALU.mult, op1=ALU.mult),
                 reads=[PA2, rinv[b], qn_t], writes=[cq[b]])
            if SUBA < 2:
                return
            cs, sn = cosT[:, t, :], sinT[:, t, :]
            x1, x2 = PA1[:, 256:288], PA1[:, 288:320]
            S.op("dve", lambda e: e.tensor_tensor(out=rt[0][b][:], in0=x1, in1=cs, op=ALU.mult), reads=[PA1, cosT], writes=[rt[0][b]])
            S.op("dve", lambda e: e.tensor_tensor(out=rt[1][b][:], in0=x2, in1=sn, op=ALU.mult), reads=[PA1, sinT], writes=[rt[1][b]])
            S.op("dve", lambda e: e.tensor_tensor(out=rt[2][b][:], in0=x2, in1=cs, op=ALU.mult), reads=[PA1, cosT], writes=[rt[2][b]])
            S.op("dve", lambda e: e.tensor_tensor(out=rt[3][b][:], in0=x1, in1=sn, op=ALU.mult), reads=[PA1, sinT], writes=[rt[3][b]])
            if SUBB < 2:
                return
            for off in (0, 64):
                S.op("pool", lambda e: e.tensor_tensor(out=kr2[b][:, off:off + 32], in0=rt[0][b][:], in1=rt[1][b][:],
                                                       op=ALU.subtract), reads=[rt[0][b], rt[1][b]], writes=[kr2[b]])
                S.op("pool", lambda e: e.tensor_tensor(out=kr2[b][:, off + 32:off + 64], in0=rt[2][b][:], in1=rt[3][b][:],
                                                       op=ALU.add), reads=[rt[2][b], rt[3][b]], writes=[kr2[b]])
            if SUBB < 3:
                return
            yield
            srcs = [(cq[b], k * 128) for k in range(3)] + [(ckv[b], k * 128) for k in range(2)] + [(kr2[b], 0)]
            for k, (src, o0) in enumerate(srcs):
                S.op("pe", lambda e: e.transpose(out=ptT[:, k * 128:(k + 1) * 128], in_=src[:, o0:o0 + 128], identity=identB[:]),
                     reads=[src, identB], writes=[ptT], inc=(k == 5))
            if SUBB < 4:
                return
            S.op("act", lambda e: e.copy(out=stg1[b][:], in_=ptT[:, 0:768].rearrange("p (c n) -> p c n", c=6)),
                 reads=[ptT], writes=[stg1[b]])
            for c in range(3):
                S.op("dve", lambda e: e.tensor_copy(out=cqT_all[:, c, sl], in_=stg1[b][:, c, :]),
                     reads=[stg1[b]], writes=[cqT_all])
            S.op("dve", lambda e: e.tensor_copy(out=krT2[:, sl], in_=stg1[b][:, 5, :]), reads=[stg1[b]], writes=[krT2])
            if SUBA < 3:
                return
            yield
            for c in range(3):
                S.op("pe", lambda e: e.matmul(PQR[:, 0:256], lhsT=stg1[b][:, c, :], rhs=wqr[:, c, :],
                                              start=(c == 0), stop=(c == 2)),
                     reads=[stg1[b], wqr], writes=[PQR], inc=(c == 2))
            for h in range(4):
                q1, q2 = PQR[:, h * 64:h * 64 + 32], PQR[:, h * 64 + 32:h * 64 + 64]
                S.op("dve", lambda e: e.tensor_tensor(out=qrt[0][b][:, h, :], in0=q1, in1=cs, op=ALU.mult), reads=[PQR, cosT], writes=[qrt[0][b]])
                S.op("dve", lambda e: e.tensor_tensor(out=qrt[1][b][:, h, :], in0=q2, in1=sn, op=ALU.mult), reads=[PQR, sinT], writes=[qrt[1][b]])
                S.op("dve", lambda e: e.tensor_tensor(out=qrt[2][b][:, h, :], in0=q2, in1=cs, op=ALU.mult), reads=[PQR, cosT], writes=[qrt[2][b]])
                S.op("dve", lambda e: e.tensor_tensor(out=qrt[3][b][:, h, :], in0=q1, in1=sn, op=ALU.mult), reads=[PQR, sinT], writes=[qrt[3][b]])
            S.op("pool", lambda e: e.tensor_tensor(out=qr[b][:, :, 0:32], in0=qrt[0][b][:], in1=qrt[1][b][:], op=ALU.subtract),
                 reads=[qrt[0][b], qrt[1][b]], writes=[qr[b]])
            S.op("pool", lambda e: e.tensor_tensor(out=qr[b][:, :, 32:64], in0=qrt[2][b][:], in1=qrt[3][b][:], op=ALU.add),
                 reads=[qrt[2][b], qrt[3][b]], writes=[qr[b]])
            qrf = qr[b][:].rearrange("p h d -> p (h d)")
            for k in range(2):
                S.op("pe", lambda e: e.transpose(out=ptT[:, 768 + k * 128:768 + (k + 1) * 128], in_=qrf[:, k * 128:(k + 1) * 128],
                                                 identity=identB[:]), reads=[qr[b], identB], writes=[ptT], inc=(k == 1))
            S.op("act", lambda e: e.copy(out=stg2[b][:], in_=ptT[:, 768:1024].rearrange("p (c n) -> p c n", c=2)),
                 reads=[ptT], writes=[stg2[b]])
            for c in range(2):
                S.op("dve", lambda e: e.tensor_copy(out=qrT_all[:, c, sl], in_=stg2[b][:, c, :]),
                     reads=[stg2[b]], writes=[qrT_all])
            if SUBA < 4:
                return
            yield
            for h in range(4):
                for c in range(2):
                    S.op("pe", lambda e: e.matmul(PK[:, h * 128:(h + 1) * 128], lhsT=wkn[:, c, h * 128:(h + 1) * 128],
                                                  rhs=stg1[b][:, 3 + c, :], start=(c == 0), stop=(c == 1)),
                         reads=[wkn, stg1[b]], writes=[PK], inc=(c == 1))
            S.op("dve", lambda e: e.tensor_copy(out=knT_sb[b][:], in_=PK[:].rearrange("p (h n) -> p h n", h=4)),
                 reads=[PK], writes=[knT_sb[b]])
            S.dma("sp", dr["knT_d"][:, :, sl].rearrange("h p n -> p h n"), knT_sb[b][:], reads=[knT_sb[b]], writes=[knT_dd])
            yield
            for c in range(2):
                S.op("pe", lambda e: e.matmul(PV[:], lhsT=stg1[b][:, 3 + c, :], rhs=wkv[:, c, :], start=(c == 0), stop=(c == 1)),
                     reads=[wkv, stg1[b]], writes=[PV], inc=(c == 1))
            S.op("dve", lambda e: e.tensor_copy(out=v_sb[b][:], in_=PV[:]), reads=[PV], writes=[v_sb[b]])
            S.dma("sp", dr["v_d"][sl, :], v_sb[b][:], reads=[v_sb[b]], writes=[v_dd])
        run_interleaved([_tile(t_) for t_ in range(NT)], 2)
        S.barrier()

    if STAGE < 2:
        return
    with ExitStack() as pctx:
        def sb(name, shape, dt):
            return Buf(pctx.enter_context(nc.sbuf_tensor(S.pfx + name, list(shape), dt)), name)
        def sb2(name, shape, dt):
            return [sb(f"{name}{i}", shape, dt) for i in range(2)]
        knT = sb("knT", [128, SEQ], BF16)
        vh = sb("vh", [128, NT, 128], BF16)
        sc = sb("sc", [128, SEQ], F32)
        Pb = sb("Pb", [128, SEQ], BF16)
        PT = sb("PT", [128, NT, 128], BF16)
        qnT = sb2("qnT", [128, 128], BF16)
        qrz = sb2("qrz", [128, 128], BF16)
        mx = sb2("mx", [128, 16], F32)
        mrow = sb2("mrow", [128, 1], F32)
        negm = sb2("negm", [128, 1], F32)
        rsum = sb2("rsum", [128, 1], F32)
        rinv2 = sb2("rinv2", [128, 1], F32)
        osb = sb2("osb", [128, 128], BF16)
        PQN = P[1]
        PS = [P[2], P[3]]
        PO = [P[4], P[5]]
        ptP = [ptB, ptT]
        it = 0
        for h in range(4):
            hb, jj = h // 2, h % 2
            S.dma("sp", knT[:], dr["knT_d"][h, :, :], writes=[knT])
            for t0 in range(0, NT, 8):
                t1 = min(NT, t0 + 8)
                S.dma("sp", vh[:, t0:t1, :],
                      dr["v_d"][t0 * 128:t1 * 128, h * 128:(h + 1) * 128].rearrange("(t p) d -> p t d", p=128),
                      writes=[vh])
            for qb in range(NT):
                b = it % 2
                it += 1
                qs = slice(qb * 128, (qb + 1) * 128)
                nkeys = (qb + 1) * 128
                for c in range(3):
                    S.op("pe", lambda e: e.matmul(PQN[:, 0:128], lhsT=wqn[:, c, h * 128:(h + 1) * 128], rhs=cqT_all[:, c, qs],
                                                  start=(c == 0), stop=(c == 2)),
                         reads=[wqn, cqT_all], writes=[PQN], inc=(c == 2))
                S.op("dve", lambda e: e.tensor_copy(out=qnT[b][:], in_=PQN[:, 0:128]), reads=[PQN], writes=[qnT[b]])
                S.op("pool", lambda e: e.tensor_scalar(out=qrz[b][:], in0=qrT_all[:, hb, qs], scalar1=hm[:, jj:jj + 1],
                                                       scalar2=None, op0=ALU.mult), reads=[qrT_all, hm], writes=[qrz[b]])
                nch = (nkeys + 511) // 512
                for kc in range(nch):
                    k0 = kc * 512
                    n = min(512, nkeys - k0)
                    pp = PS[kc % 2]
                    last = (kc == nch - 1)
                    S.op("pe", lambda e: e.matmul(pp[:, 0:n], lhsT=qnT[b][:], rhs=knT[:, k0:k0 + n], start=True, stop=False),
                         reads=[qnT[b], knT], writes=[pp], inc=False)
                    S.op("pe", lambda e: e.matmul(pp[:, 0:n], lhsT=qrz[b][:], rhs=krT2[:, k0:k0 + n], start=False, stop=(not last)),
                         reads=[qrz[b], krT2], writes=[pp], inc=(not last))
                    if last:
                        S.op("pe", lambda e: e.matmul(pp[:, n - 128:n], lhsT=identB[:], rhs=NEG[:], start=False, stop=True),
                             reads=[identB, NEG], writes=[pp])
                    S.op("dve", lambda e: e.tensor_scalar(out=sc[:, k0:k0 + n], in0=pp[:, 0:n], scalar1=1.0, scalar2=None,
                                                          op0=ALU.mult, op1=ALU.max, accum_out=mx[b][:, kc:kc + 1]),
                         reads=[pp], writes=[sc, mx[b]])
                S.op("dve", lambda e: e.tensor_reduce(out=mrow[b][:], in_=mx[b][:, 0:nch], axis=AX.X, op=ALU.max),
                     reads=[mx[b]], writes=[mrow[b]])
                S.op("dve", lambda e: e.tensor_scalar(out=negm[b][:], in0=mrow[b][:], scalar1=-SCALE, scalar2=None, op0=ALU.mult),
                     reads=[mrow[b]], writes=[negm[b]])
                S.op("act", lambda e: e.activation(out=Pb[:, 0:nkeys], in_=sc[:, 0:nkeys], func=AF.Exp, scale=SCALE,
                                                   bias=negm[b][:], accum_out=rsum[b][:]),
                     reads=[sc, negm[b]], writes=[Pb, rsum[b]])
                nkb = qb + 1
                for g0 in range(0, nkb, 8):
                    g1 = min(nkb, g0 + 8)
                    pt = ptP[(g0 // 8) % 2]
                    for kb in range(g0, g1):
                        S.op("pe", lambda e: e.transpose(out=pt[:, (kb - g0) * 128:(kb - g0 + 1) * 128],
                                                         in_=Pb[:, kb * 128:(kb + 1) * 128], identity=identB[:]),
                             reads=[Pb, identB], writes=[pt], inc=(kb == g1 - 1))
                    S.op("act", lambda e: e.copy(out=PT[:, g0:g1, :],
                                                 in_=pt[:, 0:(g1 - g0) * 128].rearrange("p (c n) -> p c n", n=128)),
                         reads=[pt], writes=[PT])
                po = PO[b]
                for kb in range(nkb):
                    S.op("pe", lambda e: e.matmul(po[:, 0:128], lhsT=PT[:, kb, :], rhs=vh[:, kb, :],
                                                  start=(kb == 0), stop=(kb == nkb - 1)),
                         reads=[PT, vh], writes=[po], inc=(kb == nkb - 1))
                S.op("dve", lambda e: e.reciprocal(out=rinv2[b][:], in_=rsum[b][:]), reads=[rsum[b]], writes=[rinv2[b]])
                S.op("dve", lambda e: e.tensor_scalar(out=osb[b][:], in0=po[:, 0:128], scalar1=rinv2[b][:], scalar2=None,
                                                      op0=ALU.mult), reads=[po, rinv2[b]], writes=[osb[b]])
                S.dma("sp", dr["out"][qs, h * 128:(h + 1) * 128], osb[b][:], reads=[osb[b]], writes=[out_d])
        S.barrier()


def build_k2(SEQ):
    nc = bass.Bass("TRN2", target_bir_lowering=False)
    dr = {}
    def din(name, shape, dt=F32):
        dr[name] = nc.dram_tensor("i_" + name, list(shape), dt, kind="ExternalInput").ap()
    NT = SEQ // 128
    din("x", [SEQ, D]); din("pos", [128, NT], I32); din("w_in", [D, 704]); din("qn", [384]); din("kvn", [256])
    din("wqn", [384, 512]); din("wqr", [384, 256]); din("wkn", [256, 512]); din("wkv", [256, 512])
    dr["out"] = nc.dram_tensor("out", [SEQ, 512], BF16, kind="ExternalOutput").ap()
    dr["knT_d"] = nc.dram_tensor("knT_d", [4, 128, SEQ], BF16, kind="Internal").ap()
    dr["v_d"] = nc.dram_tensor("v_d", [SEQ, 512], BF16, kind="Internal").ap()
    with ExitStack() as ctx:
        S = Sched(nc, ctx)
        ps = {"ptB": S.psum("ptB", [128, 1024], BF16), "ptT": S.psum("ptT", [128, 1024], BF16),
              "P": [None] + [S.psum(f"P{i}", [128, 512], F32) for i in range(1, 7)]}
        emit_mla(S, nc, SEQ, dr, ps)
        pass
    return nc


def mla_inputs(x_b, pos_b, w_in, q_norm, kv_norm, w_qb, w_kvb, g):
    SEQ = x_b.shape[0]
    H = 8
    hs = slice(g * 4, g * 4 + 4)
    w_in2 = np.ascontiguousarray(np.concatenate([w_in[:, 384:640], w_in[:, 640:704], w_in[:, 0:384]], axis=1))
    wq = w_qb.reshape(384, H, 192)[:, hs]
    wqn = np.ascontiguousarray(wq[:, :, 0:128].reshape(384, 512))
    wqr = np.ascontiguousarray(wq[:, :, 128:192].reshape(384, 256))
    wkv_ = w_kvb.reshape(256, H, 256)[:, hs]
    wkn = np.ascontiguousarray(wkv_[:, :, 0:128].reshape(256, 512))
    wkv = np.ascontiguousarray(wkv_[:, :, 128:256].reshape(256, 512))
    pos2 = np.ascontiguousarray(pos_b.reshape(SEQ // 128, 128).T.astype(np.int32))
    return {"i_x": np.ascontiguousarray(x_b), "i_pos": pos2, "i_w_in": w_in2, "i_qn": np.ascontiguousarray(q_norm),
            "i_kvn": np.ascontiguousarray(kv_norm), "i_wqn": wqn, "i_wqr": wqr, "i_wkn": wkn, "i_wkv": wkv}


def run_phase(S, pfx, fn):
    with ExitStack() as pctx:
        old = S.ctx
        S.ctx = pctx
        S.pfx = pfx
        fn()
        S.ctx = old
        S.pfx = ""


def build_fused(SEQ, CAP_, NB, depth=4):
    TOK = SEQ // 2
    NT3 = TOK // 128
    nc = bass.Bass("TRN2", target_bir_lowering=False)
    ext = {}

    def din(name, shape, dt=F32):
        ext[name] = nc.dram_tensor(name, list(shape), dt, kind="ExternalInput").ap()

    def dint(name, shape, dt):
        return nc.dram_tensor(name, list(shape), dt, kind="Internal").ap()

    din("x_full", [SEQ, D]); din("x_own", [TOK, D]); din("hgidx", [128, NT3, 2], I32)
    din("pos", [128, SEQ // 128], I32)
    for j in range((depth + 1) // 2):
        din(f"m{j}_w_fm", [D, 512]); din(f"m{j}_w_tm", [D, 1288]); din(f"m{j}_b_g", [8]); din(f"m{j}_gain", [512])
    for j in range(depth // 2):
        din(f"a{j}_w_in", [D, 704]); din(f"a{j}_qn", [384]); din(f"a{j}_kvn", [256])
        din(f"a{j}_wqn", [384, 512]); din(f"a{j}_wqr", [384, 256]); din(f"a{j}_wkn", [256, 512]); din(f"a{j}_wkv", [256, 512])
    for l in range(depth):
        din(f"l{l}_w_out", [D, D])
        for nm in ("g1", "b1", "g2", "b2"):
            din(f"l{l}_{nm}", [D])
        din(f"l{l}_w_rt", [D, E]); din(f"l{l}_b_rt", [E]); din(f"l{l}_w_gu", [E, D, 2 * D]); din(f"l{l}_b_gu", [E, 2 * D])
        din(f"l{l}_w_dn", [E, D, D]); din(f"l{l}_b_dn", [E, D])
    out_ap = nc.dram_tensor("out", [TOK, D], F32, kind="ExternalOutput").ap()
    hg_own = dint("hg_own", [SEQ, 512], BF16)
    hg_gath = dint("hg_gath", [2 * SEQ, 512], BF16)
    xn = [dint(f"xn{i}", [TOK, D], F32) for i in range(2)]
    xfull_g = dint("xfull_g", [SEQ, D], F32)
    knT_d = dint("knT_d", [4, 128, SEQ], BF16)
    v_d = dint("v_d", [SEQ, 512], BF16)
    xg = dint("xg", [E * CAP_, D], BF16)
    yg = dint("yg", [E * CAP_, D], F32)
    x1s = dint("x1s", [TOK, D], F32)
    groups = [[2 * b, 2 * b + 1] for b in range(NB)]
    CHX = min(512, TOK)
    CHH = min(2048, SEQ)

    def xmap(t):
        row = t * 128
        r = row // TOK
        lrow = row - r * TOK
        k, off = lrow // CHX, lrow % CHX
        r0 = k * 2 * CHX + r * CHX + off
        return xfull_g[r0:r0 + 128, :]
    with ExitStack() as ctx:
        S = Sched(nc, ctx)
        ptB = S.psum("ptB", [128, 1024], BF16)
        Pf = [S.psum(f"P{i}", [128, 512], F32) for i in range(1, 7)]
        ptT = S.psum("ptT", [128, 1024], BF16)
        P7 = Buf(ptT.t[:].bitcast(F32), "P7")
        ps13 = {"ptB": ptB, "P": [None] + Pf + [P7]}
        ps2 = {"ptB": ptB, "ptT": ptT, "P": [None] + Pf}
        for l in range(depth):
            j = l // 2
            xsrc = ext["x_full"] if l == 0 else xfull_g
            if l % 2 == 0:
                dr = {"x": xsrc, **({"xmap": xmap} if l > 0 else {}), "w_fm": ext[f"m{j}_w_fm"], "w_tm": ext[f"m{j}_w_tm"], "b_g": ext[f"m{j}_b_g"],
                      "gain": ext[f"m{j}_gain"], "out": hg_own}
                run_phase(S, f"L{l}m_", lambda: emit_mlstm(S, nc, SEQ, dr, ps13))
            else:
                dr = {"x": xsrc, **({"xmap": xmap} if l > 0 else {}), "pos": ext["pos"], "w_in": ext[f"a{j}_w_in"], "qn": ext[f"a{j}_qn"], "kvn": ext[f"a{j}_kvn"],
                      "wqn": ext[f"a{j}_wqn"], "wqr": ext[f"a{j}_wqr"], "wkn": ext[f"a{j}_wkn"], "wkv": ext[f"a{j}_wkv"],
                      "out": hg_own, "knT_d": knT_d, "v_d": v_d}
                run_phase(S, f"L{l}a_", lambda: emit_mla(S, nc, SEQ, dr, ps2))
            for k in range(SEQ // CHH):
                S.cc("AllGather", groups, hg_own[k * CHH:(k + 1) * CHH, :], hg_gath[k * 2 * CHH:(k + 1) * 2 * CHH, :], inc=1)
            S.barrier()
            dr = {"x": ext["x_own"] if l == 0 else xn[(l - 1) % 2], "hg_gath": hg_gath, "hgidx": ext["hgidx"],
                  "out": out_ap if l == depth - 1 else xn[l % 2], "xg": xg, "yg": yg, "x1s": x1s}
            for nm in ("w_out", "g1", "b1", "g2", "b2", "w_rt", "b_rt", "w_gu", "b_gu", "w_dn", "b_dn"):
                dr[nm] = ext[f"l{l}_{nm}"]
            run_phase(S, f"L{l}p_", lambda: emit_post(S, nc, TOK, CAP_, dr, ps13))
            if l < depth - 1:
                for k in range(TOK // CHX):
                    S.cc("AllGather", groups, xn[l % 2][k * CHX:(k + 1) * CHX, :],
                         xfull_g[k * 2 * CHX:(k + 1) * 2 * CHX, :], inc=1)
                S.barrier()
    return nc


def fused_in_maps(x, positions, ln_gain, ln_bias, mlstm_w_in, mlstm_b_gates, mlstm_norm_gain, mlstm_w_out,
                  mla_w_in, mla_q_norm, mla_kv_norm, mla_w_qb, mla_w_kvb, mla_w_out,
                  moe_w_router, moe_b_router, moe_w_gate_up, moe_b_gate_up, moe_w_down, moe_b_down):
    f32 = np.float32
    A = lambda a: np.ascontiguousarray(np.asarray(a, dtype=f32))
    x = A(x)
    positions = np.asarray(positions)
    B, S_, _ = x.shape
    TOK = S_ // 2
    NT3 = TOK // 128
    depth = ln_gain.shape[0]
    shared = {}
    for l in range(depth):
        j = l // 2
        shared[f"l{l}_w_out"] = A(mlstm_w_out[j]) if l % 2 == 0 else A(mla_w_out[j])
        shared[f"l{l}_g1"] = A(ln_gain[l, 0]); shared[f"l{l}_b1"] = A(ln_bias[l, 0])
        shared[f"l{l}_g2"] = A(ln_gain[l, 1]); shared[f"l{l}_b2"] = A(ln_bias[l, 1])
        shared[f"l{l}_w_rt"] = A(moe_w_router[l]); shared[f"l{l}_b_rt"] = A(moe_b_router[l])
        shared[f"l{l}_w_gu"] = A(moe_w_gate_up[l]); shared[f"l{l}_b_gu"] = A(moe_b_gate_up[l])
        shared[f"l{l}_w_dn"] = A(moe_w_down[l]); shared[f"l{l}_b_dn"] = A(moe_b_down[l])
    in_maps = []
    for c in range(2 * B):
        b, g = c // 2, c % 2
        m = dict(shared)
        m["x_full"] = np.ascontiguousarray(x[b])
        m["x_own"] = np.ascontiguousarray(x[b, g * TOK:(g + 1) * TOK])
        p = np.arange(128, dtype=np.int64)[:, None, None]
        i = np.arange(NT3, dtype=np.int64)[None, :, None]
        r = np.arange(2, dtype=np.int64)[None, None, :]
        chh = min(2048, S_)
        tok = g * TOK + i * 128 + p
        m["hgidx"] = np.ascontiguousarray(((tok // chh) * 2 * chh + r * chh + tok % chh).astype(np.int32))
        for j in range((depth + 1) // 2):
            mi = mlstm_inputs(x[b], A(mlstm_w_in[j]), A(mlstm_b_gates[j]), A(mlstm_norm_gain[j]), g)
            for k in ("w_fm", "w_tm", "b_g", "gain"):
                m[f"m{j}_{k}"] = mi[k]
        for j in range(depth // 2):
            ai = mla_inputs(x[b], positions[b], A(mla_w_in[j]), A(mla_q_norm[j]), A(mla_kv_norm[j]),
                            A(mla_w_qb[j]), A(mla_w_kvb[j]), g)
            m["pos"] = ai["i_pos"]
            for k in ("w_in", "qn", "kvn", "wqn", "wqr", "wkn", "wkv"):
                m[f"a{j}_{k}"] = ai["i_" + k]
        in_maps.append(m)
    return in_maps


_NC_CACHE = {}


def kernel(x, positions, ln_gain, ln_bias, mlstm_w_in, mlstm_b_gates, mlstm_norm_gain, mlstm_w_out,
           mla_w_in, mla_q_norm, mla_kv_norm, mla_w_qb, mla_w_kvb, mla_w_out,
           moe_w_router, moe_b_router, moe_w_gate_up, moe_b_gate_up, moe_w_down, moe_b_down):
    x = np.asarray(x)
    B, S_, _ = x.shape
    TOK = S_ // 2
    depth = ln_gain.shape[0]
    cap = CAP if S_ == SEQ_FULL else 128
    key = (S_, B, depth)
    if key not in _NC_CACHE:
        _NC_CACHE[key] = build_fused(S_, cap, B, depth)
    nc = _NC_CACHE[key]
    in_maps = fused_in_maps(x, positions, ln_gain, ln_bias, mlstm_w_in, mlstm_b_gates, mlstm_norm_gain, mlstm_w_out,
                            mla_w_in, mla_q_norm, mla_kv_norm, mla_w_qb, mla_w_kvb, mla_w_out,
                            moe_w_router, moe_b_router, moe_w_gate_up, moe_b_gate_up, moe_w_down, moe_b_down)
    res = run_bass_kernel_spmd(nc, in_maps, core_ids=list(range(2 * B)))
    out = np.empty((B, S_, D), dtype=np.float32)
    for c in range(2 * B):
        b, g = c // 2, c % 2
        out[b, g * TOK:(g + 1) * TOK] = res.results[c]["out"]
    return out
```

```python
import math
from contextlib import ExitStack
import numpy as np
import concourse.bass as bass
import concourse.mybir as mybir
from concourse.bass_utils import run_bass_kernel_spmd

F32 = mybir.dt.float32
BF16 = mybir.dt.bfloat16
I32 = mybir.dt.int32
U32 = mybir.dt.uint32
AF = mybir.ActivationFunctionType
ALU = mybir.AluOpType
AX = mybir.AxisListType


class Buf:
    __slots__ = ("t", "w", "r", "name")

    def __init__(self, t=None, name=""):
        self.t = t
        self.w = {}
        self.r = {}
        self.name = name

    def __getitem__(self, idx):
        return self.t[idx]


class _Eng:
    def __init__(self, name, eng, sem):
        self.name = name
        self.eng = eng
        self.sem = sem
        self.count = 0
        self.waited = {}


class Sched:
    def __init__(self, nc, ctx, n_dma_sems=12, same_engine_sync=True):
        self.nc = nc
        self.ctx = ctx
        self.same_engine_sync = same_engine_sync
        self.E = {}
        for name, eng in (("pe", nc.tensor), ("dve", nc.vector), ("act", nc.scalar),
                          ("pool", nc.gpsimd), ("sp", nc.sync)):
            sem = ctx.enter_context(nc.semaphore("s_" + name))
            self.E[name] = _Eng(name, eng, sem)
        self.dma_sems = {}
        for q in ("sp", "act", "pool"):
            lst = []
            for i in range(n_dma_sems):
                s = ctx.enter_context(nc.semaphore(f"d_{q}{i}"))
                lst.append([s, 0])
            self.dma_sems[q] = [lst, 0]
        self.ninst = 0
        self.pfx = ""

    def sbuf(self, name, shape, dt):
        t = self.ctx.enter_context(self.nc.sbuf_tensor(self.pfx + name, list(shape), dt))
        return Buf(t, name)

    def psum(self, name, shape, dt):
        t = self.ctx.enter_context(self.nc.psum_tensor(name, list(shape), dt))
        return Buf(t, name)

    @staticmethod
    def _key(sem):
        return id(sem)

    def _need(self, reads, writes):
        need = {}
        def add(d):
            for k, (s, v) in d.items():
                if k not in need or need[k][1] < v:
                    need[k] = (s, v)
        for b in reads:
            add(b.w)
        for b in writes:
            add(b.w)
            add(b.r)
        return need

    def _do_waits(self, e, need):
        for k, (s, v) in need.items():
            if s is e.sem and (e.name == "pe" or not self.same_engine_sync):
                continue
            if e.waited.get(k, 0) < v:
                e.eng.wait_ge(s, v)
                e.waited[k] = v

    def _record(self, dep_sem, dep_val, reads, writes):
        k = self._key(dep_sem)
        for b in writes:
            b.w = {k: (dep_sem, dep_val)}
            b.r = {}
        for b in reads:
            if b in writes:
                continue
            if k not in b.r or b.r[k][1] < dep_val:
                b.r[k] = (dep_sem, dep_val)

    def op(self, eng, fn, reads=(), writes=(), inc=True):
        e = self.E[eng]
        self._do_waits(e, self._need(reads, writes))
        inst = fn(e.eng)
        self.ninst += 1
        if inc:
            inst.then_inc(e.sem, 1)
            e.count += 1
            self._record(e.sem, e.count, reads, writes)
        else:
            self._record(e.sem, e.count + 1, reads, writes)
        return inst

    def dma(self, q, out, in_, reads=(), writes=(), indirect=None, **kw):
        e = self.E[q]
        lst, pos = self.dma_sems[q]
        ent = lst[pos]
        self.dma_sems[q][1] = (pos + 1) % len(lst)
        s, cnt = ent
        need = self._need(reads, writes)
        if cnt > 0:
            need[self._key(s)] = (s, cnt)
        for k, (ss, v) in need.items():
            if e.waited.get(k, 0) < v:
                e.eng.wait_ge(ss, v)
                e.waited[k] = v
        if indirect is None:
            inst = e.eng.dma_start(out=out, in_=in_, **kw)
        else:
            inst = e.eng.indirect_dma_start(out=out, in_=in_, **indirect, **kw)
        self.ninst += 1
        inst.then_inc(s, 16)
        ent[1] = cnt + 16
        self._record(s, cnt + 16, reads, writes)
        return inst

    def cc(self, kind, groups, in_ap, out_ap, reads=(), writes=(), inc=16):
        e = self.E["pool"]
        lst, pos = self.dma_sems["pool"]
        ent = lst[pos]
        self.dma_sems["pool"][1] = (pos + 1) % len(lst)
        s, cnt = ent
        need = self._need(reads, writes)
        if cnt > 0:
            need[self._key(s)] = (s, cnt)
        for k, (ss, v) in need.items():
            if e.waited.get(k, 0) < v:
                e.eng.wait_ge(ss, v)
                e.waited[k] = v
        inst = e.eng.collective_compute(kind, ALU.bypass, replica_groups=groups, ins=[in_ap], outs=[out_ap])
        self.ninst += 1
        inst.then_inc(s, inc)
        ent[1] = cnt + inc
        self._record(s, cnt + inc, reads, writes)
        return inst

    def barrier(self):
        targets = []
        for name, o in self.E.items():
            if o.count > 0:
                targets.append((o, o.sem, o.count))
        for q, (lst, _) in self.dma_sems.items():
            for s, cnt in lst:
                if cnt > 0:
                    targets.append((None, s, cnt))
        for name in ("sp", "pool", "act", "dve", "pe"):
            e = self.E[name]
            for (o, s, v) in targets:
                if o is e:
                    continue
                k = self._key(s)
                if e.waited.get(k, 0) < v:
                    e.eng.wait_ge(s, v)
                    e.waited[k] = v

    def finish(self, bufs):
        need = self._need((), bufs)
        for name in ("sp", "pool", "act", "dve", "pe"):
            e = self.E[name]
            for k, (s, v) in need.items():
                if s is e.sem:
                    continue
                if e.waited.get(k, 0) < v:
                    e.eng.wait_ge(s, v)
                    e.waited[k] = v


def run_interleaved(gens, width=2):
    it = iter(gens)
    active = []
    while True:
        while len(active) < width:
            g = next(it, None)
            if g is None:
                break
            active.append(g)
        if not active:
            break
        for g in list(active):
            try:
                next(g)
            except StopIteration:
                active.remove(g)


D = 1024
E = 32
ALPHA = 8.0 ** 0.25
LN_EPS = 1e-5
RMS_EPS = 1e-6
SCALE = 192.0 ** -0.5
STAGE = SUB = SUBA = SUBB = SUBC = 99
SEQ_FULL = 8192
TOK_CORE = 4096
CAP = 640

def emit_ln(S, z, gain, bias, out, tmp):
    st, mv, sd, rstd, xn = tmp
    for h in range(2):
        S.op("dve", lambda e: e.bn_stats(out=st[:, h, :], in_=z[:, h * 512:(h + 1) * 512]),
             reads=[z], writes=[st])
    S.op("dve", lambda e: e.bn_aggr(out=mv[:], in_=st[:].rearrange("p a b -> p (a b)")),
         reads=[st], writes=[mv])
    S.op("act", lambda e: e.activation(out=sd[:], in_=mv[:, 1:2], func=AF.Sqrt, bias=tmp_eps(S)[:], scale=1.0),
         reads=[mv], writes=[sd])
    S.op("dve", lambda e: e.reciprocal(out=rstd[:], in_=sd[:]), reads=[sd], writes=[rstd])
    S.op("dve", lambda e: e.tensor_scalar(out=sd[:], in0=mv[:, 0:1], scalar1=rstd[:], scalar2=-1.0,
                                          op0=ALU.mult, op1=ALU.mult), reads=[mv, rstd], writes=[sd])
    S.op("act", lambda e: e.activation(out=xn[:], in_=z[:], func=AF.Identity, scale=rstd[:], bias=sd[:]),
         reads=[z, rstd, sd], writes=[xn])
    S.op("dve", lambda e: e.tensor_tensor(out=xn[:], in0=xn[:], in1=gain[:], op=ALU.mult),
         reads=[xn, gain], writes=[xn])
    S.op("dve", lambda e: e.tensor_tensor(out=out[:], in0=xn[:], in1=bias[:], op=ALU.add),
         reads=[xn, bias], writes=[out])


_EPS = {}


def tmp_eps(S):
    return _EPS[id(S)]


def emit_post(S, nc, T, CAP, dr, ps):
    NT = T // 128
    RB = CAP // 128
    parts = []
    lo = 0
    while lo < CAP:
        n = min(512, CAP - lo)
        parts.append((lo, n))
        lo += n
    ctx = S.ctx
    identF = S.sbuf("identF", [128, 128], F32)
    identB = S.sbuf("identB", [128, 128], BF16)
    triS = S.sbuf("triS", [128, 128], F32)
    onesM = S.sbuf("onesM", [128, 128], F32)
    offs = S.sbuf("offs", [128, E], F32)
    epsb = S.sbuf("epsb", [128, 1], F32)
    _EPS[id(S)] = epsb
    idx_all = S.sbuf("idx_all", [128, NT, 4], I32)
    gate_all = S.sbuf("gate_all", [128, NT, 4], F32)
    wgu = [S.sbuf(f"wgu{i}", [128, 8, 2048], BF16) for i in range(2)]
    wd = [S.sbuf(f"wd{i}", [128, 8, 1024], BF16) for i in range(2)]
    bgu = [S.sbuf(f"bgu{i}", [128, 8, 2], F32) for i in range(2)]
    bdn = [S.sbuf(f"bdn{i}", [128, 1024], F32) for i in range(2)]
    lnp = [S.sbuf(f"lnp{i}", [128, 1024], F32) for i in range(4)]
    st = S.sbuf("st", [128, 2, 6], F32)
    mv = S.sbuf("mv", [128, 2], F32)
    sd = S.sbuf("sd", [128, 1], F32)
    rstd = S.sbuf("rstd", [128, 1], F32)
    xn = S.sbuf("xn", [128, 1024], F32)
    lntmp = (st, mv, sd, rstd, xn)
    xg_d = Buf(None, "xg")
    yg_d = Buf(None, "yg")
    x1_d = Buf(None, "x1s")
    out_d = Buf(None, "out")

    S.op("pool", lambda e: e.memset(identF[:], 0.0), writes=[identF])
    S.op("pool", lambda e: e.affine_select(out=identF[:], in_=identF[:], pattern=[[-1, 128]],
                                           compare_op=ALU.not_equal, fill=1.0, base=0,
                                           channel_multiplier=1), reads=[identF], writes=[identF])
    S.op("dve", lambda e: e.tensor_copy(out=identB[:], in_=identF[:]), reads=[identF], writes=[identB])
    S.op("pool", lambda e: e.memset(onesM[:], 1.0), writes=[onesM])
    S.op("pool", lambda e: e.memset(epsb[:], LN_EPS), writes=[epsb])
    S.op("pool", lambda e: e.affine_select(out=triS[:], in_=onesM[:], pattern=[[1, 128]],
                                           compare_op=ALU.is_gt, fill=0.0, base=0,
                                           channel_multiplier=-1), reads=[onesM], writes=[triS])
    S.op("pool", lambda e: e.iota(offs[:], pattern=[[CAP, E]], base=0, channel_multiplier=0,
                                  allow_small_or_imprecise_dtypes=True), writes=[offs])
    for j, nm in enumerate(("g1", "b1", "g2", "b2")):
        S.dma("sp", lnp[j][:], dr[nm].partition_broadcast(128), writes=[lnp[j]])

    with ExitStack() as pctx:
        def sb(name, shape, dt):
            return Buf(pctx.enter_context(nc.sbuf_tensor(S.pfx + name, list(shape), dt)), name)
        wout = sb("wout", [128, 8, 1024], BF16)
        wrt = sb("wrt", [128, 8, E], F32)
        brt = sb("brt", [128, E], F32)
        baseoffs = sb("baseoffs", [128, E], F32)
        hgt = [sb(f"hgt{i}", [128, 1024], BF16) for i in range(4)]
        xt = [sb(f"xt{i}", [128, 1024], F32) for i in range(4)]
        hgT = [sb(f"hgT{i}", [128, 8, 128], BF16) for i in range(2)]
        z = [sb(f"z{i}", [128, 1024], F32) for i in range(2)]
        x1 = [sb(f"x1{i}", [128, 1024], F32) for i in range(2)]
        x1b = [sb(f"x1b{i}", [128, 1024], BF16) for i in range(2)]
        x1T = [sb(f"x1T{i}", [128, 8, 128], F32) for i in range(2)]
        lg = [sb(f"lg{i}", [128, E], F32) for i in range(2)]
        top8 = [sb(f"top8{i}", [128, 8], F32) for i in range(2)]
        mask = [sb(f"mask{i}", [128, E], F32) for i in range(2)]
        nmx = [sb(f"nmx{i}", [128, 1], F32) for i in range(2)]
        ex = [sb(f"ex{i}", [128, 4], F32) for i in range(2)]
        sm = [sb(f"sm{i}", [128, 1], F32) for i in range(2)]
        rs = [sb(f"rs{i}", [128, 1], F32) for i in range(2)]
        posf = [sb(f"posf{i}", [128, E], F32) for i in range(2)]
        oh = [sb(f"oh{i}", [128, E], F32) for i in range(2)]
        junk = [sb(f"junk{i}", [128, E], F32) for i in range(2)]
        destf = [sb(f"destf{i}", [128, 4], F32) for i in range(2)]

        if "hg_gath" in dr:
            hgidx = sb("hgidx", [128, NT, 2], I32)
            S.dma("sp", hgidx[:], dr["hgidx"], writes=[hgidx])
        for c in range(0, 8, 2):
            S.dma("pool", wout[:, c:c + 2, :],
                  dr["w_out"][c * 128:(c + 2) * 128, :].rearrange("(c p) n -> p c n", p=128), writes=[wout])
        S.dma("sp", wrt[:], dr["w_rt"].rearrange("(c p) n -> p c n", p=128), writes=[wrt])
        S.dma("sp", brt[:], dr["b_rt"].partition_broadcast(128), writes=[brt])
        S.op("dve", lambda e: e.tensor_copy(out=baseoffs[:], in_=offs[:]), reads=[offs], writes=[baseoffs])

        ptB, P = ps["ptB"], ps["P"]
        def rload(i):
            sl_ = slice(i * 128, (i + 1) * 128)
            if "hg_gath" in dr:
                for r in range(2):
                    S.dma("pool", hgt[i % 4][:, r * 512:(r + 1) * 512], dr["hg_gath"], reads=[hgidx], writes=[hgt[i % 4]],
                          indirect=dict(out_offset=None,
                                        in_offset=bass.IndirectOffsetOnAxis(ap=hgidx[:, i, r:r + 1], axis=0)))
            else:
                S.dma("sp", hgt[i % 4][:], dr["hg"][sl_, :], writes=[hgt[i % 4]])
            S.dma("sp", xt[i % 4][:], dr["x"][sl_, :], writes=[xt[i % 4]])

        for i_ in range(min(2, NT)):
            rload(i_)

        def _tile(i):
            b = i % 2
            sl = slice(i * 128, (i + 1) * 128)
            if i + 2 < NT:
                rload(i + 2)
            b4 = i % 4
            for c in range(8):
                S.op("pe", lambda e: e.transpose(out=ptB[:, c * 128:(c + 1) * 128],
                                                 in_=hgt[b4][:, c * 128:(c + 1) * 128], identity=identB[:]),
                     reads=[hgt[b4], identB], writes=[ptB], inc=(c == 7))
            S.op("act", lambda e: e.copy(out=hgT[b][:], in_=ptB[:].rearrange("p (c n) -> p c n", c=8)),
                 reads=[ptB], writes=[hgT[b]])
            for h in range(2):
                pm = P[1 + h]
                for c in range(8):
                    S.op("pe", lambda e: e.matmul(pm[:], lhsT=hgT[b][:, c, :], rhs=wout[:, c, h * 512:(h + 1) * 512],
                                                  start=(c == 0), stop=(c == 7)),
                         reads=[hgT[b], wout], writes=[pm], inc=(c == 7))
                S.op("dve", lambda e: e.scalar_tensor_tensor(out=z[b][:, h * 512:(h + 1) * 512],
                                                             in0=xt[b4][:, h * 512:(h + 1) * 512], scalar=ALPHA,
                                                             in1=pm[:], op0=ALU.mult, op1=ALU.add),
                     reads=[xt[b4], pm], writes=[z[b]])
            yield
            emit_ln(S, z[b], lnp[0], lnp[1], x1[b], lntmp)
            yield
            S.dma("sp", dr["x1s"][sl, :], x1[b][:], reads=[x1[b]], writes=[x1_d])
            S.op("act", lambda e: e.copy(out=x1b[b][:], in_=x1[b][:]), reads=[x1[b]], writes=[x1b[b]])
            for h in range(2):
                pT = P[3 + h]
                for c in range(4):
                    cc = h * 4 + c
                    S.op("pe", lambda e: e.transpose(out=pT[:, c * 128:(c + 1) * 128],
                                                     in_=x1[b][:, cc * 128:(cc + 1) * 128], identity=identF[:]),
                         reads=[x1[b], identF], writes=[pT], inc=(c == 3))
                S.op("act", lambda e: e.copy(out=x1T[b][:, h * 4:(h + 1) * 4, :],
                                             in_=pT[:].rearrange("p (c n) -> p c n", c=4)),
                     reads=[pT], writes=[x1T[b]])
            yield
            pq = P[5]
            for c in range(8):
                S.op("pe", lambda e: e.matmul(pq[:, 0:E], lhsT=x1T[b][:, c, :], rhs=wrt[:, c, :],
                                              start=(c == 0), stop=(c == 7)),
                     reads=[x1T[b], wrt], writes=[pq], inc=(c == 7))
            S.op("dve", lambda e: e.tensor_tensor(out=lg[b][:], in0=pq[:, 0:E], in1=brt[:], op=ALU.add),
                 reads=[pq, brt], writes=[lg[b]])
            S.op("dve", lambda e: e.max(out=top8[b][:], in_=lg[b][:]), reads=[lg[b]], writes=[top8[b]])
            S.op("dve", lambda e: e.tensor_scalar(out=mask[b][:], in0=lg[b][:], scalar1=top8[b][:, 3:4], scalar2=None,
                                                  op0=ALU.is_ge), reads=[lg[b], top8[b]], writes=[mask[b]])
            yield
            S.op("act", lambda e: e.mul(out=nmx[b][:], in_=top8[b][:, 0:1], mul=-1.0), reads=[top8[b]], writes=[nmx[b]])
            S.op("act", lambda e: e.activation(out=ex[b][:], in_=top8[b][:, 0:4], func=AF.Exp, bias=nmx[b][:],
                                               scale=1.0, accum_out=sm[b][:]),
                 reads=[top8[b], nmx[b]], writes=[ex[b], sm[b]])
            S.op("dve", lambda e: e.reciprocal(out=rs[b][:], in_=sm[b][:]), reads=[sm[b]], writes=[rs[b]])
            S.op("dve", lambda e: e.tensor_scalar(out=gate_all[:, i, :], in0=ex[b][:], scalar1=rs[b][:], scalar2=None,
                                                  op0=ALU.mult), reads=[ex[b], rs[b]], writes=[gate_all])
            yield
            S.op("pe", lambda e: e.matmul(pq[:, 32:32 + E], lhsT=triS[:], rhs=mask[b][:], start=True, stop=True),
                 reads=[triS, mask[b]], writes=[pq], inc=False)
            S.op("pe", lambda e: e.matmul(pq[:, 64:64 + E], lhsT=onesM[:], rhs=mask[b][:], start=True, stop=True),
                 reads=[onesM, mask[b]], writes=[pq])
            S.op("dve", lambda e: e.tensor_tensor(out=posf[b][:], in0=pq[:, 32:32 + E], in1=baseoffs[:], op=ALU.add),
                 reads=[pq, baseoffs], writes=[posf[b]])
            S.op("dve", lambda e: e.tensor_tensor(out=baseoffs[:], in0=pq[:, 64:64 + E], in1=baseoffs[:], op=ALU.add),
                 reads=[pq, baseoffs], writes=[baseoffs])
            for k in range(4):
                S.op("dve", lambda e: e.scalar_tensor_tensor(out=junk[b][:], in0=lg[b][:], scalar=top8[b][:, k:k + 1],
                                                             in1=posf[b][:], op0=ALU.is_equal, op1=ALU.mult,
                                                             accum_out=destf[b][:, k:k + 1]),
                     reads=[lg[b], top8[b], posf[b]], writes=[junk[b], destf[b]])
            yield
            S.op("dve", lambda e: e.tensor_copy(out=idx_all[:, i, :], in_=destf[b][:]), reads=[destf[b]], writes=[idx_all])
            for k in range(4):
                S.dma("pool", dr["xg"], x1b[b][:], reads=[x1b[b], idx_all], writes=[],
                      indirect=dict(out_offset=bass.IndirectOffsetOnAxis(ap=idx_all[:, i, k:k + 1], axis=0),
                                    in_offset=None))
        run_interleaved([_tile(i_) for i_ in range(NT)], 2)
        S.barrier()

    with ExitStack() as pctx:
        def sb(name, shape, dt):
            return Buf(pctx.enter_context(nc.sbuf_tensor(S.pfx + name, list(shape), dt)), name)
        xgr = [sb(f"xgr{i}", [128, 1024], BF16) for i in range(2)]
        xgT = sb("xgT", [128, 8, CAP], BF16)
        hT = sb("hT", [128, 8, CAP], BF16)
        tg = [sb(f"tg{i}", [128, 512], F32) for i in range(2)]
        tsg = [sb(f"tsg{i}", [128, 512], F32) for i in range(2)]
        tu = [sb(f"tu{i}", [128, 512], F32) for i in range(2)]
        yo = [sb(f"yo{i}", [128, 1024], F32) for i in range(2)]
        ptB, P = ps["ptB"], ps["P"]
        stg = [sb(f"stg{i}", [128, 2048], F32) for i in range(3)]
        wguc = [[Buf(wgu[i].t, f"wguc{i}_{c}") for c in range(8)] for i in range(2)]
        wdc = [[Buf(wd[i].t, f"wdc{i}_{c}") for c in range(8)] for i in range(2)]
        cast_gu = ["act", "act", "dve", "act", "act", "dve", "act", "act"]
        cast_dn = ["act", "dve", "act", "dve"]
        stk = [0]

        def cast(eng, out, in_, reads, writes):
            if eng == "act":
                S.op("act", lambda e: e.copy(out=out, in_=in_), reads=reads, writes=writes)
            else:
                S.op(eng, lambda e: e.tensor_copy(out=out, in_=in_), reads=reads, writes=writes)

        def load_expert(e_):
            b = e_ % 2
            for c in range(8):
                st = stg[stk[0] % 3]
                stk[0] += 1
                S.dma("sp", st[:], dr["w_gu"][e_, c * 128:(c + 1) * 128, :], writes=[st])
                cast(cast_gu[c], wgu[b][:, c, :], st[:], [st], [wguc[b][c]])
                yield
            for k2, c in enumerate(range(0, 8, 2)):
                st = stg[stk[0] % 3]
                stk[0] += 1
                S.dma("sp", st[:].rearrange("p (c n) -> p c n", c=2),
                      dr["w_dn"][e_, c * 128:(c + 2) * 128, :].rearrange("(c p) n -> p c n", p=128), writes=[st])
                cast(cast_dn[k2], wd[b][:, c:c + 2, :], st[:].rearrange("p (c n) -> p c n", c=2), [st],
                     [wdc[b][c], wdc[b][c + 1]])
                yield
            with nc.allow_non_contiguous_dma(reason="tiny bias"):
                S.dma("sp", bgu[b][:], dr["b_gu"][e_, :].rearrange("(c p t) -> p c t", p=128, t=2), writes=[bgu[b]])
            S.dma("sp", bdn[b][:], dr["b_dn"][e_, :].partition_broadcast(128), writes=[bdn[b]])

        for _ in load_expert(0):
            pass
        cnt = 0
        for e_ in range(E):
            wb = e_ % 2
            for rb in range(RB):
                b = rb % 2
                r0 = e_ * CAP + rb * 128
                S.dma("sp", xgr[b][:], dr["xg"][r0:r0 + 128, :], writes=[xgr[b]])
                for c in range(8):
                    S.op("pe", lambda e: e.transpose(out=ptB[:, c * 128:(c + 1) * 128],
                                                     in_=xgr[b][:, c * 128:(c + 1) * 128], identity=identB[:]),
                         reads=[xgr[b], identB], writes=[ptB], inc=(c == 7))
                S.op("act", lambda e: e.copy(out=xgT[:, :, rb * 128:(rb + 1) * 128],
                                             in_=ptB[:].rearrange("p (c n) -> p c n", c=8)),
                     reads=[ptB], writes=[xgT])
            pre = load_expert(e_ + 1) if e_ + 1 < E else iter(())
            for fc in range(8):
                for (lo, n) in parts:
                    b = cnt % 2
                    cnt += 1
                    next(pre, None)
                    pg, pu = P[1 + b], P[3 + b]
                    for gi, pp in ((0, pg), (1, pu)):
                        for c in range(8):
                            S.op("pe", lambda e: e.matmul(pp[:, 0:n],
                                                          lhsT=wgu[wb][:, c, fc * 256 + gi:fc * 256 + 256:2],
                                                          rhs=xgT[:, c, lo:lo + n], start=(c == 0), stop=(c == 7)),
                                 reads=[wguc[wb][c], xgT], writes=[pp], inc=(c == 7))
                    S.op("dve", lambda e: e.tensor_scalar(out=tg[b][:, 0:n], in0=pg[:, 0:n], scalar1=bgu[wb][:, fc, 0:1],
                                                          scalar2=7.0, op0=ALU.add, op1=ALU.min),
                         reads=[pg, bgu[wb]], writes=[tg[b]])
                    S.op("act", lambda e: e.activation(out=tsg[b][:, 0:n], in_=tg[b][:, 0:n], func=AF.Gelu_apprx_sigmoid),
                         reads=[tg[b]], writes=[tsg[b]])
                    S.op("dve", lambda e: e.tensor_scalar(out=tu[b][:, 0:n], in0=pu[:, 0:n], scalar1=bgu[wb][:, fc, 1:2],
                                                          scalar2=-7.0, op0=ALU.add, op1=ALU.max),
                         reads=[pu, bgu[wb]], writes=[tu[b]])
                    S.op("dve", lambda e: e.scalar_tensor_tensor(out=tu[b][:, 0:n], in0=tu[b][:, 0:n], scalar=7.0,
                                                                 in1=tsg[b][:, 0:n], op0=ALU.min, op1=ALU.mult),
                         reads=[tu[b], tsg[b]], writes=[tu[b]])
                    S.op("dve", lambda e: e.tensor_tensor(out=hT[:, fc, lo:lo + n], in0=tu[b][:, 0:n], in1=tsg[b][:, 0:n],
                                                          op=ALU.add), reads=[tu[b], tsg[b]], writes=[hT])
            for _ in pre:
                pass
            for rb in range(RB):
                b = rb % 2
                for h in range(2):
                    py = P[5 + h]
                    for fc in range(8):
                        S.op("pe", lambda e: e.matmul(py[:], lhsT=hT[:, fc, rb * 128:(rb + 1) * 128],
                                                      rhs=wd[wb][:, fc, h * 512:(h + 1) * 512],
                                                      start=(fc == 0), stop=(fc == 7)),
                             reads=[hT, wdc[wb][fc]], writes=[py], inc=(fc == 7))
                    S.op("dve", lambda e: e.tensor_tensor(out=yo[b][:, h * 512:(h + 1) * 512], in0=py[:],
                                                          in1=bdn[wb][:, h * 512:(h + 1) * 512], op=ALU.add),
                         reads=[py, bdn[wb]], writes=[yo[b]])
                r0 = e_ * CAP + rb * 128
                S.dma("sp", dr["yg"][r0:r0 + 128, :], yo[b][:], reads=[yo[b]], writes=[yg_d])
        S.barrier()

    with ExitStack() as pctx:
        def sb(name, shape, dt):
            return Buf(pctx.enter_context(nc.sbuf_tensor(S.pfx + name, list(shape), dt)), name)
        xc = [sb(f"xc{i}", [128, 1024], F32) for i in range(2)]
        yk = [[sb(f"yk{i}_{k}", [128, 1024], F32) for k in range(4)] for i in range(2)]
        acc = [sb(f"acc{i}", [128, 1024], F32) for i in range(2)]
        x2 = [sb(f"x2{i}", [128, 1024], F32) for i in range(2)]
        def _tile(i):
            b = i % 2
            sl = slice(i * 128, (i + 1) * 128)
            S.dma("sp", xc[b][:], dr["x1s"][sl, :], writes=[xc[b]])
            for k in range(4):
                S.dma("pool", yk[b][k][:], dr["yg"], reads=[idx_all], writes=[yk[b][k]],
                      indirect=dict(out_offset=None,
                                    in_offset=bass.IndirectOffsetOnAxis(ap=idx_all[:, i, k:k + 1], axis=0)))
            yield
            S.op("dve", lambda e: e.tensor_scalar(out=acc[b][:], in0=yk[b][0][:], scalar1=gate_all[:, i, 0:1],
                                                  scalar2=None, op0=ALU.mult),
                 reads=[yk[b][0], gate_all], writes=[acc[b]])
            for k in range(1, 4):
                S.op("dve", lambda e: e.scalar_tensor_tensor(out=acc[b][:], in0=yk[b][k][:], scalar=gate_all[:, i, k:k + 1],
                                                             in1=acc[b][:], op0=ALU.mult, op1=ALU.add),
                     reads=[yk[b][k], gate_all, acc[b]], writes=[acc[b]])
            S.op("dve", lambda e: e.scalar_tensor_tensor(out=acc[b][:], in0=xc[b][:], scalar=ALPHA, in1=acc[b][:],
                                                         op0=ALU.mult, op1=ALU.add),
                 reads=[xc[b], acc[b]], writes=[acc[b]])
            yield
            emit_ln(S, acc[b], lnp[2], lnp[3], x2[b], lntmp)
            S.dma("sp", dr["out"][sl, :], x2[b][:], reads=[x2[b]], writes=[out_d])
        run_interleaved([_tile(i_) for i_ in range(NT)], 2)
        S.barrier()


def build_k3(T, CAP):
    nc = bass.Bass("TRN2", target_bir_lowering=False)
    dr = {}
    def din(name, shape, dt=F32):
        dr[name] = nc.dram_tensor(name, list(shape), dt, kind="ExternalInput").ap()
    din("x", [T, D]); din("hg", [T, D], BF16)
    din("w_out", [D, D]); din("g1", [D]); din("b1", [D]); din("g2", [D]); din("b2", [D])
    din("w_rt", [D, E]); din("b_rt", [E]); din("w_gu", [E, D, 2 * D]); din("b_gu", [E, 2 * D])
    din("w_dn", [E, D, D]); din("b_dn", [E, D])
    dr["out"] = nc.dram_tensor("out", [T, D], F32, kind="ExternalOutput").ap()
    dr["xg"] = nc.dram_tensor("xg", [E * CAP, D], BF16, kind="Internal").ap()
    dr["yg"] = nc.dram_tensor("yg", [E * CAP, D], F32, kind="Internal").ap()
    dr["x1s"] = nc.dram_tensor("x1s", [T, D], F32, kind="Internal").ap()
    with ExitStack() as ctx:
        S = Sched(nc, ctx)
        ps = {"ptB": S.psum("ptB", [128, 1024], BF16), "P": [None] + [S.psum(f"P{i}", [128, 512], F32) for i in range(1, 8)]}
        emit_post(S, nc, T, CAP, dr, ps)
        pass
    return nc


def emit_mlstm(S, nc, SEQ, dr, ps):
    NT = SEQ // 128
    ptB, PA, PB, PC, PQ, PST, PN = ps["ptB"], ps["P"][1], ps["P"][2], ps["P"][3], ps["P"][4], ps["P"][5], ps["P"][6:8]
    identF = S.sbuf("identF", [128, 128], F32)
    identB = S.sbuf("identB", [128, 128], BF16)
    triI = S.sbuf("triI", [128, 128], F32)
    onesM = S.sbuf("onesM", [128, 128], F32)
    epsb = S.sbuf("epsb", [128, 1], F32)
    wfm = S.sbuf("wfm", [128, 8, 512], BF16)
    wtm = S.sbuf("wtm", [128, 8, 1288], BF16)
    bg = S.sbuf("bg", [128, 8], F32)
    gain = S.sbuf("gain_sb", [128, 512], F32)
    Cn = [S.sbuf(f"Cn{h}", [128, 132], F32) for h in range(4)]
    Cnb = [S.sbuf(f"Cnb{h}", [128, 132], BF16) for h in range(4)]
    out_d = Buf(None, "out")

    S.op("pool", lambda e: e.memset(identF[:], 0.0), writes=[identF])
    S.op("pool", lambda e: e.affine_select(out=identF[:], in_=identF[:], pattern=[[-1, 128]],
                                           compare_op=ALU.not_equal, fill=1.0, base=0,
                                           channel_multiplier=1), reads=[identF], writes=[identF])
    S.op("dve", lambda e: e.tensor_copy(out=identB[:], in_=identF[:]), reads=[identF], writes=[identB])
    S.op("pool", lambda e: e.memset(onesM[:], 1.0), writes=[onesM])
    S.op("pool", lambda e: e.memset(epsb[:], RMS_EPS), writes=[epsb])
    oneb = S.sbuf("oneb", [128, 1], F32)
    S.op("pool", lambda e: e.memset(oneb[:], 1.0), writes=[oneb])
    S.op("pool", lambda e: e.affine_select(out=triI[:], in_=onesM[:], pattern=[[1, 128]],
                                           compare_op=ALU.is_ge, fill=0.0, base=0,
                                           channel_multiplier=-1), reads=[onesM], writes=[triI])
    hm = S.sbuf("hm", [128, 2], F32)
    S.op("pool", lambda e: e.affine_select(out=hm[:, 0:1], in_=onesM[:, 0:1], pattern=[[0, 1]],
                                           compare_op=ALU.is_ge, fill=0.0, base=63,
                                           channel_multiplier=-1), reads=[onesM], writes=[hm])
    S.op("pool", lambda e: e.affine_select(out=hm[:, 1:2], in_=onesM[:, 0:1], pattern=[[0, 1]],
                                           compare_op=ALU.is_ge, fill=0.0, base=-64,
                                           channel_multiplier=1), reads=[onesM], writes=[hm])
    for h in range(4):
        S.op("pool", lambda e: e.memset(Cn[h][:], 0.0), writes=[Cn[h]])
        S.op("pool", lambda e: e.memset(Cnb[h][:], 0.0), writes=[Cnb[h]])
    for c in range(0, 8, 2):
        S.dma("pool", wfm[:, c:c + 2, :], dr["w_fm"][c * 128:(c + 2) * 128, :].rearrange("(c p) n -> p c n", p=128),
              writes=[wfm])
        S.dma("pool", wtm[:, c:c + 2, :], dr["w_tm"][c * 128:(c + 2) * 128, :].rearrange("(c p) n -> p c n", p=128),
              writes=[wtm])
    S.dma("sp", bg[:], dr["b_g"].partition_broadcast(128), writes=[bg])
    S.dma("sp", gain[:], dr["gain"].partition_broadcast(128), writes=[gain])

    def sb2(name, shape, dt):
        return [S.sbuf(f"{name}{i}", shape, dt) for i in range(2)]
    xt = [S.sbuf(f"xt{i}", [128, 1024], F32) for i in range(4)]
    xb = sb2("xb", [128, 1024], BF16)
    xT = sb2("xT", [128, 8, 128], BF16)
    qTz = [[S.sbuf(f"qTz{i}_{h}", [128, 128], BF16) for h in range(4)] for i in range(2)]
    kT = sb2("kT", [128, 2, 128], BF16)
    gts = sb2("gts", [128, 8], F32)
    e1 = sb2("e1", [128, 4], F32)
    l1 = sb2("l1", [128, 4], F32)
    eq = sb2("eq", [128, 4], F32)
    ginb = sb2("ginb", [128, 4], F32)
    u = sb2("u", [128, 4], F32)
    ebl = sb2("ebl", [128, 4], F32)
    ktm = sb2("ktm", [128, 256], BF16)
    vx = sb2("vx", [128, 4, 132], BF16)
    og = sb2("og", [128, 512], F32)
    Sm = sb2("Sm", [128, 4, 128], BF16)
    dd = sb2("dd", [128, 4], F32)
    rr = sb2("rr", [128, 4], F32)
    fac = sb2("fac", [128, 4], F32)
    ss = sb2("ss", [128, 4], F32)
    rms = sb2("rms", [128, 4], F32)
    rinv = sb2("rinv", [128, 4], F32)
    fac2 = sb2("fac2", [128, 4], F32)
    sqj = sb2("sqj", [128, 128], F32)
    hn = sb2("hn", [128, 512], F32)
    hgo = sb2("hgo", [128, 512], BF16)
    for i in range(2):
        for h in range(4):
            S.op("pool", lambda e: e.memset(qTz[i][h][:], 0.0), writes=[qTz[i][h]])

    def xload(t):
        S.dma("sp", xt[t % 4][:], (dr["xmap"](t) if "xmap" in dr else dr["x"][t * 128:(t + 1) * 128, :]), writes=[xt[t % 4]])

    for t_ in range(min(2, NT)):
        xload(t_)

    def _tile(t):
        b = t % 2
        sl = slice(t * 128, (t + 1) * 128)
        if t + 2 < NT:
            xload(t + 2)
        S.op("act", lambda e: e.copy(out=xb[b][:], in_=xt[t % 4][:]), reads=[xt[t % 4]], writes=[xb[b]])
        for c in range(8):
            S.op("pe", lambda e: e.transpose(out=ptB[:, c * 128:(c + 1) * 128], in_=xb[b][:, c * 128:(c + 1) * 128],
                                             identity=identB[:]), reads=[xb[b], identB], writes=[ptB], inc=(c == 7))
        S.op("dve", lambda e: e.tensor_copy(out=xT[b][:], in_=ptB[:].rearrange("p (c n) -> p c n", c=8)),
             reads=[ptB], writes=[xT[b]])
        if STAGE < 1:
            return
        yield
        for j in range(4):
            for c in range(8):
                S.op("pe", lambda e: e.matmul(PQ[:, j * 128:(j + 1) * 128], lhsT=wfm[:, c, j * 128:(j + 1) * 128],
                                              rhs=xT[b][:, c, :], start=(c == 0), stop=(c == 7)),
                     reads=[wfm, xT[b]], writes=[PQ], inc=(c == 7))
        for h in range(4):
            hb, jj = h // 2, h % 2
            S.op("dve", lambda e: e.tensor_scalar(out=qTz[b][h][:], in0=PQ[:, hb * 128:(hb + 1) * 128],
                                                  scalar1=hm[:, jj:jj + 1], scalar2=0.125, op0=ALU.mult, op1=ALU.mult),
                 reads=[PQ, hm], writes=[qTz[b][h]])
        S.op("dve", lambda e: e.tensor_copy(out=kT[b][:], in_=PQ[:, 256:512].rearrange("p (c n) -> p c n", c=2)),
             reads=[PQ], writes=[kT[b]])
        if STAGE < 2:
            return
        yield
        for (pp, lo, n) in ((PA, 0, 264), (PB, 264, 512), (PC, 776, 512))[:SUB]:
            for c in range(8):
                S.op("pe", lambda e: e.matmul(pp[:, 0:n], lhsT=xT[b][:, c, :], rhs=wtm[:, c, lo:lo + n],
                                              start=(c == 0), stop=(c == 7)),
                     reads=[wtm, xT[b]], writes=[pp], inc=(c == 7))
        if SUB < 4:
            return
        S.op("dve", lambda e: e.tensor_tensor(out=gts[b][:], in0=PA[:, 256:264], in1=bg[:], op=ALU.add),
             reads=[PA, bg], writes=[gts[b]])
        if SUB < 5:
            return
        S.op("dve", lambda e: e.tensor_copy(out=ktm[b][:], in_=PA[:, 0:256]), reads=[PA], writes=[ktm[b]])
        if SUB < 6:
            return
        S.op("act", lambda e: e.activation(out=e1[b][:], in_=gts[b][:, 4:8], func=AF.Exp, scale=-1.0),
             reads=[gts[b]], writes=[e1[b]])
        S.op("act", lambda e: e.activation(out=l1[b][:], in_=e1[b][:], func=AF.Ln, bias=oneb[:], scale=1.0),
             reads=[e1[b], oneb], writes=[l1[b]])
        if STAGE < 3:
            return
        S.op("pe", lambda e: e.matmul(PA[:, 272:276], lhsT=triI[:], rhs=l1[b][:], start=True, stop=True),
             reads=[triI, l1[b]], writes=[PA], inc=False)
        S.op("pe", lambda e: e.matmul(PA[:, 280:284], lhsT=onesM[:], rhs=l1[b][:], start=True, stop=True),
             reads=[onesM, l1[b]], writes=[PA])
        S.op("act", lambda e: e.activation(out=eq[b][:], in_=PA[:, 272:276], func=AF.Exp, scale=-1.0),
             reads=[PA], writes=[eq[b]])
        S.op("dve", lambda e: e.tensor_tensor(out=ginb[b][:], in0=PA[:, 272:276], in1=gts[b][:, 0:4], op=ALU.add),
             reads=[PA, gts[b]], writes=[ginb[b]])
        S.op("act", lambda e: e.activation(out=u[b][:], in_=ginb[b][:], func=AF.Exp), reads=[ginb[b]], writes=[u[b]])
        S.op("act", lambda e: e.activation(out=ebl[b][:], in_=PA[:, 280:284], func=AF.Exp, scale=-1.0),
             reads=[PA], writes=[ebl[b]])
        for h in range(4):
            S.op("dve", lambda e: e.tensor_scalar(out=vx[b][:, h, 0:128], in0=PB[:, h * 128:(h + 1) * 128],
                                                  scalar1=u[b][:, h:h + 1], scalar2=None, op0=ALU.mult),
                 reads=[PB, u[b]], writes=[vx[b]])
        S.op("dve", lambda e: e.tensor_copy(out=vx[b][:, :, 128], in_=u[b][:]), reads=[u[b]], writes=[vx[b]])
        S.op("act", lambda e: e.activation(out=og[b][:], in_=PC[:], func=AF.Sigmoid), reads=[PC], writes=[og[b]])
        if STAGE < 4:
            return
        for h in range(4):
            hb = h // 2
            S.op("pe", lambda e: e.matmul(PST[:, h * 128:(h + 1) * 128], lhsT=kT[b][:, hb, :], rhs=qTz[b][h][:],
                                          start=True, stop=True),
                 reads=[kT[b], qTz[b][h]], writes=[PST], inc=(h == 3))
        for h in range(4):
            S.op("dve", lambda e: e.tensor_tensor(out=Sm[b][:, h, :], in0=PST[:, h * 128:(h + 1) * 128], in1=triI[:],
                                                  op=ALU.mult), reads=[PST, triI], writes=[Sm[b]])
        for h in range(4):
            hb, jj = h // 2, h % 2
            pn = PN[hb]
            S.op("pe", lambda e: e.matmul(pn[:, jj * 129:(jj + 1) * 129], lhsT=Sm[b][:, h, :], rhs=vx[b][:, h, 0:129],
                                          start=True, stop=False),
                 reads=[Sm[b], vx[b]], writes=[pn], inc=False)
            S.op("pe", lambda e: e.matmul(pn[:, jj * 129:(jj + 1) * 129], lhsT=qTz[b][h][:], rhs=Cnb[h][:, 0:129],
                                          start=False, stop=True),
                 reads=[qTz[b][h], Cnb[h]], writes=[pn])
        if STAGE < 5:
            return
        for h in range(4):
            hb, jj = h // 2, h % 2
            R = slice(0, 128)
            pd = PQ if hb == 0 else PST
            S.op("pe", lambda e: e.matmul(pd[:, jj * 129:(jj + 1) * 129], lhsT=ktm[b][:, hb * 128:(hb + 1) * 128],
                                          rhs=vx[b][:, h, 0:129], start=True, stop=True),
                 reads=[ktm[b], vx[b]], writes=[pd])
            S.op("dve", lambda e: e.tensor_tensor(out=Cn[h][R, 0:129], in0=pd[R, jj * 129:(jj + 1) * 129], in1=Cn[h][R, 0:129],
                                                  op=ALU.add), reads=[pd, Cn[h]], writes=[Cn[h]])
            S.op("act", lambda e: e.activation(out=Cn[h][R, 0:129], in_=Cn[h][R, 0:129], func=AF.Identity,
                                               scale=ebl[b][R, h:h + 1]), reads=[Cn[h], ebl[b]], writes=[Cn[h]])
            S.op("pool", lambda e: e.tensor_copy(out=Cnb[h][R, 0:129], in_=Cn[h][R, 0:129]), reads=[Cn[h]], writes=[Cnb[h]])
        if STAGE < 6:
            return
        for h in range(4):
            hb, jj = h // 2, h % 2
            pn = PN[hb]
            c0 = jj * 129
            hs = slice(h, h + 1)
            S.op("act", lambda e: e.activation(out=dd[b][:, hs], in_=pn[:, c0 + 128:c0 + 129], func=AF.Abs,
                                               scale=eq[b][:, hs]), reads=[pn, eq[b]], writes=[dd[b]])
            S.op("dve", lambda e: e.tensor_scalar(out=dd[b][:, hs], in0=dd[b][:, hs], scalar1=1.0, scalar2=None,
                                                  op0=ALU.max), reads=[dd[b]], writes=[dd[b]])
            S.op("dve", lambda e: e.reciprocal(out=rr[b][:, hs], in_=dd[b][:, hs]), reads=[dd[b]], writes=[rr[b]])
            S.op("dve", lambda e: e.tensor_tensor(out=fac[b][:, hs], in0=rr[b][:, hs], in1=eq[b][:, hs], op=ALU.mult),
                 reads=[rr[b], eq[b]], writes=[fac[b]])
            S.op("act", lambda e: e.activation(out=sqj[b][:], in_=pn[:, c0:c0 + 128], func=AF.Square,
                                               scale=fac[b][:, hs], accum_out=ss[b][:, hs]),
                 reads=[pn, fac[b]], writes=[sqj[b], ss[b]])
            S.op("act", lambda e: e.activation(out=rms[b][:, hs], in_=ss[b][:, hs], func=AF.Sqrt, bias=epsb[:],
                                               scale=1.0 / 128.0), reads=[ss[b], epsb], writes=[rms[b]])
            S.op("dve", lambda e: e.reciprocal(out=rinv[b][:, hs], in_=rms[b][:, hs]), reads=[rms[b]], writes=[rinv[b]])
            S.op("dve", lambda e: e.tensor_tensor(out=fac2[b][:, hs], in0=fac[b][:, hs], in1=rinv[b][:, hs], op=ALU.mult),
                 reads=[fac[b], rinv[b]], writes=[fac2[b]])
            S.op("dve", lambda e: e.scalar_tensor_tensor(out=hn[b][:, h * 128:(h + 1) * 128], in0=pn[:, c0:c0 + 128],
                                                         scalar=fac2[b][:, hs], in1=gain[:, h * 128:(h + 1) * 128],
                                                         op0=ALU.mult, op1=ALU.mult),
                 reads=[pn, fac2[b], gain], writes=[hn[b]])
        S.op("pool", lambda e: e.tensor_tensor(out=hgo[b][:], in0=hn[b][:], in1=og[b][:], op=ALU.mult),
             reads=[hn[b], og[b]], writes=[hgo[b]])
        S.dma("sp", dr["out"][sl, :], hgo[b][:], reads=[hgo[b]], writes=[out_d])
    run_interleaved([_tile(t_) for t_ in range(NT)], 2)
    S.barrier()


def build_k1(SEQ):
    nc = bass.Bass("TRN2", target_bir_lowering=False)
    dr = {}
    def din(name, shape, dt=F32):
        dr[name] = nc.dram_tensor(name, list(shape), dt, kind="ExternalInput").ap()
    din("x", [SEQ, D]); din("w_fm", [D, 512]); din("w_tm", [D, 1288]); din("b_g", [8]); din("gain", [512])
    dr["out"] = nc.dram_tensor("out", [SEQ, 512], BF16, kind="ExternalOutput").ap()
    with ExitStack() as ctx:
        S = Sched(nc, ctx)
        ps = {"ptB": S.psum("ptB", [128, 1024], BF16), "P": [None] + [S.psum(f"P{i}", [128, 512], F32) for i in range(1, 8)]}
        emit_mlstm(S, nc, SEQ, dr, ps)
        pass
    return nc


def mlstm_inputs(x_b, w_in, b_gates, norm_gain, g):
    H, dk, dv = 8, 64, 128
    hs = slice(g * 4, g * 4 + 4)
    wq = w_in[:, 0:512].reshape(D, H, dk)[:, hs].reshape(D, 256)
    wk = w_in[:, 512:1024].reshape(D, H, dk)[:, hs].reshape(D, 256)
    wv = w_in[:, 1024:2048].reshape(D, H, dv)[:, hs].reshape(D, 512)
    wo = w_in[:, 2048:3072].reshape(D, H, dv)[:, hs].reshape(D, 512)
    wgi = w_in[:, 3072:3080][:, hs]
    wgf = w_in[:, 3080:3088][:, hs]
    w_fm = np.ascontiguousarray(np.concatenate([wq, wk], axis=1))
    w_tm = np.ascontiguousarray(np.concatenate([wk, wgi, wgf, wv, wo], axis=1))
    b_g = np.ascontiguousarray(np.concatenate([b_gates[0:8][hs], b_gates[8:16][hs]]))
    gain = np.ascontiguousarray(norm_gain.reshape(H, dv)[hs].reshape(512))
    return {"x": np.ascontiguousarray(x_b), "w_fm": w_fm, "w_tm": w_tm, "b_g": b_g, "gain": gain}


def emit_mla(S, nc, SEQ, dr, ps):
    NT = SEQ // 128
    ptB, ptT = ps["ptB"], ps["ptT"]
    P = ps["P"]
    identF = S.sbuf("identF", [128, 128], F32)
    identB = S.sbuf("identB", [128, 128], BF16)
    onesM = S.sbuf("onesM", [128, 128], F32)
    negF = S.sbuf("negF", [128, 128], F32)
    NEG = S.sbuf("NEG", [128, 128], BF16)
    hm = S.sbuf("hm", [128, 2], F32)
    epsb = S.sbuf("epsb", [128, 1], F32)
    wqn = S.sbuf("wqn", [128, 3, 512], BF16)
    cqT_all = S.sbuf("cqT_all", [128, 3, SEQ], BF16)
    krT2 = S.sbuf("krT2", [128, SEQ], BF16)
    qrT_all = S.sbuf("qrT_all", [128, 2, SEQ], BF16)
    out_d = Buf(None, "out")
    knT_dd = Buf(None, "knT_d")
    v_dd = Buf(None, "v_d")

    S.op("pool", lambda e: e.memset(identF[:], 0.0), writes=[identF])
    S.op("pool", lambda e: e.affine_select(out=identF[:], in_=identF[:], pattern=[[-1, 128]],
                                           compare_op=ALU.not_equal, fill=1.0, base=0,
                                           channel_multiplier=1), reads=[identF], writes=[identF])
    S.op("dve", lambda e: e.tensor_copy(out=identB[:], in_=identF[:]), reads=[identF], writes=[identB])
    S.op("pool", lambda e: e.memset(onesM[:], 1.0), writes=[onesM])
    S.op("pool", lambda e: e.memset(epsb[:], RMS_EPS), writes=[epsb])
    S.op("pool", lambda e: e.memset(negF[:], -30000.0), writes=[negF])
    S.op("pool", lambda e: e.affine_select(out=negF[:], in_=negF[:], pattern=[[1, 128]],
                                           compare_op=ALU.is_gt, fill=0.0, base=0,
                                           channel_multiplier=-1), reads=[negF], writes=[negF])
    S.op("dve", lambda e: e.tensor_copy(out=NEG[:], in_=negF[:]), reads=[negF], writes=[NEG])
    S.op("pool", lambda e: e.affine_select(out=hm[:, 0:1], in_=onesM[:, 0:1], pattern=[[0, 1]],
                                           compare_op=ALU.is_ge, fill=0.0, base=63,
                                           channel_multiplier=-1), reads=[onesM], writes=[hm])
    S.op("pool", lambda e: e.affine_select(out=hm[:, 1:2], in_=onesM[:, 0:1], pattern=[[0, 1]],
                                           compare_op=ALU.is_ge, fill=0.0, base=-64,
                                           channel_multiplier=1), reads=[onesM], writes=[hm])
    S.dma("pool", wqn[:], dr["wqn"].rearrange("(c p) n -> p c n", p=128), writes=[wqn])

    with ExitStack() as pctx:
        def sb(name, shape, dt):
            return Buf(pctx.enter_context(nc.sbuf_tensor(S.pfx + name, list(shape), dt)), name)
        def sb2(name, shape, dt):
            return [sb(f"{name}{i}", shape, dt) for i in range(2)]
        win = sb("win", [128, 8, 704], BF16)
        wqr = sb("wqr", [128, 3, 256], BF16)
        wkn = sb("wkn", [128, 2, 512], BF16)
        wkv = sb("wkv", [128, 2, 512], BF16)
        qn_t = sb("qn_t", [128, 384], F32)
        kvn_t = sb("kvn_t", [128, 256], F32)
        cosT = sb("cosT", [128, NT, 32], F32)
        sinT = sb("sinT", [128, NT, 32], F32)
        for c in range(0, 8, 4):
            S.dma("pool", win[:, c:c + 4, :], dr["w_in"][c * 128:(c + 4) * 128, :].rearrange("(c p) n -> p c n", p=128),
                  writes=[win])
        S.dma("pool", wqr[:], dr["wqr"].rearrange("(c p) n -> p c n", p=128), writes=[wqr])
        S.dma("pool", wkn[:], dr["wkn"].rearrange("(c p) n -> p c n", p=128), writes=[wkn])
        S.dma("pool", wkv[:], dr["wkv"].rearrange("(c p) n -> p c n", p=128), writes=[wkv])
        S.dma("sp", qn_t[:], dr["qn"].partition_broadcast(128), writes=[qn_t])
        S.dma("sp", kvn_t[:], dr["kvn"].partition_broadcast(128), writes=[kvn_t])

        with ExitStack() as tctx:
            def tb(name, shape, dt):
                return Buf(tctx.enter_context(nc.sbuf_tensor(S.pfx + name, list(shape), dt)), name)
            posi = tb("posi", [128, NT], I32)
            posf = tb("posf", [128, NT], F32)
            iof = tb("iof", [128, 32], F32)
            invf = tb("invf", [128, 32], F32)
            rr = tb("rr", [128, NT, 32], F32)
            ri = tb("ri", [128, NT * 32], I32)
            rf = tb("rf", [128, NT * 32], F32)
            ff = tb("ff", [128, NT * 32], F32)
            mk = tb("mk", [128, NT * 32], F32)
            S.dma("sp", posi[:], dr["pos"], writes=[posi])
            S.op("dve", lambda e: e.tensor_copy(out=posf[:], in_=posi[:]), reads=[posi], writes=[posf])
            S.op("pool", lambda e: e.iota(iof[:], pattern=[[1, 32]], base=0, channel_multiplier=0,
                                          allow_small_or_imprecise_dtypes=True), writes=[iof])
            S.op("act", lambda e: e.activation(out=invf[:], in_=iof[:], func=AF.Exp, scale=-math.log(10000.0) / 32.0),
                 reads=[iof], writes=[invf])
            S.op("dve", lambda e: e.tensor_scalar(out=invf[:], in0=invf[:], scalar1=1.0 / (2.0 * math.pi), scalar2=None,
                                                  op0=ALU.mult), reads=[invf], writes=[invf])
            for t in range(NT):
                S.op("dve", lambda e: e.tensor_scalar(out=rr[:, t, :], in0=invf[:], scalar1=posf[:, t:t + 1], scalar2=None,
                                                      op0=ALU.mult), reads=[invf, posf], writes=[rr])
            rrf = rr[:].rearrange("p t i -> p (t i)")
            for (shift, outT) in ((0.0, sinT), (0.25, cosT)):
                if shift != 0.0:
                    S.op("dve", lambda e: e.tensor_scalar(out=rrf, in0=rrf, scalar1=shift, scalar2=None, op0=ALU.add),
                         reads=[rr], writes=[rr])
                S.op("dve", lambda e: e.tensor_copy(out=ri[:], in_=rrf), reads=[rr], writes=[ri])
                S.op("dve", lambda e: e.tensor_copy(out=rf[:], in_=ri[:]), reads=[ri], writes=[rf])
                S.op("dve", lambda e: e.tensor_tensor(out=ff[:], in0=rrf, in1=rf[:], op=ALU.subtract),
                     reads=[rr, rf], writes=[ff])
                S.op("dve", lambda e: e.tensor_scalar(out=mk[:], in0=ff[:], scalar1=0.5, scalar2=None, op0=ALU.is_gt),
                     reads=[ff], writes=[mk])
                S.op("dve", lambda e: e.tensor_tensor(out=ff[:], in0=ff[:], in1=mk[:], op=ALU.subtract),
                     reads=[ff, mk], writes=[ff])
                S.op("dve", lambda e: e.tensor_scalar(out=mk[:], in0=ff[:], scalar1=-0.5, scalar2=None, op0=ALU.is_lt),
                     reads=[ff], writes=[mk])
                S.op("dve", lambda e: e.tensor_tensor(out=ff[:], in0=ff[:], in1=mk[:], op=ALU.add),
                     reads=[ff, mk], writes=[ff])
                S.op("dve", lambda e: e.tensor_scalar(out=ff[:], in0=ff[:], scalar1=-0.49999, scalar2=0.49999,
                                                      op0=ALU.max, op1=ALU.min), reads=[ff], writes=[ff])
                S.op("act", lambda e: e.activation(out=outT[:].rearrange("p t i -> p (t i)"), in_=ff[:], func=AF.Sin,
                                                   scale=2.0 * math.pi), reads=[ff], writes=[outT])
            S.barrier()

        xt = [sb(f"xt{i}", [128, 1024], F32) for i in range(4)]
        xb = sb2("xb", [128, 1024], BF16)
        xT = sb2("xT", [128, 8, 128], BF16)
        junk = sb2("junk", [128, 384], F32)
        ssq = sb2("ssq", [128, 2], F32)
        rms = sb2("rms", [128, 2], F32)
        rinv = sb2("rinv", [128, 2], F32)
        ckv = sb2("ckv", [128, 256], BF16)
        cq = sb2("cq", [128, 384], BF16)
        kr2 = sb2("kr2", [128, 128], BF16)
        rt = [sb2(f"rt{k}", [128, 32], F32) for k in range(4)]
        qrt = [sb2(f"qrt{k}", [128, 4, 32], F32) for k in range(4)]
        qr = sb2("qr", [128, 4, 64], BF16)
        stg1 = sb2("stg1", [128, 6, 128], BF16)
        stg2 = sb2("stg2", [128, 2, 128], BF16)
        knT_sb = sb2("knT_sb", [128, 4, 128], BF16)
        v_sb = sb2("v_sb", [128, 512], BF16)
        PA1, PA2, PQR, PK, PV = P[1], P[2], P[3], P[4], P[5]

        def xload(t):
            S.dma("sp", xt[t % 4][:], (dr["xmap"](t) if "xmap" in dr else dr["x"][t * 128:(t + 1) * 128, :]), writes=[xt[t % 4]])

        for t_ in range(min(2, NT)):
            xload(t_)

        def _tile(t):
            if SUBA < 1:
                return
            b = t % 2
            sl = slice(t * 128, (t + 1) * 128)
            if t + 2 < NT:
                xload(t + 2)
            S.op("act", lambda e: e.copy(out=xb[b][:], in_=xt[t % 4][:]), reads=[xt[t % 4]], writes=[xb[b]])
            for c in range(8):
                S.op("pe", lambda e: e.transpose(out=ptB[:, c * 128:(c + 1) * 128], in_=xb[b][:, c * 128:(c + 1) * 128],
                                                 identity=identB[:]), reads=[xb[b], identB], writes=[ptB], inc=(c == 7))
            S.op("dve", lambda e: e.tensor_copy(out=xT[b][:], in_=ptB[:].rearrange("p (c n) -> p c n", c=8)),
                 reads=[ptB], writes=[xT[b]])
            for (pp, lo, n) in ((PA1, 0, 320), (PA2, 320, 384)):
                for c in range(8):
                    S.op("pe", lambda e: e.matmul(pp[:, 0:n], lhsT=xT[b][:, c, :], rhs=win[:, c, lo:lo + n],
                                                  start=(c == 0), stop=(c == 7)),
                         reads=[win, xT[b]], writes=[pp], inc=(c == 7))
            S.op("act", lambda e: e.activation(out=junk[b][:, 0:256], in_=PA1[:, 0:256], func=AF.Square,
                                               accum_out=ssq[b][:, 0:1]), reads=[PA1], writes=[junk[b], ssq[b]])
            S.op("act", lambda e: e.activation(out=junk[b][:, 0:384], in_=PA2[:, 0:384], func=AF.Square,
                                               accum_out=ssq[b][:, 1:2]), reads=[PA2], writes=[junk[b], ssq[b]])
            S.op("act", lambda e: e.activation(out=rms[b][:, 0:1], in_=ssq[b][:, 0:1], func=AF.Sqrt, bias=epsb[:],
                                               scale=1.0 / 256.0), reads=[ssq[b], epsb], writes=[rms[b]])
            S.op("act", lambda e: e.activation(out=rms[b][:, 1:2], in_=ssq[b][:, 1:2], func=AF.Sqrt, bias=epsb[:],
                                               scale=1.0 / 384.0), reads=[ssq[b], epsb], writes=[rms[b]])
            S.op("dve", lambda e: e.reciprocal(out=rinv[b][:], in_=rms[b][:]), reads=[rms[b]], writes=[rinv[b]])
            S.op("dve", lambda e: e.scalar_tensor_tensor(out=ckv[b][:], in0=PA1[:, 0:256], scalar=rinv[b][:, 0:1],
                                                         in1=kvn_t[:], op0=ALU.mult, op1=ALU.mult),
                 reads=[PA1, rinv[b], kvn_t], writes=[ckv[b]])
            S.op("dve", lambda e: e.scalar_tensor_tensor(out=cq[b][:], in0=PA2[:, 0:384], scalar=rinv[b][:, 1:2],
                                                         in1=qn_t[:], op0=ALU.mult, op1=ALU.mult),
                 reads=[PA2, rinv[b], qn_t], writes=[cq[b]])
            if SUBA < 2:
                return
            cs, sn = cosT[:, t, :], sinT[:, t, :]
            x1, x2 = PA1[:, 256:288], PA1[:, 288:320]
            S.op("dve", lambda e: e.tensor_tensor(out=rt[0][b][:], in0=x1, in1=cs, op=ALU.mult), reads=[PA1, cosT], writes=[rt[0][b]])
            S.op("dve", lambda e: e.tensor_tensor(out=rt[1][b][:], in0=x2, in1=sn, op=ALU.mult), reads=[PA1, sinT], writes=[rt[1][b]])
            S.op("dve", lambda e: e.tensor_tensor(out=rt[2][b][:], in0=x2, in1=cs, op=ALU.mult), reads=[PA1, cosT], writes=[rt[2][b]])
            S.op("dve", lambda e: e.tensor_tensor(out=rt[3][b][:], in0=x1, in1=sn, op=ALU.mult), reads=[PA1, sinT], writes=[rt[3][b]])
            if SUBB < 2:
                return
            for off in (0, 64):
                S.op("pool", lambda e: e.tensor_tensor(out=kr2[b][:, off:off + 32], in0=rt[0][b][:], in1=rt[1][b][:],
                                                       op=ALU.subtract), reads=[rt[0][b], rt[1][b]], writes=[kr2[b]])
                S.op("pool", lambda e: e.tensor_tensor(out=kr2[b][:, off + 32:off + 64], in0=rt[2][b][:], in1=rt[3][b][:],
                                                       op=ALU.add), reads=[rt[2][b], rt[3][b]], writes=[kr2[b]])
            if SUBB < 3:
                return
            yield
            srcs = [(cq[b], k * 128) for k in range(3)] + [(ckv[b], k * 128) for k in range(2)] + [(kr2[b], 0)]
            for k, (src, o0) in enumerate(srcs):
                S.op("pe", lambda e: e.transpose(out=ptT[:, k * 128:(k + 1) * 128], in_=src[:, o0:o0 + 128], identity=identB[:]),
                     reads=[src, identB], writes=[ptT], inc=(k == 5))
            if SUBB < 4:
                return
            S.op("act", lambda e: e.copy(out=stg1[b][:], in_=ptT[:, 0:768].rearrange("p (c n) -> p c n", c=6)),
                 reads=[ptT], writes=[stg1[b]])
            for c in range(3):
                S.op("dve", lambda e: e.tensor_copy(out=cqT_all[:, c, sl], in_=stg1[b][:, c, :]),
                     reads=[stg1[b]], writes=[cqT_all])
            S.op("dve", lambda e: e.tensor_copy(out=krT2[:, sl], in_=stg1[b][:, 5, :]), reads=[stg1[b]], writes=[krT2])
            if SUBA < 3:
                return
            yield
            for c in range(3):
                S.op("pe", lambda e: e.matmul(PQR[:, 0:256], lhsT=stg1[b][:, c, :], rhs=wqr[:, c, :],
                                              start=(c == 0), stop=(c == 2)),
                     reads=[stg1[b], wqr], writes=[PQR], inc=(c == 2))
            for h in range(4):
                q1, q2 = PQR[:, h * 64:h * 64 + 32], PQR[:, h * 64 + 32:h * 64 + 64]
                S.op("dve", lambda e: e.tensor_tensor(out=qrt[0][b][:, h, :], in0=q1, in1=cs, op=ALU.mult), reads=[PQR, cosT], writes=[qrt[0][b]])
                S.op("dve", lambda e: e.tensor_tensor(out=qrt[1][b][:, h, :], in0=q2, in1=sn, op=ALU.mult), reads=[PQR, sinT], writes=[qrt[1][b]])
                S.op("dve", lambda e: e.tensor_tensor(out=qrt[2][b][:, h, :], in0=q2, in1=cs, op=ALU.mult), reads=[PQR, cosT], writes=[qrt[2][b]])
                S.op("dve", lambda e: e.tensor_tensor(out=qrt[3][b][:, h, :], in0=q1, in1=sn, op=ALU.mult), reads=[PQR, sinT], writes=[qrt[3][b]])
            S.op("pool", lambda e: e.tensor_tensor(out=qr[b][:, :, 0:32], in0=qrt[0][b][:], in1=qrt[1][b][:], op=ALU.subtract),
                 reads=[qrt[0][b], qrt[1][b]], writes=[qr[b]])
            S.op("pool", lambda e: e.tensor_tensor(out=qr[b][:, :, 32:64], in0=qrt[2][b][:], in1=qrt[3][b][:], op=ALU.add),
                 reads=[qrt[2][b], qrt[3][b]], writes=[qr[b]])
            qrf = qr[b][:].rearrange("p h d -> p (h d)")
            for k in range(2):
                S.op("pe", lambda e: e.transpose(out=ptT[:, 768 + k * 128:768 + (k + 1) * 128], in_=qrf[:, k * 128:(k + 1) * 128],
                                                 identity=identB[:]), reads=[qr[b], identB], writes=[ptT], inc=(k == 1))
            S.op("act", lambda e: e.copy(out=stg2[b][:], in_=ptT[:, 768:1024].rearrange("p (c n) -> p c n", c=2)),
                 reads=[ptT], writes=[stg2[b]])
            for c in range(2):
                S.op("dve", lambda e: e.tensor_copy(out=qrT_all[:, c, sl], in_=stg2[b][:, c, :]),
                     reads=[stg2[b]], writes=[qrT_all])
            if SUBA < 4:
                return
            yield
            for h in range(4):
                for c in range(2):
                    S.op("pe", lambda e: e.matmul(PK[:, h * 128:(h + 1) * 128], lhsT=wkn[:, c, h * 128:(h + 1) * 128],
                                                  rhs=stg1[b][:, 3 + c, :], start=(c == 0), stop=(c == 1)),
                         reads=[wkn, stg1[b]], writes=[PK], inc=(c == 1))
            S.op("dve", lambda e: e.tensor_copy(out=knT_sb[b][:], in_=PK[:].rearrange("p (h n) -> p h n", h=4)),
                 reads=[PK], writes=[knT_sb[b]])
            S.dma("sp", dr["knT_d"][:, :, sl].rearrange("h p n -> p h n"), knT_sb[b][:], reads=[knT_sb[b]], writes=[knT_dd])
            yield
            for c in range(2):
                S.op("pe", lambda e: e.matmul(PV[:], lhsT=stg1[b][:, 3 + c, :], rhs=wkv[:, c, :], start=(c == 0), stop=(c == 1)),
                     reads=[wkv, stg1[b]], writes=[PV], inc=(c == 1))
            S.op("dve", lambda e: e.tensor_copy(out=v_sb[b][:], in_=PV[:]), reads=[PV], writes=[v_sb[b]])
            S.dma("sp", dr["v_d"][sl, :], v_sb[b][:], reads=[v_sb[b]], writes=[v_dd])
        run_interleaved([_tile(t_) for t_ in range(NT)], 2)
        S.barrier()

    if STAGE < 2:
        return
    with ExitStack() as pctx:
        def sb(name, shape, dt):
            return Buf(pctx.enter_context(nc.sbuf_tensor(S.pfx + name, list(shape), dt)), name)
        def sb2(name, shape, dt):
            return [sb(f"{name}{i}", shape, dt) for i in range(2)]
        knT = sb("knT", [128, SEQ], BF16)
        vh = sb("vh", [128, NT, 128], BF16)
        sc = sb("sc", [128, SEQ], F32)
        Pb = sb("Pb", [128, SEQ], BF16)
        PT = sb("PT", [128, NT, 128], BF16)
        qnT = sb2("qnT", [128, 128], BF16)
        qrz = sb2("qrz", [128, 128], BF16)
        mx = sb2("mx", [128, 16], F32)
        mrow = sb2("mrow", [128, 1], F32)
        negm = sb2("negm", [128, 1], F32)
        rsum = sb2("rsum", [128, 1], F32)
        rinv2 = sb2("rinv2", [128, 1], F32)
        osb = sb2("osb", [128, 128], BF16)
        PQN = P[1]
        PS = [P[2], P[3]]
        PO = [P[4], P[5]]
        ptP = [ptB, ptT]
        it = 0
        for h in range(4):
            hb, jj = h // 2, h % 2
            S.dma("sp", knT[:], dr["knT_d"][h, :, :], writes=[knT])
            for t0 in range(0, NT, 8):
                t1 = min(NT, t0 + 8)
                S.dma("sp", vh[:, t0:t1, :],
                      dr["v_d"][t0 * 128:t1 * 128, h * 128:(h + 1) * 128].rearrange("(t p) d -> p t d", p=128),
                      writes=[vh])
            for qb in range(NT):
                b = it % 2
                it += 1
                qs = slice(qb * 128, (qb + 1) * 128)
                nkeys = (qb + 1) * 128
                for c in range(3):
                    S.op("pe", lambda e: e.matmul(PQN[:, 0:128], lhsT=wqn[:, c, h * 128:(h + 1) * 128], rhs=cqT_all[:, c, qs],
                                                  start=(c == 0), stop=(c == 2)),
                         reads=[wqn, cqT_all], writes=[PQN], inc=(c == 2))
                S.op("dve", lambda e: e.tensor_copy(out=qnT[b][:], in_=PQN[:, 0:128]), reads=[PQN], writes=[qnT[b]])
                S.op("pool", lambda e: e.tensor_scalar(out=qrz[b][:], in0=qrT_all[:, hb, qs], scalar1=hm[:, jj:jj + 1],
                                                       scalar2=None, op0=ALU.mult), reads=[qrT_all, hm], writes=[qrz[b]])
                nch = (nkeys + 511) // 512
                for kc in range(nch):
                    k0 = kc * 512
                    n = min(512, nkeys - k0)
                    pp = PS[kc % 2]
                    last = (kc == nch - 1)
                    S.op("pe", lambda e: e.matmul(pp[:, 0:n], lhsT=qnT[b][:], rhs=knT[:, k0:k0 + n], start=True, stop=False),
                         reads=[qnT[b], knT], writes=[pp], inc=False)
                    S.op("pe", lambda e: e.matmul(pp[:, 0:n], lhsT=qrz[b][:], rhs=krT2[:, k0:k0 + n], start=False, stop=(not last)),
                         reads=[qrz[b], krT2], writes=[pp], inc=(not last))
                    if last:
                        S.op("pe", lambda e: e.matmul(pp[:, n - 128:n], lhsT=identB[:], rhs=NEG[:], start=False, stop=True),
                             reads=[identB, NEG], writes=[pp])
                    S.op("dve", lambda e: e.tensor_scalar(out=sc[:, k0:k0 + n], in0=pp[:, 0:n], scalar1=1.0, scalar2=None,
                                                          op0=ALU.mult, op1=ALU.max, accum_out=mx[b][:, kc:kc + 1]),
                         reads=[pp], writes=[sc, mx[b]])
                S.op("dve", lambda e: e.tensor_reduce(out=mrow[b][:], in_=mx[b][:, 0:nch], axis=AX.X, op=ALU.max),
                     reads=[mx[b]], writes=[mrow[b]])
                S.op("dve", lambda e: e.tensor_scalar(out=negm[b][:], in0=mrow[b][:], scalar1=-SCALE, scalar2=None, op0=ALU.mult),
                     reads=[mrow[b]], writes=[negm[b]])
                S.op("act", lambda e: e.activation(out=Pb[:, 0:nkeys], in_=sc[:, 0:nkeys], func=AF.Exp, scale=SCALE,
                                                   bias=negm[b][:], accum_out=rsum[b][:]),
                     reads=[sc, negm[b]], writes=[Pb, rsum[b]])
                nkb = qb + 1
                for g0 in range(0, nkb, 8):
                    g1 = min(nkb, g0 + 8)
                    pt = ptP[(g0 // 8) % 2]
                    for kb in range(g0, g1):
                        S.op("pe", lambda e: e.transpose(out=pt[:, (kb - g0) * 128:(kb - g0 + 1) * 128],
                                                         in_=Pb[:, kb * 128:(kb + 1) * 128], identity=identB[:]),
                             reads=[Pb, identB], writes=[pt], inc=(kb == g1 - 1))
                    S.op("act", lambda e: e.copy(out=PT[:, g0:g1, :],
                                                 in_=pt[:, 0:(g1 - g0) * 128].rearrange("p (c n) -> p c n", n=128)),
                         reads=[pt], writes=[PT])
                po = PO[b]
                for kb in range(nkb):
                    S.op("pe", lambda e: e.matmul(po[:, 0:128], lhsT=PT[:, kb, :], rhs=vh[:, kb, :],
                                                  start=(kb == 0), stop=(kb == nkb - 1)),
                         reads=[PT, vh], writes=[po], inc=(kb == nkb - 1))
                S.op("dve", lambda e: e.reciprocal(out=rinv2[b][:], in_=rsum[b][:]), reads=[rsum[b]], writes=[rinv2[b]])
                S.op("dve", lambda e: e.tensor_scalar(out=osb[b][:], in0=po[:, 0:128], scalar1=rinv2[b][:], scalar2=None,
                                                      op0=ALU.mult), reads=[po, rinv2[b]], writes=[osb[b]])
                S.dma("sp", dr["out"][qs, h * 128:(h + 1) * 128], osb[b][:], reads=[osb[b]], writes=[out_d])
        S.barrier()


def build_k2(SEQ):
    nc = bass.Bass("TRN2", target_bir_lowering=False)
    dr = {}
    def din(name, shape, dt=F32):
        dr[name] = nc.dram_tensor("i_" + name, list(shape), dt, kind="ExternalInput").ap()
    NT = SEQ // 128
    din("x", [SEQ, D]); din("pos", [128, NT], I32); din("w_in", [D, 704]); din("qn", [384]); din("kvn", [256])
    din("wqn", [384, 512]); din("wqr", [384, 256]); din("wkn", [256, 512]); din("wkv", [256, 512])
    dr["out"] = nc.dram_tensor("out", [SEQ, 512], BF16, kind="ExternalOutput").ap()
    dr["knT_d"] = nc.dram_tensor("knT_d", [4, 128, SEQ], BF16, kind="Internal").ap()
    dr["v_d"] = nc.dram_tensor("v_d", [SEQ, 512], BF16, kind="Internal").ap()
    with ExitStack() as ctx:
        S = Sched(nc, ctx)
        ps = {"ptB": S.psum("ptB", [128, 1024], BF16), "ptT": S.psum("ptT", [128, 1024], BF16),
              "P": [None] + [S.psum(f"P{i}", [128, 512], F32) for i in range(1, 7)]}
        emit_mla(S, nc, SEQ, dr, ps)
        pass
    return nc


def mla_inputs(x_b, pos_b, w_in, q_norm, kv_norm, w_qb, w_kvb, g):
    SEQ = x_b.shape[0]
    H = 8
    hs = slice(g * 4, g * 4 + 4)
    w_in2 = np.ascontiguousarray(np.concatenate([w_in[:, 384:640], w_in[:, 640:704], w_in[:, 0:384]], axis=1))
    wq = w_qb.reshape(384, H, 192)[:, hs]
    wqn = np.ascontiguousarray(wq[:, :, 0:128].reshape(384, 512))
    wqr = np.ascontiguousarray(wq[:, :, 128:192].reshape(384, 256))
    wkv_ = w_kvb.reshape(256, H, 256)[:, hs]
    wkn = np.ascontiguousarray(wkv_[:, :, 0:128].reshape(256, 512))
    wkv = np.ascontiguousarray(wkv_[:, :, 128:256].reshape(256, 512))
    pos2 = np.ascontiguousarray(pos_b.reshape(SEQ // 128, 128).T.astype(np.int32))
    return {"i_x": np.ascontiguousarray(x_b), "i_pos": pos2, "i_w_in": w_in2, "i_qn": np.ascontiguousarray(q_norm),
            "i_kvn": np.ascontiguousarray(kv_norm), "i_wqn": wqn, "i_wqr": wqr, "i_wkn": wkn, "i_wkv": wkv}


def run_phase(S, pfx, fn):
    with ExitStack() as pctx:
        old = S.ctx
        S.ctx = pctx
        S.pfx = pfx
        fn()
        S.ctx = old
        S.pfx = ""


def build_fused(SEQ, CAP_, NB, depth=4):
    TOK = SEQ // 2
    NT3 = TOK // 128
    nc = bass.Bass("TRN2", target_bir_lowering=False)
    ext = {}

    def din(name, shape, dt=F32):
        ext[name] = nc.dram_tensor(name, list(shape), dt, kind="ExternalInput").ap()

    def dint(name, shape, dt):
        return nc.dram_tensor(name, list(shape), dt, kind="Internal").ap()

    din("x_full", [SEQ, D]); din("x_own", [TOK, D]); din("hgidx", [128, NT3, 2], I32)
    din("pos", [128, SEQ // 128], I32)
    for j in range((depth + 1) // 2):
        din(f"m{j}_w_fm", [D, 512]); din(f"m{j}_w_tm", [D, 1288]); din(f"m{j}_b_g", [8]); din(f"m{j}_gain", [512])
    for j in range(depth // 2):
        din(f"a{j}_w_in", [D, 704]); din(f"a{j}_qn", [384]); din(f"a{j}_kvn", [256])
        din(f"a{j}_wqn", [384, 512]); din(f"a{j}_wqr", [384, 256]); din(f"a{j}_wkn", [256, 512]); din(f"a{j}_wkv", [256, 512])
    for l in range(depth):
        din(f"l{l}_w_out", [D, D])
        for nm in ("g1", "b1", "g2", "b2"):
            din(f"l{l}_{nm}", [D])
        din(f"l{l}_w_rt", [D, E]); din(f"l{l}_b_rt", [E]); din(f"l{l}_w_gu", [E, D, 2 * D]); din(f"l{l}_b_gu", [E, 2 * D])
        din(f"l{l}_w_dn", [E, D, D]); din(f"l{l}_b_dn", [E, D])
    out_ap = nc.dram_tensor("out", [TOK, D], F32, kind="ExternalOutput").ap()
    hg_own = dint("hg_own", [SEQ, 512], BF16)
    hg_gath = dint("hg_gath", [2 * SEQ, 512], BF16)
    xn = [dint(f"xn{i}", [TOK, D], F32) for i in range(2)]
    xfull_g = dint("xfull_g", [SEQ, D], F32)
    knT_d = dint("knT_d", [4, 128, SEQ], BF16)
    v_d = dint("v_d", [SEQ, 512], BF16)
    xg = dint("xg", [E * CAP_, D], BF16)
    yg = dint("yg", [E * CAP_, D], F32)
    x1s = dint("x1s", [TOK, D], F32)
    groups = [[2 * b, 2 * b + 1] for b in range(NB)]
    CHX = min(512, TOK)
    CHH = min(2048, SEQ)

    def xmap(t):
        row = t * 128
        r = row // TOK
        lrow = row - r * TOK
        k, off = lrow // CHX, lrow % CHX
        r0 = k * 2 * CHX + r * CHX + off
        return xfull_g[r0:r0 + 128, :]
    with ExitStack() as ctx:
        S = Sched(nc, ctx)
        ptB = S.psum("ptB", [128, 1024], BF16)
        Pf = [S.psum(f"P{i}", [128, 512], F32) for i in range(1, 7)]
        ptT = S.psum("ptT", [128, 1024], BF16)
        P7 = Buf(ptT.t[:].bitcast(F32), "P7")
        ps13 = {"ptB": ptB, "P": [None] + Pf + [P7]}
        ps2 = {"ptB": ptB, "ptT": ptT, "P": [None] + Pf}
        for l in range(depth):
            j = l // 2
            xsrc = ext["x_full"] if l == 0 else xfull_g
            if l % 2 == 0:
                dr = {"x": xsrc, **({"xmap": xmap} if l > 0 else {}), "w_fm": ext[f"m{j}_w_fm"], "w_tm": ext[f"m{j}_w_tm"], "b_g": ext[f"m{j}_b_g"],
                      "gain": ext[f"m{j}_gain"], "out": hg_own}
                run_phase(S, f"L{l}m_", lambda: emit_mlstm(S, nc, SEQ, dr, ps13))
            else:
                dr = {"x": xsrc, **({"xmap": xmap} if l > 0 else {}), "pos": ext["pos"], "w_in": ext[f"a{j}_w_in"], "qn": ext[f"a{j}_qn"], "kvn": ext[f"a{j}_kvn"],
                      "wqn": ext[f"a{j}_wqn"], "wqr": ext[f"a{j}_wqr"], "wkn": ext[f"a{j}_wkn"], "wkv": ext[f"a{j}_wkv"],
                      "out": hg_own, "knT_d": knT_d, "v_d": v_d}
                run_phase(S, f"L{l}a_", lambda: emit_mla(S, nc, SEQ, dr, ps2))
            for k in range(SEQ // CHH):
                S.cc("AllGather", groups, hg_own[k * CHH:(k + 1) * CHH, :], hg_gath[k * 2 * CHH:(k + 1) * 2 * CHH, :], inc=1)
            S.barrier()
            dr = {"x": ext["x_own"] if l == 0 else xn[(l - 1) % 2], "hg_gath": hg_gath, "hgidx": ext["hgidx"],
                  "out": out_ap if l == depth - 1 else xn[l % 2], "xg": xg, "yg": yg, "x1s": x1s}
            for nm in ("w_out", "g1", "b1", "g2", "b2", "w_rt", "b_rt", "w_gu", "b_gu", "w_dn", "b_dn"):
                dr[nm] = ext[f"l{l}_{nm}"]
            run_phase(S, f"L{l}p_", lambda: emit_post(S, nc, TOK, CAP_, dr, ps13))
            if l < depth - 1:
                for k in range(TOK // CHX):
                    S.cc("AllGather", groups, xn[l % 2][k * CHX:(k + 1) * CHX, :],
                         xfull_g[k * 2 * CHX:(k + 1) * 2 * CHX, :], inc=1)
                S.barrier()
    return nc


def fused_in_maps(x, positions, ln_gain, ln_bias, mlstm_w_in, mlstm_b_gates, mlstm_norm_gain, mlstm_w_out,
                  mla_w_in, mla_q_norm, mla_kv_norm, mla_w_qb, mla_w_kvb, mla_w_out,
                  moe_w_router, moe_b_router, moe_w_gate_up, moe_b_gate_up, moe_w_down, moe_b_down):
    f32 = np.float32
    A = lambda a: np.ascontiguousarray(np.asarray(a, dtype=f32))
    x = A(x)
    positions = np.asarray(positions)
    B, S_, _ = x.shape
    TOK = S_ // 2
    NT3 = TOK // 128
    depth = ln_gain.shape[0]
    shared = {}
    for l in range(depth):
        j = l // 2
        shared[f"l{l}_w_out"] = A(mlstm_w_out[j]) if l % 2 == 0 else A(mla_w_out[j])
        shared[f"l{l}_g1"] = A(ln_gain[l, 0]); shared[f"l{l}_b1"] = A(ln_bias[l, 0])
        shared[f"l{l}_g2"] = A(ln_gain[l, 1]); shared[f"l{l}_b2"] = A(ln_bias[l, 1])
        shared[f"l{l}_w_rt"] = A(moe_w_router[l]); shared[f"l{l}_b_rt"] = A(moe_b_router[l])
        shared[f"l{l}_w_gu"] = A(moe_w_gate_up[l]); shared[f"l{l}_b_gu"] = A(moe_b_gate_up[l])
        shared[f"l{l}_w_dn"] = A(moe_w_down[l]); shared[f"l{l}_b_dn"] = A(moe_b_down[l])
    in_maps = []
    for c in range(2 * B):
        b, g = c // 2, c % 2
        m = dict(shared)
        m["x_full"] = np.ascontiguousarray(x[b])
        m["x_own"] = np.ascontiguousarray(x[b, g * TOK:(g + 1) * TOK])
        p = np.arange(128, dtype=np.int64)[:, None, None]
        i = np.arange(NT3, dtype=np.int64)[None, :, None]
        r = np.arange(2, dtype=np.int64)[None, None, :]
        chh = min(2048, S_)
        tok = g * TOK + i * 128 + p
        m["hgidx"] = np.ascontiguousarray(((tok // chh) * 2 * chh + r * chh + tok % chh).astype(np.int32))
        for j in range((depth + 1) // 2):
            mi = mlstm_inputs(x[b], A(mlstm_w_in[j]), A(mlstm_b_gates[j]), A(mlstm_norm_gain[j]), g)
            for k in ("w_fm", "w_tm", "b_g", "gain"):
                m[f"m{j}_{k}"] = mi[k]
        for j in range(depth // 2):
            ai = mla_inputs(x[b], positions[b], A(mla_w_in[j]), A(mla_q_norm[j]), A(mla_kv_norm[j]),
                            A(mla_w_qb[j]), A(mla_w_kvb[j]), g)
            m["pos"] = ai["i_pos"]
            for k in ("w_in", "qn", "kvn", "wqn", "wqr", "wkn", "wkv"):
                m[f"a{j}_{k}"] = ai["i_" + k]
        in_maps.append(m)
    return in_maps


_NC_CACHE = {}


def kernel(x, positions, ln_gain, ln_bias, mlstm_w_in, mlstm_b_gates, mlstm_norm_gain, mlstm_w_out,
           mla_w_in, mla_q_norm, mla_kv_norm, mla_w_qb, mla_w_kvb, mla_w_out,
           moe_w_router, moe_b_router, moe_w_gate_up, moe_b_gate_up, moe_w_down, moe_b_down):
    x = np.asarray(x)
    B, S_, _ = x.shape
    TOK = S_ // 2
    depth = ln_gain.shape[0]
    cap = CAP if S_ == SEQ_FULL else 128
    key = (S_, B, depth)
    if key not in _NC_CACHE:
        _NC_CACHE[key] = build_fused(S_, cap, B, depth)
    nc = _NC_CACHE[key]
    in_maps = fused_in_maps(x, positions, ln_gain, ln_bias, mlstm_w_in, mlstm_b_gates, mlstm_norm_gain, mlstm_w_out,
                            mla_w_in, mla_q_norm, mla_kv_norm, mla_w_qb, mla_w_kvb, mla_w_out,
                            moe_w_router, moe_b_router, moe_w_gate_up, moe_b_gate_up, moe_w_down, moe_b_down)
    res = run_bass_kernel_spmd(nc, in_maps, core_ids=list(range(2 * B)))
    out = np.empty((B, S_, D), dtype=np.float32)
    for c in range(2 * B):
        b, g = c // 2, c % 2
        out[b, g * TOK:(g + 1) * TOK] = res.results[c]["out"]
    return out
```

```python
import math
from contextlib import ExitStack
import numpy as np
import concourse.bass as bass
import concourse.mybir as mybir
from concourse.bass_utils import run_bass_kernel_spmd

F32 = mybir.dt.float32
BF16 = mybir.dt.bfloat16
I32 = mybir.dt.int32
U32 = mybir.dt.uint32
AF = mybir.ActivationFunctionType
ALU = mybir.AluOpType
AX = mybir.AxisListType


class Buf:
    __slots__ = ("t", "w", "r", "name")

    def __init__(self, t=None, name=""):
        self.t = t
        self.w = {}
        self.r = {}
        self.name = name

    def __getitem__(self, idx):
        return self.t[idx]


class _Eng:
    def __init__(self, name, eng, sem):
        self.name = name
        self.eng = eng
        self.sem = sem
        self.count = 0
        self.waited = {}


class Sched:
    def __init__(self, nc, ctx, n_dma_sems=12, same_engine_sync=True):
        self.nc = nc
        self.ctx = ctx
        self.same_engine_sync = same_engine_sync
        self.E = {}
        for name, eng in (("pe", nc.tensor), ("dve", nc.vector), ("act", nc.scalar),
                          ("pool", nc.gpsimd), ("sp", nc.sync)):
            sem = ctx.enter_context(nc.semaphore("s_" + name))
            self.E[name] = _Eng(name, eng, sem)
        self.dma_sems = {}
        for q in ("sp", "act", "pool"):
            lst = []
            for i in range(n_dma_sems):
                s = ctx.enter_context(nc.semaphore(f"d_{q}{i}"))
                lst.append([s, 0])
            self.dma_sems[q] = [lst, 0]
        self.ninst = 0
        self.pfx = ""

    def sbuf(self, name, shape, dt):
        t = self.ctx.enter_context(self.nc.sbuf_tensor(self.pfx + name, list(shape), dt))
        return Buf(t, name)

    def psum(self, name, shape, dt):
        t = self.ctx.enter_context(self.nc.psum_tensor(name, list(shape), dt))
        return Buf(t, name)

    @staticmethod
    def _key(sem):
        return id(sem)

    def _need(self, reads, writes):
        need = {}
        def add(d):
            for k, (s, v) in d.items():
                if k not in need or need[k][1] < v:
                    need[k] = (s, v)
        for b in reads:
            add(b.w)
        for b in writes:
            add(b.w)
            add(b.r)
        return need

    def _do_waits(self, e, need):
        for k, (s, v) in need.items():
            if s is e.sem and (e.name == "pe" or not self.same_engine_sync):
                continue
            if e.waited.get(k, 0) < v:
                e.eng.wait_ge(s, v)
                e.waited[k] = v

    def _record(self, dep_sem, dep_val, reads, writes):
        k = self._key(dep_sem)
        for b in writes:
            b.w = {k: (dep_sem, dep_val)}
            b.r = {}
        for b in reads:
            if b in writes:
                continue
            if k not in b.r or b.r[k][1] < dep_val:
                b.r[k] = (dep_sem, dep_val)

    def op(self, eng, fn, reads=(), writes=(), inc=True):
        e = self.E[eng]
        self._do_waits(e, self._need(reads, writes))
        inst = fn(e.eng)
        self.ninst += 1
        if inc:
            inst.then_inc(e.sem, 1)
            e.count += 1
            self._record(e.sem, e.count, reads, writes)
        else:
            self._record(e.sem, e.count + 1, reads, writes)
        return inst

    def dma(self, q, out, in_, reads=(), writes=(), indirect=None, **kw):
        e = self.E[q]
        lst, pos = self.dma_sems[q]
        ent = lst[pos]
        self.dma_sems[q][1] = (pos + 1) % len(lst)
        s, cnt = ent
        need = self._need(reads, writes)
        if cnt > 0:
            need[self._key(s)] = (s, cnt)
        for k, (ss, v) in need.items():
            if e.waited.get(k, 0) < v:
                e.eng.wait_ge(ss, v)
                e.waited[k] = v
        if indirect is None:
            inst = e.eng.dma_start(out=out, in_=in_, **kw)
        else:
            inst = e.eng.indirect_dma_start(out=out, in_=in_, **indirect, **kw)
        self.ninst += 1
        inst.then_inc(s, 16)
        ent[1] = cnt + 16
        self._record(s, cnt + 16, reads, writes)
        return inst

    def cc(self, kind, groups, in_ap, out_ap, reads=(), writes=(), inc=16):
        e = self.E["pool"]
        lst, pos = self.dma_sems["pool"]
        ent = lst[pos]
        self.dma_sems["pool"][1] = (pos + 1) % len(lst)
        s, cnt = ent
        need = self._need(reads, writes)
        if cnt > 0:
            need[self._key(s)] = (s, cnt)
        for k, (ss, v) in need.items():
            if e.waited.get(k, 0) < v:
                e.eng.wait_ge(ss, v)
                e.waited[k] = v
        inst = e.eng.collective_compute(kind, ALU.bypass, replica_groups=groups, ins=[in_ap], outs=[out_ap])
        self.ninst += 1
        inst.then_inc(s, inc)
        ent[1] = cnt + inc
        self._record(s, cnt + inc, reads, writes)
        return inst

    def barrier(self):
        targets = []
        for name, o in self.E.items():
            if o.count > 0:
                targets.append((o, o.sem, o.count))
        for q, (lst, _) in self.dma_sems.items():
            for s, cnt in lst:
                if cnt > 0:
                    targets.append((None, s, cnt))
        for name in ("sp", "pool", "act", "dve", "pe"):
            e = self.E[name]
            for (o, s, v) in targets:
                if o is e:
                    continue
                k = self._key(s)
                if e.waited.get(k, 0) < v:
                    e.eng.wait_ge(s, v)
                    e.waited[k] = v

    def finish(self, bufs):
        need = self._need((), bufs)
        for name in ("sp", "pool", "act", "dve", "pe"):
            e = self.E[name]
            for k, (s, v) in need.items():
                if s is e.sem:
                    continue
                if e.waited.get(k, 0) < v:
                    e.eng.wait_ge(s, v)
                    e.waited[k] = v


def run_interleaved(gens, width=2):
    it = iter(gens)
    active = []
    while True:
        while len(active) < width:
            g = next(it, None)
            if g is None:
                break
            active.append(g)
        if not active:
            break
        for g in list(active):
            try:
                next(g)
            except StopIteration:
                active.remove(g)


D = 1024
E = 32
ALPHA = 8.0 ** 0.25
LN_EPS = 1e-5
RMS_EPS = 1e-6
SCALE = 192.0 ** -0.5
STAGE = SUB = SUBA = SUBB = SUBC = 99
SEQ_FULL = 8192
TOK_CORE = 4096
CAP = 640

def emit_ln(S, z, gain, bias, out, tmp):
    st, mv, sd, rstd, xn = tmp
    for h in range(2):
        S.op("dve", lambda e: e.bn_stats(out=st[:, h, :], in_=z[:, h * 512:(h + 1) * 512]),
             reads=[z], writes=[st])
    S.op("dve", lambda e: e.bn_aggr(out=mv[:], in_=st[:].rearrange("p a b -> p (a b)")),
         reads=[st], writes=[mv])
    S.op("act", lambda e: e.activation(out=sd[:], in_=mv[:, 1:2], func=AF.Sqrt, bias=tmp_eps(S)[:], scale=1.0),
         reads=[mv], writes=[sd])
    S.op("dve", lambda e: e.reciprocal(out=rstd[:], in_=sd[:]), reads=[sd], writes=[rstd])
    S.op("dve", lambda e: e.tensor_scalar(out=sd[:], in0=mv[:, 0:1], scalar1=rstd[:], scalar2=-1.0,
                                          op0=ALU.mult, op1=ALU.mult), reads=[mv, rstd], writes=[sd])
    S.op("act", lambda e: e.activation(out=xn[:], in_=z[:], func=AF.Identity, scale=rstd[:], bias=sd[:]),
         reads=[z, rstd, sd], writes=[xn])
    S.op("dve", lambda e: e.tensor_tensor(out=xn[:], in0=xn[:], in1=gain[:], op=ALU.mult),
         reads=[xn, gain], writes=[xn])
    S.op("dve", lambda e: e.tensor_tensor(out=out[:], in0=xn[:], in1=bias[:], op=ALU.add),
         reads=[xn, bias], writes=[out])


_EPS = {}


def tmp_eps(S):
    return _EPS[id(S)]


def emit_post(S, nc, T, CAP, dr, ps):
    NT = T // 128
    RB = CAP // 128
    npart = (CAP + 511) // 512
    psz = (CAP // npart + 1) // 2 * 2
    parts = []
    lo = 0
    while lo < CAP:
        n = min(psz, CAP - lo)
        parts.append((lo, n))
        lo += n
    ctx = S.ctx
    identF = S.sbuf("identF", [128, 128], F32)
    identB = S.sbuf("identB", [128, 128], BF16)
    triS = S.sbuf("triS", [128, 128], F32)
    onesM = S.sbuf("onesM", [128, 128], F32)
    offs = S.sbuf("offs", [128, E], F32)
    epsb = S.sbuf("epsb", [128, 1], F32)
    _EPS[id(S)] = epsb
    idx_all = S.sbuf("idx_all", [128, NT, 4], I32)
    gate_all = S.sbuf("gate_all", [128, NT, 4], F32)
    wgu = [S.sbuf(f"wgu{i}", [128, 8, 2048], BF16) for i in range(2)]
    wd = [S.sbuf(f"wd{i}", [128, 8, 1024], BF16) for i in range(2)]
    bgu = [S.sbuf(f"bgu{i}", [128, 8, 2], F32) for i in range(2)]
    bdn = [S.sbuf(f"bdn{i}", [128, 1024], F32) for i in range(2)]
    lnp = [S.sbuf(f"lnp{i}", [128, 1024], F32) for i in range(4)]
    st = S.sbuf("st", [128, 2, 6], F32)
    mv = S.sbuf("mv", [128, 2], F32)
    sd = S.sbuf("sd", [128, 1], F32)
    rstd = S.sbuf("rstd", [128, 1], F32)
    xn = S.sbuf("xn", [128, 1024], F32)
    lntmp = (st, mv, sd, rstd, xn)
    xg_d = Buf(None, "xg")
    yg_d = Buf(None, "yg")
    x1_d = Buf(None, "x1s")
    out_d = Buf(None, "out")

    S.op("pool", lambda e: e.memset(identF[:], 0.0), writes=[identF])
    S.op("pool", lambda e: e.affine_select(out=identF[:], in_=identF[:], pattern=[[-1, 128]],
                                           compare_op=ALU.not_equal, fill=1.0, base=0,
                                           channel_multiplier=1), reads=[identF], writes=[identF])
    S.op("dve", lambda e: e.tensor_copy(out=identB[:], in_=identF[:]), reads=[identF], writes=[identB])
    S.op("pool", lambda e: e.memset(onesM[:], 1.0), writes=[onesM])
    S.op("pool", lambda e: e.memset(epsb[:], LN_EPS), writes=[epsb])
    S.op("pool", lambda e: e.affine_select(out=triS[:], in_=onesM[:], pattern=[[1, 128]],
                                           compare_op=ALU.is_gt, fill=0.0, base=0,
                                           channel_multiplier=-1), reads=[onesM], writes=[triS])
    S.op("pool", lambda e: e.iota(offs[:], pattern=[[CAP, E]], base=0, channel_multiplier=0,
                                  allow_small_or_imprecise_dtypes=True), writes=[offs])
    for j, nm in enumerate(("g1", "b1", "g2", "b2")):
        S.dma("sp", lnp[j][:], dr[nm].partition_broadcast(128), writes=[lnp[j]])

    with ExitStack() as pctx:
        def sb(name, shape, dt):
            return Buf(pctx.enter_context(nc.sbuf_tensor(S.pfx + name, list(shape), dt)), name)
        wout = sb("wout", [128, 8, 1024], BF16)
        wrt = sb("wrt", [128, 8, E], F32)
        brt = sb("brt", [128, E], F32)
        baseoffs = sb("baseoffs", [128, E], F32)
        hgt = [sb(f"hgt{i}", [128, 1024], BF16) for i in range(4)]
        xt = [sb(f"xt{i}", [128, 1024], F32) for i in range(4)]
        hgT = [sb(f"hgT{i}", [128, 8, 128], BF16) for i in range(2)]
        z = [sb(f"z{i}", [128, 1024], F32) for i in range(2)]
        x1 = [sb(f"x1{i}", [128, 1024], F32) for i in range(2)]
        x1b = [sb(f"x1b{i}", [128, 1024], BF16) for i in range(2)]
        x1T = [sb(f"x1T{i}", [128, 8, 128], F32) for i in range(2)]
        lg = [sb(f"lg{i}", [128, E], F32) for i in range(2)]
        top8 = [sb(f"top8{i}", [128, 8], F32) for i in range(2)]
        mask = [sb(f"mask{i}", [128, E], F32) for i in range(2)]
        nmx = [sb(f"nmx{i}", [128, 1], F32) for i in range(2)]
        ex = [sb(f"ex{i}", [128, 4], F32) for i in range(2)]
        sm = [sb(f"sm{i}", [128, 1], F32) for i in range(2)]
        rs = [sb(f"rs{i}", [128, 1], F32) for i in range(2)]
        posf = [sb(f"posf{i}", [128, E], F32) for i in range(2)]
        oh = [sb(f"oh{i}", [128, E], F32) for i in range(2)]
        junk = [sb(f"junk{i}", [128, E], F32) for i in range(2)]
        destf = [sb(f"destf{i}", [128, 4], F32) for i in range(2)]

        if "hg_gath" in dr:
            hgidx = sb("hgidx", [128, NT, 2], I32)
            S.dma("sp", hgidx[:], dr["hgidx"], writes=[hgidx])
        for c in range(0, 8, 2):
            S.dma("pool", wout[:, c:c + 2, :],
                  dr["w_out"][c * 128:(c + 2) * 128, :].rearrange("(c p) n -> p c n", p=128), writes=[wout])
        S.dma("sp", wrt[:], dr["w_rt"].rearrange("(c p) n -> p c n", p=128), writes=[wrt])
        S.dma("sp", brt[:], dr["b_rt"].partition_broadcast(128), writes=[brt])
        S.op("dve", lambda e: e.tensor_copy(out=baseoffs[:], in_=offs[:]), reads=[offs], writes=[baseoffs])

        ptB, P = ps["ptB"], ps["P"]
        def rload(i):
            sl_ = slice(i * 128, (i + 1) * 128)
            if "hg_gath" in dr:
                for r in range(2):
                    S.dma("pool", hgt[i % 4][:, r * 512:(r + 1) * 512], dr["hg_gath"], reads=[hgidx], writes=[hgt[i % 4]],
                          indirect=dict(out_offset=None,
                                        in_offset=bass.IndirectOffsetOnAxis(ap=hgidx[:, i, r:r + 1], axis=0)))
            else:
                S.dma("sp", hgt[i % 4][:], dr["hg"][sl_, :], writes=[hgt[i % 4]])
            S.dma("sp", xt[i % 4][:], dr["x"][sl_, :], writes=[xt[i % 4]])

        for i_ in range(min(2, NT)):
            rload(i_)

        def _tile(i):
            b = i % 2
            sl = slice(i * 128, (i + 1) * 128)
            if i + 2 < NT:
                rload(i + 2)
            b4 = i % 4
            for c in range(8):
                S.op("pe", lambda e: e.transpose(out=ptB[:, c * 128:(c + 1) * 128],
                                                 in_=hgt[b4][:, c * 128:(c + 1) * 128], identity=identB[:]),
                     reads=[hgt[b4], identB], writes=[ptB], inc=(c == 7))
            S.op("act", lambda e: e.copy(out=hgT[b][:], in_=ptB[:].rearrange("p (c n) -> p c n", c=8)),
                 reads=[ptB], writes=[hgT[b]])
            for h in range(2):
                pm = P[1 + h]
                for c in range(8):
                    S.op("pe", lambda e: e.matmul(pm[:], lhsT=hgT[b][:, c, :], rhs=wout[:, c, h * 512:(h + 1) * 512],
                                                  start=(c == 0), stop=(c == 7)),
                         reads=[hgT[b], wout], writes=[pm], inc=(c == 7))
                S.op("dve", lambda e: e.scalar_tensor_tensor(out=z[b][:, h * 512:(h + 1) * 512],
                                                             in0=xt[b4][:, h * 512:(h + 1) * 512], scalar=ALPHA,
                                                             in1=pm[:], op0=ALU.mult, op1=ALU.add),
                     reads=[xt[b4], pm], writes=[z[b]])
            yield
            emit_ln(S, z[b], lnp[0], lnp[1], x1[b], lntmp)
            yield
            S.dma("sp", dr["x1s"][sl, :], x1[b][:], reads=[x1[b]], writes=[x1_d])
            S.op("act", lambda e: e.copy(out=x1b[b][:], in_=x1[b][:]), reads=[x1[b]], writes=[x1b[b]])
            for h in range(2):
                pT = P[3 + h]
                for c in range(4):
                    cc = h * 4 + c
                    S.op("pe", lambda e: e.transpose(out=pT[:, c * 128:(c + 1) * 128],
                                                     in_=x1[b][:, cc * 128:(cc + 1) * 128], identity=identF[:]),
                         reads=[x1[b], identF], writes=[pT], inc=(c == 3))
                S.op("act", lambda e: e.copy(out=x1T[b][:, h * 4:(h + 1) * 4, :],
                                             in_=pT[:].rearrange("p (c n) -> p c n", c=4)),
                     reads=[pT], writes=[x1T[b]])
            yield
            pq = P[5]
            for c in range(8):
                S.op("pe", lambda e: e.matmul(pq[:, 0:E], lhsT=x1T[b][:, c, :], rhs=wrt[:, c, :],
                                              start=(c == 0), stop=(c == 7)),
                     reads=[x1T[b], wrt], writes=[pq], inc=(c == 7))
            S.op("dve", lambda e: e.tensor_tensor(out=lg[b][:], in0=pq[:, 0:E], in1=brt[:], op=ALU.add),
                 reads=[pq, brt], writes=[lg[b]])
            S.op("dve", lambda e: e.max(out=top8[b][:], in_=lg[b][:]), reads=[lg[b]], writes=[top8[b]])
            S.op("dve", lambda e: e.tensor_scalar(out=mask[b][:], in0=lg[b][:], scalar1=top8[b][:, 3:4], scalar2=None,
                                                  op0=ALU.is_ge), reads=[lg[b], top8[b]], writes=[mask[b]])
            yield
            S.op("act", lambda e: e.mul(out=nmx[b][:], in_=top8[b][:, 0:1], mul=-1.0), reads=[top8[b]], writes=[nmx[b]])
            S.op("act", lambda e: e.activation(out=ex[b][:], in_=top8[b][:, 0:4], func=AF.Exp, bias=nmx[b][:],
                                               scale=1.0, accum_out=sm[b][:]),
                 reads=[top8[b], nmx[b]], writes=[ex[b], sm[b]])
            S.op("dve", lambda e: e.reciprocal(out=rs[b][:], in_=sm[b][:]), reads=[sm[b]], writes=[rs[b]])
            S.op("dve", lambda e: e.tensor_scalar(out=gate_all[:, i, :], in0=ex[b][:], scalar1=rs[b][:], scalar2=None,
                                                  op0=ALU.mult), reads=[ex[b], rs[b]], writes=[gate_all])
            yield
            S.op("pe", lambda e: e.matmul(pq[:, 32:32 + E], lhsT=triS[:], rhs=mask[b][:], start=True, stop=True),
                 reads=[triS, mask[b]], writes=[pq], inc=False)
            S.op("pe", lambda e: e.matmul(pq[:, 64:64 + E], lhsT=onesM[:], rhs=mask[b][:], start=True, stop=True),
                 reads=[onesM, mask[b]], writes=[pq])
            S.op("dve", lambda e: e.tensor_tensor(out=posf[b][:], in0=pq[:, 32:32 + E], in1=baseoffs[:], op=ALU.add),
                 reads=[pq, baseoffs], writes=[posf[b]])
            S.op("dve", lambda e: e.tensor_tensor(out=baseoffs[:], in0=pq[:, 64:64 + E], in1=baseoffs[:], op=ALU.add),
                 reads=[pq, baseoffs], writes=[baseoffs])
            for k in range(4):
                S.op("dve", lambda e: e.scalar_tensor_tensor(out=junk[b][:], in0=lg[b][:], scalar=top8[b][:, k:k + 1],
                                                             in1=posf[b][:], op0=ALU.is_equal, op1=ALU.mult,
                                                             accum_out=destf[b][:, k:k + 1]),
                     reads=[lg[b], top8[b], posf[b]], writes=[junk[b], destf[b]])
            yield
            S.op("dve", lambda e: e.tensor_copy(out=idx_all[:, i, :], in_=destf[b][:]), reads=[destf[b]], writes=[idx_all])
            for k in range(4):
                S.dma("pool", dr["xg"], x1b[b][:], reads=[x1b[b], idx_all], writes=[],
                      indirect=dict(out_offset=bass.IndirectOffsetOnAxis(ap=idx_all[:, i, k:k + 1], axis=0),
                                    in_offset=None))
        run_interleaved([_tile(i_) for i_ in range(NT)], 2)
        S.barrier()

    with ExitStack() as pctx:
        def sb(name, shape, dt):
            return Buf(pctx.enter_context(nc.sbuf_tensor(S.pfx + name, list(shape), dt)), name)
        xgr = [sb(f"xgr{i}", [128, 1024], BF16) for i in range(2)]
        xgT = sb("xgT", [128, 8, CAP], BF16)
        hT = sb("hT", [128, 8, CAP], BF16)
        tg = [sb(f"tg{i}", [128, 512], F32) for i in range(2)]
        tsg = [sb(f"tsg{i}", [128, 512], F32) for i in range(2)]
        tu = [sb(f"tu{i}", [128, 512], F32) for i in range(2)]
        yo = [sb(f"yo{i}", [128, 1024], F32) for i in range(2)]
        ptB, P = ps["ptB"], ps["P"]
        stg = [sb(f"stg{i}", [128, 2048], F32) for i in range(3)]
        wguc = [[Buf(wgu[i].t, f"wguc{i}_{c}") for c in range(8)] for i in range(2)]
        wdc = [[Buf(wd[i].t, f"wdc{i}_{c}") for c in range(8)] for i in range(2)]
        cast_gu = ["act", "act", "dve", "act", "act", "dve", "act", "act"]
        cast_dn = ["act", "dve", "act", "dve"]
        stk = [0]

        def cast(eng, out, in_, reads, writes):
            if eng == "act":
                S.op("act", lambda e: e.copy(out=out, in_=in_), reads=reads, writes=writes)
            else:
                S.op(eng, lambda e: e.tensor_copy(out=out, in_=in_), reads=reads, writes=writes)

        def load_expert(e_):
            b = e_ % 2
            for c in range(8):
                st = stg[stk[0] % 3]
                stk[0] += 1
                S.dma("sp", st[:], dr["w_gu"][e_, c * 128:(c + 1) * 128, :], writes=[st])
                cast(cast_gu[c], wgu[b][:, c, :], st[:], [st], [wguc[b][c]])
                yield
            for k2, c in enumerate(range(0, 8, 2)):
                st = stg[stk[0] % 3]
                stk[0] += 1
                S.dma("sp", st[:].rearrange("p (c n) -> p c n", c=2),
                      dr["w_dn"][e_, c * 128:(c + 2) * 128, :].rearrange("(c p) n -> p c n", p=128), writes=[st])
                cast(cast_dn[k2], wd[b][:, c:c + 2, :], st[:].rearrange("p (c n) -> p c n", c=2), [st],
                     [wdc[b][c], wdc[b][c + 1]])
                yield
            with nc.allow_non_contiguous_dma(reason="tiny bias"):
                S.dma("sp", bgu[b][:], dr["b_gu"][e_, :].rearrange("(c p t) -> p c t", p=128, t=2), writes=[bgu[b]])
            S.dma("sp", bdn[b][:], dr["b_dn"][e_, :].partition_broadcast(128), writes=[bdn[b]])

        for _ in load_expert(0):
            pass
        cnt = 0
        for e_ in range(E):
            wb = e_ % 2
            for rb in range(RB):
                b = rb % 2
                r0 = e_ * CAP + rb * 128
                S.dma("sp", xgr[b][:], dr["xg"][r0:r0 + 128, :], writes=[xgr[b]])
                for c in range(8):
                    S.op("pe", lambda e: e.transpose(out=ptB[:, c * 128:(c + 1) * 128],
                                                     in_=xgr[b][:, c * 128:(c + 1) * 128], identity=identB[:]),
                         reads=[xgr[b], identB], writes=[ptB], inc=(c == 7))
                S.op("act", lambda e: e.copy(out=xgT[:, :, rb * 128:(rb + 1) * 128],
                                             in_=ptB[:].rearrange("p (c n) -> p c n", c=8)),
                     reads=[ptB], writes=[xgT])
            pre = load_expert(e_ + 1) if e_ + 1 < E else iter(())
            for fc in range(8):
                for (lo, n) in parts:
                    b = cnt % 2
                    cnt += 1
                    next(pre, None)
                    pg, pu = P[1 + b], P[3 + b]
                    for gi, pp in ((0, pg), (1, pu)):
                        for c in range(8):
                            S.op("pe", lambda e: e.matmul(pp[:, 0:n],
                                                          lhsT=wgu[wb][:, c, fc * 256 + gi:fc * 256 + 256:2],
                                                          rhs=xgT[:, c, lo:lo + n], start=(c == 0), stop=(c == 7)),
                                 reads=[wguc[wb][c], xgT], writes=[pp], inc=(c == 7))
                    S.op("dve", lambda e: e.tensor_scalar(out=tg[b][:, 0:n], in0=pg[:, 0:n], scalar1=bgu[wb][:, fc, 0:1],
                                                          scalar2=7.0, op0=ALU.add, op1=ALU.min),
                         reads=[pg, bgu[wb]], writes=[tg[b]])
                    S.op("act", lambda e: e.activation(out=tsg[b][:, 0:n], in_=tg[b][:, 0:n], func=AF.Gelu_apprx_sigmoid),
                         reads=[tg[b]], writes=[tsg[b]])
                    S.op("dve", lambda e: e.tensor_scalar(out=tu[b][:, 0:n], in0=pu[:, 0:n], scalar1=bgu[wb][:, fc, 1:2],
                                                          scalar2=-7.0, op0=ALU.add, op1=ALU.max),
                         reads=[pu, bgu[wb]], writes=[tu[b]])
                    S.op("dve", lambda e: e.scalar_tensor_tensor(out=tu[b][:, 0:n], in0=tu[b][:, 0:n], scalar=7.0,
                                                                 in1=tsg[b][:, 0:n], op0=ALU.min, op1=ALU.mult),
                         reads=[tu[b], tsg[b]], writes=[tu[b]])
                    S.op("dve", lambda e: e.tensor_tensor(out=hT[:, fc, lo:lo + n], in0=tu[b][:, 0:n], in1=tsg[b][:, 0:n],
                                                          op=ALU.add), reads=[tu[b], tsg[b]], writes=[hT])
            for _ in pre:
                pass
            for rb in range(RB):
                b = rb % 2
                for h in range(2):
                    py = P[5 + h]
                    for fc in range(8):
                        S.op("pe", lambda e: e.matmul(py[:], lhsT=hT[:, fc, rb * 128:(rb + 1) * 128],
                                                      rhs=wd[wb][:, fc, h * 512:(h + 1) * 512],
                                                      start=(fc == 0), stop=(fc == 7)),
                             reads=[hT, wdc[wb][fc]], writes=[py], inc=(fc == 7))
                    S.op("dve", lambda e: e.tensor_tensor(out=yo[b][:, h * 512:(h + 1) * 512], in0=py[:],
                                                          in1=bdn[wb][:, h * 512:(h + 1) * 512], op=ALU.add),
                         reads=[py, bdn[wb]], writes=[yo[b]])
                r0 = e_ * CAP + rb * 128
                S.dma("sp", dr["yg"][r0:r0 + 128, :], yo[b][:], reads=[yo[b]], writes=[yg_d])
        S.barrier()

    with ExitStack() as pctx:
        def sb(name, shape, dt):
            return Buf(pctx.enter_context(nc.sbuf_tensor(S.pfx + name, list(shape), dt)), name)
        xc = [sb(f"xc{i}", [128, 1024], F32) for i in range(2)]
        yk = [[sb(f"yk{i}_{k}", [128, 1024], F32) for k in range(4)] for i in range(2)]
        acc = [sb(f"acc{i}", [128, 1024], F32) for i in range(2)]
        x2 = [sb(f"x2{i}", [128, 1024], F32) for i in range(2)]
        x2b = [sb(f"x2b{i}", [128, 1024], BF16) for i in range(2)]
        def _tile(i):
            b = i % 2
            sl = slice(i * 128, (i + 1) * 128)
            S.dma("sp", xc[b][:], dr["x1s"][sl, :], writes=[xc[b]])
            for k in range(4):
                S.dma("pool", yk[b][k][:], dr["yg"], reads=[idx_all], writes=[yk[b][k]],
                      indirect=dict(out_offset=None,
                                    in_offset=bass.IndirectOffsetOnAxis(ap=idx_all[:, i, k:k + 1], axis=0)))
            yield
            S.op("dve", lambda e: e.tensor_scalar(out=acc[b][:], in0=yk[b][0][:], scalar1=gate_all[:, i, 0:1],
                                                  scalar2=None, op0=ALU.mult),
                 reads=[yk[b][0], gate_all], writes=[acc[b]])
            for k in range(1, 4):
                S.op("dve", lambda e: e.scalar_tensor_tensor(out=acc[b][:], in0=yk[b][k][:], scalar=gate_all[:, i, k:k + 1],
                                                             in1=acc[b][:], op0=ALU.mult, op1=ALU.add),
                     reads=[yk[b][k], gate_all, acc[b]], writes=[acc[b]])
            S.op("dve", lambda e: e.scalar_tensor_tensor(out=acc[b][:], in0=xc[b][:], scalar=ALPHA, in1=acc[b][:],
                                                         op0=ALU.mult, op1=ALU.add),
                 reads=[xc[b], acc[b]], writes=[acc[b]])
            yield
            emit_ln(S, acc[b], lnp[2], lnp[3], x2[b], lntmp)
            S.dma("sp", dr["out"][sl, :], x2[b][:], reads=[x2[b]], writes=[out_d])
            if "outb" in dr:
                S.op("act", lambda e: e.copy(out=x2b[b][:], in_=x2[b][:]), reads=[x2[b]], writes=[x2b[b]])
                S.dma("sp", dr["outb"][sl, :], x2b[b][:], reads=[x2b[b]], writes=[out_d])
        run_interleaved([_tile(i_) for i_ in range(NT)], 2)
        S.barrier()


def build_k3(T, CAP):
    nc = bass.Bass("TRN2", target_bir_lowering=False)
    dr = {}
    def din(name, shape, dt=F32):
        dr[name] = nc.dram_tensor(name, list(shape), dt, kind="ExternalInput").ap()
    din("x", [T, D]); din("hg", [T, D], BF16)
    din("w_out", [D, D]); din("g1", [D]); din("b1", [D]); din("g2", [D]); din("b2", [D])
    din("w_rt", [D, E]); din("b_rt", [E]); din("w_gu", [E, D, 2 * D]); din("b_gu", [E, 2 * D])
    din("w_dn", [E, D, D]); din("b_dn", [E, D])
    dr["out"] = nc.dram_tensor("out", [T, D], F32, kind="ExternalOutput").ap()
    dr["xg"] = nc.dram_tensor("xg", [E * CAP, D], BF16, kind="Internal").ap()
    dr["yg"] = nc.dram_tensor("yg", [E * CAP, D], F32, kind="Internal").ap()
    dr["x1s"] = nc.dram_tensor("x1s", [T, D], F32, kind="Internal").ap()
    with ExitStack() as ctx:
        S = Sched(nc, ctx)
        ps = {"ptB": S.psum("ptB", [128, 1024], BF16), "P": [None] + [S.psum(f"P{i}", [128, 512], F32) for i in range(1, 8)]}
        emit_post(S, nc, T, CAP, dr, ps)
        pass
    return nc


def emit_mlstm(S, nc, SEQ, dr, ps):
    NT = SEQ // 128
    ptB, PA, PB, PC, PQ, PST, PN = ps["ptB"], ps["P"][1], ps["P"][2], ps["P"][3], ps["P"][4], ps["P"][5], ps["P"][6:8]
    identF = S.sbuf("identF", [128, 128], F32)
    identB = S.sbuf("identB", [128, 128], BF16)
    triI = S.sbuf("triI", [128, 128], F32)
    onesM = S.sbuf("onesM", [128, 128], F32)
    epsb = S.sbuf("epsb", [128, 1], F32)
    wfm = S.sbuf("wfm", [128, 8, 512], BF16)
    wtm = S.sbuf("wtm", [128, 8, 1288], BF16)
    bg = S.sbuf("bg", [128, 8], F32)
    gain = S.sbuf("gain_sb", [128, 512], F32)
    Cn = [S.sbuf(f"Cn{h}", [128, 132], F32) for h in range(4)]
    Cnb = [S.sbuf(f"Cnb{h}", [128, 132], BF16) for h in range(4)]
    out_d = Buf(None, "out")

    S.op("pool", lambda e: e.memset(identF[:], 0.0), writes=[identF])
    S.op("pool", lambda e: e.affine_select(out=identF[:], in_=identF[:], pattern=[[-1, 128]],
                                           compare_op=ALU.not_equal, fill=1.0, base=0,
                                           channel_multiplier=1), reads=[identF], writes=[identF])
    S.op("dve", lambda e: e.tensor_copy(out=identB[:], in_=identF[:]), reads=[identF], writes=[identB])
    S.op("pool", lambda e: e.memset(onesM[:], 1.0), writes=[onesM])
    S.op("pool", lambda e: e.memset(epsb[:], RMS_EPS), writes=[epsb])
    oneb = S.sbuf("oneb", [128, 1], F32)
    S.op("pool", lambda e: e.memset(oneb[:], 1.0), writes=[oneb])
    S.op("pool", lambda e: e.affine_select(out=triI[:], in_=onesM[:], pattern=[[1, 128]],
                                           compare_op=ALU.is_ge, fill=0.0, base=0,
                                           channel_multiplier=-1), reads=[onesM], writes=[triI])
    hm = S.sbuf("hm", [128, 2], F32)
    S.op("pool", lambda e: e.affine_select(out=hm[:, 0:1], in_=onesM[:, 0:1], pattern=[[0, 1]],
                                           compare_op=ALU.is_ge, fill=0.0, base=63,
                                           channel_multiplier=-1), reads=[onesM], writes=[hm])
    S.op("pool", lambda e: e.affine_select(out=hm[:, 1:2], in_=onesM[:, 0:1], pattern=[[0, 1]],
                                           compare_op=ALU.is_ge, fill=0.0, base=-64,
                                           channel_multiplier=1), reads=[onesM], writes=[hm])
    for h in range(4):
        S.op("pool", lambda e: e.memset(Cn[h][:], 0.0), writes=[Cn[h]])
        S.op("pool", lambda e: e.memset(Cnb[h][:], 0.0), writes=[Cnb[h]])
    for c in range(0, 8, 2):
        S.dma("pool", wfm[:, c:c + 2, :], dr["w_fm"][c * 128:(c + 2) * 128, :].rearrange("(c p) n -> p c n", p=128),
              writes=[wfm])
        S.dma("pool", wtm[:, c:c + 2, :], dr["w_tm"][c * 128:(c + 2) * 128, :].rearrange("(c p) n -> p c n", p=128),
              writes=[wtm])
    S.dma("sp", bg[:], dr["b_g"].partition_broadcast(128), writes=[bg])
    S.dma("sp", gain[:], dr["gain"].partition_broadcast(128), writes=[gain])

    def sb2(name, shape, dt):
        return [S.sbuf(f"{name}{i}", shape, dt) for i in range(2)]
    xt = [S.sbuf(f"xt{i}", [128, 1024], F32) for i in range(4)]
    xb = [S.sbuf(f"xb{i}", [128, 1024], BF16) for i in range(4)]
    xT = sb2("xT", [128, 8, 128], BF16)
    qTz = [[S.sbuf(f"qTz{i}_{h}", [128, 128], BF16) for h in range(4)] for i in range(2)]
    kT = sb2("kT", [128, 2, 128], BF16)
    gts = sb2("gts", [128, 8], F32)
    e1 = sb2("e1", [128, 4], F32)
    l1 = sb2("l1", [128, 4], F32)
    eq = sb2("eq", [128, 4], F32)
    ginb = sb2("ginb", [128, 4], F32)
    u = sb2("u", [128, 4], F32)
    ebl = sb2("ebl", [128, 4], F32)
    ktm = sb2("ktm", [128, 256], BF16)
    vx = sb2("vx", [128, 4, 132], BF16)
    og = sb2("og", [128, 512], F32)
    Sm = sb2("Sm", [128, 4, 128], BF16)
    dd = sb2("dd", [128, 4], F32)
    rr = sb2("rr", [128, 4], F32)
    fac = sb2("fac", [128, 4], F32)
    ss = sb2("ss", [128, 4], F32)
    rms = sb2("rms", [128, 4], F32)
    rinv = sb2("rinv", [128, 4], F32)
    fac2 = sb2("fac2", [128, 4], F32)
    sqj = sb2("sqj", [128, 128], F32)
    hn = sb2("hn", [128, 512], F32)
    hgo = sb2("hgo", [128, 512], BF16)
    for i in range(2):
        for h in range(4):
            S.op("pool", lambda e: e.memset(qTz[i][h][:], 0.0), writes=[qTz[i][h]])

    def xload(t):
        if "xmapb" in dr:
            S.dma("sp", xb[t % 4][:], dr["xmapb"](t), writes=[xb[t % 4]])
        else:
            S.dma("sp", xt[t % 4][:], dr["x"][t * 128:(t + 1) * 128, :], writes=[xt[t % 4]])

    for t_ in range(min(2, NT)):
        xload(t_)

    def _tile(t):
        b = t % 2
        sl = slice(t * 128, (t + 1) * 128)
        if t + 2 < NT:
            xload(t + 2)
        if "xmapb" not in dr:
            S.op("act", lambda e: e.copy(out=xb[t % 4][:], in_=xt[t % 4][:]), reads=[xt[t % 4]], writes=[xb[t % 4]])
        for c in range(8):
            S.op("pe", lambda e: e.transpose(out=ptB[:, c * 128:(c + 1) * 128], in_=xb[t % 4][:, c * 128:(c + 1) * 128],
                                             identity=identB[:]), reads=[xb[t % 4], identB], writes=[ptB], inc=(c == 7))
        S.op("dve", lambda e: e.tensor_copy(out=xT[b][:], in_=ptB[:].rearrange("p (c n) -> p c n", c=8)),
             reads=[ptB], writes=[xT[b]])
        if STAGE < 1:
            return
        yield
        for j in range(4):
            for c in range(8):
                S.op("pe", lambda e: e.matmul(PQ[:, j * 128:(j + 1) * 128], lhsT=wfm[:, c, j * 128:(j + 1) * 128],
                                              rhs=xT[b][:, c, :], start=(c == 0), stop=(c == 7)),
                     reads=[wfm, xT[b]], writes=[PQ], inc=(c == 7))
        for h in range(4):
            hb, jj = h // 2, h % 2
            S.op("dve", lambda e: e.tensor_scalar(out=qTz[b][h][:], in0=PQ[:, hb * 128:(hb + 1) * 128],
                                                  scalar1=hm[:, jj:jj + 1], scalar2=0.125, op0=ALU.mult, op1=ALU.mult),
                 reads=[PQ, hm], writes=[qTz[b][h]])
        S.op("dve", lambda e: e.tensor_copy(out=kT[b][:], in_=PQ[:, 256:512].rearrange("p (c n) -> p c n", c=2)),
             reads=[PQ], writes=[kT[b]])
        if STAGE < 2:
            return
        yield
        for (pp, lo, n) in ((PA, 0, 264), (PB, 264, 512), (PC, 776, 512))[:SUB]:
            for c in range(8):
                S.op("pe", lambda e: e.matmul(pp[:, 0:n], lhsT=xT[b][:, c, :], rhs=wtm[:, c, lo:lo + n],
                                              start=(c == 0), stop=(c == 7)),
                     reads=[wtm, xT[b]], writes=[pp], inc=(c == 7))
        if SUB < 4:
            return
        S.op("dve", lambda e: e.tensor_tensor(out=gts[b][:], in0=PA[:, 256:264], in1=bg[:], op=ALU.add),
             reads=[PA, bg], writes=[gts[b]])
        if SUB < 5:
            return
        S.op("dve", lambda e: e.tensor_copy(out=ktm[b][:], in_=PA[:, 0:256]), reads=[PA], writes=[ktm[b]])
        if SUB < 6:
            return
        S.op("act", lambda e: e.activation(out=e1[b][:], in_=gts[b][:, 4:8], func=AF.Exp, scale=-1.0),
             reads=[gts[b]], writes=[e1[b]])
        S.op("act", lambda e: e.activation(out=l1[b][:], in_=e1[b][:], func=AF.Ln, bias=oneb[:], scale=1.0),
             reads=[e1[b], oneb], writes=[l1[b]])
        if STAGE < 3:
            return
        S.op("pe", lambda e: e.matmul(PA[:, 272:276], lhsT=triI[:], rhs=l1[b][:], start=True, stop=True),
             reads=[triI, l1[b]], writes=[PA], inc=False)
        S.op("pe", lambda e: e.matmul(PA[:, 280:284], lhsT=onesM[:], rhs=l1[b][:], start=True, stop=True),
             reads=[onesM, l1[b]], writes=[PA])
        S.op("act", lambda e: e.activation(out=eq[b][:], in_=PA[:, 272:276], func=AF.Exp, scale=-1.0),
             reads=[PA], writes=[eq[b]])
        S.op("dve", lambda e: e.tensor_tensor(out=ginb[b][:], in0=PA[:, 272:276], in1=gts[b][:, 0:4], op=ALU.add),
             reads=[PA, gts[b]], writes=[ginb[b]])
        S.op("act", lambda e: e.activation(out=u[b][:], in_=ginb[b][:], func=AF.Exp), reads=[ginb[b]], writes=[u[b]])
        S.op("act", lambda e: e.activation(out=ebl[b][:], in_=PA[:, 280:284], func=AF.Exp, scale=-1.0),
             reads=[PA], writes=[ebl[b]])
        for h in range(4):
            S.op("dve", lambda e: e.tensor_scalar(out=vx[b][:, h, 0:128], in0=PB[:, h * 128:(h + 1) * 128],
                                                  scalar1=u[b][:, h:h + 1], scalar2=None, op0=ALU.mult),
                 reads=[PB, u[b]], writes=[vx[b]])
        S.op("dve", lambda e: e.tensor_copy(out=vx[b][:, :, 128], in_=u[b][:]), reads=[u[b]], writes=[vx[b]])
        S.op("act", lambda e: e.activation(out=og[b][:], in_=PC[:], func=AF.Sigmoid), reads=[PC], writes=[og[b]])
        if STAGE < 4:
            return
        for h in range(4):
            hb = h // 2
            S.op("pe", lambda e: e.matmul(PST[:, h * 128:(h + 1) * 128], lhsT=kT[b][:, hb, :], rhs=qTz[b][h][:],
                                          start=True, stop=True),
                 reads=[kT[b], qTz[b][h]], writes=[PST], inc=(h == 3))
        for h in range(4):
            S.op("dve", lambda e: e.tensor_tensor(out=Sm[b][:, h, :], in0=PST[:, h * 128:(h + 1) * 128], in1=triI[:],
                                                  op=ALU.mult), reads=[PST, triI], writes=[Sm[b]])
        for h in range(4):
            hb, jj = h // 2, h % 2
            pn = PN[hb]
            S.op("pe", lambda e: e.matmul(pn[:, jj * 129:(jj + 1) * 129], lhsT=Sm[b][:, h, :], rhs=vx[b][:, h, 0:129],
                                          start=True, stop=False),
                 reads=[Sm[b], vx[b]], writes=[pn], inc=False)
            S.op("pe", lambda e: e.matmul(pn[:, jj * 129:(jj + 1) * 129], lhsT=qTz[b][h][:], rhs=Cnb[h][:, 0:129],
                                          start=False, stop=True),
                 reads=[qTz[b][h], Cnb[h]], writes=[pn])
        if STAGE < 5:
            return
        for h in range(4):
            hb, jj = h // 2, h % 2
            R = slice(0, 128)
            pd = PQ if hb == 0 else PST
            S.op("pe", lambda e: e.matmul(pd[:, jj * 129:(jj + 1) * 129], lhsT=ktm[b][:, hb * 128:(hb + 1) * 128],
                                          rhs=vx[b][:, h, 0:129], start=True, stop=True),
                 reads=[ktm[b], vx[b]], writes=[pd])
            S.op("dve", lambda e: e.tensor_tensor(out=Cn[h][R, 0:129], in0=pd[R, jj * 129:(jj + 1) * 129], in1=Cn[h][R, 0:129],
                                                  op=ALU.add), reads=[pd, Cn[h]], writes=[Cn[h]])
            S.op("act", lambda e: e.activation(out=Cn[h][R, 0:129], in_=Cn[h][R, 0:129], func=AF.Identity,
                                               scale=ebl[b][R, h:h + 1]), reads=[Cn[h], ebl[b]], writes=[Cn[h]])
            S.op("pool", lambda e: e.tensor_copy(out=Cnb[h][R, 0:129], in_=Cn[h][R, 0:129]), reads=[Cn[h]], writes=[Cnb[h]])
        if STAGE < 6:
            return
        for h in range(4):
            hb, jj = h // 2, h % 2
            pn = PN[hb]
            c0 = jj * 129
            hs = slice(h, h + 1)
            S.op("act", lambda e: e.activation(out=dd[b][:, hs], in_=pn[:, c0 + 128:c0 + 129], func=AF.Abs,
                                               scale=eq[b][:, hs]), reads=[pn, eq[b]], writes=[dd[b]])
            S.op("dve", lambda e: e.tensor_scalar(out=dd[b][:, hs], in0=dd[b][:, hs], scalar1=1.0, scalar2=None,
                                                  op0=ALU.max), reads=[dd[b]], writes=[dd[b]])
            S.op("dve", lambda e: e.reciprocal(out=rr[b][:, hs], in_=dd[b][:, hs]), reads=[dd[b]], writes=[rr[b]])
            S.op("dve", lambda e: e.tensor_tensor(out=fac[b][:, hs], in0=rr[b][:, hs], in1=eq[b][:, hs], op=ALU.mult),
                 reads=[rr[b], eq[b]], writes=[fac[b]])
            S.op("act", lambda e: e.activation(out=sqj[b][:], in_=pn[:, c0:c0 + 128], func=AF.Square,
                                               scale=fac[b][:, hs], accum_out=ss[b][:, hs]),
                 reads=[pn, fac[b]], writes=[sqj[b], ss[b]])
            S.op("act", lambda e: e.activation(out=rms[b][:, hs], in_=ss[b][:, hs], func=AF.Sqrt, bias=epsb[:],
                                               scale=1.0 / 128.0), reads=[ss[b], epsb], writes=[rms[b]])
            S.op("dve", lambda e: e.reciprocal(out=rinv[b][:, hs], in_=rms[b][:, hs]), reads=[rms[b]], writes=[rinv[b]])
            S.op("dve", lambda e: e.tensor_tensor(out=fac2[b][:, hs], in0=fac[b][:, hs], in1=rinv[b][:, hs], op=ALU.mult),
                 reads=[fac[b], rinv[b]], writes=[fac2[b]])
            S.op("dve", lambda e: e.scalar_tensor_tensor(out=hn[b][:, h * 128:(h + 1) * 128], in0=pn[:, c0:c0 + 128],
                                                         scalar=fac2[b][:, hs], in1=gain[:, h * 128:(h + 1) * 128],
                                                         op0=ALU.mult, op1=ALU.mult),
                 reads=[pn, fac2[b], gain], writes=[hn[b]])
        S.op("pool", lambda e: e.tensor_tensor(out=hgo[b][:], in0=hn[b][:], in1=og[b][:], op=ALU.mult),
             reads=[hn[b], og[b]], writes=[hgo[b]])
        S.dma("sp", dr["out"][sl, :], hgo[b][:], reads=[hgo[b]], writes=[out_d])
    run_interleaved([_tile(t_) for t_ in range(NT)], 2)
    S.barrier()


def build_k1(SEQ):
    nc = bass.Bass("TRN2", target_bir_lowering=False)
    dr = {}
    def din(name, shape, dt=F32):
        dr[name] = nc.dram_tensor(name, list(shape), dt, kind="ExternalInput").ap()
    din("x", [SEQ, D]); din("w_fm", [D, 512]); din("w_tm", [D, 1288]); din("b_g", [8]); din("gain", [512])
    dr["out"] = nc.dram_tensor("out", [SEQ, 512], BF16, kind="ExternalOutput").ap()
    with ExitStack() as ctx:
        S = Sched(nc, ctx)
        ps = {"ptB": S.psum("ptB", [128, 1024], BF16), "P": [None] + [S.psum(f"P{i}", [128, 512], F32) for i in range(1, 8)]}
        emit_mlstm(S, nc, SEQ, dr, ps)
        pass
    return nc


def mlstm_inputs(x_b, w_in, b_gates, norm_gain, g):
    H, dk, dv = 8, 64, 128
    hs = slice(g * 4, g * 4 + 4)
    wq = w_in[:, 0:512].reshape(D, H, dk)[:, hs].reshape(D, 256)
    wk = w_in[:, 512:1024].reshape(D, H, dk)[:, hs].reshape(D, 256)
    wv = w_in[:, 1024:2048].reshape(D, H, dv)[:, hs].reshape(D, 512)
    wo = w_in[:, 2048:3072].reshape(D, H, dv)[:, hs].reshape(D, 512)
    wgi = w_in[:, 3072:3080][:, hs]
    wgf = w_in[:, 3080:3088][:, hs]
    w_fm = np.ascontiguousarray(np.concatenate([wq, wk], axis=1))
    w_tm = np.ascontiguousarray(np.concatenate([wk, wgi, wgf, wv, wo], axis=1))
    b_g = np.ascontiguousarray(np.concatenate([b_gates[0:8][hs], b_gates[8:16][hs]]))
    gain = np.ascontiguousarray(norm_gain.reshape(H, dv)[hs].reshape(512))
    return {"x": np.ascontiguousarray(x_b), "w_fm": w_fm, "w_tm": w_tm, "b_g": b_g, "gain": gain}


def emit_mla(S, nc, SEQ, dr, ps):
    NT = SEQ // 128
    ptB, ptT = ps["ptB"], ps["ptT"]
    P = ps["P"]
    identF = S.sbuf("identF", [128, 128], F32)
    identB = S.sbuf("identB", [128, 128], BF16)
    onesM = S.sbuf("onesM", [128, 128], F32)
    negF = S.sbuf("negF", [128, 128], F32)
    NEG = S.sbuf("NEG", [128, 128], BF16)
    hm = S.sbuf("hm", [128, 2], F32)
    epsb = S.sbuf("epsb", [128, 1], F32)
    wqn = S.sbuf("wqn", [128, 3, 512], BF16)
    cqT_all = S.sbuf("cqT_all", [128, 3, SEQ], BF16)
    krT2 = S.sbuf("krT2", [128, SEQ], BF16)
    qrT_all = S.sbuf("qrT_all", [128, 2, SEQ], BF16)
    out_d = Buf(None, "out")
    knT_dd = Buf(None, "knT_d")
    v_dd = Buf(None, "v_d")

    S.op("pool", lambda e: e.memset(identF[:], 0.0), writes=[identF])
    S.op("pool", lambda e: e.affine_select(out=identF[:], in_=identF[:], pattern=[[-1, 128]],
                                           compare_op=ALU.not_equal, fill=1.0, base=0,
                                           channel_multiplier=1), reads=[identF], writes=[identF])
    S.op("dve", lambda e: e.tensor_copy(out=identB[:], in_=identF[:]), reads=[identF], writes=[identB])
    S.op("pool", lambda e: e.memset(onesM[:], 1.0), writes=[onesM])
    S.op("pool", lambda e: e.memset(epsb[:], RMS_EPS), writes=[epsb])
    S.op("pool", lambda e: e.memset(negF[:], -30000.0), writes=[negF])
    S.op("pool", lambda e: e.affine_select(out=negF[:], in_=negF[:], pattern=[[1, 128]],
                                           compare_op=ALU.is_gt, fill=0.0, base=0,
                                           channel_multiplier=-1), reads=[negF], writes=[negF])
    S.op("dve", lambda e: e.tensor_copy(out=NEG[:], in_=negF[:]), reads=[negF], writes=[NEG])
    S.op("pool", lambda e: e.affine_select(out=hm[:, 0:1], in_=onesM[:, 0:1], pattern=[[0, 1]],
                                           compare_op=ALU.is_ge, fill=0.0, base=63,
                                           channel_multiplier=-1), reads=[onesM], writes=[hm])
    S.op("pool", lambda e: e.affine_select(out=hm[:, 1:2], in_=onesM[:, 0:1], pattern=[[0, 1]],
                                           compare_op=ALU.is_ge, fill=0.0, base=-64,
                                           channel_multiplier=1), reads=[onesM], writes=[hm])
    S.dma("pool", wqn[:], dr["wqn"].rearrange("(c p) n -> p c n", p=128), writes=[wqn])

    with ExitStack() as pctx:
        def sb(name, shape, dt):
            return Buf(pctx.enter_context(nc.sbuf_tensor(S.pfx + name, list(shape), dt)), name)
        def sb2(name, shape, dt):
            return [sb(f"{name}{i}", shape, dt) for i in range(2)]
        win = sb("win", [128, 8, 704], BF16)
        wqr = sb("wqr", [128, 3, 256], BF16)
        wkn = sb("wkn", [128, 2, 512], BF16)
        wkv = sb("wkv", [128, 2, 512], BF16)
        qn_t = sb("qn_t", [128, 384], F32)
        kvn_t = sb("kvn_t", [128, 256], F32)
        cosT = sb("cosT", [128, NT, 32], F32)
        sinT = sb("sinT", [128, NT, 32], F32)
        for c in range(0, 8, 4):
            S.dma("pool", win[:, c:c + 4, :], dr["w_in"][c * 128:(c + 4) * 128, :].rearrange("(c p) n -> p c n", p=128),
                  writes=[win])
        S.dma("pool", wqr[:], dr["wqr"].rearrange("(c p) n -> p c n", p=128), writes=[wqr])
        S.dma("pool", wkn[:], dr["wkn"].rearrange("(c p) n -> p c n", p=128), writes=[wkn])
        S.dma("pool", wkv[:], dr["wkv"].rearrange("(c p) n -> p c n", p=128), writes=[wkv])
        S.dma("sp", qn_t[:], dr["qn"].partition_broadcast(128), writes=[qn_t])
        S.dma("sp", kvn_t[:], dr["kvn"].partition_broadcast(128), writes=[kvn_t])

        with ExitStack() as tctx:
            def tb(name, shape, dt):
                return Buf(tctx.enter_context(nc.sbuf_tensor(S.pfx + name, list(shape), dt)), name)
            posi = tb("posi", [128, NT], I32)
            posf = tb("posf", [128, NT], F32)
            iof = tb("iof", [128, 32], F32)
            invf = tb("invf", [128, 32], F32)
            rr = tb("rr", [128, NT, 32], F32)
            ri = tb("ri", [128, NT * 32], I32)
            rf = tb("rf", [128, NT * 32], F32)
            ff = tb("ff", [128, NT * 32], F32)
            mk = tb("mk", [128, NT * 32], F32)
            S.dma("sp", posi[:], dr["pos"], writes=[posi])
            S.op("dve", lambda e: e.tensor_copy(out=posf[:], in_=posi[:]), reads=[posi], writes=[posf])
            S.op("pool", lambda e: e.iota(iof[:], pattern=[[1, 32]], base=0, channel_multiplier=0,
                                          allow_small_or_imprecise_dtypes=True), writes=[iof])
            S.op("act", lambda e: e.activation(out=invf[:], in_=iof[:], func=AF.Exp, scale=-math.log(10000.0) / 32.0),
                 reads=[iof], writes=[invf])
            S.op("dve", lambda e: e.tensor_scalar(out=invf[:], in0=invf[:], scalar1=1.0 / (2.0 * math.pi), scalar2=None,
                                                  op0=ALU.mult), reads=[invf], writes=[invf])
            for t in range(NT):
                S.op("dve", lambda e: e.tensor_scalar(out=rr[:, t, :], in0=invf[:], scalar1=posf[:, t:t + 1], scalar2=None,
                                                      op0=ALU.mult), reads=[invf, posf], writes=[rr])
            rrf = rr[:].rearrange("p t i -> p (t i)")
            for (shift, outT) in ((0.0, sinT), (0.25, cosT)):
                if shift != 0.0:
                    S.op("dve", lambda e: e.tensor_scalar(out=rrf, in0=rrf, scalar1=shift, scalar2=None, op0=ALU.add),
                         reads=[rr], writes=[rr])
                S.op("dve", lambda e: e.tensor_copy(out=ri[:], in_=rrf), reads=[rr], writes=[ri])
                S.op("dve", lambda e: e.tensor_copy(out=rf[:], in_=ri[:]), reads=[ri], writes=[rf])
                S.op("dve", lambda e: e.tensor_tensor(out=ff[:], in0=rrf, in1=rf[:], op=ALU.subtract),
                     reads=[rr, rf], writes=[ff])
                S.op("dve", lambda e: e.tensor_scalar(out=mk[:], in0=ff[:], scalar1=0.5, scalar2=None, op0=ALU.is_gt),
                     reads=[ff], writes=[mk])
                S.op("dve", lambda e: e.tensor_tensor(out=ff[:], in0=ff[:], in1=mk[:], op=ALU.subtract),
                     reads=[ff, mk], writes=[ff])
                S.op("dve", lambda e: e.tensor_scalar(out=mk[:], in0=ff[:], scalar1=-0.5, scalar2=None, op0=ALU.is_lt),
                     reads=[ff], writes=[mk])
                S.op("dve", lambda e: e.tensor_tensor(out=ff[:], in0=ff[:], in1=mk[:], op=ALU.add),
                     reads=[ff, mk], writes=[ff])
                S.op("dve", lambda e: e.tensor_scalar(out=ff[:], in0=ff[:], scalar1=-0.49999, scalar2=0.49999,
                                                      op0=ALU.max, op1=ALU.min), reads=[ff], writes=[ff])
                S.op("act", lambda e: e.activation(out=outT[:].rearrange("p t i -> p (t i)"), in_=ff[:], func=AF.Sin,
                                                   scale=2.0 * math.pi), reads=[ff], writes=[outT])
            S.barrier()

        xt = [sb(f"xt{i}", [128, 1024], F32) for i in range(4)]
        xb = [sb(f"xb{i}", [128, 1024], BF16) for i in range(4)]
        xT = sb2("xT", [128, 8, 128], BF16)
        junk = sb2("junk", [128, 384], F32)
        ssq = sb2("ssq", [128, 2], F32)
        rms = sb2("rms", [128, 2], F32)
        rinv = sb2("rinv", [128, 2], F32)
        ckv = sb2("ckv", [128, 256], BF16)
        cq = sb2("cq", [128, 384], BF16)
        kr2 = sb2("kr2", [128, 128], BF16)
        rt = [sb2(f"rt{k}", [128, 32], F32) for k in range(4)]
        qrt = [sb2(f"qrt{k}", [128, 4, 32], F32) for k in range(4)]
        qr = sb2("qr", [128, 4, 64], BF16)
        stg1 = sb2("stg1", [128, 6, 128], BF16)
        stg2 = sb2("stg2", [128, 2, 128], BF16)
        knT_sb = sb2("knT_sb", [128, 4, 128], BF16)
        v_sb = sb2("v_sb", [128, 512], BF16)
        PA1, PA2, PQR, PK, PV = P[1], P[2], P[3], P[4], P[5]

        def xload(t):
            if "xmapb" in dr:
                S.dma("sp", xb[t % 4][:], dr["xmapb"](t), writes=[xb[t % 4]])
            else:
                S.dma("sp", xt[t % 4][:], dr["x"][t * 128:(t + 1) * 128, :], writes=[xt[t % 4]])

        for t_ in range(min(2, NT)):
            xload(t_)

        def _tile(t):
            if SUBA < 1:
                return
            b = t % 2
            sl = slice(t * 128, (t + 1) * 128)
            if t + 2 < NT:
                xload(t + 2)
            if "xmapb" not in dr:
                S.op("act", lambda e: e.copy(out=xb[t % 4][:], in_=xt[t % 4][:]), reads=[xt[t % 4]], writes=[xb[t % 4]])
            for c in range(8):
                S.op("pe", lambda e: e.transpose(out=ptB[:, c * 128:(c + 1) * 128], in_=xb[t % 4][:, c * 128:(c + 1) * 128],
                                                 identity=identB[:]), reads=[xb[t % 4], identB], writes=[ptB], inc=(c == 7))
            S.op("dve", lambda e: e.tensor_copy(out=xT[b][:], in_=ptB[:].rearrange("p (c n) -> p c n", c=8)),
                 reads=[ptB], writes=[xT[b]])
            for (pp, lo, n) in ((PA1, 0, 320), (PA2, 320, 384)):
                for c in range(8):
                    S.op("pe", lambda e: e.matmul(pp[:, 0:n], lhsT=xT[b][:, c, :], rhs=win[:, c, lo:lo + n],
                                                  start=(c == 0), stop=(c == 7)),
                         reads=[win, xT[b]], writes=[pp], inc=(c == 7))
            S.op("act", lambda e: e.activation(out=junk[b][:, 0:256], in_=PA1[:, 0:256], func=AF.Square,
                                               accum_out=ssq[b][:, 0:1]), reads=[PA1], writes=[junk[b], ssq[b]])
            S.op("act", lambda e: e.activation(out=junk[b][:, 0:384], in_=PA2[:, 0:384], func=AF.Square,
                                               accum_out=ssq[b][:, 1:2]), reads=[PA2], writes=[junk[b], ssq[b]])
            S.op("act", lambda e: e.activation(out=rms[b][:, 0:1], in_=ssq[b][:, 0:1], func=AF.Sqrt, bias=epsb[:],
                                               scale=1.0 / 256.0), reads=[ssq[b], epsb], writes=[rms[b]])
            S.op("act", lambda e: e.activation(out=rms[b][:, 1:2], in_=ssq[b][:, 1:2], func=AF.Sqrt, bias=epsb[:],
                                               scale=1.0 / 384.0), reads=[ssq[b], epsb], writes=[rms[b]])
            S.op("dve", lambda e: e.reciprocal(out=rinv[b][:], in_=rms[b][:]), reads=[rms[b]], writes=[rinv[b]])
            S.op("dve", lambda e: e.scalar_tensor_tensor(out=ckv[b][:], in0=PA1[:, 0:256], scalar=rinv[b][:, 0:1],
                                                         in1=kvn_t[:], op0=ALU.mult, op1=ALU.mult),
                 reads=[PA1, rinv[b], kvn_t], writes=[ckv[b]])
            S.op("dve", lambda e: e.scalar_tensor_tensor(out=cq[b][:], in0=PA2[:, 0:384], scalar=rinv[b][:, 1:2],
                                                         in1=qn_t[:], op0=ALU.mult, op1=ALU.mult),
                 reads=[PA2, rinv[b], qn_t], writes=[cq[b]])
            if SUBA < 2:
                return
            cs, sn = cosT[:, t, :], sinT[:, t, :]
            x1, x2 = PA1[:, 256:288], PA1[:, 288:320]
            S.op("dve", lambda e: e.tensor_tensor(out=rt[0][b][:], in0=x1, in1=cs, op=ALU.mult), reads=[PA1, cosT], writes=[rt[0][b]])
            S.op("dve", lambda e: e.tensor_tensor(out=rt[1][b][:], in0=x2, in1=sn, op=ALU.mult), reads=[PA1, sinT], writes=[rt[1][b]])
            S.op("dve", lambda e: e.tensor_tensor(out=rt[2][b][:], in0=x2, in1=cs, op=ALU.mult), reads=[PA1, cosT], writes=[rt[2][b]])
            S.op("dve", lambda e: e.tensor_tensor(out=rt[3][b][:], in0=x1, in1=sn, op=ALU.mult), reads=[PA1, sinT], writes=[rt[3][b]])
            if SUBB < 2:
                return
            for off in (0, 64):
                S.op("pool", lambda e: e.tensor_tensor(out=kr2[b][:, off:off + 32], in0=rt[0][b][:], in1=rt[1][b][:],
                                                       op=ALU.subtract), reads=[rt[0][b], rt[1][b]], writes=[kr2[b]])
                S.op("pool", lambda e: e.tensor_tensor(out=kr2[b][:, off + 32:off + 64], in0=rt[2][b][:], in1=rt[3][b][:],
                                                       op=ALU.add), reads=[rt[2][b], rt[3][b]], writes=[kr2[b]])
            if SUBB < 3:
                return
            yield
            srcs = [(cq[b], k * 128) for k in range(3)] + [(ckv[b], k * 128) for k in range(2)] + [(kr2[b], 0)]
            for k, (src, o0) in enumerate(srcs):
                S.op("pe", lambda e: e.transpose(out=ptT[:, k * 128:(k + 1) * 128], in_=src[:, o0:o0 + 128], identity=identB[:]),
                     reads=[src, identB], writes=[ptT], inc=(k == 5))
            if SUBB < 4:
                return
            S.op("act", lambda e: e.copy(out=stg1[b][:], in_=ptT[:, 0:768].rearrange("p (c n) -> p c n", c=6)),
                 reads=[ptT], writes=[stg1[b]])
            for c in range(3):
                S.op("dve", lambda e: e.tensor_copy(out=cqT_all[:, c, sl], in_=stg1[b][:, c, :]),
                     reads=[stg1[b]], writes=[cqT_all])
            S.op("dve", lambda e: e.tensor_copy(out=krT2[:, sl], in_=stg1[b][:, 5, :]), reads=[stg1[b]], writes=[krT2])
            if SUBA < 3:
                return
            yield
            for c in range(3):
                S.op("pe", lambda e: e.matmul(PQR[:, 0:256], lhsT=stg1[b][:, c, :], rhs=wqr[:, c, :],
                                              start=(c == 0), stop=(c == 2)),
                     reads=[stg1[b], wqr], writes=[PQR], inc=(c == 2))
            for h in range(4):
                q1, q2 = PQR[:, h * 64:h * 64 + 32], PQR[:, h * 64 + 32:h * 64 + 64]
                S.op("dve", lambda e: e.tensor_tensor(out=qrt[0][b][:, h, :], in0=q1, in1=cs, op=ALU.mult), reads=[PQR, cosT], writes=[qrt[0][b]])
                S.op("dve", lambda e: e.tensor_tensor(out=qrt[1][b][:, h, :], in0=q2, in1=sn, op=ALU.mult), reads=[PQR, sinT], writes=[qrt[1][b]])
                S.op("dve", lambda e: e.tensor_tensor(out=qrt[2][b][:, h, :], in0=q2, in1=cs, op=ALU.mult), reads=[PQR, cosT], writes=[qrt[2][b]])
                S.op("dve", lambda e: e.tensor_tensor(out=qrt[3][b][:, h, :], in0=q1, in1=sn, op=ALU.mult), reads=[PQR, sinT], writes=[qrt[3][b]])
            S.op("pool", lambda e: e.tensor_tensor(out=qr[b][:, :, 0:32], in0=qrt[0][b][:], in1=qrt[1][b][:], op=ALU.subtract),
                 reads=[qrt[0][b], qrt[1][b]], writes=[qr[b]])
            S.op("pool", lambda e: e.tensor_tensor(out=qr[b][:, :, 32:64], in0=qrt[2][b][:], in1=qrt[3][b][:], op=ALU.add),
                 reads=[qrt[2][b], qrt[3][b]], writes=[qr[b]])
            qrf = qr[b][:].rearrange("p h d -> p (h d)")
            for k in range(2):
                S.op("pe", lambda e: e.transpose(out=ptT[:, 768 + k * 128:768 + (k + 1) * 128], in_=qrf[:, k * 128:(k + 1) * 128],
                                                 identity=identB[:]), reads=[qr[b], identB], writes=[ptT], inc=(k == 1))
            S.op("act", lambda e: e.copy(out=stg2[b][:], in_=ptT[:, 768:1024].rearrange("p (c n) -> p c n", c=2)),
                 reads=[ptT], writes=[stg2[b]])
            for c in range(2):
                S.op("dve", lambda e: e.tensor_copy(out=qrT_all[:, c, sl], in_=stg2[b][:, c, :]),
                     reads=[stg2[b]], writes=[qrT_all])
            if SUBA < 4:
                return
            yield
            for h in range(4):
                for c in range(2):
                    S.op("pe", lambda e: e.matmul(PK[:, h * 128:(h + 1) * 128], lhsT=wkn[:, c, h * 128:(h + 1) * 128],
                                                  rhs=stg1[b][:, 3 + c, :], start=(c == 0), stop=(c == 1)),
                         reads=[wkn, stg1[b]], writes=[PK], inc=(c == 1))
            S.op("dve", lambda e: e.tensor_copy(out=knT_sb[b][:], in_=PK[:].rearrange("p (h n) -> p h n", h=4)),
                 reads=[PK], writes=[knT_sb[b]])
            S.dma("sp", dr["knT_d"][:, :, sl].rearrange("h p n -> p h n"), knT_sb[b][:], reads=[knT_sb[b]], writes=[knT_dd])
            yield
            for c in range(2):
                S.op("pe", lambda e: e.matmul(PV[:], lhsT=stg1[b][:, 3 + c, :], rhs=wkv[:, c, :], start=(c == 0), stop=(c == 1)),
                     reads=[wkv, stg1[b]], writes=[PV], inc=(c == 1))
            S.op("dve", lambda e: e.tensor_copy(out=v_sb[b][:], in_=PV[:]), reads=[PV], writes=[v_sb[b]])
            S.dma("sp", dr["v_d"][sl, :], v_sb[b][:], reads=[v_sb[b]], writes=[v_dd])
        run_interleaved([_tile(t_) for t_ in range(NT)], 2)
        S.barrier()

    if STAGE < 2:
        return
    with ExitStack() as pctx:
        def sb(name, shape, dt):
            return Buf(pctx.enter_context(nc.sbuf_tensor(S.pfx + name, list(shape), dt)), name)
        def sb2(name, shape, dt):
            return [sb(f"{name}{i}", shape, dt) for i in range(2)]
        knT = sb("knT", [128, SEQ], BF16)
        vh = sb("vh", [128, NT, 128], BF16)
        sc = sb("sc", [128, SEQ], F32)
        Pb = sb("Pb", [128, SEQ], BF16)
        PT = sb("PT", [128, NT, 128], BF16)
        qnT = sb2("qnT", [128, 128], BF16)
        qrz = sb2("qrz", [128, 128], BF16)
        mx = sb2("mx", [128, 16], F32)
        mrow = sb2("mrow", [128, 1], F32)
        negm = sb2("negm", [128, 1], F32)
        rsum = sb2("rsum", [128, 1], F32)
        rinv2 = sb2("rinv2", [128, 1], F32)
        osb = sb2("osb", [128, 128], BF16)
        PQN = P[1]
        PS = [P[2], P[3]]
        PO = [P[4], P[5]]
        ptP = [ptB, ptT]
        it = 0
        for h in range(4):
            hb, jj = h // 2, h % 2
            S.dma("sp", knT[:], dr["knT_d"][h, :, :], writes=[knT])
            for t0 in range(0, NT, 8):
                t1 = min(NT, t0 + 8)
                S.dma("sp", vh[:, t0:t1, :],
                      dr["v_d"][t0 * 128:t1 * 128, h * 128:(h + 1) * 128].rearrange("(t p) d -> p t d", p=128),
                      writes=[vh])
            for qb in range(NT):
                b = it % 2
                it += 1
                qs = slice(qb * 128, (qb + 1) * 128)
                nkeys = (qb + 1) * 128
                for c in range(3):
                    S.op("pe", lambda e: e.matmul(PQN[:, 0:128], lhsT=wqn[:, c, h * 128:(h + 1) * 128], rhs=cqT_all[:, c, qs],
                                                  start=(c == 0), stop=(c == 2)),
                         reads=[wqn, cqT_all], writes=[PQN], inc=(c == 2))
                S.op("dve", lambda e: e.tensor_copy(out=qnT[b][:], in_=PQN[:, 0:128]), reads=[PQN], writes=[qnT[b]])
                S.op("pool", lambda e: e.tensor_scalar(out=qrz[b][:], in0=qrT_all[:, hb, qs], scalar1=hm[:, jj:jj + 1],
                                                       scalar2=None, op0=ALU.mult), reads=[qrT_all, hm], writes=[qrz[b]])
                nch = (nkeys + 511) // 512
                for kc in range(nch):
                    k0 = kc * 512
                    n = min(512, nkeys - k0)
                    pp = PS[kc % 2]
                    last = (kc == nch - 1)
                    S.op("pe", lambda e: e.matmul(pp[:, 0:n], lhsT=qnT[b][:], rhs=knT[:, k0:k0 + n], start=True, stop=False),
                         reads=[qnT[b], knT], writes=[pp], inc=False)
                    S.op("pe", lambda e: e.matmul(pp[:, 0:n], lhsT=qrz[b][:], rhs=krT2[:, k0:k0 + n], start=False, stop=(not last)),
                         reads=[qrz[b], krT2], writes=[pp], inc=(not last))
                    if last:
                        S.op("pe", lambda e: e.matmul(pp[:, n - 128:n], lhsT=identB[:], rhs=NEG[:], start=False, stop=True),
                             reads=[identB, NEG], writes=[pp])
                    S.op("dve", lambda e: e.tensor_scalar(out=sc[:, k0:k0 + n], in0=pp[:, 0:n], scalar1=1.0, scalar2=None,
                                                          op0=ALU.mult, op1=ALU.max, accum_out=mx[b][:, kc:kc + 1]),
                         reads=[pp], writes=[sc, mx[b]])
                S.op("dve", lambda e: e.tensor_reduce(out=mrow[b][:], in_=mx[b][:, 0:nch], axis=AX.X, op=ALU.max),
                     reads=[mx[b]], writes=[mrow[b]])
                S.op("dve", lambda e: e.tensor_scalar(out=negm[b][:], in0=mrow[b][:], scalar1=-SCALE, scalar2=None, op0=ALU.mult),
                     reads=[mrow[b]], writes=[negm[b]])
                S.op("act", lambda e: e.activation(out=Pb[:, 0:nkeys], in_=sc[:, 0:nkeys], func=AF.Exp, scale=SCALE,
                                                   bias=negm[b][:], accum_out=rsum[b][:]),
                     reads=[sc, negm[b]], writes=[Pb, rsum[b]])
                nkb = qb + 1
                for g0 in range(0, nkb, 8):
                    g1 = min(nkb, g0 + 8)
                    pt = ptP[(g0 // 8) % 2]
                    for kb in range(g0, g1):
                        S.op("pe", lambda e: e.transpose(out=pt[:, (kb - g0) * 128:(kb - g0 + 1) * 128],
                                                         in_=Pb[:, kb * 128:(kb + 1) * 128], identity=identB[:]),
                             reads=[Pb, identB], writes=[pt], inc=(kb == g1 - 1))
                    S.op("act", lambda e: e.copy(out=PT[:, g0:g1, :],
                                                 in_=pt[:, 0:(g1 - g0) * 128].rearrange("p (c n) -> p c n", n=128)),
                         reads=[pt], writes=[PT])
                po = PO[b]
                for kb in range(nkb):
                    S.op("pe", lambda e: e.matmul(po[:, 0:128], lhsT=PT[:, kb, :], rhs=vh[:, kb, :],
                                                  start=(kb == 0), stop=(kb == nkb - 1)),
                         reads=[PT, vh], writes=[po], inc=(kb == nkb - 1))
                S.op("dve", lambda e: e.reciprocal(out=rinv2[b][:], in_=rsum[b][:]), reads=[rsum[b]], writes=[rinv2[b]])
                S.op("dve", lambda e: e.tensor_scalar(out=osb[b][:], in0=po[:, 0:128], scalar1=rinv2[b][:], scalar2=None,
                                                      op0=ALU.mult), reads=[po, rinv2[b]], writes=[osb[b]])
                S.dma("sp", dr["out"][qs, h * 128:(h + 1) * 128], osb[b][:], reads=[osb[b]], writes=[out_d])
        S.barrier()


def build_k2(SEQ):
    nc = bass.Bass("TRN2", target_bir_lowering=False)
    dr = {}
    def din(name, shape, dt=F32):
        dr[name] = nc.dram_tensor("i_" + name, list(shape), dt, kind="ExternalInput").ap()
    NT = SEQ // 128
    din("x", [SEQ, D]); din("pos", [128, NT], I32); din("w_in", [D, 704]); din("qn", [384]); din("kvn", [256])
    din("wqn", [384, 512]); din("wqr", [384, 256]); din("wkn", [256, 512]); din("wkv", [256, 512])
    dr["out"] = nc.dram_tensor("out", [SEQ, 512], BF16, kind="ExternalOutput").ap()
    dr["knT_d"] = nc.dram_tensor("knT_d", [4, 128, SEQ], BF16, kind="Internal").ap()
    dr["v_d"] = nc.dram_tensor("v_d", [SEQ, 512], BF16, kind="Internal").ap()
    with ExitStack() as ctx:
        S = Sched(nc, ctx)
        ps = {"ptB": S.psum("ptB", [128, 1024], BF16), "ptT": S.psum("ptT", [128, 1024], BF16),
              "P": [None] + [S.psum(f"P{i}", [128, 512], F32) for i in range(1, 7)]}
        emit_mla(S, nc, SEQ, dr, ps)
        pass
    return nc


def mla_inputs(x_b, pos_b, w_in, q_norm, kv_norm, w_qb, w_kvb, g):
    SEQ = x_b.shape[0]
    H = 8
    hs = slice(g * 4, g * 4 + 4)
    w_in2 = np.ascontiguousarray(np.concatenate([w_in[:, 384:640], w_in[:, 640:704], w_in[:, 0:384]], axis=1))
    wq = w_qb.reshape(384, H, 192)[:, hs]
    wqn = np.ascontiguousarray(wq[:, :, 0:128].reshape(384, 512))
    wqr = np.ascontiguousarray(wq[:, :, 128:192].reshape(384, 256))
    wkv_ = w_kvb.reshape(256, H, 256)[:, hs]
    wkn = np.ascontiguousarray(wkv_[:, :, 0:128].reshape(256, 512))
    wkv = np.ascontiguousarray(wkv_[:, :, 128:256].reshape(256, 512))
    pos2 = np.ascontiguousarray(pos_b.reshape(SEQ // 128, 128).T.astype(np.int32))
    return {"i_x": np.ascontiguousarray(x_b), "i_pos": pos2, "i_w_in": w_in2, "i_qn": np.ascontiguousarray(q_norm),
            "i_kvn": np.ascontiguousarray(kv_norm), "i_wqn": wqn, "i_wqr": wqr, "i_wkn": wkn, "i_wkv": wkv}


def run_phase(S, pfx, fn):
    with ExitStack() as pctx:
        old = S.ctx
        S.ctx = pctx
        S.pfx = pfx
        fn()
        S.ctx = old
        S.pfx = ""


def build_fused(SEQ, CAP_, NB, depth=4):
    TOK = SEQ // 2
    NT3 = TOK // 128
    nc = bass.Bass("TRN2", target_bir_lowering=False)
    ext = {}

    def din(name, shape, dt=F32):
        ext[name] = nc.dram_tensor(name, list(shape), dt, kind="ExternalInput").ap()

    def dint(name, shape, dt):
        return nc.dram_tensor(name, list(shape), dt, kind="Internal").ap()

    din("x_full", [SEQ, D]); din("x_own", [TOK, D]); din("hgidx", [128, NT3, 2], I32)
    din("pos", [128, SEQ // 128], I32)
    for j in range((depth + 1) // 2):
        din(f"m{j}_w_fm", [D, 512]); din(f"m{j}_w_tm", [D, 1288]); din(f"m{j}_b_g", [8]); din(f"m{j}_gain", [512])
    for j in range(depth // 2):
        din(f"a{j}_w_in", [D, 704]); din(f"a{j}_qn", [384]); din(f"a{j}_kvn", [256])
        din(f"a{j}_wqn", [384, 512]); din(f"a{j}_wqr", [384, 256]); din(f"a{j}_wkn", [256, 512]); din(f"a{j}_wkv", [256, 512])
    for l in range(depth):
        din(f"l{l}_w_out", [D, D])
        for nm in ("g1", "b1", "g2", "b2"):
            din(f"l{l}_{nm}", [D])
        din(f"l{l}_w_rt", [D, E]); din(f"l{l}_b_rt", [E]); din(f"l{l}_w_gu", [E, D, 2 * D]); din(f"l{l}_b_gu", [E, 2 * D])
        din(f"l{l}_w_dn", [E, D, D]); din(f"l{l}_b_dn", [E, D])
    out_ap = nc.dram_tensor("out", [TOK, D], F32, kind="ExternalOutput").ap()
    hg_own = dint("hg_own", [SEQ, 512], BF16)
    hg_gath = dint("hg_gath", [2 * SEQ, 512], BF16)
    xn = [dint(f"xn{i}", [TOK, D], F32) for i in range(2)]
    xnb = dint("xnb", [TOK, D], BF16)
    xfull_gb = dint("xfull_gb", [SEQ, D], BF16)
    knT_d = dint("knT_d", [4, 128, SEQ], BF16)
    v_d = dint("v_d", [SEQ, 512], BF16)
    xg = dint("xg", [E * CAP_, D], BF16)
    yg = dint("yg", [E * CAP_, D], F32)
    x1s = dint("x1s", [TOK, D], F32)
    groups = [[2 * b, 2 * b + 1] for b in range(NB)]
    CHX = min(1024, TOK)
    CHH = min(2048, SEQ)

    def xmap(t):
        row = t * 128
        r = row // TOK
        lrow = row - r * TOK
        k, off = lrow // CHX, lrow % CHX
        r0 = k * 2 * CHX + r * CHX + off
        return xfull_gb[r0:r0 + 128, :]
    with ExitStack() as ctx:
        S = Sched(nc, ctx)
        ptB = S.psum("ptB", [128, 1024], BF16)
        Pf = [S.psum(f"P{i}", [128, 512], F32) for i in range(1, 7)]
        ptT = S.psum("ptT", [128, 1024], BF16)
        P7 = Buf(ptT.t[:].bitcast(F32), "P7")
        ps13 = {"ptB": ptB, "P": [None] + Pf + [P7]}
        ps2 = {"ptB": ptB, "ptT": ptT, "P": [None] + Pf}
        for l in range(depth):
            j = l // 2
            xsrc = ext["x_full"]
            if l % 2 == 0:
                dr = {"x": xsrc, **({"xmapb": xmap} if l > 0 else {}), "w_fm": ext[f"m{j}_w_fm"], "w_tm": ext[f"m{j}_w_tm"], "b_g": ext[f"m{j}_b_g"],
                      "gain": ext[f"m{j}_gain"], "out": hg_own}
                run_phase(S, f"L{l}m_", lambda: emit_mlstm(S, nc, SEQ, dr, ps13))
            else:
                dr = {"x": xsrc, **({"xmapb": xmap} if l > 0 else {}), "pos": ext["pos"], "w_in": ext[f"a{j}_w_in"], "qn": ext[f"a{j}_qn"], "kvn": ext[f"a{j}_kvn"],
                      "wqn": ext[f"a{j}_wqn"], "wqr": ext[f"a{j}_wqr"], "wkn": ext[f"a{j}_wkn"], "wkv": ext[f"a{j}_wkv"],
                      "out": hg_own, "knT_d": knT_d, "v_d": v_d}
                run_phase(S, f"L{l}a_", lambda: emit_mla(S, nc, SEQ, dr, ps2))
            for k in range(SEQ // CHH):
                S.cc("AllGather", groups, hg_own[k * CHH:(k + 1) * CHH, :], hg_gath[k * 2 * CHH:(k + 1) * 2 * CHH, :], inc=1)
            S.barrier()
            dr = {"x": ext["x_own"] if l == 0 else xn[(l - 1) % 2], "hg_gath": hg_gath, "hgidx": ext["hgidx"],
                  "out": out_ap if l == depth - 1 else xn[l % 2], "xg": xg, "yg": yg, "x1s": x1s}
            if l < depth - 1:
                dr["outb"] = xnb
            for nm in ("w_out", "g1", "b1", "g2", "b2", "w_rt", "b_rt", "w_gu", "b_gu", "w_dn", "b_dn"):
                dr[nm] = ext[f"l{l}_{nm}"]
            run_phase(S, f"L{l}p_", lambda: emit_post(S, nc, TOK, CAP_, dr, ps13))
            if l < depth - 1:
                for k in range(TOK // CHX):
                    S.cc("AllGather", groups, xnb[k * CHX:(k + 1) * CHX, :],
                         xfull_gb[k * 2 * CHX:(k + 1) * 2 * CHX, :], inc=1)
                S.barrier()
    return nc


def fused_in_maps(x, positions, ln_gain, ln_bias, mlstm_w_in, mlstm_b_gates, mlstm_norm_gain, mlstm_w_out,
                  mla_w_in, mla_q_norm, mla_kv_norm, mla_w_qb, mla_w_kvb, mla_w_out,
                  moe_w_router, moe_b_router, moe_w_gate_up, moe_b_gate_up, moe_w_down, moe_b_down):
    f32 = np.float32
    A = lambda a: np.ascontiguousarray(np.asarray(a, dtype=f32))
    x = A(x)
    positions = np.asarray(positions)
    B, S_, _ = x.shape
    TOK = S_ // 2
    NT3 = TOK // 128
    depth = ln_gain.shape[0]
    shared = {}
    for l in range(depth):
        j = l // 2
        shared[f"l{l}_w_out"] = A(mlstm_w_out[j]) if l % 2 == 0 else A(mla_w_out[j])
        shared[f"l{l}_g1"] = A(ln_gain[l, 0]); shared[f"l{l}_b1"] = A(ln_bias[l, 0])
        shared[f"l{l}_g2"] = A(ln_gain[l, 1]); shared[f"l{l}_b2"] = A(ln_bias[l, 1])
        shared[f"l{l}_w_rt"] = A(moe_w_router[l]); shared[f"l{l}_b_rt"] = A(moe_b_router[l])
        shared[f"l{l}_w_gu"] = A(moe_w_gate_up[l]); shared[f"l{l}_b_gu"] = A(moe_b_gate_up[l])
        shared[f"l{l}_w_dn"] = A(moe_w_down[l]); shared[f"l{l}_b_dn"] = A(moe_b_down[l])
    in_maps = []
    for c in range(2 * B):
        b, g = c // 2, c % 2
        m = dict(shared)
        m["x_full"] = np.ascontiguousarray(x[b])
        m["x_own"] = np.ascontiguousarray(x[b, g * TOK:(g + 1) * TOK])
        p = np.arange(128, dtype=np.int64)[:, None, None]
        i = np.arange(NT3, dtype=np.int64)[None, :, None]
        r = np.arange(2, dtype=np.int64)[None, None, :]
        chh = min(2048, S_)
        tok = g * TOK + i * 128 + p
        m["hgidx"] = np.ascontiguousarray(((tok // chh) * 2 * chh + r * chh + tok % chh).astype(np.int32))
        for j in range((depth + 1) // 2):
            mi = mlstm_inputs(x[b], A(mlstm_w_in[j]), A(mlstm_b_gates[j]), A(mlstm_norm_gain[j]), g)
            for k in ("w_fm", "w_tm", "b_g", "gain"):
                m[f"m{j}_{k}"] = mi[k]
        for j in range(depth // 2):
            ai = mla_inputs(x[b], positions[b], A(mla_w_in[j]), A(mla_q_norm[j]), A(mla_kv_norm[j]),
                            A(mla_w_qb[j]), A(mla_w_kvb[j]), g)
            m["pos"] = ai["i_pos"]
            for k in ("w_in", "qn", "kvn", "wqn", "wqr", "wkn", "wkv"):
                m[f"a{j}_{k}"] = ai["i_" + k]
        in_maps.append(m)
    return in_maps


_NC_CACHE = {}


def kernel(x, positions, ln_gain, ln_bias, mlstm_w_in, mlstm_b_gates, mlstm_norm_gain, mlstm_w_out,
           mla_w_in, mla_q_norm, mla_kv_norm, mla_w_qb, mla_w_kvb, mla_w_out,
           moe_w_router, moe_b_router, moe_w_gate_up, moe_b_gate_up, moe_w_down, moe_b_down):
    x = np.asarray(x)
    B, S_, _ = x.shape
    TOK = S_ // 2
    depth = ln_gain.shape[0]
    cap = CAP if S_ == SEQ_FULL else 128
    key = (S_, B, depth)
    if key not in _NC_CACHE:
        _NC_CACHE[key] = build_fused(S_, cap, B, depth)
    nc = _NC_CACHE[key]
    in_maps = fused_in_maps(x, positions, ln_gain, ln_bias, mlstm_w_in, mlstm_b_gates, mlstm_norm_gain, mlstm_w_out,
                            mla_w_in, mla_q_norm, mla_kv_norm, mla_w_qb, mla_w_kvb, mla_w_out,
                            moe_w_router, moe_b_router, moe_w_gate_up, moe_b_gate_up, moe_w_down, moe_b_down)
    res = run_bass_kernel_spmd(nc, in_maps, core_ids=list(range(2 * B)))
    out = np.empty((B, S_, D), dtype=np.float32)
    for c in range(2 * B):
        b, g = c // 2, c % 2
        out[b, g * TOK:(g + 1) * TOK] = res.results[c]["out"]
    return out
```
